# Optimizing a Trainium2 kernel written in Bass

```python
import math
import jax, jax.numpy as jnp
from jax import lax
import numpy as np

D_MODEL = 1024
BATCH = 4
SEQ = 4096
DEPTH = 4

N_MIXERS = 3
EPS = 1e-6
CONV_WIDTH = 3
N_HEADS = 16
N_KV_HEADS = 4
HEAD_DIM = D_MODEL // N_HEADS
IDX_HEADS = 8
IDX_DIM = 64
TOPK_MAX = 256
Q_BLOCK = 128
REL_BUCKETS = 32
REL_MAX_DIST = 128
ATTN_IN_SIZES = [N_HEADS * HEAD_DIM, N_KV_HEADS * HEAD_DIM, N_KV_HEADS * HEAD_DIM, IDX_HEADS * IDX_DIM, IDX_DIM, IDX_HEADS]
ATTN_IN_DIM = sum(ATTN_IN_SIZES)
SSM_GROUP = 16
SSM_GROUPS = D_MODEL // SSM_GROUP
SSM_STATE = 64
D_FF = ((8 * D_MODEL // 3 + 127) // 128) * 128
N_EXPERTS = 8
TOP_K_EXPERTS = 2
N_CONV_LAYERS = (DEPTH + 2) // 3
N_ATTN_LAYERS = (DEPTH + 1) // 3
N_SSM_LAYERS = DEPTH // 3
N_DENSE_LAYERS = (DEPTH + 1) // 2
N_MOE_LAYERS = DEPTH // 2

kernel_name = 'hybrid_conv_dsa_s5_moe_trunk'


def rms_norm(x, g):
    x32 = x.astype(jnp.float32)
    y = x32 * lax.rsqrt(jnp.mean(x32 * x32, axis=-1, keepdims=True) + EPS)
    return (y * g.astype(jnp.float32)).astype(x.dtype)


def modulate(h, shift, scale):
    return h * (1 + scale[:, None, :]) + shift[:, None, :]


def short_conv_mixer(h, w_in, w_conv, w_out):
    b_gate, c_gate, v = jnp.split(h @ w_in, 3, axis=-1)
    u = c_gate * v
    conv = lax.conv_general_dilated(
        u, w_conv[:, None, :], window_strides=(1,), padding=[(CONV_WIDTH - 1, 0)],
        dimension_numbers=('NWC', 'WIO', 'NWC'), feature_group_count=u.shape[-1])
    return (b_gate * conv) @ w_out


def rel_bucket(dist):
    max_exact = REL_BUCKETS // 2
    d = jnp.maximum(dist, 1).astype(jnp.float32)
    large = max_exact + (jnp.log(d / max_exact) / math.log(REL_MAX_DIST / max_exact)
                         * (REL_BUCKETS - max_exact)).astype(jnp.int32)
    large = jnp.minimum(large, REL_BUCKETS - 1)
    return jnp.where(dist < max_exact, dist, large)


def dsa_attention(h, w_in, q_gain, k_gain, w_out, rel_bias):
    bsz, seq_len, _ = h.shape
    f32 = jnp.float32
    top_k = min(TOPK_MAX, seq_len // 4)
    n_rep = N_HEADS // N_KV_HEADS
    splits = [int(s) for s in np.cumsum(ATTN_IN_SIZES)[:-1]]
    q, k, v, qi, ki, wi = jnp.split(h @ w_in, splits, axis=-1)
    q = rms_norm(q.reshape(bsz, seq_len, N_KV_HEADS, n_rep, HEAD_DIM), q_gain)
    k = rms_norm(k.reshape(bsz, seq_len, N_KV_HEADS, HEAD_DIM), k_gain)
    v = v.reshape(bsz, seq_len, N_KV_HEADS, HEAD_DIM)
    qi = qi.reshape(bsz, seq_len, IDX_HEADS, IDX_DIM)
    wi = wi * (IDX_HEADS ** -0.5 * IDX_DIM ** -0.5)
    key_pos = jnp.arange(seq_len)
    gather = jax.vmap(lambda arr, idx: arr[idx])
    bias_table = rel_bias.astype(f32)

    def block(start):
        qpos = start + jnp.arange(Q_BLOCK)
        qb = lax.dynamic_slice_in_dim(q, start, Q_BLOCK, axis=1)
        qib = lax.dynamic_slice_in_dim(qi, start, Q_BLOCK, axis=1)
        wib = lax.dynamic_slice_in_dim(wi, start, Q_BLOCK, axis=1)
        dots = jax.nn.relu(jnp.einsum('bthd,bsd->bths', qib, ki).astype(f32))
        score = jnp.einsum('bths,bth->bts', dots, wib.astype(f32))
        score = jnp.where(key_pos[None, None, :] <= qpos[None, :, None], score, -jnp.inf)
        _, sel = lax.top_k(score, top_k)
        valid = sel <= qpos[None, :, None]
        kg = gather(k, sel)
        vg = gather(v, sel)
        logits = jnp.einsum('btngd,btknd->btngk', qb, kg).astype(f32) * (HEAD_DIM ** -0.5)
        bias = bias_table[rel_bucket(jnp.maximum(qpos[None, :, None] - sel, 0))]
        bias = bias.reshape(bsz, Q_BLOCK, top_k, N_KV_HEADS, n_rep).transpose(0, 1, 3, 4, 2)
        logits = jnp.where(valid[:, :, None, None, :], logits + bias, -jnp.inf)
        p = jax.nn.softmax(logits, axis=-1).astype(v.dtype)
        o = jnp.einsum('btngk,btknd->btngd', p, vg)
        return o.reshape(bsz, Q_BLOCK, N_HEADS * HEAD_DIM)

    starts = jnp.arange(seq_len // Q_BLOCK, dtype=jnp.int32) * Q_BLOCK
    out = lax.map(block, starts)
    out = out.transpose(1, 0, 2, 3).reshape(bsz, seq_len, N_HEADS * HEAD_DIM)
    return out @ w_out


def s5_mixer(h, lam_re, lam_im, log_step, b_re, b_im, c_re, c_im, d_skip, w_glu):
    bsz, seq_len, _ = h.shape
    f32 = jnp.float32
    lam = lax.complex(jnp.minimum(lam_re.astype(f32), -1e-4), lam_im.astype(f32))
    step = jnp.exp(log_step.astype(f32))[:, None]
    lam_bar = jnp.exp(lam * step)
    b_bar = ((lam_bar - 1.0) / lam)[..., None] * lax.complex(b_re.astype(f32), b_im.astype(f32))
    c_mat = lax.complex(c_re.astype(f32), c_im.astype(f32))
    u = h.astype(f32).reshape(bsz, seq_len, SSM_GROUPS, SSM_GROUP)
    bu = jnp.einsum('blgc,gpc->blgp', u.astype(jnp.complex64), b_bar)
    a = jnp.broadcast_to(lam_bar, (1, seq_len, SSM_GROUPS, SSM_STATE))

    def combine(left, right):
        a_l, b_l = left
        a_r, b_r = right
        return a_r * a_l, a_r * b_l + b_r

    _, states = lax.associative_scan(combine, (a, bu), axis=1)
    y = jnp.einsum('blgp,gcp->blgc', states, c_mat).real.reshape(bsz, seq_len, D_MODEL)
    y = y + d_skip.astype(f32) * h.astype(f32)
    g = jax.nn.gelu(y).astype(h.dtype)
    lin, gate = jnp.split(g @ w_glu, 2, axis=-1)
    return lin * jax.nn.sigmoid(gate)


def swiglu(h, w_gu, w_down):
    g, u = jnp.split(h @ w_gu, 2, axis=-1)
    return (jax.nn.silu(g) * u) @ w_down


def moe_swiglu(h, router_w, router_b, w_gu, w_down):
    bsz, seq_len, d = h.shape
    f32 = jnp.float32
    t = h.reshape(-1, d)
    logits = t.astype(f32) @ router_w.astype(f32) + router_b.astype(f32)
    probs = jax.nn.softmax(logits, axis=-1)
    top_p, top_i = lax.top_k(probs, TOP_K_EXPERTS)
    top_p = top_p / jnp.sum(top_p, axis=-1, keepdims=True)
    gates = jnp.sum(jax.nn.one_hot(top_i, N_EXPERTS, dtype=f32) * top_p[..., None], axis=1)
    out = jnp.zeros_like(t)
    for e in range(N_EXPERTS):
        out = out + gates[:, e:e + 1].astype(t.dtype) * swiglu(t, w_gu[e], w_down[e])
    return out.reshape(bsz, seq_len, d)


def setup_inputs(seed: int = 0) -> dict:
    key = jax.random.key(seed)
    ks = jax.random.split(key, 32)
    f32 = jnp.float32

    def nrm(k, shape, scale):
        return jax.random.normal(k, shape, f32) * scale

    n_idx = jnp.arange(SSM_STATE, dtype=f32)
    return {
        'x': nrm(ks[0], (BATCH, SEQ, D_MODEL), 1.0),
        'c': nrm(ks[1], (BATCH, D_MODEL), 1.0),
        'ada_w': nrm(ks[2], (DEPTH, D_MODEL, 6 * D_MODEL), 0.5 * D_MODEL ** -0.5),
        'ada_b': nrm(ks[3], (DEPTH, 6 * D_MODEL), 0.02),
        'norm_g': 1.0 + nrm(ks[4], (DEPTH, 2, D_MODEL), 0.02),
        'conv_w_in': nrm(ks[5], (N_CONV_LAYERS, D_MODEL, 3 * D_MODEL), D_MODEL ** -0.5),
        'conv_w': nrm(ks[6], (N_CONV_LAYERS, CONV_WIDTH, D_MODEL), CONV_WIDTH ** -0.5),
        'conv_w_out': nrm(ks[7], (N_CONV_LAYERS, D_MODEL, D_MODEL), D_MODEL ** -0.5),
        'attn_w_in': nrm(ks[8], (N_ATTN_LAYERS, D_MODEL, ATTN_IN_DIM), D_MODEL ** -0.5),
        'attn_q_gain': 1.0 + nrm(ks[9], (N_ATTN_LAYERS, HEAD_DIM), 0.02),
        'attn_k_gain': 1.0 + nrm(ks[10], (N_ATTN_LAYERS, HEAD_DIM), 0.02),
        'attn_w_out': nrm(ks[11], (N_ATTN_LAYERS, N_HEADS * HEAD_DIM, D_MODEL), (N_HEADS * HEAD_DIM) ** -0.5),
        'rel_bias': nrm(ks[12], (REL_BUCKETS, N_HEADS), 0.5),
        'ssm_lambda_re': -0.5 + nrm(ks[13], (N_SSM_LAYERS, SSM_GROUPS, SSM_STATE), 0.01),
        'ssm_lambda_im': math.pi * n_idx + nrm(ks[14], (N_SSM_LAYERS, SSM_GROUPS, SSM_STATE), 0.01),
        'ssm_log_step': jax.random.uniform(ks[15], (N_SSM_LAYERS, SSM_GROUPS), f32, math.log(1e-3), math.log(1e-1)),
        'ssm_b_re': nrm(ks[16], (N_SSM_LAYERS, SSM_GROUPS, SSM_STATE, SSM_GROUP), SSM_GROUP ** -0.5),
        'ssm_b_im': nrm(ks[17], (N_SSM_LAYERS, SSM_GROUPS, SSM_STATE, SSM_GROUP), SSM_GROUP ** -0.5),
        'ssm_c_re': nrm(ks[18], (N_SSM_LAYERS, SSM_GROUPS, SSM_GROUP, SSM_STATE), SSM_STATE ** -0.5),
        'ssm_c_im': nrm(ks[19], (N_SSM_LAYERS, SSM_GROUPS, SSM_GROUP, SSM_STATE), SSM_STATE ** -0.5),
        'ssm_d': nrm(ks[20], (N_SSM_LAYERS, D_MODEL), 1.0),
        'ssm_w_glu': nrm(ks[21], (N_SSM_LAYERS, D_MODEL, 2 * D_MODEL), D_MODEL ** -0.5),
        'ffn_w_gu': nrm(ks[22], (N_DENSE_LAYERS, D_MODEL, 2 * D_FF), D_MODEL ** -0.5),
        'ffn_w_down': nrm(ks[23], (N_DENSE_LAYERS, D_FF, D_MODEL), D_FF ** -0.5),
        'moe_router_w': nrm(ks[24], (N_MOE_LAYERS, D_MODEL, N_EXPERTS), D_MODEL ** -0.5),
        'moe_router_b': nrm(ks[25], (N_MOE_LAYERS, N_EXPERTS), 0.01),
        'moe_w_gu': nrm(ks[26], (N_MOE_LAYERS, N_EXPERTS, D_MODEL, 2 * D_FF), D_MODEL ** -0.5),
        'moe_w_down': nrm(ks[27], (N_MOE_LAYERS, N_EXPERTS, D_FF, D_MODEL), D_FF ** -0.5),
    }


def reference(x, c, ada_w, ada_b, norm_g, conv_w_in, conv_w, conv_w_out, attn_w_in, attn_q_gain,
              attn_k_gain, attn_w_out, rel_bias, ssm_lambda_re, ssm_lambda_im, ssm_log_step, ssm_b_re,
              ssm_b_im, ssm_c_re, ssm_c_im, ssm_d, ssm_w_glu, ffn_w_gu, ffn_w_down, moe_router_w,
              moe_router_b, moe_w_gu, moe_w_down):
    cond = jax.nn.silu(c)
    for i in range(DEPTH):
        mod = cond @ ada_w[i] + ada_b[i]
        sh1, sc1, g1, sh2, sc2, g2 = jnp.split(mod, 6, axis=-1)
        h = modulate(rms_norm(x, norm_g[i, 0]), sh1, sc1)
        j = i // N_MIXERS
        if i % N_MIXERS == 0:
            y = short_conv_mixer(h, conv_w_in[j], conv_w[j], conv_w_out[j])
        elif i % N_MIXERS == 1:
            y = dsa_attention(h, attn_w_in[j], attn_q_gain[j], attn_k_gain[j], attn_w_out[j], rel_bias)
        else:
            y = s5_mixer(h, ssm_lambda_re[j], ssm_lambda_im[j], ssm_log_step[j], ssm_b_re[j], ssm_b_im[j],
                         ssm_c_re[j], ssm_c_im[j], ssm_d[j], ssm_w_glu[j])
        x = x + g1[:, None, :] * y
        h = modulate(rms_norm(x, norm_g[i, 1]), sh2, sc2)
        if i % 2 == 0:
            y = swiglu(h, ffn_w_gu[i // 2], ffn_w_down[i // 2])
        else:
            y = moe_swiglu(h, moe_router_w[i // 2], moe_router_b[i // 2], moe_w_gu[i // 2], moe_w_down[i // 2])
        x = x + g2[:, None, :] * y
    return x
```

```python
import contextlib
from types import FunctionType
import numpy as np
import concourse.bass as bass
import concourse.mybir as mybir
from concourse.bass_utils import run_bass_kernel_spmd

F32 = mybir.dt.float32
BF16 = mybir.dt.bfloat16
AF = mybir.ActivationFunctionType
ALU = mybir.AluOpType
AX = mybir.AxisListType

S = 4096
D = 1024
DC = 8
P = 128
DFF = 2816
FC = 22
NE = 8
EPS = 1e-6
NBLK = S // 512

ENG = ['pe', 'act', 'dve', 'pool', 'sp']
SELF_SYNC = True


class Sched:
    def __init__(self, nc, stack):
        self.nc = nc
        self.stack = stack
        self.streams = {e: [] for e in ENG}
        self.sem = {e: stack.enter_context(nc.semaphore('s_' + e)) for e in ENG}
        self.count = {e: 0 for e in ENG}
        self.dsem = {}
        self.seen = {e: {} for e in ENG}
        self.hist = {}
        self.buf = {}
        self.ninst = 0

    def _semof(self, key):
        if isinstance(key, tuple):
            return self.dsem[key[1]][0]
        return self.sem[key]

    def _wait(self, eng, toks):
        best = {}
        for (k, v) in toks:
            if v > best.get(k, 0):
                best[k] = v
        seen = self.seen[eng]
        for k, v in best.items():
            if seen.get(k, 0) >= v:
                continue
            if k == eng and (eng == 'pe' or not SELF_SYNC):
                continue
            s = self._semof(k)
            self.streams[eng].append(lambda e, s=s, v=v: e.wait_ge(s, v))
            self.ninst += 1
            h = self.hist.get((k, v))
            if h:
                for kk, vv in h.items():
                    if vv > seen.get(kk, 0):
                        seen[kk] = vv
            if v > seen.get(k, 0):
                seen[k] = v

    def _deps(self, reads, writes):
        toks = []
        for b in reads:
            st = self.buf.get(b)
            if st and st[0]:
                toks.append(st[0])
        for b in writes:
            st = self.buf.get(b)
            if st:
                if st[0]:
                    toks.append(st[0])
                toks.extend(st[1].items())
        return toks

    def _record(self, tok, reads, writes):
        for b in reads:
            st = self.buf.setdefault(b, [None, {}])
            if tok[1] > st[1].get(tok[0], 0):
                st[1][tok[0]] = tok[1]
        for b in writes:
            self.buf[b] = [tok, {}]

    def op(self, eng, fns, reads=(), writes=()):
        if not isinstance(fns, (list, tuple)):
            fns = [fns]
        self._wait(eng, self._deps(reads, writes))
        self.count[eng] += 1
        tok = (eng, self.count[eng])
        sem = self.sem[eng]
        for f in fns[:-1]:
            self.streams[eng].append(f)
        last = fns[-1]
        self.streams[eng].append(lambda e, f=last, s=sem: f(e).then_inc(s, 1))
        self.ninst += len(fns)
        self.hist[tok] = dict(self.seen[eng])
        self._record(tok, reads, writes)
        return tok

    def dma(self, queue, chan, out, in_, reads=(), writes=(), **kw):
        if chan == 'const':
            self.nconst = getattr(self, 'nconst', 0) + 1
            chan = 'const%d' % self.nconst
        if chan not in self.dsem:
            self.dsem[chan] = [self.stack.enter_context(self.nc.semaphore('d_' + str(chan))), 0]
        self._wait(queue, self._deps(reads, writes))
        ds = self.dsem[chan]
        ds[1] += 16
        tok = (('dma', chan), ds[1])
        s = ds[0]
        self.streams[queue].append(lambda e, s=s, o=out, i=in_, kw=kw: e.dma_start(
            out=(o() if isinstance(o, FunctionType) else o), in_=(i() if isinstance(i, FunctionType) else i), **kw).then_inc(s, 16))
        self.ninst += 1
        self.hist[tok] = dict(self.seen[queue])
        self._record(tok, reads, writes)
        return tok

    def barrier(self):
        toks = []
        for b, st in self.buf.items():
            if st[0]:
                toks.append(st[0])
            toks.extend(st[1].items())
        for eng in ENG:
            self._wait(eng, toks)

    def finish(self, eng='sp'):
        toks = []
        for b, st in self.buf.items():
            if st[0]:
                toks.append(st[0])
            toks.extend(st[1].items())
        self._wait(eng, toks)

    def emit(self):
        nc = self.nc
        with nc.Block() as block:
            @block.tensor
            def _(e):
                for f in self.streams['pe']:
                    f(e)

            @block.scalar
            def _(e):
                for f in self.streams['act']:
                    f(e)

            @block.vector
            def _(e):
                for f in self.streams['dve']:
                    f(e)

            @block.gpsimd
            def _(e):
                for f in self.streams['pool']:
                    f(e)

            @block.sync
            def _(e):
                for f in self.streams['sp']:
                    f(e)


class Builder:
    def __init__(self, cfg):
        self.cfg = cfg
        self.nc = bass.Bass("TRN2", target_bir_lowering=False)
        self.stack = contextlib.ExitStack()
        self.sc = Sched(self.nc, self.stack)
        self.din = {}
        self.psn = 0
        self.uid = 0

    def inp(self, name, shape, dtype=F32):
        t = self.nc.dram_tensor(name, list(shape), dtype, kind="ExternalInput").ap()
        self.din[name] = t
        return t

    def sb(self, name, shape, dtype, st=None):
        self.uid += 1
        return (st or self.stack).enter_context(self.nc.sbuf_tensor('sb%d_%s' % (self.uid, name), list(shape), dtype))

    def bank(self):
        rot = getattr(self, 'rot', None) or list(range(8))
        i = rot[self.psn % len(rot)]
        self.psn += 1
        return ('ps', i), self.ps[i]

    def setup_common(self):
        nc = self.nc
        self.ps = [self.stack.enter_context(nc.psum_tensor('ps%d' % i, [P, 512], F32)) for i in range(8)]
        self.ident = self.sb('ident', [P, P], F32)
        self.ones = self.sb('ones', [P, P], F32)
        d_ident = self.inp('ident', [P, P])
        self.sc.dma('sp', 'const', self.ident[:], d_ident[:, :], writes=['ident'])
        self.sc.op('dve', lambda e: e.memset(self.ones[:], 1.0), writes=['ones'])

    def build_mod(self):
        sc = self.sc
        cT = self.inp('cT', [P, DC])
        ada_w = self.inp('ada_w', [4, D, 6 * D])
        ada_bT = self.inp('ada_bT', [P, 4, 48])
        norm_gT = self.inp('norm_gT', [P, 4, 2, DC])
        self.cond = self.sb('cond', [P, DC], F32)
        self.modT = self.sb('modT', [P, 4, 48], F32)
        self.adab = self.sb('adab', [P, 4, 48], F32)
        self.ng = self.sb('ng', [P, 4, 2, DC], F32)
        self.gs = self.sb('gs', [P, 4, 2, DC], F32)
        craw = self.sb('craw', [P, DC], F32)
        sig = self.sb('csig', [P, DC], F32)
        sc.dma('sp', 'const', craw[:], cT[:, :], writes=['craw'])
        sc.dma('sp', 'const', self.adab[:], ada_bT[:, :, :], writes=['adab'])
        sc.dma('sp', 'const', self.ng[:], norm_gT[:, :, :, :], writes=['ng'])
        sc.op('act', lambda e: e.activation(out=sig[:], in_=craw[:], func=AF.Sigmoid), reads=['craw'], writes=['csig'])
        sc.op('dve', lambda e: e.tensor_tensor(out=self.cond[:], in0=craw[:], in1=sig[:], op=ALU.mult),
              reads=['craw', 'csig'], writes=['cond'])
        st = contextlib.ExitStack()
        wsl = [self.sb('adaw%d' % i, [P, DC, 1024], F32, st) for i in range(2)]
        n = 0
        for i in range(4):
            pname, pt = self.bank()
            for k in range(6):
                slot = n % 2
                n += 1
                for c in range(DC):
                    sc.dma('sp', 'adaw%d' % slot, wsl[slot][:, c, :],
                           ada_w[i, c * P:(c + 1) * P, k * 1024:(k + 1) * 1024], writes=['adaw%d' % slot])
                fns = []
                for nn in range(8):
                    for c in range(DC):
                        fns.append(lambda e, pt=pt, slot=slot, nn=nn, c=c, k=k:
                                   e.matmul(pt[:, k * 8 + nn:k * 8 + nn + 1], lhsT=wsl[slot][:, c, nn * P:(nn + 1) * P],
                                            rhs=self.cond[:, c:c + 1], start=(c == 0), stop=(c == DC - 1)))
                sc.op('pe', fns, reads=['adaw%d' % slot, 'cond'], writes=[pname])
            sc.op('dve', lambda e, pt=pt, i=i: e.tensor_tensor(out=self.modT[:, i, :], in0=pt[:, 0:48], in1=self.adab[:, i, :], op=ALU.add),
                  reads=[pname, 'adab'], writes=[('modT', i)])
            for j, k in ((0, 1), (1, 4)):
                sc.op('dve', lambda e, i=i, j=j, k=k: e.scalar_tensor_tensor(
                    out=self.gs[:, i, j, :], in0=self.modT[:, i, k * 8:(k + 1) * 8], scalar=1.0, in1=self.ng[:, i, j, :],
                    op0=ALU.add, op1=ALU.mult), reads=[('modT', i), 'ng'], writes=[('modT', i)])
        sc.barrier()
        st.close()

    def modv(self, i, k, c):
        return self.modT[:, i, k * 8 + c:k * 8 + c + 1]

    def alloc_stream(self):
        self.xT_d = self.nc.dram_tensor('xT_scratch', [D, S], F32).ap()
        self.xh_d = self.nc.dram_tensor('xh_scratch', [D, S // 2], F32).ap()
        self.rd_mode = 'full'
        self.wr_mode = 'full'
        self.ntok = S
        self.sc.streams['sp'].append(lambda e: setattr(self, 'rv', e.partition_id() // 4))
        d_rankf = self.inp('rankf', [P, 1])
        self.rankf = self.sb('rankf', [P, 1], F32)
        self.sc.dma('sp', 'const', self.rankf[:], d_rankf[:, :], writes=['rankf'])
        self.xin = self.inp('x', [S, D])
        self.xblk = [self.sb('xblk%d' % i, [P, DC, 512], F32) for i in range(2)]
        self.sq = self.sb('sq', [P, 512], F32)
        self.rstd = self.sb('rstd', [P, 512], F32)
        self.tmp32 = [self.sb('tmp32_%d' % i, [P, 512], F32) for i in range(3)]
        self.xn = 0

    def xd(self, c, blk, write):
        mode = self.wr_mode if write else self.rd_mode
        if mode == 'full':
            return self.xT_d[c * P:(c + 1) * P, blk * 512:(blk + 1) * 512], ('xTd', c, blk)
        if mode == 'half':
            return self.xh_d[c * P:(c + 1) * P, blk * 512:(blk + 1) * 512], ('xh', c, blk)
        if mode == 'dynfull':
            return self.xs_d[c * P:(c + 1) * P, (blk + 1) * 512:(blk + 2) * 512], 'xs'
        raise ValueError(mode)

    def load_xT(self, blk, from_input):
        sc = self.sc
        slot = self.xn % 2
        self.xn += 1
        name = 'xblk%d' % slot
        t = self.xblk[slot]
        if from_input:
            for j in range(4):
                sc.dma('sp', 'xtok', self.xtok[:, j, :], self.xin[blk * 512 + j * P: blk * 512 + (j + 1) * P, :],
                       writes=[('xtok', j)])
            for c in range(DC):
                pname, pt = self.bank()
                fns = [lambda e, pt=pt, j=j, c=c: e.transpose(out=pt[:, j * P:(j + 1) * P], in_=self.xtok[:, j, c * P:(c + 1) * P],
                                                              identity=self.ident[:]) for j in range(4)]
                sc.op('pe', fns, reads=[('xtok', j) for j in range(4)] + ['ident'], writes=[pname])
                eng = 'act' if c % 2 == 0 else 'dve'
                if eng == 'act':
                    sc.op('act', lambda e, pt=pt, t=t, c=c: e.copy(out=t[:, c, :], in_=pt[:]), reads=[pname], writes=[(name, c)])
                else:
                    sc.op('dve', lambda e, pt=pt, t=t, c=c: e.tensor_copy(out=t[:, c, :], in_=pt[:]), reads=[pname], writes=[(name, c)])
        else:
            for c in range(DC):
                ap, tn = self.xd(c, blk, False)
                sc.dma('sp', name, t[:, c, :], ap, reads=[tn], writes=[(name, c)])
        return name, t

    def store_xT(self, blk, name, t, c):
        ap, tn = self.xd(c, blk, True)
        self.sc.dma('sp', 'st_' + name, ap, t[:, c, :], reads=[(name, c)], writes=[tn])

    def norm_mod(self, li, j, name, t, hT, hname, hoff, h32=None):
        sc = self.sc
        pname, pt = self.bank()
        for c in range(DC):
            sc.op('act', lambda e, c=c: e.activation(out=self.sq[:], in_=t[:, c, :], func=AF.Square), reads=[(name, c)], writes=['sq'])
            sc.op('pe', lambda e, c=c, pt=pt: e.matmul(pt[:], lhsT=self.ones[:], rhs=self.sq[:], start=(c == 0), stop=(c == DC - 1)),
                  reads=['sq', 'ones'], writes=[pname])
        sc.op('act', lambda e, pt=pt: e.activation(out=self.rstd[:], in_=pt[:], func=AF.Sqrt, scale=1.0 / D, bias=self.epsb[:]),
              reads=[pname, 'epsb'], writes=['rstd'])
        sc.op('dve', lambda e: e.reciprocal(out=self.rstd[:], in_=self.rstd[:]), reads=['rstd'], writes=['rstd'])
        kshift = 0 if j == 0 else 3
        for c in range(DC):
            tm = self.tmp32[c % 2]
            tn = 'tmp32_%d' % (c % 2)
            sc.op('dve', lambda e, c=c, tm=tm: e.tensor_tensor(out=tm[:], in0=t[:, c, :], in1=self.rstd[:], op=ALU.mult),
                  reads=[(name, c), 'rstd'], writes=[tn])
            if h32 is not None:
                sc.op('pool', lambda e, c=c, tm=tm: e.tensor_scalar(out=h32[:, c, :], in0=tm[:], scalar1=self.gs[:, li, j, c:c + 1],
                                                                   scalar2=self.modv(li, kshift, c), op0=ALU.mult, op1=ALU.add),
                      reads=[tn, ('modT', li)], writes=[('h32', c)])
            sc.op('dve', lambda e, c=c, tm=tm: e.tensor_scalar(out=hT[:, c, hoff:hoff + 512], in0=tm[:], scalar1=self.gs[:, li, j, c:c + 1],
                                                              scalar2=self.modv(li, kshift, c), op0=ALU.mult, op1=ALU.add),
                  reads=[tn, ('modT', li)], writes=[(hname, c, hoff)])

    def load_w_cast(self, chan, dst, src, rows_split, writes):
        n = dst.shape[-1]
        step = 2048
        for a in range(0, n, step):
            b = min(n, a + step)
            self.sc.dma('pool', chan, dst[:, a:b], src[:, a:b], writes=writes)

    def conv_setup(self):
        self.d_conv_w_in = self.inp('conv_w_in', [2, D, 3 * D])
        self.d_conv_w_out = self.inp('conv_w_out', [2, D, D])
        d_cw = self.inp('conv_wT', [P, 2, 3, DC])
        self.cwT = self.sb('cwT', [P, 2, 3, DC], F32)
        self.sc.dma('sp', 'const', self.cwT[:], d_cw[:, :, :, :], writes=['cwT'])

    def conv_mixer(self, li, j, first):
        sc = self.sc
        st = contextlib.ExitStack()
        nc = self.nc
        win = self.sb('cw_in', [P, DC, 3 * D], BF16, st)
        wout = self.sb('cw_out', [P, DC, D], BF16, st)
        hT = self.sb('hTc', [P, DC, 512], BF16, st)
        bT = self.sb('bTc', [P, DC, 512], F32, st)
        uT = self.sb('uTc', [P, DC, 514], F32, st)
        zT = self.sb('zTc', [P, DC, 512], BF16, st)
        if first:
            self.xtok = self.sb('xtok', [P, 4, D], F32, st)
        L = 'L%d' % li
        for c in range(DC):
            self.load_w_cast('cwin', win[:, c, :], self.d_conv_w_in[j, c * P:(c + 1) * P, :], None, writes=[(L, 'cwin')])
            self.load_w_cast('cwout', wout[:, c, :], self.d_conv_w_out[j, c * P:(c + 1) * P, :], None, writes=[(L, 'cwout')])
        sc.op('pool', lambda e: e.memset(uT[:, :, 0:2], 0.0), writes=[(L, 'uT', c) for c in range(DC)])
        split = (self.rd_mode == 'dynfull')
        for blk in ([-1] if split else []) + list(range(self.ntok // 512)):
            name, t = self.load_xT(blk, first)
            self.norm_mod(li, 0, name, t, hT, (L, 'hT'), 0)
            hreads = [((L, 'hT'), c, 0) for c in range(DC)] + [(L, 'cwin')]
            for c in range(DC):
                pts = []
                for kind in ((1, 2) if blk < 0 else (0, 1, 2)):
                    pname, pt = self.bank()
                    fns = [lambda e, pt=pt, k=k, kind=kind, c=c: e.matmul(
                        pt[:], lhsT=win[:, k, kind * D + c * P: kind * D + (c + 1) * P], rhs=hT[:, k, :],
                        start=(k == 0), stop=(k == DC - 1)) for k in range(DC)]
                    sc.op('pe', fns, reads=hreads, writes=[pname])
                    pts.append((pname, pt))
                if blk < 0:
                    (pcn, pc), (pvn, pv) = pts
                else:
                    (pbn, pb), (pcn, pc), (pvn, pv) = pts
                    sc.op('act', lambda e, pb=pb, c=c: e.activation(out=bT[:, c, :], in_=pb[:], func=AF.Copy), reads=[pbn], writes=[(L, 'bT', c)])
                tm = self.tmp32[2]
                sc.op('act', lambda e, pc=pc, tm=tm: e.activation(out=tm[:], in_=pc[:], func=AF.Copy), reads=[pcn], writes=['tmp32_2'])
                sc.op('dve', lambda e, pv=pv, tm=tm, c=c: e.tensor_tensor(out=uT[:, c, 2:514], in0=pv[:], in1=tm[:], op=ALU.mult),
                      reads=[pvn, 'tmp32_2'], writes=[(L, 'uT', c)])
            if blk < 0:
                for c in range(DC):
                    sc.op('dve', lambda e, c=c: e.tensor_scalar(out=uT[:, c, 0:2], in0=uT[:, c, 512:514], scalar1=self.rankf[:, 0:1], scalar2=None, op0=ALU.mult),
                          reads=[(L, 'uT', c), 'rankf'], writes=[(L, 'uT', c)])
                continue
            for c in range(DC):
                tm = self.tmp32[c % 2]
                tn = 'tmp32_%d' % (c % 2)
                sc.op('dve', lambda e, c=c, tm=tm: e.tensor_scalar(out=tm[:], in0=uT[:, c, 2:514], scalar1=self.cwT[:, j, 2, c:c + 1],
                                                                  scalar2=None, op0=ALU.mult), reads=[(L, 'uT', c), 'cwT'], writes=[tn])
                sc.op('dve', lambda e, c=c, tm=tm: e.scalar_tensor_tensor(out=tm[:], in0=uT[:, c, 1:513], scalar=self.cwT[:, j, 1, c:c + 1],
                                                                         in1=tm[:], op0=ALU.mult, op1=ALU.add),
                      reads=[(L, 'uT', c), tn], writes=[tn])
                sc.op('dve', lambda e, c=c, tm=tm: e.scalar_tensor_tensor(out=tm[:], in0=uT[:, c, 0:512], scalar=self.cwT[:, j, 0, c:c + 1],
                                                                         in1=tm[:], op0=ALU.mult, op1=ALU.add),
                      reads=[(L, 'uT', c), tn], writes=[tn])
                sc.op('dve', lambda e, c=c, tm=tm: e.tensor_tensor(out=zT[:, c, :], in0=tm[:], in1=bT[:, c, :], op=ALU.mult),
                      reads=[tn, (L, 'bT', c)], writes=[(L, 'zT', c)])
                sc.op('pool', lambda e, c=c: e.tensor_copy(out=uT[:, c, 0:2], in_=uT[:, c, 512:514]),
                      reads=[(L, 'uT', c)], writes=[(L, 'uT', c)])
            zreads = [(L, 'zT', c) for c in range(DC)] + [(L, 'cwout')]
            for c in range(DC):
                pname, pt = self.bank()
                fns = [lambda e, pt=pt, k=k, c=c: e.matmul(pt[:], lhsT=wout[:, k, c * P:(c + 1) * P], rhs=zT[:, k, :],
                                                           start=(k == 0), stop=(k == DC - 1)) for k in range(DC)]
                sc.op('pe', fns, reads=zreads, writes=[pname])
                sc.op('dve', lambda e, pt=pt, c=c, t=t: e.scalar_tensor_tensor(out=t[:, c, :], in0=pt[:], scalar=self.modv(li, 2, c),
                                                                              in1=t[:, c, :], op0=ALU.mult, op1=ALU.add),
                      reads=[pname, (name, c), ('modT', li)], writes=[(name, c)])
                self.store_xT(blk, name, t, c)
        sc.barrier()
        st.close()

    def ffn_setup(self):
        self.d_ffn_gu = self.inp('ffn_gu_r', [2, FC, P, DC * 256])
        self.d_ffn_dn = self.inp('ffn_dn_r', [2, DC, P, FC * P])
        self.d_moe_gu = self.inp('moe_gu_r', [2, NE, FC, P, DC * 256])
        self.d_moe_dn = self.inp('moe_dn_r', [2, NE, DC, P, FC * P])
        d_rw = self.inp('moe_rwT', [P, 2, DC, NE])
        d_rb = self.inp('moe_rbB', [P, 2, NE])
        d_sel = self.inp('sel8', [NE, NE, P])
        self.rw = self.sb('rw', [P, 2, DC, NE], F32)
        self.rb = self.sb('rb', [P, 2, NE], F32)
        self.sel = self.sb('sel', [NE, NE, P], F32)
        self.sc.dma('sp', 'const', self.rw[:], d_rw[:, :, :, :], writes=['rw'])
        self.sc.dma('sp', 'const', self.rb[:], d_rb[:, :, :], writes=['rb'])
        self.sc.dma('sp', 'const', self.sel[:], d_sel[:, :, :], writes=['sel'])

    def router(self, li, L, h32, half, gT, sm):
        sc = self.sc
        jl = li // 2
        lg, ex, gt, m8, sc1 = sm
        prn, pr = self.bank()
        h32r = [('h32', c) for c in range(DC)]
        for tt in range(4):
            fns = [lambda e, pr=pr, tt=tt, c=c: e.matmul(pr[:, tt * 8:(tt + 1) * 8], lhsT=h32[:, c, tt * P:(tt + 1) * P],
                                                        rhs=self.rw[:, jl, c, :], start=(c == 0), stop=(c == DC - 1))
                   for c in range(DC)]
            sc.op('pe', fns, reads=h32r + ['rw'], writes=[prn])
        ptn, ptT = self.bank()
        for tt in range(4):
            R = [(L, 'rt')]
            sc.op('dve', lambda e, tt=tt, pr=pr: e.tensor_tensor(out=lg[:, 0:8], in0=pr[:, tt * 8:(tt + 1) * 8], in1=self.rb[:, jl, :], op=ALU.add),
                  reads=[prn, 'rb'], writes=R)
            sc.op('dve', lambda e: e.tensor_reduce(out=sc1[:, 0:1], in_=lg[:, 0:8], axis=AX.X, op=ALU.max), reads=R, writes=R)
            sc.op('dve', lambda e: e.tensor_scalar(out=sc1[:, 0:1], in0=sc1[:, 0:1], scalar1=-1.0, scalar2=None, op0=ALU.mult), reads=R, writes=R)
            sc.op('act', lambda e: e.activation(out=ex[:, 0:8], in_=lg[:, 0:8], func=AF.Exp, bias=sc1[:, 0:1], scale=1.0), reads=R, writes=R)
            sc.op('dve', lambda e: e.max(out=m8[:, 0:8], in_=ex[:, 0:8]), reads=R, writes=R)
            sc.op('dve', lambda e: e.tensor_tensor(out=sc1[:, 1:2], in0=m8[:, 0:1], in1=m8[:, 1:2], op=ALU.add), reads=R, writes=R)
            sc.op('dve', lambda e: e.reciprocal(out=sc1[:, 1:2], in_=sc1[:, 1:2]), reads=R, writes=R)
            sc.op('dve', lambda e: e.tensor_scalar(out=gt[:, 0:8], in0=ex[:, 0:8], scalar1=m8[:, 1:2], scalar2=None, op0=ALU.is_ge), reads=R, writes=R)
            sc.op('dve', lambda e: e.tensor_tensor(out=gt[:, 0:8], in0=gt[:, 0:8], in1=ex[:, 0:8], op=ALU.mult), reads=R, writes=R)
            sc.op('dve', lambda e: e.tensor_scalar(out=gt[:, 0:8], in0=gt[:, 0:8], scalar1=sc1[:, 1:2], scalar2=None, op0=ALU.mult), reads=R, writes=R)
            sc.op('pe', lambda e, tt=tt, ptT=ptT: e.transpose(out=ptT[0:8, tt * P:(tt + 1) * P], in_=gt[:, 0:8], identity=self.ident[:]),
                  reads=R + ['ident'], writes=[ptn])
        sc.op('act', lambda e, ptT=ptT, half=half: e.activation(out=gT[0:8, half * 512:(half + 1) * 512], in_=ptT[0:8, :], func=AF.Copy),
              reads=[ptn], writes=[(L, 'gT', half)])

    def ffn(self, li, moe):
        sc = self.sc
        st = contextlib.ExitStack()
        L = 'F%d' % li
        TB = 1024
        hT = self.sb('hTf', [P, DC, TB], BF16, st)
        actT = self.sb('actT', [P, FC, TB], BF16, st)
        wgu = [self.sb('wgu%d' % i, [P, DC * 256], BF16, st) for i in range(2)]
        wd = [self.sb('wd%d' % i, [P, FC * P], BF16, st) for i in range(2)]
        xs = [self.sb('xs%d' % i, [P, 512], F32, st) for i in range(2)]
        if moe:
            h32 = self.sb('h32', [P, DC, 512], F32, st)
            yacc = self.sb('yacc', [P, DC, TB], F32, st)
            Gb = [self.sb('Gb%d' % i, [P, TB], F32, st) for i in range(2)]
            gT = self.sb('gT', [NE, TB], F32, st)
            sm = [self.sb('rsm%d' % i, [P, 8], F32, st) for i in range(5)]
        jl = li // 2
        nw = 0
        nd = 0
        nx = 0
        ng = 0
        for sbi in range(self.ntok // TB):
            for half in range(2):
                blk = sbi * 2 + half
                name, t = self.load_xT(blk, False)
                self.norm_mod(li, 1, name, t, hT, (L, 'hT'), half * 512, h32=(h32 if moe else None))
                if moe:
                    self.router(li, L, h32, half, gT, sm)
            for ex in (range(NE) if moe else [None]):
                if moe:
                    gsl = ng % 2
                    ng += 1
                    gbn = (L, 'Gb', gsl)
                    for half in range(2):
                        pn, pt = self.bank()
                        sc.op('pe', lambda e, pt=pt, ex=ex, half=half: e.matmul(pt[:], lhsT=self.sel[0:8, ex, :], rhs=gT[0:8, half * 512:(half + 1) * 512],
                                                                               start=True, stop=True),
                              reads=['sel', (L, 'gT', half)], writes=[pn])
                        sc.op('act', lambda e, pt=pt, gsl=gsl, half=half: e.activation(out=Gb[gsl][:, half * 512:(half + 1) * 512], in_=pt[:], func=AF.Copy),
                              reads=[pn], writes=[(gbn, half)])
                for f in range(FC):
                    slot = nw % 2
                    nw += 1
                    wn = (L, 'wgu', slot)
                    src = self.d_moe_gu[jl, ex, f, :, :] if moe else self.d_ffn_gu[jl, f, :, :]
                    self.load_w_cast('wgu%d' % slot, wgu[slot][:, :], src, None, writes=[wn])
                    for half in range(2):
                        hreads = [((L, 'hT'), c, half * 512) for c in range(DC)] + [wn]
                        pgn, pg = self.bank()
                        pun, pu = self.bank()
                        for (pn, pt, off) in ((pgn, pg, 0), (pun, pu, 128)):
                            fns = [lambda e, pt=pt, k=k, off=off, slot=slot, half=half: e.matmul(
                                pt[:], lhsT=wgu[slot][:, k * 256 + off: k * 256 + off + 128], rhs=hT[:, k, half * 512:(half + 1) * 512],
                                start=(k == 0), stop=(k == DC - 1)) for k in range(DC)]
                            sc.op('pe', fns, reads=hreads, writes=[pn])
                        tm = self.tmp32[2]
                        sc.op('act', lambda e, pg=pg, tm=tm: e.activation(out=tm[:], in_=pg[:], func=AF.Silu), reads=[pgn], writes=['tmp32_2'])
                        sc.op('dve', lambda e, pu=pu, tm=tm, f=f, half=half: e.tensor_tensor(
                            out=actT[:, f, half * 512:(half + 1) * 512], in0=pu[:], in1=tm[:], op=ALU.mult),
                            reads=[pun, 'tmp32_2'], writes=[(L, 'act', f, half)])
                for c in range(DC):
                    slot = nd % 2
                    nd += 1
                    wn = (L, 'wd', slot)
                    src = self.d_moe_dn[jl, ex, c, :, :] if moe else self.d_ffn_dn[jl, c, :, :]
                    self.load_w_cast('wd%d' % slot, wd[slot][:, :], src, None, writes=[wn])
                    for half in range(2):
                        blk = sbi * 2 + half
                        areads = [(L, 'act', f, half) for f in range(FC)] + [wn]
                        pn, pt = self.bank()
                        fns = [lambda e, pt=pt, f=f, slot=slot, half=half: e.matmul(
                            pt[:], lhsT=wd[slot][:, f * P:(f + 1) * P], rhs=actT[:, f, half * 512:(half + 1) * 512],
                            start=(f == 0), stop=(f == FC - 1)) for f in range(FC)]
                        sc.op('pe', fns, reads=areads, writes=[pn])
                        if not moe:
                            self.resid(li, 5, xs, nx, c, blk, pn, pt, None, None)
                            nx += 1
                        else:
                            yn = (L, 'yacc', c, half)
                            ysl = yacc[:, c, half * 512:(half + 1) * 512]
                            gsl_ap = Gb[gsl][:, half * 512:(half + 1) * 512]
                            if ex == 0:
                                sc.op('dve', lambda e, pt=pt, ysl=ysl, g=gsl_ap: e.tensor_tensor(out=ysl, in0=pt[:], in1=g, op=ALU.mult),
                                      reads=[pn, (gbn, half)], writes=[yn])
                            else:
                                tm = self.tmp32[c % 2]
                                tn = 'tmp32_%d' % (c % 2)
                                sc.op('dve', lambda e, pt=pt, tm=tm, g=gsl_ap: e.tensor_tensor(out=tm[:], in0=pt[:], in1=g, op=ALU.mult),
                                      reads=[pn, (gbn, half)], writes=[tn])
                                sc.op('pool', lambda e, tm=tm, ysl=ysl: e.tensor_tensor(out=ysl, in0=ysl, in1=tm[:], op=ALU.add),
                                      reads=[tn, yn], writes=[yn])
            if moe:
                for c in range(DC):
                    for half in range(2):
                        blk = sbi * 2 + half
                        self.resid(li, 5, xs, nx, c, blk, (L, 'yacc', c, half), None, yacc[:, c, half * 512:(half + 1) * 512], None)
                        nx += 1
        sc.barrier()
        st.close()

    def resid(self, li, k, xs, nx, c, blk, srcname, pt, src_ap, _):
        sc = self.sc
        xsl = nx % 2
        xn = 'xs%d' % xsl
        xt = xs[xsl]
        src = pt[:] if pt is not None else src_ap
        rap, rtn = self.xd(c, blk, False)
        wap, wtn = self.xd(c, blk, True)
        sc.dma('sp', xn, xt[:], rap, reads=[rtn], writes=[xn])
        sc.op('dve', lambda e, src=src, xt=xt, c=c: e.scalar_tensor_tensor(
            out=xt[:], in0=src, scalar=self.modv(li, k, c), in1=xt[:], op0=ALU.mult, op1=ALU.add),
            reads=[srcname, xn, ('modT', li)], writes=[xn])
        sc.dma('sp', 'st_' + xn, wap, xt[:], reads=[xn], writes=[wtn])

    def attn_setup(self):
        self.d_awq = self.inp('attn_wq_perm', [D, 1024])
        self.d_awk = self.inp('attn_wk', [D, 256])
        self.d_awv = self.inp('attn_wv', [D, 256])
        self.d_awqi = self.inp('attn_wqi', [D, 512])
        self.d_awki2 = self.inp('attn_wki2', [D, 128])
        self.d_awwi = self.inp('attn_wwi', [D, 8])
        self.d_awout = self.inp('attn_wout_r', [64, 16, D])
        self.d_gainT = self.inp('attn_gainT', [P, 2])
        self.d_biasT = self.inp('attn_biasT', [P, 32, P])
        self.d_b31 = self.inp('attn_b31B', [P, 16])
        self.d_cmask = self.inp('cmask', [P, P])
        self.d_bones = self.inp('blockones', [P, P])
        self.d_selrow = self.inp('selrow', [65, 64])

    def head_norm(self, pn, pt, gain_ap, out_ap, tagw):
        sc = self.sc
        qs = self.tmp32[2]
        sc.op('act', lambda e, pt=pt, qs=qs: e.activation(out=qs[:], in_=pt[:], func=AF.Copy), reads=[pn], writes=['tmp32_2'])
        sc.op('act', lambda e, qs=qs: e.activation(out=self.sq[:], in_=qs[:], func=AF.Square), reads=['tmp32_2'], writes=['sq'])
        p2n, p2 = self.bank()
        sc.op('pe', lambda e, p2=p2: e.matmul(p2[:], lhsT=self.bones[:], rhs=self.sq[:], start=True, stop=True), reads=['sq', 'bones'], writes=[p2n])
        sc.op('act', lambda e, p2=p2: e.activation(out=self.rstd[:], in_=p2[:], func=AF.Sqrt, scale=1.0 / 64, bias=self.epsb[:]),
              reads=[p2n, 'epsb'], writes=['rstd'])
        sc.op('dve', lambda e: e.reciprocal(out=self.rstd[:], in_=self.rstd[:]), reads=['rstd'], writes=['rstd'])
        sc.op('dve', lambda e, qs=qs, g=gain_ap, o=out_ap: e.scalar_tensor_tensor(out=o, in0=qs[:], scalar=g, in1=self.rstd[:], op0=ALU.mult, op1=ALU.mult),
              reads=['tmp32_2', 'rstd', 'again'], writes=tagw)

    def attn_mixer(self, li):
        sc = self.sc
        st = contextlib.ExitStack()
        L = 'A'
        NI = 24
        KT = self.sb('KT', [P, 2, S], BF16, st)
        VA = self.sb('VA', [P, 32, 4, 65], BF16, st)
        KI = self.sb('KI', [P, S], BF16, st)
        hT = self.sb('hTa', [P, DC, 512], BF16, st)
        wA = self.sb('wA', [P, DC * 1024], BF16, st)
        QT = self.sb('QT', [P, 8, 512], BF16, st)
        QI = self.sb('QI', [P, 4, 512], BF16, st)
        WI = self.sb('WI', [P, 4, 8], F32, st)
        score = self.sb('score', [P, S], F32, st)
        nmq = self.sb('nmq', [P, S], BF16, st)
        nmT = [self.sb('nmT%d' % i, [P, 32, P], BF16, st) for i in range(2)]
        pexp = [self.sb('pexp%d' % i, [P, 512], BF16, st) for i in range(3)]
        OTn = self.sb('OTn', [64, 16, 512], BF16, st)
        osb = self.sb('osb', [65, 512], F32, st)
        rinv = self.sq
        Rt = self.tmp32[0:2]
        biasS = self.sb('biasS', [P, 32, P], BF16, st)
        self.bones = self.sb('bones', [P, P], F32, st)
        identb = self.sb('identb', [P, P], BF16, st)
        cmask = self.sb('cmaskS', [P, P], F32, st)
        selrow = self.sb('selrowS', [65, 64], F32, st)
        again = self.sb('again', [P, 2], F32, st)
        b31 = self.sb('b31', [P, 16], F32, st)
        bs = [self.sb('bsm%d' % i, [P, 1], F32, st) for i in range(6)]
        sc.dma('sp', 'const', self.bones[:], self.d_bones[:, :], writes=['bones'])
        sc.dma('sp', 'const', cmask[:], self.d_cmask[:, :], writes=['cmask'])
        sc.dma('sp', 'const', selrow[:], self.d_selrow[:, :], writes=['selrow'])
        sc.dma('sp', 'const', again[:], self.d_gainT[:, :], writes=['again'])
        sc.dma('sp', 'const', b31[:], self.d_b31[:, :], writes=['b31'])
        sc.op('dve', lambda e: e.tensor_copy(out=identb[:], in_=self.ident[:]), reads=['ident'], writes=['identb'])
        sc.op('dve', lambda e: e.tensor_scalar(out=again[:, 0:1], in0=again[:, 0:1], scalar1=0.125, scalar2=None, op0=ALU.mult),
              reads=['again'], writes=['again'])
        sc.op('pool', lambda e: e.memset(VA[:, :, :, 64:65], 1.0), writes=[(L, 'VAones')])
        for half in range(4):
            sc.dma('sp', 'bstage', score[:, 0:1024].rearrange('p (a b) -> p a b', b=P), self.d_biasT[:, half * 8:(half + 1) * 8, :],
                   writes=[(L, 'bstage')])
            for i in range(8):
                kh = half * 8 + i
                h = kh % 16
                sc.op('dve', lambda e, i=i, kh=kh, h=h: e.tensor_scalar(out=biasS[:, kh, :], in0=score[:, i * P:(i + 1) * P], scalar1=b31[:, h:h + 1],
                                                                      scalar2=None, op0=ALU.subtract), reads=[(L, 'bstage'), 'b31'], writes=[(L, 'biasS')])
        wk = wA[:, 0:DC * 256].rearrange('p (c n) -> p c n', c=DC)
        wv = wA[:, DC * 256:DC * 512].rearrange('p (c n) -> p c n', c=DC)
        wki = wA[:, DC * 512:DC * 640].rearrange('p (c n) -> p c n', c=DC)
        for c in range(DC):
            sc.dma('pool', 'wA', wk[:, c, :], self.d_awk[c * P:(c + 1) * P, :], writes=[(L, 'wA')])
            sc.dma('pool', 'wA', wv[:, c, :], self.d_awv[c * P:(c + 1) * P, :], writes=[(L, 'wA')])
            sc.dma('pool', 'wA', wki[:, c, :], self.d_awki2[c * P:(c + 1) * P, :], writes=[(L, 'wA')])
        for blk in range(NBLK):
            name, t = self.load_xT(blk, False)
            self.norm_mod(li, 0, name, t, hT, (L, 'hT'), 0)
            hreads = [((L, 'hT'), c, 0) for c in range(DC)] + [(L, 'wA')]
            for m in range(2):
                pn, pt = self.bank()
                fns = [lambda e, pt=pt, k=k, m=m: e.matmul(pt[:], lhsT=wk[:, k, m * P:(m + 1) * P], rhs=hT[:, k, :], start=(k == 0), stop=(k == DC - 1))
                       for k in range(DC)]
                sc.op('pe', fns, reads=hreads, writes=[pn])
                self.head_norm(pn, pt, again[:, 1:2], KT[:, m, blk * 512:(blk + 1) * 512], [(L, 'KT', m, blk)])
            pn, pt = self.bank()
            fns = [lambda e, pt=pt, k=k: e.matmul(pt[:], lhsT=wki[:, k, :], rhs=hT[:, k, :], start=(k == 0), stop=(k == DC - 1)) for k in range(DC)]
            sc.op('pe', fns, reads=hreads, writes=[pn])
            sc.op('act', lambda e, pt=pt, blk=blk: e.activation(out=KI[:, blk * 512:(blk + 1) * 512], in_=pt[:], func=AF.Copy), reads=[pn], writes=[(L, 'KI', blk)])
            for tt in range(4):
                pn, pt = self.bank()
                fns = [lambda e, pt=pt, k=k, tt=tt: e.matmul(pt[:, 0:256], lhsT=hT[:, k, tt * P:(tt + 1) * P], rhs=wv[:, k, :], start=(k == 0), stop=(k == DC - 1))
                       for k in range(DC)]
                sc.op('pe', fns, reads=hreads, writes=[pn])
                sc.op('dve', lambda e, pt=pt, blk=blk, tt=tt: e.tensor_copy(out=VA[:, blk * 4 + tt, :, 0:64], in_=pt[:, 0:256].rearrange('p (a b) -> p a b', b=64)),
                      reads=[pn], writes=[(L, 'VA', blk * 4 + tt)])
        if self.cfg.get('astage', 9) < 1:
            sc.barrier(); st.close(); return
        wq = wA[:, :].rearrange('p (c n) -> p c n', c=DC)
        wqi = wA[:, 0:DC * 512].rearrange('p (c n) -> p c n', c=DC)
        wwi = wA[:, DC * 512:DC * 520].rearrange('p (c n) -> p c n', c=DC)
        wout = wA[0:64, 0:16 * 512].rearrange('p (h n) -> p h n', h=16)
        self.abank = [6, 7]
        self.nab = 0
        self.rot = [0, 1, 2, 3, 4, 5]
        nmn = 0
        npx = 0
        nrt = 0
        for blk in range(self.cfg.get('anblk', NBLK)):
            for c in range(DC):
                sc.dma('pool', 'wA', wq[:, c, :], self.d_awq[c * P:(c + 1) * P, :], writes=[(L, 'wA')])
            name, t = self.load_xT(blk, False)
            self.norm_mod(li, 0, name, t, hT, (L, 'hT'), 0)
            hreads = [((L, 'hT'), c, 0) for c in range(DC)]
            for m in range(8):
                pn, pt = self.bank()
                fns = [lambda e, pt=pt, k=k, m=m: e.matmul(pt[:], lhsT=wq[:, k, m * P:(m + 1) * P], rhs=hT[:, k, :], start=(k == 0), stop=(k == DC - 1))
                       for k in range(DC)]
                sc.op('pe', fns, reads=hreads + [(L, 'wA')], writes=[pn])
                self.head_norm(pn, pt, again[:, 0:1], QT[:, m, :], [(L, 'QT', m)])
            for c in range(DC):
                sc.dma('pool', 'wA', wqi[:, c, :], self.d_awqi[c * P:(c + 1) * P, :], writes=[(L, 'wA')])
                sc.dma('pool', 'wA', wwi[:, c, :], self.d_awwi[c * P:(c + 1) * P, :], writes=[(L, 'wA')])
            for m in range(4):
                pn, pt = self.bank()
                fns = [lambda e, pt=pt, k=k, m=m: e.matmul(pt[:], lhsT=wqi[:, k, m * P:(m + 1) * P], rhs=hT[:, k, :], start=(k == 0), stop=(k == DC - 1))
                       for k in range(DC)]
                sc.op('pe', fns, reads=hreads + [(L, 'wA')], writes=[pn])
                sc.op('act', lambda e, pt=pt, m=m: e.activation(out=QI[:, m, :], in_=pt[:], func=AF.Copy), reads=[pn], writes=[(L, 'QI', m)])
            pn, pt = self.bank()
            for qb in range(4):
                fns = [lambda e, pt=pt, k=k, qb=qb: e.matmul(pt[:, qb * 8:(qb + 1) * 8], lhsT=hT[:, k, qb * P:(qb + 1) * P], rhs=wwi[:, k, :],
                                                            start=(k == 0), stop=(k == DC - 1)) for k in range(DC)]
                sc.op('pe', fns, reads=hreads + [(L, 'wA')], writes=[pn])
            sc.op('dve', lambda e, pt=pt: e.tensor_scalar(out=WI[:, :, :], in0=pt[:, 0:32].rearrange('p (a b) -> p a b', b=8), scalar1=0.04419417382415922,
                                                        scalar2=None, op0=ALU.mult), reads=[pn], writes=[(L, 'WI')])
            for qb in range(4 if self.cfg.get('astage', 9) >= 2 else 0):
                gq = blk * 4 + qb
                nk = gq + 1
                W = nk * P
                for k0 in range(0, W, 512):
                    n = min(512, W - k0)
                    for ih in range(8):
                        b0 = (ih % 2) * 64
                        pn, pt = self.bank()
                        sc.op('pe', lambda e, pt=pt, ih=ih, b0=b0, qb=qb, k0=k0, n=n: e.matmul(
                            pt[:, 0:n], lhsT=QI[b0:b0 + 64, ih // 2, qb * P:(qb + 1) * P], rhs=KI[b0:b0 + 64, k0:k0 + n], start=True, stop=True),
                            reads=[(L, 'QI', ih // 2)] + [(L, 'KI', kb) for kb in range(k0 // 512, (k0 + n + 511) // 512)], writes=[pn])
                        rs = nrt % 2
                        nrt += 1
                        rn = 'tmp32_%d' % rs
                        sc.op('act', lambda e, pt=pt, rs=rs, n=n: e.activation(out=Rt[rs][:, 0:n], in_=pt[:, 0:n], func=AF.Relu), reads=[pn], writes=[rn])
                        if ih == 0:
                            sc.op('dve', lambda e, rs=rs, n=n, k0=k0, qb=qb: e.tensor_scalar(out=score[:, k0:k0 + n], in0=Rt[rs][:, 0:n], scalar1=WI[:, qb, 0:1],
                                                                                      scalar2=None, op0=ALU.mult), reads=[rn, (L, 'WI')], writes=[(L, 'score')])
                        else:
                            sc.op('dve', lambda e, rs=rs, n=n, k0=k0, qb=qb, ih=ih: e.scalar_tensor_tensor(
                                out=score[:, k0:k0 + n], in0=Rt[rs][:, 0:n], scalar=WI[:, qb, ih:ih + 1], in1=score[:, k0:k0 + n], op0=ALU.mult, op1=ALU.add),
                                reads=[rn, (L, 'WI'), (L, 'score')], writes=[(L, 'score')])
                SR = [(L, 'score'), (L, 'bis')]
                mx, lo, w0, mid, cnt, ff = bs
                if nk >= 3:
                    sc.op('dve', lambda e, W=W: e.tensor_reduce(out=mx[:], in_=score[:, 0:W], axis=AX.X, op=ALU.max), reads=SR, writes=[(L, 'bis')])
                    sc.op('dve', lambda e, W=W: e.tensor_reduce(out=lo[:], in_=score[:, 0:W], axis=AX.X, op=ALU.min), reads=SR, writes=[(L, 'bis')])
                    sc.op('dve', lambda e: e.tensor_tensor(out=w0[:], in0=mx[:], in1=lo[:], op=ALU.subtract), reads=SR, writes=[(L, 'bis')])
                else:
                    sc.op('dve', lambda e: e.memset(lo[:], -1e29), reads=SR, writes=[(L, 'bis')])
                sc.op('dve', lambda e, W=W: e.tensor_tensor(out=score[:, W - P:W], in0=score[:, W - P:W], in1=cmask[:], op=ALU.add),
                      reads=SR + ['cmask'], writes=SR)
                if nk >= 3:
                    for it in range(NI):
                        cst = 2.0 ** (-(it + 1))
                        sc.op('dve', lambda e, cst=cst: e.scalar_tensor_tensor(out=mid[:], in0=w0[:], scalar=cst, in1=lo[:], op0=ALU.mult, op1=ALU.add),
                              reads=SR, writes=[(L, 'bis')])
                        sc.op('dve', lambda e, W=W: e.tensor_scalar(out=nmq[:, 0:W], in0=score[:, 0:W], scalar1=mid[:, 0:1], scalar2=0.0, op0=ALU.is_ge,
                                                                   op1=ALU.add, accum_out=cnt[:, 0:1]), reads=SR + [(L, 'nmq')], writes=[(L, 'bis'), (L, 'nmq')])
                        sc.op('dve', lambda e, cst=cst: e.tensor_scalar(out=ff[:], in0=cnt[:], scalar1=256.0, scalar2=cst, op0=ALU.is_ge, op1=ALU.mult),
                              reads=SR, writes=[(L, 'bis')])
                        sc.op('dve', lambda e: e.scalar_tensor_tensor(out=lo[:], in0=ff[:], scalar=w0[:, 0:1], in1=lo[:], op0=ALU.mult, op1=ALU.add),
                              reads=SR, writes=[(L, 'bis')])
                sc.op('dve', lambda e, W=W: e.tensor_scalar(out=nmq[:, 0:W], in0=score[:, 0:W], scalar1=lo[:, 0:1], scalar2=-30000.0, op0=ALU.is_lt, op1=ALU.mult),
                      reads=SR + [(L, 'nmq')], writes=[(L, 'nmq')])
                if self.cfg.get('astage', 9) < 3:
                    continue
                ms = nmn % 2
                nmn += 1
                mn_ = (L, 'nmT', ms)
                for j0 in range(0, nk, 8):
                    jn = min(8, nk - j0)
                    pn, pt = self.bank()
                    ptb = pt[:].bitcast(BF16)
                    fns = [lambda e, ptb=ptb, j=j, j0=j0: e.transpose(out=ptb[:, (j - j0) * P:(j - j0 + 1) * P], in_=nmq[:, j * P:(j + 1) * P], identity=identb[:])
                           for j in range(j0, j0 + jn)]
                    sc.op('pe', fns, reads=[(L, 'nmq'), 'identb'], writes=[pn])
                    sc.op('act', lambda e, ptb=ptb, ms=ms, j0=j0, jn=jn: e.activation(out=nmT[ms][:, j0:j0 + jn, :], in_=ptb[:, 0:jn * P].rearrange('p (a b) -> p a b', b=P),
                                                                                func=AF.Copy), reads=[pn], writes=[(mn_, j0)])
                for kvh in range(4 if self.cfg.get('astage', 9) >= 4 else 0):
                    ab = self.abank[self.nab % 2]
                    self.nab += 1
                    pon = ('ps', ab)
                    po = self.ps[ab]
                    b0 = (kvh % 2) * 64
                    for j in range(nk):
                        pn, pt = self.bank()
                        near = (nk - 1 - j) if (nk - 1 - j) < 2 else None
                        fns = []
                        for g in range(4):
                            h = kvh * 4 + g
                            fns.append(lambda e, pt=pt, g=g, j=j, b0=b0, kvh=kvh, qb=qb: e.matmul(
                                pt[:, g * P:(g + 1) * P], lhsT=KT[b0:b0 + 64, kvh // 2, j * P:(j + 1) * P],
                                rhs=QT[b0:b0 + 64, (kvh // 2) * 4 + g, qb * P:(qb + 1) * P], start=True, stop=False))
                            if near is not None:
                                fns.append(lambda e, pt=pt, g=g, h=h, near=near: e.matmul(
                                    pt[:, g * P:(g + 1) * P], lhsT=identb[:], rhs=biasS[:, near * 16 + h, :], start=False, stop=False))
                            fns.append(lambda e, pt=pt, g=g, j=j, ms=ms: e.matmul(
                                pt[:, g * P:(g + 1) * P], lhsT=identb[:], rhs=nmT[ms][:, j, :], start=False, stop=True))
                        sc.op('pe', fns, reads=[(L, 'KT', kvh // 2, j // 4), (mn_, (j // 8) * 8), 'identb', (L, 'biasS')] +
                              [(L, 'QT', (kvh // 2) * 4 + g) for g in range(4)], writes=[pn])
                        px = npx % 3
                        npx += 1
                        pxn = 'pexp%d' % px
                        sc.op('act', lambda e, pt=pt, px=px: e.activation(out=pexp[px][:], in_=pt[:], func=AF.Exp), reads=[pn], writes=[pxn])
                        sc.op('pe', lambda e, po=po, px=px, j=j, kvh=kvh, nk=nk: e.matmul(po[0:65, :], lhsT=VA[:, j, kvh, :], rhs=pexp[px][:],
                                                                                     start=(j == 0), stop=(j == nk - 1)),
                              reads=[pxn, (L, 'VA', j), (L, 'VAones')], writes=[pon])
                    sc.op('act', lambda e, po=po: e.activation(out=osb[:], in_=po[0:65, :], func=AF.Copy), reads=[pon], writes=['osb'])
                    pn, pt = self.bank()
                    sc.op('pe', lambda e, pt=pt: e.matmul(pt[0:64, :], lhsT=selrow[:], rhs=osb[:], start=True, stop=True), reads=['osb', 'selrow'], writes=[pn])
                    sc.op('dve', lambda e, pt=pt: e.reciprocal(out=rinv[0:64, :], in_=pt[0:64, :]), reads=[pn], writes=['sq'])
                    sc.op('dve', lambda e, kvh=kvh, qb=qb: e.tensor_tensor(out=OTn[:, kvh * 4:(kvh + 1) * 4, qb * P:(qb + 1) * P],
                                                                       in0=osb[0:64, :].rearrange('p (a b) -> p a b', b=P),
                                                                       in1=rinv[0:64, :].rearrange('p (a b) -> p a b', b=P), op=ALU.mult),
                          reads=['osb', 'sq'], writes=[(L, 'OTn', kvh, qb)])
            if self.cfg.get('astage', 9) < 5:
                continue
            oreads = [(L, 'OTn', kvh, qb) for kvh in range(4) for qb in range(4)] + [(L, 'wA')]
            for c in range(DC):
                if c % 4 == 0:
                    for h in range(16):
                        sc.dma('pool', 'wA', wout[:, h, :], self.d_awout[:, h, (c // 4) * 512:(c // 4 + 1) * 512], writes=[(L, 'wA')])
                pn, pt = self.bank()
                fns = [lambda e, pt=pt, h=h, c=c: e.matmul(pt[:], lhsT=wout[:, h, (c % 4) * P:(c % 4 + 1) * P], rhs=OTn[:, h, :], start=(h == 0), stop=(h == 15))
                       for h in range(16)]
                sc.op('pe', fns, reads=oreads, writes=[pn])
                sc.op('dve', lambda e, pt=pt, c=c, t=t: e.scalar_tensor_tensor(out=t[:, c, :], in0=pt[:], scalar=self.modv(li, 2, c), in1=t[:, c, :],
                                                                              op0=ALU.mult, op1=ALU.add), reads=[pn, (name, c), ('modT', li)], writes=[(name, c)])
                self.store_xT(blk, name, t, c)
        self.rot = list(range(8))
        sc.barrier()
        st.close()

    def ssm_setup(self):
        self.d_lamT = self.inp('ssm_lamT', [P, 32, 2])
        self.d_lstepT = self.inp('ssm_lstepT', [P, 32])
        self.d_Bblk = self.inp('ssm_Bblk', [32, P, 256])
        self.d_Cblk = self.inp('ssm_Cblk', [32, P, 256])
        self.d_dT = self.inp('ssm_dT', [P, DC])
        self.d_glu = self.inp('ssm_glu_r', [DC, P, DC * 256])

    def ssm_mixer(self, li):
        import math
        sc = self.sc
        st = contextlib.ExitStack()
        L = 'S'
        LC = 128
        I32 = mybir.dt.int32
        Ec = self.sb('Ec', [P, 32, LC], F32, st)
        En = self.sb('En', [P, 32, LC], F32, st)
        Bb = self.sb('Bb', [P, 32, 256], BF16, st)
        Cb = self.sb('Cb', [P, 32, 256], BF16, st)
        sm = {n: self.sb('ss_' + n, [P, 32], F32, st) for n in
              ['lr', 'li', 'stp', 'th', 'rr', 'u', 'f', 'g', 'sn', 'cs', 'x', 'y', 'den', 'cr', 'ci', 'cL', 'nL', 'ire', 'iim', 'e1c', 'e1n', 't1', 't2']}
        ni = self.sb('ss_ni', [P, 32], I32, st)
        lam = self.sb('ss_lam', [P, 32, 2], F32, st)
        dT = self.sb('ss_dT', [P, DC], F32, st)
        cs1 = self.sb('ss_c1', [P, 4], F32, st)
        SM = [(L, 'sm')]

        def dv(fn, extra_r=(), extra_w=()):
            sc.op('dve', fn, reads=SM + list(extra_r), writes=SM + list(extra_w))

        def ac(fn):
            sc.op('act', fn, reads=SM, writes=SM)
        sc.dma('sp', 'const', lam[:], self.d_lamT[:, :, :], writes=SM)
        sc.dma('sp', 'const', sm['stp'][:], self.d_lstepT[:, :], writes=SM)
        sc.dma('sp', 'const', dT[:], self.d_dT[:, :], writes=[(L, 'dT')])
        for k in range(32):
            sc.dma('pool', 'Bb', Bb[:, k, :], self.d_Bblk[k, :, :], writes=[(L, 'Bb')])
        dv(lambda e: e.tensor_scalar(out=sm['lr'][:], in0=lam[:, :, 0], scalar1=-1e-4, scalar2=None, op0=ALU.min))
        dv(lambda e: e.tensor_copy(out=sm['li'][:], in_=lam[:, :, 1]))
        ac(lambda e: e.activation(out=sm['stp'][:], in_=sm['stp'][:], func=AF.Exp))
        dv(lambda e: e.tensor_tensor(out=sm['th'][:], in0=sm['li'][:], in1=sm['stp'][:], op=ALU.mult))
        dv(lambda e: e.tensor_tensor(out=sm['rr'][:], in0=sm['lr'][:], in1=sm['stp'][:], op=ALU.mult))
        ac(lambda e: e.activation(out=sm['rr'][:], in_=sm['rr'][:], func=AF.Exp))

        def sincos(dst, off):
            dv(lambda e: e.tensor_scalar(out=sm['u'][:], in0=sm['th'][:], scalar1=1.0 / (2 * math.pi), scalar2=off, op0=ALU.mult, op1=ALU.add))
            dv(lambda e: e.tensor_copy(out=ni[:], in_=sm['u'][:]))
            dv(lambda e: e.tensor_copy(out=sm['f'][:], in_=ni[:]))
            dv(lambda e: e.tensor_tensor(out=sm['f'][:], in0=sm['u'][:], in1=sm['f'][:], op=ALU.subtract))
            dv(lambda e: e.tensor_scalar(out=sm['g'][:], in0=sm['f'][:], scalar1=0.5, scalar2=None, op0=ALU.is_gt))
            dv(lambda e: e.tensor_tensor(out=sm['f'][:], in0=sm['f'][:], in1=sm['g'][:], op=ALU.subtract))
            dv(lambda e: e.tensor_scalar(out=sm['g'][:], in0=sm['f'][:], scalar1=-0.5, scalar2=None, op0=ALU.is_lt))
            dv(lambda e: e.tensor_tensor(out=sm['f'][:], in0=sm['f'][:], in1=sm['g'][:], op=ALU.add))
            ac(lambda e, dst=dst: e.activation(out=sm[dst][:], in_=sm['f'][:], func=AF.Sin, scale=-2 * math.pi))
        sincos('sn', 64.5)
        sincos('cs', 64.75)
        dv(lambda e: e.tensor_tensor(out=sm['x'][:], in0=sm['rr'][:], in1=sm['cs'][:], op=ALU.mult))
        dv(lambda e: e.tensor_scalar(out=sm['x'][:], in0=sm['x'][:], scalar1=-1.0, scalar2=None, op0=ALU.add))
        dv(lambda e: e.tensor_tensor(out=sm['y'][:], in0=sm['rr'][:], in1=sm['sn'][:], op=ALU.mult))
        dv(lambda e: e.tensor_tensor(out=sm['den'][:], in0=sm['lr'][:], in1=sm['lr'][:], op=ALU.mult))
        dv(lambda e: e.tensor_tensor(out=sm['t1'][:], in0=sm['li'][:], in1=sm['li'][:], op=ALU.mult))
        dv(lambda e: e.tensor_tensor(out=sm['den'][:], in0=sm['den'][:], in1=sm['t1'][:], op=ALU.add))
        dv(lambda e: e.reciprocal(out=sm['den'][:], in_=sm['den'][:]))
        dv(lambda e: e.tensor_tensor(out=sm['cr'][:], in0=sm['x'][:], in1=sm['lr'][:], op=ALU.mult))
        dv(lambda e: e.tensor_tensor(out=sm['t1'][:], in0=sm['y'][:], in1=sm['li'][:], op=ALU.mult))
        dv(lambda e: e.tensor_tensor(out=sm['cr'][:], in0=sm['cr'][:], in1=sm['t1'][:], op=ALU.add))
        dv(lambda e: e.tensor_tensor(out=sm['cr'][:], in0=sm['cr'][:], in1=sm['den'][:], op=ALU.mult))
        dv(lambda e: e.tensor_tensor(out=sm['ci'][:], in0=sm['y'][:], in1=sm['lr'][:], op=ALU.mult))
        dv(lambda e: e.tensor_tensor(out=sm['t1'][:], in0=sm['x'][:], in1=sm['li'][:], op=ALU.mult))
        dv(lambda e: e.tensor_tensor(out=sm['ci'][:], in0=sm['ci'][:], in1=sm['t1'][:], op=ALU.subtract))
        dv(lambda e: e.tensor_tensor(out=sm['ci'][:], in0=sm['ci'][:], in1=sm['den'][:], op=ALU.mult))
        st2 = contextlib.ExitStack()
        Tc = self.sb('Tc', [P, LC, 32], F32, st2)
        Tn = self.sb('Tn', [P, LC, 32], F32, st2)
        U1 = self.sb('U1', [P, LC // 2, 32], F32, st2)
        U2 = self.sb('U2', [P, LC // 2, 32], F32, st2)
        cst1 = self.sb('tp1s', [P, 512], F32, st2)
        cst2 = self.sb('tp2s', [P, 512], F32, st2)
        dv(lambda e: e.tensor_copy(out=sm['e1c'][:], in_=sm['cs'][:]))
        dv(lambda e: e.tensor_scalar(out=sm['e1n'][:], in0=sm['sn'][:], scalar1=-1.0, scalar2=None, op0=ALU.mult))
        dv(lambda e: e.memset(Tc[:, 0, :], 1.0), extra_w=[(L, 'T')])
        dv(lambda e: e.memset(Tn[:, 0, :], 0.0), extra_w=[(L, 'T')])
        TT = [(L, 'T')]
        n = 1
        while n < LC:
            ecb = sm['e1c'][:, :].unsqueeze(1).to_broadcast([P, n, 32])
            enb = sm['e1n'][:, :].unsqueeze(1).to_broadcast([P, n, 32])
            dv(lambda e, n=n, ecb=ecb: e.tensor_tensor(out=Tc[:, n:2 * n, :], in0=Tc[:, 0:n, :], in1=ecb, op=ALU.mult), TT, TT)
            dv(lambda e, n=n, enb=enb: e.tensor_tensor(out=U1[:, 0:n, :], in0=Tn[:, 0:n, :], in1=enb, op=ALU.mult), TT, TT)
            dv(lambda e, n=n: e.tensor_tensor(out=Tc[:, n:2 * n, :], in0=Tc[:, n:2 * n, :], in1=U1[:, 0:n, :], op=ALU.subtract), TT, TT)
            dv(lambda e, n=n, enb=enb: e.tensor_tensor(out=Tn[:, n:2 * n, :], in0=Tc[:, 0:n, :], in1=enb, op=ALU.mult), TT, TT)
            dv(lambda e, n=n, ecb=ecb: e.tensor_tensor(out=U2[:, 0:n, :], in0=Tn[:, 0:n, :], in1=ecb, op=ALU.mult), TT, TT)
            dv(lambda e, n=n: e.tensor_tensor(out=Tn[:, n:2 * n, :], in0=Tn[:, n:2 * n, :], in1=U2[:, 0:n, :], op=ALU.add), TT, TT)
            dv(lambda e: e.tensor_tensor(out=sm['t1'][:], in0=sm['e1c'][:], in1=sm['e1c'][:], op=ALU.mult))
            dv(lambda e: e.tensor_tensor(out=sm['t2'][:], in0=sm['e1n'][:], in1=sm['e1n'][:], op=ALU.mult))
            dv(lambda e: e.tensor_tensor(out=sm['e1n'][:], in0=sm['e1c'][:], in1=sm['e1n'][:], op=ALU.mult))
            dv(lambda e: e.tensor_scalar(out=sm['e1n'][:], in0=sm['e1n'][:], scalar1=2.0, scalar2=None, op0=ALU.mult))
            dv(lambda e: e.tensor_tensor(out=sm['e1c'][:], in0=sm['t1'][:], in1=sm['t2'][:], op=ALU.subtract))
            n *= 2
        dv(lambda e: e.tensor_copy(out=sm['cL'][:], in_=sm['e1c'][:]))
        dv(lambda e: e.tensor_copy(out=sm['nL'][:], in_=sm['e1n'][:]))
        dv(lambda e: e.memset(sm['ire'][:], 0.0))
        dv(lambda e: e.memset(sm['iim'][:], 0.0))
        dv(lambda e: e.tensor_copy(out=Ec[:, :, :], in_=Tc[:, :, :].rearrange('p t k -> p k t')), TT, [(L, 'E')])
        dv(lambda e: e.tensor_copy(out=En[:, :, :], in_=Tn[:, :, :].rearrange('p t k -> p k t')), TT, [(L, 'E')])
        for k in range(32):
            sc.dma('sp', 'cstage', cst1[:, 0:256], self.d_Cblk[k, :, :], writes=['cst1'])
            crk = sm['cr'][:, k:k + 1]
            cik = sm['ci'][:, k:k + 1]
            sc.op('dve', lambda e, cik=cik: e.tensor_scalar(out=cst2[:, 0:128], in0=cst1[:, 128:256], scalar1=cik, scalar2=None, op0=ALU.mult),
                  reads=['cst1'] + SM, writes=['cst2'])
            sc.op('dve', lambda e, k=k, crk=crk: e.scalar_tensor_tensor(out=Cb[:, k, 0:128], in0=cst1[:, 0:128], scalar=crk, in1=cst2[:, 0:128], op0=ALU.mult, op1=ALU.subtract),
                  reads=['cst1', 'cst2'] + SM, writes=[(L, 'Cb')])
            sc.op('dve', lambda e, crk=crk: e.tensor_scalar(out=cst2[:, 128:256], in0=cst1[:, 128:256], scalar1=crk, scalar2=None, op0=ALU.mult),
                  reads=['cst1'] + SM, writes=['cst2'])
            sc.op('dve', lambda e, k=k, cik=cik: e.scalar_tensor_tensor(out=Cb[:, k, 128:256], in0=cst1[:, 0:128], scalar=cik, in1=cst2[:, 128:256], op0=ALU.mult, op1=ALU.add),
                  reads=['cst1', 'cst2'] + SM, writes=[(L, 'Cb')])
        if self.cfg.get('sdebug'):
            dbg = self.nc.dram_tensor('dbg', [P, 7 * 32 + 512 + 512], F32, kind="ExternalOutput").ap()
            for i, nme in enumerate(['sn', 'cs', 'rr', 'cr', 'ci', 'cL', 'nL']):
                sc.dma('sp', 'const', dbg[:, i * 32:(i + 1) * 32], sm[nme][:], reads=SM, writes=[('dbg', i)])
            sc.dma('sp', 'const', dbg[:, 224:224 + 256], Ec[:, 0:2, :], reads=[(L, 'E')], writes=[('dbg', 10)])
            sc.dma('sp', 'const', dbg[:, 224 + 256:224 + 512], En[:, 0:2, :], reads=[(L, 'E')], writes=[('dbg', 11)])
            sc.dma('sp', 'const', dbg[:, 224 + 512:224 + 768], Cb[:, 0, :], reads=[(L, 'Cb')], writes=[('dbg', 12)], allow_dtype=True) if False else None
        sc.barrier()
        st2.close()
        hT = self.sb('hTs', [P, DC, 512], BF16, st)
        h32 = self.sb('h32s', [P, DC, 512], F32, st)
        GT = self.sb('GTs', [P, DC, 512], BF16, st)
        Sb = self.sb('Sb', [P, 4, 2, 512], BF16, st)
        wgl = [self.sb('wgl%d' % i, [P, DC * 256], BF16, st) for i in range(2)]
        bre = self.sb('bre', [P, 512], F32, st)
        bim = self.sb('bim', [P, 512], F32, st)
        wre = self.sb('wre', [P, 512], F32, st)
        wim = self.sb('wim', [P, 512], F32, st)
        tq = self.sb('tq', [P, 512], F32, st)
        tp1 = self.sb('tp1', [P, 512], F32, st)
        tp2 = self.sb('tp2', [P, 512], F32, st)
        nw = 0
        for blk in range(self.cfg.get('snblk', NBLK)):
            name, t = self.load_xT(blk, False)
            self.norm_mod(li, 0, name, t, hT, (L, 'hT'), 0, h32=h32)
            for j in range(DC):
                for kk in range(4):
                    k = 4 * j + kk
                    pxr_n, pxr = self.bank()
                    pxi_n, pxi = self.bank()
                    hr = [((L, 'hT'), j, 0), (L, 'Bb')]
                    sc.op('pe', lambda e, pxr=pxr, k=k, j=j: e.matmul(pxr[:], lhsT=Bb[:, k, 0:128], rhs=hT[:, j, :], start=True, stop=True), reads=hr, writes=[pxr_n])
                    sc.op('pe', lambda e, pxi=pxi, k=k, j=j: e.matmul(pxi[:], lhsT=Bb[:, k, 128:256], rhs=hT[:, j, :], start=True, stop=True), reads=hr, writes=[pxi_n])
                    cb = Ec[:, k, :].unsqueeze(1).to_broadcast([P, 4, LC])
                    nb = En[:, k, :].unsqueeze(1).to_broadcast([P, 4, LC])

                    def v3(ap):
                        return ap.rearrange('p (a b) -> p a b', b=LC)
                    ER = [(L, 'E')]
                    sc.op('dve', lambda e, pxr=pxr, cb=cb: e.tensor_tensor(out=v3(bre[:, :]), in0=v3(pxr[:, :]), in1=cb, op=ALU.mult), reads=[pxr_n] + ER, writes=['bre'])
                    sc.op('dve', lambda e, pxi=pxi, nb=nb: e.tensor_tensor(out=v3(tq[:, :]), in0=v3(pxi[:, :]), in1=nb, op=ALU.mult), reads=[pxi_n] + ER, writes=['tq'])
                    sc.op('dve', lambda e: e.tensor_tensor(out=bre[:], in0=bre[:], in1=tq[:], op=ALU.subtract), reads=['bre', 'tq'], writes=['bre'])
                    sc.op('dve', lambda e, pxi=pxi, cb=cb: e.tensor_tensor(out=v3(bim[:, :]), in0=v3(pxi[:, :]), in1=cb, op=ALU.mult), reads=[pxi_n] + ER, writes=['bim'])
                    sc.op('dve', lambda e, pxr=pxr, nb=nb: e.tensor_tensor(out=v3(tq[:, :]), in0=v3(pxr[:, :]), in1=nb, op=ALU.mult), reads=[pxr_n] + ER, writes=['tq'])
                    sc.op('dve', lambda e: e.tensor_tensor(out=bim[:], in0=bim[:], in1=tq[:], op=ALU.add), reads=['bim', 'tq'], writes=['bim'])
                    rb = sm['rr'][:, k:k + 1].to_broadcast([P, LC])
                    cLk = sm['cL'][:, k:k + 1]
                    nLk = sm['nL'][:, k:k + 1]
                    irek = sm['ire'][:, k:k + 1]
                    iimk = sm['iim'][:, k:k + 1]
                    CR = [(L, 'carry', k)]
                    for ch in range(4):
                        lo_, hi_ = ch * LC, (ch + 1) * LC
                        sc.op('dve', lambda e, rb=rb, irek=irek, lo_=lo_, hi_=hi_: e.tensor_tensor_scan(out=wre[:, lo_:hi_], data0=rb, data1=bre[:, lo_:hi_], initial=irek,
                                                                                                 op0=ALU.mult, op1=ALU.add), reads=['bre'] + CR + SM, writes=['wre'])
                        sc.op('dve', lambda e, rb=rb, iimk=iimk, lo_=lo_, hi_=hi_: e.tensor_tensor_scan(out=wim[:, lo_:hi_], data0=rb, data1=bim[:, lo_:hi_], initial=iimk,
                                                                                                 op0=ALU.mult, op1=ALU.add), reads=['bim'] + CR + SM, writes=['wim'])
                        wrl = wre[:, hi_ - 1:hi_]
                        wil = wim[:, hi_ - 1:hi_]
                        sc.op('dve', lambda e, nLk=nLk, wil=wil: e.tensor_tensor(out=cs1[:, 0:1], in0=wil, in1=nLk, op=ALU.mult), reads=['wim'] + SM, writes=['cs1'])
                        sc.op('dve', lambda e, cLk=cLk, wrl=wrl, irek=irek: e.scalar_tensor_tensor(out=irek, in0=wrl, scalar=cLk, in1=cs1[:, 0:1], op0=ALU.mult, op1=ALU.add),
                              reads=['wre', 'cs1'] + SM, writes=CR)
                        sc.op('dve', lambda e, nLk=nLk, wrl=wrl: e.tensor_tensor(out=cs1[:, 1:2], in0=wrl, in1=nLk, op=ALU.mult), reads=['wre'] + SM, writes=['cs1'])
                        sc.op('dve', lambda e, cLk=cLk, wil=wil, iimk=iimk: e.scalar_tensor_tensor(out=iimk, in0=wil, scalar=cLk, in1=cs1[:, 1:2], op0=ALU.mult, op1=ALU.subtract),
                              reads=['wim', 'cs1'] + SM, writes=CR)
                    if self.cfg.get('sdebug') and blk == 0 and k == 1:
                        dbg2 = self.nc.dram_tensor('dbg2', [P, 4, 512], F32, kind="ExternalOutput").ap()
                        sc.dma('sp', 'const', dbg2[:, 0, :], bre[:], reads=['bre'], writes=[('dbg2', 0)])
                        sc.dma('sp', 'const', dbg2[:, 1, :], bim[:], reads=['bim'], writes=[('dbg2', 1)])
                        sc.dma('sp', 'const', dbg2[:, 2, :], wre[:], reads=['wre'], writes=[('dbg2', 2)])
                        sc.dma('sp', 'const', dbg2[:, 3, :], wim[:], reads=['wim'], writes=[('dbg2', 3)])
                    sc.op('pool', lambda e, cb=cb: e.tensor_tensor(out=v3(tp1[:, :]), in0=v3(wre[:, :]), in1=cb, op=ALU.mult), reads=['wre'] + ER, writes=['tp1'])
                    sc.op('pool', lambda e, nb=nb: e.tensor_tensor(out=v3(tp2[:, :]), in0=v3(wim[:, :]), in1=nb, op=ALU.mult), reads=['wim'] + ER, writes=['tp2'])
                    sc.op('pool', lambda e, kk=kk: e.tensor_tensor(out=Sb[:, kk, 0, :], in0=tp1[:], in1=tp2[:], op=ALU.add), reads=['tp1', 'tp2'], writes=[(L, 'Sb', kk)])
                    sc.op('pool', lambda e, nb=nb: e.tensor_tensor(out=v3(tp1[:, :]), in0=v3(wre[:, :]), in1=nb, op=ALU.mult), reads=['wre'] + ER, writes=['tp1'])
                    sc.op('pool', lambda e, cb=cb: e.tensor_tensor(out=v3(tp2[:, :]), in0=v3(wim[:, :]), in1=cb, op=ALU.mult), reads=['wim'] + ER, writes=['tp2'])
                    sc.op('pool', lambda e, kk=kk: e.tensor_tensor(out=Sb[:, kk, 1, :], in0=tp1[:], in1=tp2[:], op=ALU.subtract), reads=['tp1', 'tp2'], writes=[(L, 'Sb', kk)])
                if self.cfg.get('sdebug') and blk == 0 and j == 0:
                    dbg3 = self.nc.dram_tensor('dbg3', [P, 2, 512], F32, kind="ExternalOutput").ap()
                    sc.dma('pool', 'dbg3', dbg3[:, 0, :], Sb[:, 1, 0, :], reads=[(L, 'Sb', 1)], writes=[('dbg3', 0)])
                    sc.dma('pool', 'dbg3', dbg3[:, 1, :], Sb[:, 1, 1, :], reads=[(L, 'Sb', 1)], writes=[('dbg3', 1)])
                pyn, py = self.bank()
                fns = []
                for kk in range(4):
                    k = 4 * j + kk
                    fns.append(lambda e, py=py, k=k, kk=kk: e.matmul(py[:], lhsT=Cb[:, k, 0:128], rhs=Sb[:, kk, 0, :], start=(kk == 0), stop=False))
                    fns.append(lambda e, py=py, k=k, kk=kk: e.matmul(py[:], lhsT=Cb[:, k, 128:256], rhs=Sb[:, kk, 1, :], start=False, stop=(kk == 3)))
                sc.op('pe', fns, reads=[(L, 'Sb', kk) for kk in range(4)] + [(L, 'Cb')], writes=[pyn])
                if self.cfg.get('sdebug') == 2 and blk == 0 and j == 0:
                    sc.op('act', lambda e, py=py: e.activation(out=bre[:], in_=py[:], func=AF.Copy), reads=[pyn, 'bre'], writes=['bre'])
                    sc.dma('sp', 'const', dbg2[:, 4, :], bre[:], reads=['bre'], writes=[('dbg2', 4)])
                z = self.tmp32[0]
                w_ = self.tmp32[1]
                sc.op('dve', lambda e, py=py, j=j, z=z: e.scalar_tensor_tensor(out=z[:], in0=h32[:, j, :], scalar=dT[:, j:j + 1], in1=py[:], op0=ALU.mult, op1=ALU.add),
                      reads=[pyn, ('h32', j), (L, 'dT')], writes=['tmp32_0'])
                if self.cfg.get('sdebug') and blk == 0 and j == 0:
                    dbg4 = self.nc.dram_tensor('dbg4', [P, 3, 512], F32, kind="ExternalOutput").ap()
                    sc.dma('sp', 'const', dbg4[:, 0, :], z[:], reads=['tmp32_0'], writes=[('dbg4', 0)])
                    sc.dma('pool', 'dbg4b', dbg4[:, 1, 0:256], Cb[:, 1, :], reads=[(L, 'Cb')], writes=[('dbg4', 1)])
                    sc.dma('pool', 'dbg4b', dbg4[:, 1, 256:512], Cb[:, 1, :], reads=[(L, 'Cb')], writes=[('dbg4', 3)])
                sc.op('act', lambda e, z=z, w_=w_: e.activation(out=w_[:], in_=z[:], func=AF.Square), reads=['tmp32_0'], writes=['tmp32_1'])
                sc.op('dve', lambda e, w_=w_: e.tensor_scalar(out=w_[:], in0=w_[:], scalar1=0.044715, scalar2=1.0, op0=ALU.mult, op1=ALU.add), reads=['tmp32_1'], writes=['tmp32_1'])
                sc.op('dve', lambda e, z=z, w_=w_: e.tensor_tensor(out=w_[:], in0=w_[:], in1=z[:], op=ALU.mult), reads=['tmp32_0', 'tmp32_1'], writes=['tmp32_1'])
                sc.op('act', lambda e, w_=w_: e.activation(out=w_[:], in_=w_[:], func=AF.Sigmoid, scale=1.5957691216057308), reads=['tmp32_1'], writes=['tmp32_1'])
                sc.op('dve', lambda e, z=z, w_=w_, j=j: e.tensor_tensor(out=GT[:, j, :], in0=z[:], in1=w_[:], op=ALU.mult), reads=['tmp32_0', 'tmp32_1'], writes=[(L, 'GT', j)])
            if self.cfg.get('sdebug') and blk == 0:
                sc.dma('pool', 'dbg4b', dbg4[:, 2, :], GT[:, 0, :], reads=[(L, 'GT', 0)], writes=[('dbg4', 2)])
            greads = [(L, 'GT', j) for j in range(DC)]
            for c in range(DC):
                slot = nw % 2
                nw += 1
                wn = (L, 'wgl', slot)
                self.load_w_cast('wgl%d' % slot, wgl[slot][:, :], self.d_glu[c, :, :], None, writes=[wn])
                pln, pl = self.bank()
                pgn, pg = self.bank()
                for (pn, pt, off) in ((pln, pl, 0), (pgn, pg, 128)):
                    fns = [lambda e, pt=pt, kq=kq, off=off, slot=slot: e.matmul(pt[:], lhsT=wgl[slot][:, kq * 256 + off: kq * 256 + off + 128], rhs=GT[:, kq, :],
                                                                              start=(kq == 0), stop=(kq == DC - 1)) for kq in range(DC)]
                    sc.op('pe', fns, reads=greads + [wn], writes=[pn])
                tm = self.tmp32[2]
                sc.op('act', lambda e, pg=pg, tm=tm: e.activation(out=tm[:], in_=pg[:], func=AF.Sigmoid), reads=[pgn], writes=['tmp32_2'])
                sc.op('dve', lambda e, pl=pl, tm=tm: e.tensor_tensor(out=tm[:], in0=pl[:], in1=tm[:], op=ALU.mult), reads=[pln, 'tmp32_2'], writes=['tmp32_2'])
                sc.op('dve', lambda e, tm=tm, c=c, t=t: e.scalar_tensor_tensor(out=t[:, c, :], in0=tm[:], scalar=self.modv(li, 2, c), in1=t[:, c, :], op0=ALU.mult, op1=ALU.add),
                      reads=['tmp32_2', (name, c), ('modT', li)], writes=[(name, c)])
                self.store_xT(blk, name, t, c)
        sc.barrier()
        st.close()

    def epilogue(self):
        sc = self.sc
        self.out = self.nc.dram_tensor('out', [self.ntok, D], F32, kind="ExternalOutput").ap()
        otok = [self.sb('otok%d' % i, [P, D], F32) for i in range(2)]
        n = 0
        for blk in range(self.ntok // 512):
            name, t = self.load_xT(blk, False)
            for j in range(4):
                slot = n % 2
                n += 1
                on = 'otok%d' % slot
                for hh in range(2):
                    pname, pt = self.bank()
                    fns = [lambda e, pt=pt, j=j, c=c, hh=hh, t=t: e.transpose(out=pt[:, (c - hh * 4) * P:(c - hh * 4 + 1) * P],
                                                                      in_=t[:, c, j * P:(j + 1) * P], identity=self.ident[:])
                           for c in range(hh * 4, hh * 4 + 4)]
                    sc.op('pe', fns, reads=[(name, c) for c in range(DC)] + ['ident'], writes=[pname])
                    if hh == 0:
                        sc.op('act', lambda e, pt=pt, slot=slot: e.activation(out=otok[slot][:, 0:512], in_=pt[:], func=AF.Copy),
                              reads=[pname], writes=[(on, 0)])
                    else:
                        sc.op('dve', lambda e, pt=pt, slot=slot: e.tensor_copy(out=otok[slot][:, 512:1024], in_=pt[:]),
                              reads=[pname], writes=[(on, 1)])
                sc.dma('sp', 'st_' + on, self.out[blk * 512 + j * P: blk * 512 + (j + 1) * P, :], otok[slot][:],
                       reads=[(on, 0), (on, 1)], writes=[('out', blk, j)])

    def build(self):
        cfg = self.cfg
        self.setup_common()
        self.epsb = self.sb('epsb', [P, 1], F32)
        self.sc.op('dve', lambda e: e.memset(self.epsb[:], EPS), writes=['epsb'])
        self.build_mod()
        self.alloc_stream()
        self.conv_setup()
        self.ffn_setup()
        self.attn_setup()
        self.ssm_setup()
        steps = cfg.get('steps', ['m0', 'f0', 'm1', 'f1', 'm2', 'f2', 'm3', 'f3'])
        first = True
        if steps[0][0] != 'm' or int(steps[0][1]) % 3 != 0:
            st = contextlib.ExitStack()
            self.xtok = self.sb('xtok', [P, 4, D], F32, st)
            for blk in range(NBLK):
                name, t = self.load_xT(blk, True)
                for c in range(DC):
                    self.store_xT(blk, name, t, c)
            self.sc.barrier()
            st.close()
            first = False
        tail_split = cfg.get('tail_split', False)
        for s in steps:
            li = int(s[1])
            if tail_split and s == 'm3':
                self.sc.barrier()
                self.xs_d = self.nc.dram_tensor('xs_scratch', [D, S // 2 + 512], F32).ap()
                self.sc.dma('sp', 'xstage', self.xs_d[:, 512:512 + S // 2], (lambda: self.xT_d[:, bass.ds(self.rv * 2048, 2048)]), writes=['xs'])
                self.sc.dma('sp', 'xstage', self.xs_d[:, 0:512], (lambda: self.xT_d[:, bass.ds(self.rv * 1536, 512)]), writes=['xs'])
                self.rd_mode, self.wr_mode, self.ntok = 'dynfull', 'half', S // 2
            if tail_split and s == 'f3':
                self.rd_mode, self.wr_mode, self.ntok = 'half', 'half', S // 2
            if s[0] == 'm':
                if li % 3 == 0:
                    self.conv_mixer(li, li // 3, first)
                elif li % 3 == 1:
                    self.attn_mixer(li)
                else:
                    self.ssm_mixer(li)
                first = False
            else:
                self.ffn(li, li % 2 == 1)
        self.epilogue()
        self.sc.finish('sp')
        self.sc.emit()
        return self.nc


def host_layout(inputs, b):
    f = np.float32
    m = {}
    m['ident'] = np.eye(P, dtype=f)
    m['x'] = np.ascontiguousarray(inputs['x'][b])
    m['cT'] = np.ascontiguousarray(inputs['c'][b].reshape(DC, P).T)
    m['ada_w'] = inputs['ada_w']
    m['ada_bT'] = np.ascontiguousarray(inputs['ada_b'].reshape(4, 48, P).transpose(2, 0, 1))
    m['norm_gT'] = np.ascontiguousarray(inputs['norm_g'].reshape(4, 2, DC, P).transpose(3, 0, 1, 2))
    m['conv_w_in'] = inputs['conv_w_in']
    m['conv_w_out'] = inputs['conv_w_out']
    m['conv_wT'] = np.ascontiguousarray(inputs['conv_w'].reshape(2, 3, DC, P).transpose(3, 0, 1, 2))
    m['rankf'] = np.zeros((P, 1), dtype=f)
    return m


def rel_bucket_np(dist):
    import math
    d = np.maximum(dist, 1).astype(np.float32)
    large = 16 + (np.log(d / np.float32(16)) / np.float32(math.log(128 / 16)) * np.float32(16)).astype(np.int32)
    large = np.minimum(large, 31)
    return np.where(dist < 16, dist, large)


def ssm_layout(inputs):
    f = np.float32
    m = {}
    lre = inputs['ssm_lambda_re'][0]
    lim = inputs['ssm_lambda_im'][0]
    lamT = np.empty((P, 32, 2), dtype=f)
    lst = np.empty((P, 32), dtype=f)
    Bb = np.zeros((32, P, 2, P), dtype=f)
    Cb = np.zeros((32, P, 2, P), dtype=f)
    bre = inputs['ssm_b_re'][0]
    bim = inputs['ssm_b_im'][0]
    cre = inputs['ssm_c_re'][0]
    cim = inputs['ssm_c_im'][0]
    for k in range(32):
        for gg in range(2):
            g = 2 * k + gg
            lamT[gg * 64:(gg + 1) * 64, k, 0] = lre[g]
            lamT[gg * 64:(gg + 1) * 64, k, 1] = lim[g]
            lst[gg * 64:(gg + 1) * 64, k] = inputs['ssm_log_step'][0, g]
            r0 = (g % 8) * 16
            Bb[k, r0:r0 + 16, 0, gg * 64:(gg + 1) * 64] = bre[g].T
            Bb[k, r0:r0 + 16, 1, gg * 64:(gg + 1) * 64] = bim[g].T
            Cb[k, gg * 64:(gg + 1) * 64, 0, r0:r0 + 16] = cre[g].T
            Cb[k, gg * 64:(gg + 1) * 64, 1, r0:r0 + 16] = cim[g].T
    m['ssm_lamT'] = lamT
    m['ssm_lstepT'] = lst
    m['ssm_Bblk'] = Bb.reshape(32, P, 256)
    m['ssm_Cblk'] = Cb.reshape(32, P, 256)
    m['ssm_dT'] = np.ascontiguousarray(inputs['ssm_d'][0].reshape(DC, P).T)
    w = inputs['ssm_w_glu'][0]
    lin = w[:, :D].reshape(DC, P, DC, P)
    gate = w[:, D:].reshape(DC, P, DC, P)
    r = np.empty((DC, P, DC, 2, P), dtype=f)
    r[:, :, :, 0, :] = lin.transpose(2, 1, 0, 3)
    r[:, :, :, 1, :] = gate.transpose(2, 1, 0, 3)
    m['ssm_glu_r'] = r.reshape(DC, P, DC * 256)
    return m


def attn_layout(inputs):
    f = np.float32
    m = {}
    w = inputs['attn_w_in'][0]
    wq = w[:, 0:1024].reshape(D, 16, 64)
    order = []
    for pair in range(2):
        for g in range(4):
            order += [(2 * pair) * 4 + g, (2 * pair + 1) * 4 + g]
    m['attn_wq_perm'] = np.ascontiguousarray(wq[:, order, :]).reshape(D, 1024)
    m['attn_wk'] = np.ascontiguousarray(w[:, 1024:1280])
    m['attn_wv'] = np.ascontiguousarray(w[:, 1280:1536])
    m['attn_wqi'] = np.ascontiguousarray(w[:, 1536:2048])
    ki = w[:, 2048:2112]
    m['attn_wki2'] = np.ascontiguousarray(np.concatenate([ki, ki], axis=1))
    m['attn_wwi'] = np.ascontiguousarray(w[:, 2112:2120])
    m['attn_wout_r'] = np.ascontiguousarray(inputs['attn_w_out'][0].reshape(16, 64, D).transpose(1, 0, 2))
    qg = inputs['attn_q_gain'][0]
    kg = inputs['attn_k_gain'][0]
    m['attn_gainT'] = np.ascontiguousarray(np.stack([np.tile(qg, 2), np.tile(kg, 2)], axis=1))
    rb = inputs['rel_bias']
    sl = np.arange(P)[:, None]
    tl = np.arange(P)[None, :]
    bt = np.empty((P, 32, P), dtype=f)
    for kind in range(2):
        dist = np.maximum(tl - sl + kind * P, 0)
        bk = rel_bucket_np(dist)
        for h in range(16):
            bt[:, kind * 16 + h, :] = rb[bk, h]
    m['attn_biasT'] = bt
    m['attn_b31B'] = np.ascontiguousarray(np.broadcast_to(rb[31][None, :], (P, 16)))
    cm = np.zeros((P, P), dtype=f)
    cm[np.arange(P)[None, :] > np.arange(P)[:, None]] = -1e30
    m['cmask'] = cm
    bo = np.zeros((P, P), dtype=f)
    bo[0:64, 0:64] = 1.0
    bo[64:128, 64:128] = 1.0
    m['blockones'] = bo
    sr = np.zeros((65, 64), dtype=f)
    sr[64, :] = 1.0
    m['selrow'] = sr
    return m


_shared_cache = {}


def shared_layout(inputs):
    f = np.float32
    m = {}
    gu = inputs['ffn_w_gu']
    g = gu[:, :, :DFF].reshape(2, DC, P, FC, P)
    u = gu[:, :, DFF:].reshape(2, DC, P, FC, P)
    gu_r = np.stack([g, u], axis=4)
    m['ffn_gu_r'] = np.ascontiguousarray(gu_r.transpose(0, 3, 2, 1, 4, 5)).reshape(2, FC, P, DC * 256)
    dn = inputs['ffn_w_down'].reshape(2, FC, P, DC, P)
    m['ffn_dn_r'] = np.ascontiguousarray(dn.transpose(0, 3, 2, 1, 4)).reshape(2, DC, P, FC * P)
    gu = inputs['moe_w_gu']
    g = gu[:, :, :, :DFF].reshape(2, NE, DC, P, FC, P)
    u = gu[:, :, :, DFF:].reshape(2, NE, DC, P, FC, P)
    r = np.empty((2, NE, FC, P, DC, 2, P), dtype=f)
    r[:, :, :, :, :, 0, :] = g.transpose(0, 1, 4, 3, 2, 5)
    r[:, :, :, :, :, 1, :] = u.transpose(0, 1, 4, 3, 2, 5)
    m['moe_gu_r'] = r.reshape(2, NE, FC, P, DC * 256)
    dn = inputs['moe_w_down'].reshape(2, NE, FC, P, DC, P)
    m['moe_dn_r'] = np.ascontiguousarray(dn.transpose(0, 1, 4, 3, 2, 5)).reshape(2, NE, DC, P, FC * P)
    m['moe_rwT'] = np.ascontiguousarray(inputs['moe_router_w'].reshape(2, DC, P, NE).transpose(2, 0, 1, 3))
    m['moe_rbB'] = np.ascontiguousarray(np.broadcast_to(inputs['moe_router_b'][None], (P, 2, NE)))
    m.update(attn_layout(inputs))
    m.update(ssm_layout(inputs))
    sel = np.zeros((NE, NE, P), dtype=f)
    for e in range(NE):
        sel[e, e, :] = 1.0
    m['sel8'] = sel
    return m


def kernel(**inputs):
    inputs = {k: np.asarray(v) for k, v in inputs.items()}
    b = Builder({'tail_split': True})
    nc = b.build()
    shared = shared_layout(inputs)
    in_maps = []
    for core in range(8):
        m = host_layout(inputs, core % 4)
        m['rankf'] = np.full((P, 1), float(core // 4), dtype=np.float32)
        m.update(shared)
        in_maps.append({k: m[k] for k in b.din})
    res = run_bass_kernel_spmd(nc, in_maps, core_ids=list(range(8)))
    out = np.stack([np.concatenate([res.results[i]['out'], res.results[i + 4]['out']], axis=0) for i in range(4)], axis=0)
    return out.astype(np.float32)
```

```python
import contextlib
from types import FunctionType
import numpy as np
import concourse.bass as bass
import concourse.mybir as mybir
from concourse.bass_utils import run_bass_kernel_spmd

F32 = mybir.dt.float32
BF16 = mybir.dt.bfloat16
AF = mybir.ActivationFunctionType
ALU = mybir.AluOpType
AX = mybir.AxisListType

S = 4096
D = 1024
DC = 8
P = 128
DFF = 2816
FC = 22
NE = 8
EPS = 1e-6
NBLK = S // 512

ENG = ['pe', 'act', 'dve', 'pool', 'sp']
SELF_SYNC = True


class Sched:
    def __init__(self, nc, stack):
        self.nc = nc
        self.stack = stack
        self.streams = {e: [] for e in ENG}
        self.sem = {e: stack.enter_context(nc.semaphore('s_' + e)) for e in ENG}
        self.count = {e: 0 for e in ENG}
        self.dsem = {}
        self.seen = {e: {} for e in ENG}
        self.hist = {}
        self.buf = {}
        self.ninst = 0

    def _semof(self, key):
        if isinstance(key, tuple):
            return self.dsem[key[1]][0]
        return self.sem[key]

    def _wait(self, eng, toks):
        best = {}
        for (k, v) in toks:
            if v > best.get(k, 0):
                best[k] = v
        seen = self.seen[eng]
        for k, v in best.items():
            if seen.get(k, 0) >= v:
                continue
            if k == eng and (eng == 'pe' or not SELF_SYNC):
                continue
            s = self._semof(k)
            self.streams[eng].append(lambda e, s=s, v=v: e.wait_ge(s, v))
            self.ninst += 1
            h = self.hist.get((k, v))
            if h:
                for kk, vv in h.items():
                    if vv > seen.get(kk, 0):
                        seen[kk] = vv
            if v > seen.get(k, 0):
                seen[k] = v

    def _deps(self, reads, writes):
        toks = []
        for b in reads:
            st = self.buf.get(b)
            if st and st[0]:
                toks.append(st[0])
        for b in writes:
            st = self.buf.get(b)
            if st:
                if st[0]:
                    toks.append(st[0])
                toks.extend(st[1].items())
        return toks

    def _record(self, tok, reads, writes):
        for b in reads:
            st = self.buf.setdefault(b, [None, {}])
            if tok[1] > st[1].get(tok[0], 0):
                st[1][tok[0]] = tok[1]
        for b in writes:
            self.buf[b] = [tok, {}]

    def op(self, eng, fns, reads=(), writes=()):
        if not isinstance(fns, (list, tuple)):
            fns = [fns]
        self._wait(eng, self._deps(reads, writes))
        self.count[eng] += 1
        tok = (eng, self.count[eng])
        sem = self.sem[eng]
        for f in fns[:-1]:
            self.streams[eng].append(f)
        last = fns[-1]
        self.streams[eng].append(lambda e, f=last, s=sem: f(e).then_inc(s, 1))
        self.ninst += len(fns)
        self.hist[tok] = dict(self.seen[eng])
        self._record(tok, reads, writes)
        return tok

    def dma(self, queue, chan, out, in_, reads=(), writes=(), **kw):
        if chan == 'const':
            self.nconst = getattr(self, 'nconst', 0) + 1
            chan = 'const%d' % self.nconst
        if chan not in self.dsem:
            self.dsem[chan] = [self.stack.enter_context(self.nc.semaphore('d_' + str(chan))), 0]
        self._wait(queue, self._deps(reads, writes))
        ds = self.dsem[chan]
        ds[1] += 16
        tok = (('dma', chan), ds[1])
        s = ds[0]
        self.streams[queue].append(lambda e, s=s, o=out, i=in_, kw=kw: e.dma_start(
            out=(o() if isinstance(o, FunctionType) else o), in_=(i() if isinstance(i, FunctionType) else i), **kw).then_inc(s, 16))
        self.ninst += 1
        self.hist[tok] = dict(self.seen[queue])
        self._record(tok, reads, writes)
        return tok

    def barrier(self):
        toks = []
        for b, st in self.buf.items():
            if st[0]:
                toks.append(st[0])
            toks.extend(st[1].items())
        for eng in ENG:
            self._wait(eng, toks)

    def finish(self, eng='sp'):
        toks = []
        for b, st in self.buf.items():
            if st[0]:
                toks.append(st[0])
            toks.extend(st[1].items())
        self._wait(eng, toks)

    def emit(self):
        nc = self.nc
        with nc.Block() as block:
            @block.tensor
            def _(e):
                for f in self.streams['pe']:
                    f(e)

            @block.scalar
            def _(e):
                for f in self.streams['act']:
                    f(e)

            @block.vector
            def _(e):
                for f in self.streams['dve']:
                    f(e)

            @block.gpsimd
            def _(e):
                for f in self.streams['pool']:
                    f(e)

            @block.sync
            def _(e):
                for f in self.streams['sp']:
                    f(e)


class Builder:
    def __init__(self, cfg):
        self.cfg = cfg
        self.nc = bass.Bass("TRN2", target_bir_lowering=False)
        self.stack = contextlib.ExitStack()
        self.sc = Sched(self.nc, self.stack)
        self.din = {}
        self.psn = 0
        self.uid = 0

    def inp(self, name, shape, dtype=F32):
        t = self.nc.dram_tensor(name, list(shape), dtype, kind="ExternalInput").ap()
        self.din[name] = t
        return t

    def sb(self, name, shape, dtype, st=None):
        self.uid += 1
        return (st or self.stack).enter_context(self.nc.sbuf_tensor('sb%d_%s' % (self.uid, name), list(shape), dtype))

    def bank(self):
        rot = getattr(self, 'rot', None) or list(range(8))
        i = rot[self.psn % len(rot)]
        self.psn += 1
        return ('ps', i), self.ps[i]

    def setup_common(self):
        nc = self.nc
        self.ps = [self.stack.enter_context(nc.psum_tensor('ps%d' % i, [P, 512], F32)) for i in range(8)]
        self.ident = self.sb('ident', [P, P], F32)
        self.ones = self.sb('ones', [P, P], F32)
        d_ident = self.inp('ident', [P, P])
        self.sc.dma('sp', 'const', self.ident[:], d_ident[:, :], writes=['ident'])
        self.sc.op('dve', lambda e: e.memset(self.ones[:], 1.0), writes=['ones'])

    def build_mod(self):
        sc = self.sc
        cT = self.inp('cT', [P, DC])
        ada_w = self.inp('ada_w', [4, D, 6 * D])
        ada_bT = self.inp('ada_bT', [P, 4, 48])
        norm_gT = self.inp('norm_gT', [P, 4, 2, DC])
        self.cond = self.sb('cond', [P, DC], F32)
        self.modT = self.sb('modT', [P, 4, 48], F32)
        self.adab = self.sb('adab', [P, 4, 48], F32)
        self.ng = self.sb('ng', [P, 4, 2, DC], F32)
        self.gs = self.sb('gs', [P, 4, 2, DC], F32)
        craw = self.sb('craw', [P, DC], F32)
        sig = self.sb('csig', [P, DC], F32)
        sc.dma('sp', 'const', craw[:], cT[:, :], writes=['craw'])
        sc.dma('sp', 'const', self.adab[:], ada_bT[:, :, :], writes=['adab'])
        sc.dma('sp', 'const', self.ng[:], norm_gT[:, :, :, :], writes=['ng'])
        sc.op('act', lambda e: e.activation(out=sig[:], in_=craw[:], func=AF.Sigmoid), reads=['craw'], writes=['csig'])
        sc.op('dve', lambda e: e.tensor_tensor(out=self.cond[:], in0=craw[:], in1=sig[:], op=ALU.mult),
              reads=['craw', 'csig'], writes=['cond'])
        st = contextlib.ExitStack()
        wsl = [self.sb('adaw%d' % i, [P, DC, 1024], F32, st) for i in range(2)]
        n = 0
        for i in range(4):
            pname, pt = self.bank()
            for k in range(6):
                slot = n % 2
                n += 1
                for c in range(DC):
                    sc.dma('sp', 'adaw%d' % slot, wsl[slot][:, c, :],
                           ada_w[i, c * P:(c + 1) * P, k * 1024:(k + 1) * 1024], writes=['adaw%d' % slot])
                fns = []
                for nn in range(8):
                    for c in range(DC):
                        fns.append(lambda e, pt=pt, slot=slot, nn=nn, c=c, k=k:
                                   e.matmul(pt[:, k * 8 + nn:k * 8 + nn + 1], lhsT=wsl[slot][:, c, nn * P:(nn + 1) * P],
                                            rhs=self.cond[:, c:c + 1], start=(c == 0), stop=(c == DC - 1)))
                sc.op('pe', fns, reads=['adaw%d' % slot, 'cond'], writes=[pname])
            sc.op('dve', lambda e, pt=pt, i=i: e.tensor_tensor(out=self.modT[:, i, :], in0=pt[:, 0:48], in1=self.adab[:, i, :], op=ALU.add),
                  reads=[pname, 'adab'], writes=[('modT', i)])
            for j, k in ((0, 1), (1, 4)):
                sc.op('dve', lambda e, i=i, j=j, k=k: e.scalar_tensor_tensor(
                    out=self.gs[:, i, j, :], in0=self.modT[:, i, k * 8:(k + 1) * 8], scalar=1.0, in1=self.ng[:, i, j, :],
                    op0=ALU.add, op1=ALU.mult), reads=[('modT', i), 'ng'], writes=[('modT', i)])
        sc.barrier()
        st.close()

    def modv(self, i, k, c):
        return self.modT[:, i, k * 8 + c:k * 8 + c + 1]

    def alloc_stream(self):
        self.xT_d = self.nc.dram_tensor('xT_scratch', [D, S], F32).ap()
        self.xh_d = self.nc.dram_tensor('xh_scratch', [D, S // 2], F32).ap()
        self.rd_mode = 'full'
        self.wr_mode = 'full'
        self.ntok = S
        self.sc.streams['sp'].append(lambda e: setattr(self, 'rv', e.partition_id() // 4))
        d_rankf = self.inp('rankf', [P, 1])
        self.rankf = self.sb('rankf', [P, 1], F32)
        self.sc.dma('sp', 'const', self.rankf[:], d_rankf[:, :], writes=['rankf'])
        self.xin = self.inp('x', [S, D])
        self.xblk = [self.sb('xblk%d' % i, [P, DC, 512], F32) for i in range(2)]
        self.sq = self.sb('sq', [P, 512], F32)
        self.rstd = self.sb('rstd', [P, 512], F32)
        self.tmp32 = [self.sb('tmp32_%d' % i, [P, 512], F32) for i in range(3)]
        self.xn = 0

    def xd(self, c, blk, write):
        mode = self.wr_mode if write else self.rd_mode
        if mode == 'full':
            return self.xT_d[c * P:(c + 1) * P, blk * 512:(blk + 1) * 512], ('xTd', c, blk)
        if mode == 'half':
            return self.xh_d[c * P:(c + 1) * P, blk * 512:(blk + 1) * 512], ('xh', c, blk)
        if mode == 'dynfull':
            return self.xs_d[c * P:(c + 1) * P, (blk + 1) * 512:(blk + 2) * 512], 'xs'
        raise ValueError(mode)

    def load_xT(self, blk, from_input):
        sc = self.sc
        slot = self.xn % 2
        self.xn += 1
        name = 'xblk%d' % slot
        t = self.xblk[slot]
        if from_input:
            for j in range(4):
                sc.dma('sp', 'xtok', self.xtok[:, j, :], self.xin[blk * 512 + j * P: blk * 512 + (j + 1) * P, :],
                       writes=[('xtok', j)])
            for c in range(DC):
                pname, pt = self.bank()
                fns = [lambda e, pt=pt, j=j, c=c: e.transpose(out=pt[:, j * P:(j + 1) * P], in_=self.xtok[:, j, c * P:(c + 1) * P],
                                                              identity=self.ident[:]) for j in range(4)]
                sc.op('pe', fns, reads=[('xtok', j) for j in range(4)] + ['ident'], writes=[pname])
                eng = 'act' if c % 2 == 0 else 'dve'
                if eng == 'act':
                    sc.op('act', lambda e, pt=pt, t=t, c=c: e.copy(out=t[:, c, :], in_=pt[:]), reads=[pname], writes=[(name, c)])
                else:
                    sc.op('dve', lambda e, pt=pt, t=t, c=c: e.tensor_copy(out=t[:, c, :], in_=pt[:]), reads=[pname], writes=[(name, c)])
        else:
            for c in range(DC):
                ap, tn = self.xd(c, blk, False)
                sc.dma('sp', name, t[:, c, :], ap, reads=[tn], writes=[(name, c)])
        return name, t

    def store_xT(self, blk, name, t, c):
        ap, tn = self.xd(c, blk, True)
        self.sc.dma('sp', 'st_' + name, ap, t[:, c, :], reads=[(name, c)], writes=[tn])

    def norm_mod(self, li, j, name, t, hT, hname, hoff, h32=None):
        sc = self.sc
        pname, pt = self.bank()
        for c in range(DC):
            sc.op('act', lambda e, c=c: e.activation(out=self.sq[:], in_=t[:, c, :], func=AF.Square), reads=[(name, c)], writes=['sq'])
            sc.op('pe', lambda e, c=c, pt=pt: e.matmul(pt[:], lhsT=self.ones[:], rhs=self.sq[:], start=(c == 0), stop=(c == DC - 1)),
                  reads=['sq', 'ones'], writes=[pname])
        sc.op('act', lambda e, pt=pt: e.activation(out=self.rstd[:], in_=pt[:], func=AF.Sqrt, scale=1.0 / D, bias=self.epsb[:]),
              reads=[pname, 'epsb'], writes=['rstd'])
        sc.op('dve', lambda e: e.reciprocal(out=self.rstd[:], in_=self.rstd[:]), reads=['rstd'], writes=['rstd'])
        kshift = 0 if j == 0 else 3
        for c in range(DC):
            tm = self.tmp32[c % 2]
            tn = 'tmp32_%d' % (c % 2)
            sc.op('dve', lambda e, c=c, tm=tm: e.tensor_tensor(out=tm[:], in0=t[:, c, :], in1=self.rstd[:], op=ALU.mult),
                  reads=[(name, c), 'rstd'], writes=[tn])
            if h32 is not None:
                sc.op('pool', lambda e, c=c, tm=tm: e.tensor_scalar(out=h32[:, c, :], in0=tm[:], scalar1=self.gs[:, li, j, c:c + 1],
                                                                   scalar2=self.modv(li, kshift, c), op0=ALU.mult, op1=ALU.add),
                      reads=[tn, ('modT', li)], writes=[('h32', c)])
            sc.op('dve', lambda e, c=c, tm=tm: e.tensor_scalar(out=hT[:, c, hoff:hoff + 512], in0=tm[:], scalar1=self.gs[:, li, j, c:c + 1],
                                                              scalar2=self.modv(li, kshift, c), op0=ALU.mult, op1=ALU.add),
                  reads=[tn, ('modT', li)], writes=[(hname, c, hoff)])

    def load_w_cast(self, chan, dst, src, rows_split, writes):
        n = dst.shape[-1]
        step = 2048
        for a in range(0, n, step):
            b = min(n, a + step)
            self.sc.dma('pool', chan, dst[:, a:b], src[:, a:b], writes=writes)

    def conv_setup(self):
        self.d_conv_w_in = self.inp('conv_w_in', [2, D, 3 * D])
        self.d_conv_w_out = self.inp('conv_w_out', [2, D, D])
        d_cw = self.inp('conv_wT', [P, 2, 3, DC])
        self.cwT = self.sb('cwT', [P, 2, 3, DC], F32)
        self.sc.dma('sp', 'const', self.cwT[:], d_cw[:, :, :, :], writes=['cwT'])

    def conv_mixer(self, li, j, first):
        sc = self.sc
        st = contextlib.ExitStack()
        nc = self.nc
        win = self.sb('cw_in', [P, DC, 3 * D], BF16, st)
        wout = self.sb('cw_out', [P, DC, D], BF16, st)
        hT = self.sb('hTc', [P, DC, 512], BF16, st)
        bT = self.sb('bTc', [P, DC, 512], F32, st)
        uT = self.sb('uTc', [P, DC, 514], F32, st)
        zT = self.sb('zTc', [P, DC, 512], BF16, st)
        if first:
            self.xtok = self.sb('xtok', [P, 4, D], F32, st)
        L = 'L%d' % li
        for c in range(DC):
            self.load_w_cast('cwin', win[:, c, :], self.d_conv_w_in[j, c * P:(c + 1) * P, :], None, writes=[(L, 'cwin')])
            self.load_w_cast('cwout', wout[:, c, :], self.d_conv_w_out[j, c * P:(c + 1) * P, :], None, writes=[(L, 'cwout')])
        sc.op('pool', lambda e: e.memset(uT[:, :, 0:2], 0.0), writes=[(L, 'uT', c) for c in range(DC)])
        split = (self.rd_mode == 'dynfull')
        for blk in ([-1] if split else []) + list(range(self.ntok // 512)):
            name, t = self.load_xT(blk, first)
            self.norm_mod(li, 0, name, t, hT, (L, 'hT'), 0)
            hreads = [((L, 'hT'), c, 0) for c in range(DC)] + [(L, 'cwin')]
            for c in range(DC):
                pts = []
                for kind in ((1, 2) if blk < 0 else (0, 1, 2)):
                    pname, pt = self.bank()
                    fns = [lambda e, pt=pt, k=k, kind=kind, c=c: e.matmul(
                        pt[:], lhsT=win[:, k, kind * D + c * P: kind * D + (c + 1) * P], rhs=hT[:, k, :],
                        start=(k == 0), stop=(k == DC - 1)) for k in range(DC)]
                    sc.op('pe', fns, reads=hreads, writes=[pname])
                    pts.append((pname, pt))
                if blk < 0:
                    (pcn, pc), (pvn, pv) = pts
                else:
                    (pbn, pb), (pcn, pc), (pvn, pv) = pts
                    sc.op('act', lambda e, pb=pb, c=c: e.activation(out=bT[:, c, :], in_=pb[:], func=AF.Copy), reads=[pbn], writes=[(L, 'bT', c)])
                tm = self.tmp32[2]
                sc.op('act', lambda e, pc=pc, tm=tm: e.activation(out=tm[:], in_=pc[:], func=AF.Copy), reads=[pcn], writes=['tmp32_2'])
                sc.op('dve', lambda e, pv=pv, tm=tm, c=c: e.tensor_tensor(out=uT[:, c, 2:514], in0=pv[:], in1=tm[:], op=ALU.mult),
                      reads=[pvn, 'tmp32_2'], writes=[(L, 'uT', c)])
            if blk < 0:
                for c in range(DC):
                    sc.op('dve', lambda e, c=c: e.tensor_scalar(out=uT[:, c, 0:2], in0=uT[:, c, 512:514], scalar1=self.rankf[:, 0:1], scalar2=None, op0=ALU.mult),
                          reads=[(L, 'uT', c), 'rankf'], writes=[(L, 'uT', c)])
                continue
            for c in range(DC):
                tm = self.tmp32[c % 2]
                tn = 'tmp32_%d' % (c % 2)
                sc.op('dve', lambda e, c=c, tm=tm: e.tensor_scalar(out=tm[:], in0=uT[:, c, 2:514], scalar1=self.cwT[:, j, 2, c:c + 1],
                                                                  scalar2=None, op0=ALU.mult), reads=[(L, 'uT', c), 'cwT'], writes=[tn])
                sc.op('dve', lambda e, c=c, tm=tm: e.scalar_tensor_tensor(out=tm[:], in0=uT[:, c, 1:513], scalar=self.cwT[:, j, 1, c:c + 1],
                                                                         in1=tm[:], op0=ALU.mult, op1=ALU.add),
                      reads=[(L, 'uT', c), tn], writes=[tn])
                sc.op('dve', lambda e, c=c, tm=tm: e.scalar_tensor_tensor(out=tm[:], in0=uT[:, c, 0:512], scalar=self.cwT[:, j, 0, c:c + 1],
                                                                         in1=tm[:], op0=ALU.mult, op1=ALU.add),
                      reads=[(L, 'uT', c), tn], writes=[tn])
                sc.op('dve', lambda e, c=c, tm=tm: e.tensor_tensor(out=zT[:, c, :], in0=tm[:], in1=bT[:, c, :], op=ALU.mult),
                      reads=[tn, (L, 'bT', c)], writes=[(L, 'zT', c)])
                sc.op('pool', lambda e, c=c: e.tensor_copy(out=uT[:, c, 0:2], in_=uT[:, c, 512:514]),
                      reads=[(L, 'uT', c)], writes=[(L, 'uT', c)])
            zreads = [(L, 'zT', c) for c in range(DC)] + [(L, 'cwout')]
            for c in range(DC):
                pname, pt = self.bank()
                fns = [lambda e, pt=pt, k=k, c=c: e.matmul(pt[:], lhsT=wout[:, k, c * P:(c + 1) * P], rhs=zT[:, k, :],
                                                           start=(k == 0), stop=(k == DC - 1)) for k in range(DC)]
                sc.op('pe', fns, reads=zreads, writes=[pname])
                sc.op('dve', lambda e, pt=pt, c=c, t=t: e.scalar_tensor_tensor(out=t[:, c, :], in0=pt[:], scalar=self.modv(li, 2, c),
                                                                              in1=t[:, c, :], op0=ALU.mult, op1=ALU.add),
                      reads=[pname, (name, c), ('modT', li)], writes=[(name, c)])
                self.store_xT(blk, name, t, c)
        sc.barrier()
        st.close()

    def ffn_setup(self):
        self.d_ffn_gu = self.inp('ffn_gu_r', [2, FC, P, DC * 256])
        self.d_ffn_dn = self.inp('ffn_dn_r', [2, DC, P, FC * P])
        self.d_moe_gu = self.inp('moe_gu_r', [2, NE, FC, P, DC * 256])
        self.d_moe_dn = self.inp('moe_dn_r', [2, NE, DC, P, FC * P])
        d_rw = self.inp('moe_rwT', [P, 2, DC, NE])
        d_rb = self.inp('moe_rbB', [P, 2, NE])
        d_sel = self.inp('sel8', [NE, NE, P])
        self.rw = self.sb('rw', [P, 2, DC, NE], F32)
        self.rb = self.sb('rb', [P, 2, NE], F32)
        self.sel = self.sb('sel', [NE, NE, P], F32)
        self.sc.dma('sp', 'const', self.rw[:], d_rw[:, :, :, :], writes=['rw'])
        self.sc.dma('sp', 'const', self.rb[:], d_rb[:, :, :], writes=['rb'])
        self.sc.dma('sp', 'const', self.sel[:], d_sel[:, :, :], writes=['sel'])

    def router(self, li, L, h32, half, gT, sm):
        sc = self.sc
        jl = li // 2
        lg, ex, gt, m8, sc1 = sm
        prn, pr = self.bank()
        h32r = [('h32', c) for c in range(DC)]
        for tt in range(4):
            fns = [lambda e, pr=pr, tt=tt, c=c: e.matmul(pr[:, tt * 8:(tt + 1) * 8], lhsT=h32[:, c, tt * P:(tt + 1) * P],
                                                        rhs=self.rw[:, jl, c, :], start=(c == 0), stop=(c == DC - 1))
                   for c in range(DC)]
            sc.op('pe', fns, reads=h32r + ['rw'], writes=[prn])
        ptn, ptT = self.bank()
        for tt in range(4):
            R = [(L, 'rt')]
            sc.op('dve', lambda e, tt=tt, pr=pr: e.tensor_tensor(out=lg[:, 0:8], in0=pr[:, tt * 8:(tt + 1) * 8], in1=self.rb[:, jl, :], op=ALU.add),
                  reads=[prn, 'rb'], writes=R)
            sc.op('dve', lambda e: e.tensor_reduce(out=sc1[:, 0:1], in_=lg[:, 0:8], axis=AX.X, op=ALU.max), reads=R, writes=R)
            sc.op('dve', lambda e: e.tensor_scalar(out=sc1[:, 0:1], in0=sc1[:, 0:1], scalar1=-1.0, scalar2=None, op0=ALU.mult), reads=R, writes=R)
            sc.op('act', lambda e: e.activation(out=ex[:, 0:8], in_=lg[:, 0:8], func=AF.Exp, bias=sc1[:, 0:1], scale=1.0), reads=R, writes=R)
            sc.op('dve', lambda e: e.max(out=m8[:, 0:8], in_=ex[:, 0:8]), reads=R, writes=R)
            sc.op('dve', lambda e: e.tensor_tensor(out=sc1[:, 1:2], in0=m8[:, 0:1], in1=m8[:, 1:2], op=ALU.add), reads=R, writes=R)
            sc.op('dve', lambda e: e.reciprocal(out=sc1[:, 1:2], in_=sc1[:, 1:2]), reads=R, writes=R)
            sc.op('dve', lambda e: e.tensor_scalar(out=gt[:, 0:8], in0=ex[:, 0:8], scalar1=m8[:, 1:2], scalar2=None, op0=ALU.is_ge), reads=R, writes=R)
            sc.op('dve', lambda e: e.tensor_tensor(out=gt[:, 0:8], in0=gt[:, 0:8], in1=ex[:, 0:8], op=ALU.mult), reads=R, writes=R)
            sc.op('dve', lambda e: e.tensor_scalar(out=gt[:, 0:8], in0=gt[:, 0:8], scalar1=sc1[:, 1:2], scalar2=None, op0=ALU.mult), reads=R, writes=R)
            sc.op('pe', lambda e, tt=tt, ptT=ptT: e.transpose(out=ptT[0:8, tt * P:(tt + 1) * P], in_=gt[:, 0:8], identity=self.ident[:]),
                  reads=R + ['ident'], writes=[ptn])
        sc.op('act', lambda e, ptT=ptT, half=half: e.activation(out=gT[0:8, half * 512:(half + 1) * 512], in_=ptT[0:8, :], func=AF.Copy),
              reads=[ptn], writes=[(L, 'gT', half)])

    def ffn(self, li, moe):
        sc = self.sc
        st = contextlib.ExitStack()
        L = 'F%d' % li
        TB = 1024
        hT = self.sb('hTf', [P, DC, TB], BF16, st)
        actT = self.sb('actT', [P, FC, TB], BF16, st)
        wgu = [self.sb('wgu%d' % i, [P, DC * 256], BF16, st) for i in range(2)]
        wd = [self.sb('wd%d' % i, [P, FC * P], BF16, st) for i in range(2)]
        xs = [self.sb('xs%d' % i, [P, 512], F32, st) for i in range(2)]
        if moe:
            h32 = self.sb('h32', [P, DC, 512], F32, st)
            yacc = self.sb('yacc', [P, DC, TB], F32, st)
            Gb = [self.sb('Gb%d' % i, [P, TB], F32, st) for i in range(2)]
            gT = self.sb('gT', [NE, TB], F32, st)
            sm = [self.sb('rsm%d' % i, [P, 8], F32, st) for i in range(5)]
        jl = li // 2
        nw = 0
        nd = 0
        nx = 0
        ng = 0
        for sbi in range(self.ntok // TB):
            for half in range(2):
                blk = sbi * 2 + half
                name, t = self.load_xT(blk, False)
                self.norm_mod(li, 1, name, t, hT, (L, 'hT'), half * 512, h32=(h32 if moe else None))
                if moe:
                    self.router(li, L, h32, half, gT, sm)
            for ex in (range(NE) if moe else [None]):
                if moe:
                    gsl = ng % 2
                    ng += 1
                    gbn = (L, 'Gb', gsl)
                    for half in range(2):
                        pn, pt = self.bank()
                        sc.op('pe', lambda e, pt=pt, ex=ex, half=half: e.matmul(pt[:], lhsT=self.sel[0:8, ex, :], rhs=gT[0:8, half * 512:(half + 1) * 512],
                                                                               start=True, stop=True),
                              reads=['sel', (L, 'gT', half)], writes=[pn])
                        sc.op('act', lambda e, pt=pt, gsl=gsl, half=half: e.activation(out=Gb[gsl][:, half * 512:(half + 1) * 512], in_=pt[:], func=AF.Copy),
                              reads=[pn], writes=[(gbn, half)])
                for f in range(FC):
                    slot = nw % 2
                    nw += 1
                    wn = (L, 'wgu', slot)
                    src = self.d_moe_gu[jl, ex, f, :, :] if moe else self.d_ffn_gu[jl, f, :, :]
                    self.load_w_cast('wgu%d' % slot, wgu[slot][:, :], src, None, writes=[wn])
                    for half in range(2):
                        hreads = [((L, 'hT'), c, half * 512) for c in range(DC)] + [wn]
                        pgn, pg = self.bank()
                        pun, pu = self.bank()
                        for (pn, pt, off) in ((pgn, pg, 0), (pun, pu, 128)):
                            fns = [lambda e, pt=pt, k=k, off=off, slot=slot, half=half: e.matmul(
                                pt[:], lhsT=wgu[slot][:, k * 256 + off: k * 256 + off + 128], rhs=hT[:, k, half * 512:(half + 1) * 512],
                                start=(k == 0), stop=(k == DC - 1)) for k in range(DC)]
                            sc.op('pe', fns, reads=hreads, writes=[pn])
                        tm = self.tmp32[2]
                        sc.op('act', lambda e, pg=pg, tm=tm: e.activation(out=tm[:], in_=pg[:], func=AF.Silu), reads=[pgn], writes=['tmp32_2'])
                        sc.op('dve', lambda e, pu=pu, tm=tm, f=f, half=half: e.tensor_tensor(
                            out=actT[:, f, half * 512:(half + 1) * 512], in0=pu[:], in1=tm[:], op=ALU.mult),
                            reads=[pun, 'tmp32_2'], writes=[(L, 'act', f, half)])
                for c in range(DC):
                    slot = nd % 2
                    nd += 1
                    wn = (L, 'wd', slot)
                    src = self.d_moe_dn[jl, ex, c, :, :] if moe else self.d_ffn_dn[jl, c, :, :]
                    self.load_w_cast('wd%d' % slot, wd[slot][:, :], src, None, writes=[wn])
                    for half in range(2):
                        blk = sbi * 2 + half
                        areads = [(L, 'act', f, half) for f in range(FC)] + [wn]
                        pn, pt = self.bank()
                        fns = [lambda e, pt=pt, f=f, slot=slot, half=half: e.matmul(
                            pt[:], lhsT=wd[slot][:, f * P:(f + 1) * P], rhs=actT[:, f, half * 512:(half + 1) * 512],
                            start=(f == 0), stop=(f == FC - 1)) for f in range(FC)]
                        sc.op('pe', fns, reads=areads, writes=[pn])
                        if not moe:
                            self.resid(li, 5, xs, nx, c, blk, pn, pt, None, None)
                            nx += 1
                        else:
                            yn = (L, 'yacc', c, half)
                            ysl = yacc[:, c, half * 512:(half + 1) * 512]
                            gsl_ap = Gb[gsl][:, half * 512:(half + 1) * 512]
                            if ex == 0:
                                sc.op('dve', lambda e, pt=pt, ysl=ysl, g=gsl_ap: e.tensor_tensor(out=ysl, in0=pt[:], in1=g, op=ALU.mult),
                                      reads=[pn, (gbn, half)], writes=[yn])
                            else:
                                tm = self.tmp32[c % 2]
                                tn = 'tmp32_%d' % (c % 2)
                                sc.op('dve', lambda e, pt=pt, tm=tm, g=gsl_ap: e.tensor_tensor(out=tm[:], in0=pt[:], in1=g, op=ALU.mult),
                                      reads=[pn, (gbn, half)], writes=[tn])
                                sc.op('pool', lambda e, tm=tm, ysl=ysl: e.tensor_tensor(out=ysl, in0=ysl, in1=tm[:], op=ALU.add),
                                      reads=[tn, yn], writes=[yn])
            if moe:
                for c in range(DC):
                    for half in range(2):
                        blk = sbi * 2 + half
                        self.resid(li, 5, xs, nx, c, blk, (L, 'yacc', c, half), None, yacc[:, c, half * 512:(half + 1) * 512], None)
                        nx += 1
        sc.barrier()
        st.close()

    def resid(self, li, k, xs, nx, c, blk, srcname, pt, src_ap, _):
        sc = self.sc
        xsl = nx % 2
        xn = 'xs%d' % xsl
        xt = xs[xsl]
        src = pt[:] if pt is not None else src_ap
        rap, rtn = self.xd(c, blk, False)
        wap, wtn = self.xd(c, blk, True)
        sc.dma('sp', xn, xt[:], rap, reads=[rtn], writes=[xn])
        sc.op('dve', lambda e, src=src, xt=xt, c=c: e.scalar_tensor_tensor(
            out=xt[:], in0=src, scalar=self.modv(li, k, c), in1=xt[:], op0=ALU.mult, op1=ALU.add),
            reads=[srcname, xn, ('modT', li)], writes=[xn])
        sc.dma('sp', 'st_' + xn, wap, xt[:], reads=[xn], writes=[wtn])

    def attn_setup(self):
        self.d_awq = self.inp('attn_wq_perm', [D, 1024])
        self.d_awk = self.inp('attn_wk', [D, 256])
        self.d_awv = self.inp('attn_wv', [D, 256])
        self.d_awqi = self.inp('attn_wqi', [D, 512])
        self.d_awki2 = self.inp('attn_wki2', [D, 128])
        self.d_awwi = self.inp('attn_wwi', [D, 8])
        self.d_awout = self.inp('attn_wout_r', [64, 16, D])
        self.d_gainT = self.inp('attn_gainT', [P, 2])
        self.d_biasT = self.inp('attn_biasT', [P, 32, P])
        self.d_b31 = self.inp('attn_b31B', [P, 16])
        self.d_cmask = self.inp('cmask', [P, P])
        self.d_bones = self.inp('blockones', [P, P])
        self.d_selrow = self.inp('selrow', [65, 64])

    def head_norm(self, pn, pt, gain_ap, out_ap, tagw):
        sc = self.sc
        qs = self.tmp32[2]
        sc.op('act', lambda e, pt=pt, qs=qs: e.activation(out=qs[:], in_=pt[:], func=AF.Copy), reads=[pn], writes=['tmp32_2'])
        sc.op('act', lambda e, qs=qs: e.activation(out=self.sq[:], in_=qs[:], func=AF.Square), reads=['tmp32_2'], writes=['sq'])
        p2n, p2 = self.bank()
        sc.op('pe', lambda e, p2=p2: e.matmul(p2[:], lhsT=self.bones[:], rhs=self.sq[:], start=True, stop=True), reads=['sq', 'bones'], writes=[p2n])
        sc.op('act', lambda e, p2=p2: e.activation(out=self.rstd[:], in_=p2[:], func=AF.Sqrt, scale=1.0 / 64, bias=self.epsb[:]),
              reads=[p2n, 'epsb'], writes=['rstd'])
        sc.op('dve', lambda e: e.reciprocal(out=self.rstd[:], in_=self.rstd[:]), reads=['rstd'], writes=['rstd'])
        sc.op('dve', lambda e, qs=qs, g=gain_ap, o=out_ap: e.scalar_tensor_tensor(out=o, in0=qs[:], scalar=g, in1=self.rstd[:], op0=ALU.mult, op1=ALU.mult),
              reads=['tmp32_2', 'rstd', 'again'], writes=tagw)

    def attn_mixer(self, li):
        sc = self.sc
        st = contextlib.ExitStack()
        L = 'A'
        NI = 24
        KT = self.sb('KT', [P, 2, S], BF16, st)
        VA = self.sb('VA', [P, 32, 4, 65], BF16, st)
        KI = self.sb('KI', [P, S], BF16, st)
        hT = self.sb('hTa', [P, DC, 512], BF16, st)
        wA = self.sb('wA', [P, DC * 1024], BF16, st)
        QT = self.sb('QT', [P, 8, 512], BF16, st)
        QI = self.sb('QI', [P, 4, 512], BF16, st)
        WI = self.sb('WI', [P, 4, 8], F32, st)
        score = self.sb('score', [P, S], F32, st)
        nmq = self.sb('nmq', [P, S], BF16, st)
        nmT = [self.sb('nmT%d' % i, [P, 32, P], BF16, st) for i in range(2)]
        pexp = [self.sb('pexp%d' % i, [P, 512], BF16, st) for i in range(3)]
        OTn = self.sb('OTn', [64, 16, 512], BF16, st)
        osb = self.sb('osb', [65, 512], F32, st)
        rbc = self.sb('rbc', [64, 512], F32, st)
        Rt = self.tmp32[0:2]
        biasS = self.sb('biasS', [P, 32, P], BF16, st)
        self.bones = self.sb('bones', [P, P], F32, st)
        identb = self.sb('identb', [P, P], BF16, st)
        cmask = self.sb('cmaskS', [P, P], F32, st)
        selrow = self.sb('selrowS', [65, 64], F32, st)
        again = self.sb('again', [P, 2], F32, st)
        b31 = self.sb('b31', [P, 16], F32, st)
        bs = [self.sb('bsm%d' % i, [P, 1], F32, st) for i in range(6)]
        sc.dma('sp', 'const', self.bones[:], self.d_bones[:, :], writes=['bones'])
        sc.dma('sp', 'const', cmask[:], self.d_cmask[:, :], writes=['cmask'])
        sc.dma('sp', 'const', selrow[:], self.d_selrow[:, :], writes=['selrow'])
        sc.dma('sp', 'const', again[:], self.d_gainT[:, :], writes=['again'])
        sc.dma('sp', 'const', b31[:], self.d_b31[:, :], writes=['b31'])
        sc.op('dve', lambda e: e.tensor_copy(out=identb[:], in_=self.ident[:]), reads=['ident'], writes=['identb'])
        sc.op('dve', lambda e: e.tensor_scalar(out=again[:, 0:1], in0=again[:, 0:1], scalar1=0.125, scalar2=None, op0=ALU.mult),
              reads=['again'], writes=['again'])
        sc.op('pool', lambda e: e.memset(VA[:, :, :, 64:65], 1.0), writes=[(L, 'VAones')])
        for half in range(4):
            sc.dma('sp', 'bstage', score[:, 0:1024].rearrange('p (a b) -> p a b', b=P), self.d_biasT[:, half * 8:(half + 1) * 8, :],
                   writes=[(L, 'bstage')])
            for i in range(8):
                kh = half * 8 + i
                h = kh % 16
                sc.op('dve', lambda e, i=i, kh=kh, h=h: e.tensor_scalar(out=biasS[:, kh, :], in0=score[:, i * P:(i + 1) * P], scalar1=b31[:, h:h + 1],
                                                                      scalar2=None, op0=ALU.subtract), reads=[(L, 'bstage'), 'b31'], writes=[(L, 'biasS')])
        wk = wA[:, 0:DC * 256].rearrange('p (c n) -> p c n', c=DC)
        wv = wA[:, DC * 256:DC * 512].rearrange('p (c n) -> p c n', c=DC)
        wki = wA[:, DC * 512:DC * 640].rearrange('p (c n) -> p c n', c=DC)
        for c in range(DC):
            sc.dma('pool', 'wA', wk[:, c, :], self.d_awk[c * P:(c + 1) * P, :], writes=[(L, 'wA')])
            sc.dma('pool', 'wA', wv[:, c, :], self.d_awv[c * P:(c + 1) * P, :], writes=[(L, 'wA')])
            sc.dma('pool', 'wA', wki[:, c, :], self.d_awki2[c * P:(c + 1) * P, :], writes=[(L, 'wA')])
        for blk in range(NBLK):
            name, t = self.load_xT(blk, False)
            self.norm_mod(li, 0, name, t, hT, (L, 'hT'), 0)
            hreads = [((L, 'hT'), c, 0) for c in range(DC)] + [(L, 'wA')]
            for m in range(2):
                pn, pt = self.bank()
                fns = [lambda e, pt=pt, k=k, m=m: e.matmul(pt[:], lhsT=wk[:, k, m * P:(m + 1) * P], rhs=hT[:, k, :], start=(k == 0), stop=(k == DC - 1))
                       for k in range(DC)]
                sc.op('pe', fns, reads=hreads, writes=[pn])
                self.head_norm(pn, pt, again[:, 1:2], KT[:, m, blk * 512:(blk + 1) * 512], [(L, 'KT', m, blk)])
            pn, pt = self.bank()
            fns = [lambda e, pt=pt, k=k: e.matmul(pt[:], lhsT=wki[:, k, :], rhs=hT[:, k, :], start=(k == 0), stop=(k == DC - 1)) for k in range(DC)]
            sc.op('pe', fns, reads=hreads, writes=[pn])
            sc.op('act', lambda e, pt=pt, blk=blk: e.activation(out=KI[:, blk * 512:(blk + 1) * 512], in_=pt[:], func=AF.Copy), reads=[pn], writes=[(L, 'KI', blk)])
            for tt in range(4):
                pn, pt = self.bank()
                fns = [lambda e, pt=pt, k=k, tt=tt: e.matmul(pt[:, 0:256], lhsT=hT[:, k, tt * P:(tt + 1) * P], rhs=wv[:, k, :], start=(k == 0), stop=(k == DC - 1))
                       for k in range(DC)]
                sc.op('pe', fns, reads=hreads, writes=[pn])
                sc.op('dve', lambda e, pt=pt, blk=blk, tt=tt: e.tensor_copy(out=VA[:, blk * 4 + tt, :, 0:64], in_=pt[:, 0:256].rearrange('p (a b) -> p a b', b=64)),
                      reads=[pn], writes=[(L, 'VA', blk * 4 + tt)])
        if self.cfg.get('astage', 9) < 1:
            sc.barrier(); st.close(); return
        wq = wA[:, :].rearrange('p (c n) -> p c n', c=DC)
        wqi = wA[:, 0:DC * 512].rearrange('p (c n) -> p c n', c=DC)
        wwi = wA[:, DC * 512:DC * 520].rearrange('p (c n) -> p c n', c=DC)
        wout = wA[0:64, 0:16 * 512].rearrange('p (h n) -> p h n', h=16)
        self.abank = [6, 7]
        self.nab = 0
        self.rot = [0, 1, 2, 3, 4, 5]
        nmn = 0
        npx = 0
        nrt = 0
        for blk in range(self.cfg.get('anblk', NBLK)):
            for c in range(DC):
                sc.dma('pool', 'wA', wq[:, c, :], self.d_awq[c * P:(c + 1) * P, :], writes=[(L, 'wA')])
            name, t = self.load_xT(blk, False)
            self.norm_mod(li, 0, name, t, hT, (L, 'hT'), 0)
            hreads = [((L, 'hT'), c, 0) for c in range(DC)]
            for m in range(8):
                pn, pt = self.bank()
                fns = [lambda e, pt=pt, k=k, m=m: e.matmul(pt[:], lhsT=wq[:, k, m * P:(m + 1) * P], rhs=hT[:, k, :], start=(k == 0), stop=(k == DC - 1))
                       for k in range(DC)]
                sc.op('pe', fns, reads=hreads + [(L, 'wA')], writes=[pn])
                self.head_norm(pn, pt, again[:, 0:1], QT[:, m, :], [(L, 'QT', m)])
            for c in range(DC):
                sc.dma('pool', 'wA', wqi[:, c, :], self.d_awqi[c * P:(c + 1) * P, :], writes=[(L, 'wA')])
                sc.dma('pool', 'wA', wwi[:, c, :], self.d_awwi[c * P:(c + 1) * P, :], writes=[(L, 'wA')])
            for m in range(4):
                pn, pt = self.bank()
                fns = [lambda e, pt=pt, k=k, m=m: e.matmul(pt[:], lhsT=wqi[:, k, m * P:(m + 1) * P], rhs=hT[:, k, :], start=(k == 0), stop=(k == DC - 1))
                       for k in range(DC)]
                sc.op('pe', fns, reads=hreads + [(L, 'wA')], writes=[pn])
                sc.op('act', lambda e, pt=pt, m=m: e.activation(out=QI[:, m, :], in_=pt[:], func=AF.Copy), reads=[pn], writes=[(L, 'QI', m)])
            pn, pt = self.bank()
            for qb in range(4):
                fns = [lambda e, pt=pt, k=k, qb=qb: e.matmul(pt[:, qb * 8:(qb + 1) * 8], lhsT=hT[:, k, qb * P:(qb + 1) * P], rhs=wwi[:, k, :],
                                                            start=(k == 0), stop=(k == DC - 1)) for k in range(DC)]
                sc.op('pe', fns, reads=hreads + [(L, 'wA')], writes=[pn])
            sc.op('dve', lambda e, pt=pt: e.tensor_scalar(out=WI[:, :, :], in0=pt[:, 0:32].rearrange('p (a b) -> p a b', b=8), scalar1=0.04419417382415922,
                                                        scalar2=None, op0=ALU.mult), reads=[pn], writes=[(L, 'WI')])
            def idx_bis(qb):
                gq = blk * 4 + qb
                nk = gq + 1
                W = nk * P
                nonlocal nrt
                for k0 in range(0, W, 512):
                    n = min(512, W - k0)
                    for ih in range(8):
                        b0 = (ih % 2) * 64
                        pn, pt = self.bank()
                        sc.op('pe', lambda e, pt=pt, ih=ih, b0=b0, qb=qb, k0=k0, n=n: e.matmul(
                            pt[:, 0:n], lhsT=QI[b0:b0 + 64, ih // 2, qb * P:(qb + 1) * P], rhs=KI[b0:b0 + 64, k0:k0 + n], start=True, stop=True),
                            reads=[(L, 'QI', ih // 2)] + [(L, 'KI', kb) for kb in range(k0 // 512, (k0 + n + 511) // 512)], writes=[pn])
                        rs = nrt % 2
                        nrt += 1
                        rn = 'tmp32_%d' % rs
                        sc.op('act', lambda e, pt=pt, rs=rs, n=n: e.activation(out=Rt[rs][:, 0:n], in_=pt[:, 0:n], func=AF.Relu), reads=[pn], writes=[rn])
                        if ih == 0:
                            sc.op('dve', lambda e, rs=rs, n=n, k0=k0, qb=qb: e.tensor_scalar(out=score[:, k0:k0 + n], in0=Rt[rs][:, 0:n], scalar1=WI[:, qb, 0:1],
                                                                                      scalar2=None, op0=ALU.mult), reads=[rn, (L, 'WI')], writes=[(L, 'score')])
                        else:
                            sc.op('dve', lambda e, rs=rs, n=n, k0=k0, qb=qb, ih=ih: e.scalar_tensor_tensor(
                                out=score[:, k0:k0 + n], in0=Rt[rs][:, 0:n], scalar=WI[:, qb, ih:ih + 1], in1=score[:, k0:k0 + n], op0=ALU.mult, op1=ALU.add),
                                reads=[rn, (L, 'WI'), (L, 'score')], writes=[(L, 'score')])
                SR = [(L, 'score'), (L, 'bis')]
                mx, lo, w0, mid, cnt, ff = bs
                if nk >= 3:
                    sc.op('dve', lambda e, W=W: e.tensor_reduce(out=mx[:], in_=score[:, 0:W], axis=AX.X, op=ALU.max), reads=SR, writes=[(L, 'bis')])
                    sc.op('dve', lambda e, W=W: e.tensor_reduce(out=lo[:], in_=score[:, 0:W], axis=AX.X, op=ALU.min), reads=SR, writes=[(L, 'bis')])
                    sc.op('dve', lambda e: e.tensor_tensor(out=w0[:], in0=mx[:], in1=lo[:], op=ALU.subtract), reads=SR, writes=[(L, 'bis')])
                else:
                    sc.op('dve', lambda e: e.memset(lo[:], -1e29), reads=SR, writes=[(L, 'bis')])
                sc.op('dve', lambda e, W=W: e.tensor_tensor(out=score[:, W - P:W], in0=score[:, W - P:W], in1=cmask[:], op=ALU.add),
                      reads=SR + ['cmask'], writes=SR)
                if nk >= 3:
                    for it in range(NI):
                        cst = 2.0 ** (-(it + 1))
                        sc.op('dve', lambda e, cst=cst: e.scalar_tensor_tensor(out=mid[:], in0=w0[:], scalar=cst, in1=lo[:], op0=ALU.mult, op1=ALU.add),
                              reads=SR, writes=[(L, 'bis')])
                        sc.op('dve', lambda e, W=W: e.tensor_scalar(out=nmq[:, 0:W], in0=score[:, 0:W], scalar1=mid[:, 0:1], scalar2=0.0, op0=ALU.is_ge,
                                                                   op1=ALU.add, accum_out=cnt[:, 0:1]), reads=SR + [(L, 'nmq')], writes=[(L, 'bis'), (L, 'nmq')])
                        sc.op('dve', lambda e, cst=cst: e.tensor_scalar(out=ff[:], in0=cnt[:], scalar1=256.0, scalar2=cst, op0=ALU.is_ge, op1=ALU.mult),
                              reads=SR, writes=[(L, 'bis')])
                        sc.op('dve', lambda e: e.scalar_tensor_tensor(out=lo[:], in0=ff[:], scalar=w0[:, 0:1], in1=lo[:], op0=ALU.mult, op1=ALU.add),
                              reads=SR, writes=[(L, 'bis')])
                sc.op('dve', lambda e, W=W: e.tensor_scalar(out=nmq[:, 0:W], in0=score[:, 0:W], scalar1=lo[:, 0:1], scalar2=-30000.0, op0=ALU.is_lt, op1=ALU.mult),
                      reads=SR + [(L, 'nmq')], writes=[(L, 'nmq')])
            def tr(qb):
                gq = blk * 4 + qb
                nk = gq + 1
                W = nk * P
                ms = gq % 2
                mn_ = (L, 'nmT', ms)
                for j0 in range(0, nk, 8):
                    jn = min(8, nk - j0)
                    pn, pt = self.bank()
                    ptb = pt[:].bitcast(BF16)
                    fns = [lambda e, ptb=ptb, j=j, j0=j0: e.transpose(out=ptb[:, (j - j0) * P:(j - j0 + 1) * P], in_=nmq[:, j * P:(j + 1) * P], identity=identb[:])
                           for j in range(j0, j0 + jn)]
                    sc.op('pe', fns, reads=[(L, 'nmq'), 'identb'], writes=[pn])
                    sc.op('act', lambda e, ptb=ptb, ms=ms, j0=j0, jn=jn: e.activation(out=nmT[ms][:, j0:j0 + jn, :], in_=ptb[:, 0:jn * P].rearrange('p (a b) -> p a b', b=P),
                                                                                func=AF.Copy), reads=[pn], writes=[(mn_, j0)])
            def att(qb):
                gq = blk * 4 + qb
                nk = gq + 1
                W = nk * P
                nonlocal npx
                ms = gq % 2
                mn_ = (L, 'nmT', ms)
                for kvh in range(4 if self.cfg.get('astage', 9) >= 4 else 0):
                    ab = self.abank[self.nab % 2]
                    self.nab += 1
                    pon = ('ps', ab)
                    po = self.ps[ab]
                    b0 = (kvh % 2) * 64
                    stb = {}

                    def emit_st(j, kvh=kvh, b0=b0, stb=stb):
                        pn, pt = self.bank()
                        stb[j] = (pn, pt)
                        near = (nk - 1 - j) if (nk - 1 - j) < 2 else None
                        fns = []
                        for g in range(4):
                            h = kvh * 4 + g
                            fns.append(lambda e, pt=pt, g=g, j=j, b0=b0, kvh=kvh, qb=qb: e.matmul(
                                pt[:, g * P:(g + 1) * P], lhsT=KT[b0:b0 + 64, kvh // 2, j * P:(j + 1) * P],
                                rhs=QT[b0:b0 + 64, (kvh // 2) * 4 + g, qb * P:(qb + 1) * P], start=True, stop=False))
                            if near is not None:
                                fns.append(lambda e, pt=pt, g=g, h=h, near=near: e.matmul(
                                    pt[:, g * P:(g + 1) * P], lhsT=identb[:], rhs=biasS[:, near * 16 + h, :], start=False, stop=False))
                            fns.append(lambda e, pt=pt, g=g, j=j, ms=ms: e.matmul(
                                pt[:, g * P:(g + 1) * P], lhsT=identb[:], rhs=nmT[ms][:, j, :], start=False, stop=True))
                        sc.op('pe', fns, reads=[(L, 'KT', kvh // 2, j // 4), (mn_, (j // 8) * 8), 'identb', (L, 'biasS')] +
                              [(L, 'QT', (kvh // 2) * 4 + g) for g in range(4)], writes=[pn])
                    emit_st(0)
                    for j in range(nk):
                        if j + 1 < nk:
                            emit_st(j + 1)
                        pn, pt = stb[j]
                        px = npx % 3
                        npx += 1
                        pxn = 'pexp%d' % px
                        sc.op('act', lambda e, pt=pt, px=px: e.activation(out=pexp[px][:], in_=pt[:], func=AF.Exp), reads=[pn], writes=[pxn])
                        sc.op('pe', lambda e, po=po, px=px, j=j, kvh=kvh, nk=nk: e.matmul(po[0:65, :], lhsT=VA[:, j, kvh, :], rhs=pexp[px][:],
                                                                                     start=(j == 0), stop=(j == nk - 1)),
                              reads=[pxn, (L, 'VA', j), (L, 'VAones')], writes=[pon])
                    sc.op('act', lambda e, po=po: e.activation(out=osb[:], in_=po[0:65, :], func=AF.Copy), reads=[pon], writes=['osb'])
                    sc.op('act', lambda e: e.activation(out=osb[64:65, :], in_=osb[64:65, :], func=AF.Ln), reads=['osb'], writes=['osb'])
                    sc.op('act', lambda e: e.activation(out=osb[64:65, :], in_=osb[64:65, :], func=AF.Exp, scale=-1.0), reads=['osb'], writes=['osb'])
                    pn, pt = self.bank()
                    sc.op('pe', lambda e, pt=pt: e.matmul(pt[0:64, :], lhsT=selrow[:], rhs=osb[:], start=True, stop=True), reads=['osb', 'selrow'], writes=[pn])
                    sc.op('act', lambda e, pt=pt: e.activation(out=rbc[0:64, :], in_=pt[0:64, :], func=AF.Copy), reads=[pn], writes=['rbc'])
                    sc.op('pool', lambda e, kvh=kvh, qb=qb: e.tensor_tensor(out=OTn[:, kvh * 4:(kvh + 1) * 4, qb * P:(qb + 1) * P],
                                                                        in0=osb[0:64, :].rearrange('p (a b) -> p a b', b=P),
                                                                        in1=rbc[0:64, :].rearrange('p (a b) -> p a b', b=P), op=ALU.mult),
                          reads=['osb', 'rbc'], writes=[(L, 'OTn', kvh, qb)])
            nq = 4 if self.cfg.get('astage', 9) >= 2 else 0
            if nq:
                idx_bis(0)
                tr(0)
            for qb in range(nq):
                if qb + 1 < nq:
                    idx_bis(qb + 1)
                att(qb)
                if qb + 1 < nq:
                    tr(qb + 1)
            if self.cfg.get('astage', 9) < 5:
                continue
            oreads = [(L, 'OTn', kvh, qb) for kvh in range(4) for qb in range(4)] + [(L, 'wA')]
            for c in range(DC):
                if c % 4 == 0:
                    for h in range(16):
                        sc.dma('pool', 'wA', wout[:, h, :], self.d_awout[:, h, (c // 4) * 512:(c // 4 + 1) * 512], writes=[(L, 'wA')])
                pn, pt = self.bank()
                fns = [lambda e, pt=pt, h=h, c=c: e.matmul(pt[:], lhsT=wout[:, h, (c % 4) * P:(c % 4 + 1) * P], rhs=OTn[:, h, :], start=(h == 0), stop=(h == 15))
                       for h in range(16)]
                sc.op('pe', fns, reads=oreads, writes=[pn])
                sc.op('dve', lambda e, pt=pt, c=c, t=t: e.scalar_tensor_tensor(out=t[:, c, :], in0=pt[:], scalar=self.modv(li, 2, c), in1=t[:, c, :],
                                                                              op0=ALU.mult, op1=ALU.add), reads=[pn, (name, c), ('modT', li)], writes=[(name, c)])
                self.store_xT(blk, name, t, c)
        self.rot = list(range(8))
        sc.barrier()
        st.close()

    def ssm_setup(self):
        self.d_lamT = self.inp('ssm_lamT', [P, 32, 2])
        self.d_lstepT = self.inp('ssm_lstepT', [P, 32])
        self.d_Bblk = self.inp('ssm_Bblk', [32, P, 256])
        self.d_Cblk = self.inp('ssm_Cblk', [32, P, 256])
        self.d_dT = self.inp('ssm_dT', [P, DC])
        self.d_glu = self.inp('ssm_glu_r', [DC, P, DC * 256])

    def ssm_mixer(self, li):
        import math
        sc = self.sc
        st = contextlib.ExitStack()
        L = 'S'
        LC = 128
        I32 = mybir.dt.int32
        Ec = self.sb('Ec', [P, 32, LC], F32, st)
        En = self.sb('En', [P, 32, LC], F32, st)
        Bb = self.sb('Bb', [P, 32, 256], BF16, st)
        Cb = self.sb('Cb', [P, 32, 256], BF16, st)
        sm = {n: self.sb('ss_' + n, [P, 32], F32, st) for n in
              ['lr', 'li', 'stp', 'th', 'rr', 'u', 'f', 'g', 'sn', 'cs', 'x', 'y', 'den', 'cr', 'ci', 'cL', 'nL', 'ire', 'iim', 'e1c', 'e1n', 't1', 't2']}
        ni = self.sb('ss_ni', [P, 32], I32, st)
        lam = self.sb('ss_lam', [P, 32, 2], F32, st)
        dT = self.sb('ss_dT', [P, DC], F32, st)
        cs1 = self.sb('ss_c1', [P, 4], F32, st)
        SM = [(L, 'sm')]

        def dv(fn, extra_r=(), extra_w=()):
            sc.op('dve', fn, reads=SM + list(extra_r), writes=SM + list(extra_w))

        def ac(fn):
            sc.op('act', fn, reads=SM, writes=SM)
        sc.dma('sp', 'const', lam[:], self.d_lamT[:, :, :], writes=SM)
        sc.dma('sp', 'const', sm['stp'][:], self.d_lstepT[:, :], writes=SM)
        sc.dma('sp', 'const', dT[:], self.d_dT[:, :], writes=[(L, 'dT')])
        for k in range(32):
            sc.dma('pool', 'Bb', Bb[:, k, :], self.d_Bblk[k, :, :], writes=[(L, 'Bb')])
        dv(lambda e: e.tensor_scalar(out=sm['lr'][:], in0=lam[:, :, 0], scalar1=-1e-4, scalar2=None, op0=ALU.min))
        dv(lambda e: e.tensor_copy(out=sm['li'][:], in_=lam[:, :, 1]))
        ac(lambda e: e.activation(out=sm['stp'][:], in_=sm['stp'][:], func=AF.Exp))
        dv(lambda e: e.tensor_tensor(out=sm['th'][:], in0=sm['li'][:], in1=sm['stp'][:], op=ALU.mult))
        dv(lambda e: e.tensor_tensor(out=sm['rr'][:], in0=sm['lr'][:], in1=sm['stp'][:], op=ALU.mult))
        ac(lambda e: e.activation(out=sm['rr'][:], in_=sm['rr'][:], func=AF.Exp))

        def sincos(dst, off):
            dv(lambda e: e.tensor_scalar(out=sm['u'][:], in0=sm['th'][:], scalar1=1.0 / (2 * math.pi), scalar2=off, op0=ALU.mult, op1=ALU.add))
            dv(lambda e: e.tensor_copy(out=ni[:], in_=sm['u'][:]))
            dv(lambda e: e.tensor_copy(out=sm['f'][:], in_=ni[:]))
            dv(lambda e: e.tensor_tensor(out=sm['f'][:], in0=sm['u'][:], in1=sm['f'][:], op=ALU.subtract))
            dv(lambda e: e.tensor_scalar(out=sm['g'][:], in0=sm['f'][:], scalar1=0.5, scalar2=None, op0=ALU.is_gt))
            dv(lambda e: e.tensor_tensor(out=sm['f'][:], in0=sm['f'][:], in1=sm['g'][:], op=ALU.subtract))
            dv(lambda e: e.tensor_scalar(out=sm['g'][:], in0=sm['f'][:], scalar1=-0.5, scalar2=None, op0=ALU.is_lt))
            dv(lambda e: e.tensor_tensor(out=sm['f'][:], in0=sm['f'][:], in1=sm['g'][:], op=ALU.add))
            ac(lambda e, dst=dst: e.activation(out=sm[dst][:], in_=sm['f'][:], func=AF.Sin, scale=-2 * math.pi))
        sincos('sn', 64.5)
        sincos('cs', 64.75)
        dv(lambda e: e.tensor_tensor(out=sm['x'][:], in0=sm['rr'][:], in1=sm['cs'][:], op=ALU.mult))
        dv(lambda e: e.tensor_scalar(out=sm['x'][:], in0=sm['x'][:], scalar1=-1.0, scalar2=None, op0=ALU.add))
        dv(lambda e: e.tensor_tensor(out=sm['y'][:], in0=sm['rr'][:], in1=sm['sn'][:], op=ALU.mult))
        dv(lambda e: e.tensor_tensor(out=sm['den'][:], in0=sm['lr'][:], in1=sm['lr'][:], op=ALU.mult))
        dv(lambda e: e.tensor_tensor(out=sm['t1'][:], in0=sm['li'][:], in1=sm['li'][:], op=ALU.mult))
        dv(lambda e: e.tensor_tensor(out=sm['den'][:], in0=sm['den'][:], in1=sm['t1'][:], op=ALU.add))
        dv(lambda e: e.reciprocal(out=sm['den'][:], in_=sm['den'][:]))
        dv(lambda e: e.tensor_tensor(out=sm['cr'][:], in0=sm['x'][:], in1=sm['lr'][:], op=ALU.mult))
        dv(lambda e: e.tensor_tensor(out=sm['t1'][:], in0=sm['y'][:], in1=sm['li'][:], op=ALU.mult))
        dv(lambda e: e.tensor_tensor(out=sm['cr'][:], in0=sm['cr'][:], in1=sm['t1'][:], op=ALU.add))
        dv(lambda e: e.tensor_tensor(out=sm['cr'][:], in0=sm['cr'][:], in1=sm['den'][:], op=ALU.mult))
        dv(lambda e: e.tensor_tensor(out=sm['ci'][:], in0=sm['y'][:], in1=sm['lr'][:], op=ALU.mult))
        dv(lambda e: e.tensor_tensor(out=sm['t1'][:], in0=sm['x'][:], in1=sm['li'][:], op=ALU.mult))
        dv(lambda e: e.tensor_tensor(out=sm['ci'][:], in0=sm['ci'][:], in1=sm['t1'][:], op=ALU.subtract))
        dv(lambda e: e.tensor_tensor(out=sm['ci'][:], in0=sm['ci'][:], in1=sm['den'][:], op=ALU.mult))
        st2 = contextlib.ExitStack()
        Tc = self.sb('Tc', [P, LC, 32], F32, st2)
        Tn = self.sb('Tn', [P, LC, 32], F32, st2)
        U1 = self.sb('U1', [P, LC // 2, 32], F32, st2)
        U2 = self.sb('U2', [P, LC // 2, 32], F32, st2)
        cst1 = self.sb('tp1s', [P, 512], F32, st2)
        cst2 = self.sb('tp2s', [P, 512], F32, st2)
        dv(lambda e: e.tensor_copy(out=sm['e1c'][:], in_=sm['cs'][:]))
        dv(lambda e: e.tensor_scalar(out=sm['e1n'][:], in0=sm['sn'][:], scalar1=-1.0, scalar2=None, op0=ALU.mult))
        dv(lambda e: e.memset(Tc[:, 0, :], 1.0), extra_w=[(L, 'T')])
        dv(lambda e: e.memset(Tn[:, 0, :], 0.0), extra_w=[(L, 'T')])
        TT = [(L, 'T')]
        n = 1
        while n < LC:
            ecb = sm['e1c'][:, :].unsqueeze(1).to_broadcast([P, n, 32])
            enb = sm['e1n'][:, :].unsqueeze(1).to_broadcast([P, n, 32])
            dv(lambda e, n=n, ecb=ecb: e.tensor_tensor(out=Tc[:, n:2 * n, :], in0=Tc[:, 0:n, :], in1=ecb, op=ALU.mult), TT, TT)
            dv(lambda e, n=n, enb=enb: e.tensor_tensor(out=U1[:, 0:n, :], in0=Tn[:, 0:n, :], in1=enb, op=ALU.mult), TT, TT)
            dv(lambda e, n=n: e.tensor_tensor(out=Tc[:, n:2 * n, :], in0=Tc[:, n:2 * n, :], in1=U1[:, 0:n, :], op=ALU.subtract), TT, TT)
            dv(lambda e, n=n, enb=enb: e.tensor_tensor(out=Tn[:, n:2 * n, :], in0=Tc[:, 0:n, :], in1=enb, op=ALU.mult), TT, TT)
            dv(lambda e, n=n, ecb=ecb: e.tensor_tensor(out=U2[:, 0:n, :], in0=Tn[:, 0:n, :], in1=ecb, op=ALU.mult), TT, TT)
            dv(lambda e, n=n: e.tensor_tensor(out=Tn[:, n:2 * n, :], in0=Tn[:, n:2 * n, :], in1=U2[:, 0:n, :], op=ALU.add), TT, TT)
            dv(lambda e: e.tensor_tensor(out=sm['t1'][:], in0=sm['e1c'][:], in1=sm['e1c'][:], op=ALU.mult))
            dv(lambda e: e.tensor_tensor(out=sm['t2'][:], in0=sm['e1n'][:], in1=sm['e1n'][:], op=ALU.mult))
            dv(lambda e: e.tensor_tensor(out=sm['e1n'][:], in0=sm['e1c'][:], in1=sm['e1n'][:], op=ALU.mult))
            dv(lambda e: e.tensor_scalar(out=sm['e1n'][:], in0=sm['e1n'][:], scalar1=2.0, scalar2=None, op0=ALU.mult))
            dv(lambda e: e.tensor_tensor(out=sm['e1c'][:], in0=sm['t1'][:], in1=sm['t2'][:], op=ALU.subtract))
            n *= 2
        dv(lambda e: e.tensor_copy(out=sm['cL'][:], in_=sm['e1c'][:]))
        dv(lambda e: e.tensor_copy(out=sm['nL'][:], in_=sm['e1n'][:]))
        dv(lambda e: e.memset(sm['ire'][:], 0.0))
        dv(lambda e: e.memset(sm['iim'][:], 0.0))
        dv(lambda e: e.tensor_copy(out=Ec[:, :, :], in_=Tc[:, :, :].rearrange('p t k -> p k t')), TT, [(L, 'E')])
        dv(lambda e: e.tensor_copy(out=En[:, :, :], in_=Tn[:, :, :].rearrange('p t k -> p k t')), TT, [(L, 'E')])
        for k in range(32):
            sc.dma('sp', 'cstage', cst1[:, 0:256], self.d_Cblk[k, :, :], writes=['cst1'])
            crk = sm['cr'][:, k:k + 1]
            cik = sm['ci'][:, k:k + 1]
            sc.op('dve', lambda e, cik=cik: e.tensor_scalar(out=cst2[:, 0:128], in0=cst1[:, 128:256], scalar1=cik, scalar2=None, op0=ALU.mult),
                  reads=['cst1'] + SM, writes=['cst2'])
            sc.op('dve', lambda e, k=k, crk=crk: e.scalar_tensor_tensor(out=Cb[:, k, 0:128], in0=cst1[:, 0:128], scalar=crk, in1=cst2[:, 0:128], op0=ALU.mult, op1=ALU.subtract),
                  reads=['cst1', 'cst2'] + SM, writes=[(L, 'Cb')])
            sc.op('dve', lambda e, crk=crk: e.tensor_scalar(out=cst2[:, 128:256], in0=cst1[:, 128:256], scalar1=crk, scalar2=None, op0=ALU.mult),
                  reads=['cst1'] + SM, writes=['cst2'])
            sc.op('dve', lambda e, k=k, cik=cik: e.scalar_tensor_tensor(out=Cb[:, k, 128:256], in0=cst1[:, 0:128], scalar=cik, in1=cst2[:, 128:256], op0=ALU.mult, op1=ALU.add),
                  reads=['cst1', 'cst2'] + SM, writes=[(L, 'Cb')])
        if self.cfg.get('sdebug'):
            dbg = self.nc.dram_tensor('dbg', [P, 7 * 32 + 512 + 512], F32, kind="ExternalOutput").ap()
            for i, nme in enumerate(['sn', 'cs', 'rr', 'cr', 'ci', 'cL', 'nL']):
                sc.dma('sp', 'const', dbg[:, i * 32:(i + 1) * 32], sm[nme][:], reads=SM, writes=[('dbg', i)])
            sc.dma('sp', 'const', dbg[:, 224:224 + 256], Ec[:, 0:2, :], reads=[(L, 'E')], writes=[('dbg', 10)])
            sc.dma('sp', 'const', dbg[:, 224 + 256:224 + 512], En[:, 0:2, :], reads=[(L, 'E')], writes=[('dbg', 11)])
            sc.dma('sp', 'const', dbg[:, 224 + 512:224 + 768], Cb[:, 0, :], reads=[(L, 'Cb')], writes=[('dbg', 12)], allow_dtype=True) if False else None
        sc.barrier()
        st2.close()
        hT = self.sb('hTs', [P, DC, 512], BF16, st)
        h32 = self.sb('h32s', [P, DC, 512], F32, st)
        GT = self.sb('GTs', [P, DC, 512], BF16, st)
        Sb = self.sb('Sb', [P, 4, 2, 512], BF16, st)
        wgl = [self.sb('wgl%d' % i, [P, DC * 256], BF16, st) for i in range(2)]
        bre = self.sb('bre', [P, 512], F32, st)
        bim = self.sb('bim', [P, 512], F32, st)
        wre = self.sb('wre', [P, 512], F32, st)
        wim = self.sb('wim', [P, 512], F32, st)
        tq = self.sb('tq', [P, 512], F32, st)
        tp1 = self.sb('tp1', [P, 512], F32, st)
        tp2 = self.sb('tp2', [P, 512], F32, st)
        nw = 0
        for blk in range(self.cfg.get('snblk', NBLK)):
            name, t = self.load_xT(blk, False)
            self.norm_mod(li, 0, name, t, hT, (L, 'hT'), 0, h32=h32)
            for j in range(DC):
                for kk in range(4):
                    k = 4 * j + kk
                    pxr_n, pxr = self.bank()
                    pxi_n, pxi = self.bank()
                    hr = [((L, 'hT'), j, 0), (L, 'Bb')]
                    sc.op('pe', lambda e, pxr=pxr, k=k, j=j: e.matmul(pxr[:], lhsT=Bb[:, k, 0:128], rhs=hT[:, j, :], start=True, stop=True), reads=hr, writes=[pxr_n])
                    sc.op('pe', lambda e, pxi=pxi, k=k, j=j: e.matmul(pxi[:], lhsT=Bb[:, k, 128:256], rhs=hT[:, j, :], start=True, stop=True), reads=hr, writes=[pxi_n])
                    cb = Ec[:, k, :].unsqueeze(1).to_broadcast([P, 4, LC])
                    nb = En[:, k, :].unsqueeze(1).to_broadcast([P, 4, LC])

                    def v3(ap):
                        return ap.rearrange('p (a b) -> p a b', b=LC)
                    ER = [(L, 'E')]
                    sc.op('dve', lambda e, pxr=pxr, cb=cb: e.tensor_tensor(out=v3(bre[:, :]), in0=v3(pxr[:, :]), in1=cb, op=ALU.mult), reads=[pxr_n] + ER, writes=['bre'])
                    sc.op('dve', lambda e, pxi=pxi, nb=nb: e.tensor_tensor(out=v3(tq[:, :]), in0=v3(pxi[:, :]), in1=nb, op=ALU.mult), reads=[pxi_n] + ER, writes=['tq'])
                    sc.op('dve', lambda e: e.tensor_tensor(out=bre[:], in0=bre[:], in1=tq[:], op=ALU.subtract), reads=['bre', 'tq'], writes=['bre'])
                    sc.op('dve', lambda e, pxi=pxi, cb=cb: e.tensor_tensor(out=v3(bim[:, :]), in0=v3(pxi[:, :]), in1=cb, op=ALU.mult), reads=[pxi_n] + ER, writes=['bim'])
                    sc.op('dve', lambda e, pxr=pxr, nb=nb: e.tensor_tensor(out=v3(tq[:, :]), in0=v3(pxr[:, :]), in1=nb, op=ALU.mult), reads=[pxr_n] + ER, writes=['tq'])
                    sc.op('dve', lambda e: e.tensor_tensor(out=bim[:], in0=bim[:], in1=tq[:], op=ALU.add), reads=['bim', 'tq'], writes=['bim'])
                    rb = sm['rr'][:, k:k + 1].to_broadcast([P, LC])
                    cLk = sm['cL'][:, k:k + 1]
                    nLk = sm['nL'][:, k:k + 1]
                    irek = sm['ire'][:, k:k + 1]
                    iimk = sm['iim'][:, k:k + 1]
                    CR = [(L, 'carry', k)]
                    for ch in range(4):
                        lo_, hi_ = ch * LC, (ch + 1) * LC
                        sc.op('dve', lambda e, rb=rb, irek=irek, lo_=lo_, hi_=hi_: e.tensor_tensor_scan(out=wre[:, lo_:hi_], data0=rb, data1=bre[:, lo_:hi_], initial=irek,
                                                                                                 op0=ALU.mult, op1=ALU.add), reads=['bre'] + CR + SM, writes=['wre'])
                        sc.op('dve', lambda e, rb=rb, iimk=iimk, lo_=lo_, hi_=hi_: e.tensor_tensor_scan(out=wim[:, lo_:hi_], data0=rb, data1=bim[:, lo_:hi_], initial=iimk,
                                                                                                 op0=ALU.mult, op1=ALU.add), reads=['bim'] + CR + SM, writes=['wim'])
                        wrl = wre[:, hi_ - 1:hi_]
                        wil = wim[:, hi_ - 1:hi_]
                        sc.op('dve', lambda e, nLk=nLk, wil=wil: e.tensor_tensor(out=cs1[:, 0:1], in0=wil, in1=nLk, op=ALU.mult), reads=['wim'] + SM, writes=['cs1'])
                        sc.op('dve', lambda e, cLk=cLk, wrl=wrl, irek=irek: e.scalar_tensor_tensor(out=irek, in0=wrl, scalar=cLk, in1=cs1[:, 0:1], op0=ALU.mult, op1=ALU.add),
                              reads=['wre', 'cs1'] + SM, writes=CR)
                        sc.op('dve', lambda e, nLk=nLk, wrl=wrl: e.tensor_tensor(out=cs1[:, 1:2], in0=wrl, in1=nLk, op=ALU.mult), reads=['wre'] + SM, writes=['cs1'])
                        sc.op('dve', lambda e, cLk=cLk, wil=wil, iimk=iimk: e.scalar_tensor_tensor(out=iimk, in0=wil, scalar=cLk, in1=cs1[:, 1:2], op0=ALU.mult, op1=ALU.subtract),
                              reads=['wim', 'cs1'] + SM, writes=CR)
                    if self.cfg.get('sdebug') and blk == 0 and k == 1:
                        dbg2 = self.nc.dram_tensor('dbg2', [P, 4, 512], F32, kind="ExternalOutput").ap()
                        sc.dma('sp', 'const', dbg2[:, 0, :], bre[:], reads=['bre'], writes=[('dbg2', 0)])
                        sc.dma('sp', 'const', dbg2[:, 1, :], bim[:], reads=['bim'], writes=[('dbg2', 1)])
                        sc.dma('sp', 'const', dbg2[:, 2, :], wre[:], reads=['wre'], writes=[('dbg2', 2)])
                        sc.dma('sp', 'const', dbg2[:, 3, :], wim[:], reads=['wim'], writes=[('dbg2', 3)])
                    sc.op('pool', lambda e, cb=cb: e.tensor_tensor(out=v3(tp1[:, :]), in0=v3(wre[:, :]), in1=cb, op=ALU.mult), reads=['wre'] + ER, writes=['tp1'])
                    sc.op('pool', lambda e, nb=nb: e.tensor_tensor(out=v3(tp2[:, :]), in0=v3(wim[:, :]), in1=nb, op=ALU.mult), reads=['wim'] + ER, writes=['tp2'])
                    sc.op('pool', lambda e, kk=kk: e.tensor_tensor(out=Sb[:, kk, 0, :], in0=tp1[:], in1=tp2[:], op=ALU.add), reads=['tp1', 'tp2'], writes=[(L, 'Sb', kk)])
                    sc.op('pool', lambda e, nb=nb: e.tensor_tensor(out=v3(tp1[:, :]), in0=v3(wre[:, :]), in1=nb, op=ALU.mult), reads=['wre'] + ER, writes=['tp1'])
                    sc.op('pool', lambda e, cb=cb: e.tensor_tensor(out=v3(tp2[:, :]), in0=v3(wim[:, :]), in1=cb, op=ALU.mult), reads=['wim'] + ER, writes=['tp2'])
                    sc.op('pool', lambda e, kk=kk: e.tensor_tensor(out=Sb[:, kk, 1, :], in0=tp1[:], in1=tp2[:], op=ALU.subtract), reads=['tp1', 'tp2'], writes=[(L, 'Sb', kk)])
                if self.cfg.get('sdebug') and blk == 0 and j == 0:
                    dbg3 = self.nc.dram_tensor('dbg3', [P, 2, 512], F32, kind="ExternalOutput").ap()
                    sc.dma('pool', 'dbg3', dbg3[:, 0, :], Sb[:, 1, 0, :], reads=[(L, 'Sb', 1)], writes=[('dbg3', 0)])
                    sc.dma('pool', 'dbg3', dbg3[:, 1, :], Sb[:, 1, 1, :], reads=[(L, 'Sb', 1)], writes=[('dbg3', 1)])
                pyn, py = self.bank()
                fns = []
                for kk in range(4):
                    k = 4 * j + kk
                    fns.append(lambda e, py=py, k=k, kk=kk: e.matmul(py[:], lhsT=Cb[:, k, 0:128], rhs=Sb[:, kk, 0, :], start=(kk == 0), stop=False))
                    fns.append(lambda e, py=py, k=k, kk=kk: e.matmul(py[:], lhsT=Cb[:, k, 128:256], rhs=Sb[:, kk, 1, :], start=False, stop=(kk == 3)))
                sc.op('pe', fns, reads=[(L, 'Sb', kk) for kk in range(4)] + [(L, 'Cb')], writes=[pyn])
                if self.cfg.get('sdebug') == 2 and blk == 0 and j == 0:
                    sc.op('act', lambda e, py=py: e.activation(out=bre[:], in_=py[:], func=AF.Copy), reads=[pyn, 'bre'], writes=['bre'])
                    sc.dma('sp', 'const', dbg2[:, 4, :], bre[:], reads=['bre'], writes=[('dbg2', 4)])
                z = self.tmp32[0]
                w_ = self.tmp32[1]
                sc.op('dve', lambda e, py=py, j=j, z=z: e.scalar_tensor_tensor(out=z[:], in0=h32[:, j, :], scalar=dT[:, j:j + 1], in1=py[:], op0=ALU.mult, op1=ALU.add),
                      reads=[pyn, ('h32', j), (L, 'dT')], writes=['tmp32_0'])
                if self.cfg.get('sdebug') and blk == 0 and j == 0:
                    dbg4 = self.nc.dram_tensor('dbg4', [P, 3, 512], F32, kind="ExternalOutput").ap()
                    sc.dma('sp', 'const', dbg4[:, 0, :], z[:], reads=['tmp32_0'], writes=[('dbg4', 0)])
                    sc.dma('pool', 'dbg4b', dbg4[:, 1, 0:256], Cb[:, 1, :], reads=[(L, 'Cb')], writes=[('dbg4', 1)])
                    sc.dma('pool', 'dbg4b', dbg4[:, 1, 256:512], Cb[:, 1, :], reads=[(L, 'Cb')], writes=[('dbg4', 3)])
                sc.op('act', lambda e, z=z, w_=w_: e.activation(out=w_[:], in_=z[:], func=AF.Square), reads=['tmp32_0'], writes=['tmp32_1'])
                sc.op('dve', lambda e, w_=w_: e.tensor_scalar(out=w_[:], in0=w_[:], scalar1=0.044715, scalar2=1.0, op0=ALU.mult, op1=ALU.add), reads=['tmp32_1'], writes=['tmp32_1'])
                sc.op('dve', lambda e, z=z, w_=w_: e.tensor_tensor(out=w_[:], in0=w_[:], in1=z[:], op=ALU.mult), reads=['tmp32_0', 'tmp32_1'], writes=['tmp32_1'])
                sc.op('act', lambda e, w_=w_: e.activation(out=w_[:], in_=w_[:], func=AF.Sigmoid, scale=1.5957691216057308), reads=['tmp32_1'], writes=['tmp32_1'])
                sc.op('dve', lambda e, z=z, w_=w_, j=j: e.tensor_tensor(out=GT[:, j, :], in0=z[:], in1=w_[:], op=ALU.mult), reads=['tmp32_0', 'tmp32_1'], writes=[(L, 'GT', j)])
            if self.cfg.get('sdebug') and blk == 0:
                sc.dma('pool', 'dbg4b', dbg4[:, 2, :], GT[:, 0, :], reads=[(L, 'GT', 0)], writes=[('dbg4', 2)])
            greads = [(L, 'GT', j) for j in range(DC)]
            for c in range(DC):
                slot = nw % 2
                nw += 1
                wn = (L, 'wgl', slot)
                self.load_w_cast('wgl%d' % slot, wgl[slot][:, :], self.d_glu[c, :, :], None, writes=[wn])
                pln, pl = self.bank()
                pgn, pg = self.bank()
                for (pn, pt, off) in ((pln, pl, 0), (pgn, pg, 128)):
                    fns = [lambda e, pt=pt, kq=kq, off=off, slot=slot: e.matmul(pt[:], lhsT=wgl[slot][:, kq * 256 + off: kq * 256 + off + 128], rhs=GT[:, kq, :],
                                                                              start=(kq == 0), stop=(kq == DC - 1)) for kq in range(DC)]
                    sc.op('pe', fns, reads=greads + [wn], writes=[pn])
                tm = self.tmp32[2]
                sc.op('act', lambda e, pg=pg, tm=tm: e.activation(out=tm[:], in_=pg[:], func=AF.Sigmoid), reads=[pgn], writes=['tmp32_2'])
                sc.op('dve', lambda e, pl=pl, tm=tm: e.tensor_tensor(out=tm[:], in0=pl[:], in1=tm[:], op=ALU.mult), reads=[pln, 'tmp32_2'], writes=['tmp32_2'])
                sc.op('dve', lambda e, tm=tm, c=c, t=t: e.scalar_tensor_tensor(out=t[:, c, :], in0=tm[:], scalar=self.modv(li, 2, c), in1=t[:, c, :], op0=ALU.mult, op1=ALU.add),
                      reads=['tmp32_2', (name, c), ('modT', li)], writes=[(name, c)])
                self.store_xT(blk, name, t, c)
        sc.barrier()
        st.close()

    def epilogue(self):
        sc = self.sc
        self.out = self.nc.dram_tensor('out', [self.ntok, D], F32, kind="ExternalOutput").ap()
        otok = [self.sb('otok%d' % i, [P, D], F32) for i in range(2)]
        n = 0
        for blk in range(self.ntok // 512):
            name, t = self.load_xT(blk, False)
            for j in range(4):
                slot = n % 2
                n += 1
                on = 'otok%d' % slot
                for hh in range(2):
                    pname, pt = self.bank()
                    fns = [lambda e, pt=pt, j=j, c=c, hh=hh, t=t: e.transpose(out=pt[:, (c - hh * 4) * P:(c - hh * 4 + 1) * P],
                                                                      in_=t[:, c, j * P:(j + 1) * P], identity=self.ident[:])
                           for c in range(hh * 4, hh * 4 + 4)]
                    sc.op('pe', fns, reads=[(name, c) for c in range(DC)] + ['ident'], writes=[pname])
                    if hh == 0:
                        sc.op('act', lambda e, pt=pt, slot=slot: e.activation(out=otok[slot][:, 0:512], in_=pt[:], func=AF.Copy),
                              reads=[pname], writes=[(on, 0)])
                    else:
                        sc.op('dve', lambda e, pt=pt, slot=slot: e.tensor_copy(out=otok[slot][:, 512:1024], in_=pt[:]),
                              reads=[pname], writes=[(on, 1)])
                sc.dma('sp', 'st_' + on, self.out[blk * 512 + j * P: blk * 512 + (j + 1) * P, :], otok[slot][:],
                       reads=[(on, 0), (on, 1)], writes=[('out', blk, j)])

    def build(self):
        cfg = self.cfg
        self.setup_common()
        self.epsb = self.sb('epsb', [P, 1], F32)
        self.sc.op('dve', lambda e: e.memset(self.epsb[:], EPS), writes=['epsb'])
        self.build_mod()
        self.alloc_stream()
        self.conv_setup()
        self.ffn_setup()
        self.attn_setup()
        self.ssm_setup()
        steps = cfg.get('steps', ['m0', 'f0', 'm1', 'f1', 'm2', 'f2', 'm3', 'f3'])
        first = True
        if steps[0][0] != 'm' or int(steps[0][1]) % 3 != 0:
            st = contextlib.ExitStack()
            self.xtok = self.sb('xtok', [P, 4, D], F32, st)
            for blk in range(NBLK):
                name, t = self.load_xT(blk, True)
                for c in range(DC):
                    self.store_xT(blk, name, t, c)
            self.sc.barrier()
            st.close()
            first = False
        tail_split = cfg.get('tail_split', False)
        for s in steps:
            li = int(s[1])
            if tail_split and s == 'm3':
                self.sc.barrier()
                self.xs_d = self.nc.dram_tensor('xs_scratch', [D, S // 2 + 512], F32).ap()
                self.sc.dma('sp', 'xstage', self.xs_d[:, 512:512 + S // 2], (lambda: self.xT_d[:, bass.ds(self.rv * 2048, 2048)]), writes=['xs'])
                self.sc.dma('sp', 'xstage', self.xs_d[:, 0:512], (lambda: self.xT_d[:, bass.ds(self.rv * 1536, 512)]), writes=['xs'])
                self.rd_mode, self.wr_mode, self.ntok = 'dynfull', 'half', S // 2
            if tail_split and s == 'f3':
                self.rd_mode, self.wr_mode, self.ntok = 'half', 'half', S // 2
            if s[0] == 'm':
                if li % 3 == 0:
                    self.conv_mixer(li, li // 3, first)
                elif li % 3 == 1:
                    self.attn_mixer(li)
                else:
                    self.ssm_mixer(li)
                first = False
            else:
                self.ffn(li, li % 2 == 1)
        self.epilogue()
        self.sc.finish('sp')
        self.sc.emit()
        return self.nc


def host_layout(inputs, b):
    f = np.float32
    m = {}
    m['ident'] = np.eye(P, dtype=f)
    m['x'] = np.ascontiguousarray(inputs['x'][b])
    m['cT'] = np.ascontiguousarray(inputs['c'][b].reshape(DC, P).T)
    m['ada_w'] = inputs['ada_w']
    m['ada_bT'] = np.ascontiguousarray(inputs['ada_b'].reshape(4, 48, P).transpose(2, 0, 1))
    m['norm_gT'] = np.ascontiguousarray(inputs['norm_g'].reshape(4, 2, DC, P).transpose(3, 0, 1, 2))
    m['conv_w_in'] = inputs['conv_w_in']
    m['conv_w_out'] = inputs['conv_w_out']
    m['conv_wT'] = np.ascontiguousarray(inputs['conv_w'].reshape(2, 3, DC, P).transpose(3, 0, 1, 2))
    m['rankf'] = np.zeros((P, 1), dtype=f)
    return m


def rel_bucket_np(dist):
    import math
    d = np.maximum(dist, 1).astype(np.float32)
    large = 16 + (np.log(d / np.float32(16)) / np.float32(math.log(128 / 16)) * np.float32(16)).astype(np.int32)
    large = np.minimum(large, 31)
    return np.where(dist < 16, dist, large)


def ssm_layout(inputs):
    f = np.float32
    m = {}
    lre = inputs['ssm_lambda_re'][0]
    lim = inputs['ssm_lambda_im'][0]
    lamT = np.empty((P, 32, 2), dtype=f)
    lst = np.empty((P, 32), dtype=f)
    Bb = np.zeros((32, P, 2, P), dtype=f)
    Cb = np.zeros((32, P, 2, P), dtype=f)
    bre = inputs['ssm_b_re'][0]
    bim = inputs['ssm_b_im'][0]
    cre = inputs['ssm_c_re'][0]
    cim = inputs['ssm_c_im'][0]
    for k in range(32):
        for gg in range(2):
            g = 2 * k + gg
            lamT[gg * 64:(gg + 1) * 64, k, 0] = lre[g]
            lamT[gg * 64:(gg + 1) * 64, k, 1] = lim[g]
            lst[gg * 64:(gg + 1) * 64, k] = inputs['ssm_log_step'][0, g]
            r0 = (g % 8) * 16
            Bb[k, r0:r0 + 16, 0, gg * 64:(gg + 1) * 64] = bre[g].T
            Bb[k, r0:r0 + 16, 1, gg * 64:(gg + 1) * 64] = bim[g].T
            Cb[k, gg * 64:(gg + 1) * 64, 0, r0:r0 + 16] = cre[g].T
            Cb[k, gg * 64:(gg + 1) * 64, 1, r0:r0 + 16] = cim[g].T
    m['ssm_lamT'] = lamT
    m['ssm_lstepT'] = lst
    m['ssm_Bblk'] = Bb.reshape(32, P, 256)
    m['ssm_Cblk'] = Cb.reshape(32, P, 256)
    m['ssm_dT'] = np.ascontiguousarray(inputs['ssm_d'][0].reshape(DC, P).T)
    w = inputs['ssm_w_glu'][0]
    lin = w[:, :D].reshape(DC, P, DC, P)
    gate = w[:, D:].reshape(DC, P, DC, P)
    r = np.empty((DC, P, DC, 2, P), dtype=f)
    r[:, :, :, 0, :] = lin.transpose(2, 1, 0, 3)
    r[:, :, :, 1, :] = gate.transpose(2, 1, 0, 3)
    m['ssm_glu_r'] = r.reshape(DC, P, DC * 256)
    return m


def attn_layout(inputs):
    f = np.float32
    m = {}
    w = inputs['attn_w_in'][0]
    wq = w[:, 0:1024].reshape(D, 16, 64)
    order = []
    for pair in range(2):
        for g in range(4):
            order += [(2 * pair) * 4 + g, (2 * pair + 1) * 4 + g]
    m['attn_wq_perm'] = np.ascontiguousarray(wq[:, order, :]).reshape(D, 1024)
    m['attn_wk'] = np.ascontiguousarray(w[:, 1024:1280])
    m['attn_wv'] = np.ascontiguousarray(w[:, 1280:1536])
    m['attn_wqi'] = np.ascontiguousarray(w[:, 1536:2048])
    ki = w[:, 2048:2112]
    m['attn_wki2'] = np.ascontiguousarray(np.concatenate([ki, ki], axis=1))
    m['attn_wwi'] = np.ascontiguousarray(w[:, 2112:2120])
    m['attn_wout_r'] = np.ascontiguousarray(inputs['attn_w_out'][0].reshape(16, 64, D).transpose(1, 0, 2))
    qg = inputs['attn_q_gain'][0]
    kg = inputs['attn_k_gain'][0]
    m['attn_gainT'] = np.ascontiguousarray(np.stack([np.tile(qg, 2), np.tile(kg, 2)], axis=1))
    rb = inputs['rel_bias']
    sl = np.arange(P)[:, None]
    tl = np.arange(P)[None, :]
    bt = np.empty((P, 32, P), dtype=f)
    for kind in range(2):
        dist = np.maximum(tl - sl + kind * P, 0)
        bk = rel_bucket_np(dist)
        for h in range(16):
            bt[:, kind * 16 + h, :] = rb[bk, h]
    m['attn_biasT'] = bt
    m['attn_b31B'] = np.ascontiguousarray(np.broadcast_to(rb[31][None, :], (P, 16)))
    cm = np.zeros((P, P), dtype=f)
    cm[np.arange(P)[None, :] > np.arange(P)[:, None]] = -1e30
    m['cmask'] = cm
    bo = np.zeros((P, P), dtype=f)
    bo[0:64, 0:64] = 1.0
    bo[64:128, 64:128] = 1.0
    m['blockones'] = bo
    sr = np.zeros((65, 64), dtype=f)
    sr[64, :] = 1.0
    m['selrow'] = sr
    return m


_shared_cache = {}


def shared_layout(inputs):
    f = np.float32
    m = {}
    gu = inputs['ffn_w_gu']
    g = gu[:, :, :DFF].reshape(2, DC, P, FC, P)
    u = gu[:, :, DFF:].reshape(2, DC, P, FC, P)
    gu_r = np.stack([g, u], axis=4)
    m['ffn_gu_r'] = np.ascontiguousarray(gu_r.transpose(0, 3, 2, 1, 4, 5)).reshape(2, FC, P, DC * 256)
    dn = inputs['ffn_w_down'].reshape(2, FC, P, DC, P)
    m['ffn_dn_r'] = np.ascontiguousarray(dn.transpose(0, 3, 2, 1, 4)).reshape(2, DC, P, FC * P)
    gu = inputs['moe_w_gu']
    g = gu[:, :, :, :DFF].reshape(2, NE, DC, P, FC, P)
    u = gu[:, :, :, DFF:].reshape(2, NE, DC, P, FC, P)
    r = np.empty((2, NE, FC, P, DC, 2, P), dtype=f)
    r[:, :, :, :, :, 0, :] = g.transpose(0, 1, 4, 3, 2, 5)
    r[:, :, :, :, :, 1, :] = u.transpose(0, 1, 4, 3, 2, 5)
    m['moe_gu_r'] = r.reshape(2, NE, FC, P, DC * 256)
    dn = inputs['moe_w_down'].reshape(2, NE, FC, P, DC, P)
    m['moe_dn_r'] = np.ascontiguousarray(dn.transpose(0, 1, 4, 3, 2, 5)).reshape(2, NE, DC, P, FC * P)
    m['moe_rwT'] = np.ascontiguousarray(inputs['moe_router_w'].reshape(2, DC, P, NE).transpose(2, 0, 1, 3))
    m['moe_rbB'] = np.ascontiguousarray(np.broadcast_to(inputs['moe_router_b'][None], (P, 2, NE)))
    m.update(attn_layout(inputs))
    m.update(ssm_layout(inputs))
    sel = np.zeros((NE, NE, P), dtype=f)
    for e in range(NE):
        sel[e, e, :] = 1.0
    m['sel8'] = sel
    return m


def kernel(**inputs):
    inputs = {k: np.asarray(v) for k, v in inputs.items()}
    b = Builder({'tail_split': True})
    nc = b.build()
    shared = shared_layout(inputs)
    in_maps = []
    for core in range(8):
        m = host_layout(inputs, core % 4)
        m['rankf'] = np.full((P, 1), float(core // 4), dtype=np.float32)
        m.update(shared)
        in_maps.append({k: m[k] for k in b.din})
    res = run_bass_kernel_spmd(nc, in_maps, core_ids=list(range(8)))
    out = np.stack([np.concatenate([res.results[i]['out'], res.results[i + 4]['out']], axis=0) for i in range(4)], axis=0)
    return out.astype(np.float32)
```

```python
import contextlib
from types import FunctionType
import numpy as np
import concourse.bass as bass
import concourse.mybir as mybir
from concourse.bass_utils import run_bass_kernel_spmd

F32 = mybir.dt.float32
BF16 = mybir.dt.bfloat16
AF = mybir.ActivationFunctionType
ALU = mybir.AluOpType
AX = mybir.AxisListType

S = 4096
D = 1024
DC = 8
P = 128
DFF = 2816
FC = 22
NE = 8
EPS = 1e-6
NBLK = S // 512

ENG = ['pe', 'act', 'dve', 'pool', 'sp']
SELF_SYNC = True


class Sched:
    def __init__(self, nc, stack):
        self.nc = nc
        self.stack = stack
        self.streams = {e: [] for e in ENG}
        self.sem = {e: stack.enter_context(nc.semaphore('s_' + e)) for e in ENG}
        self.count = {e: 0 for e in ENG}
        self.dsem = {}
        self.seen = {e: {} for e in ENG}
        self.hist = {}
        self.buf = {}
        self.ninst = 0

    def _semof(self, key):
        if isinstance(key, tuple):
            return self.dsem[key[1]][0]
        return self.sem[key]

    def _wait(self, eng, toks):
        best = {}
        for (k, v) in toks:
            if v > best.get(k, 0):
                best[k] = v
        seen = self.seen[eng]
        for k, v in best.items():
            if seen.get(k, 0) >= v:
                continue
            if k == eng and (eng == 'pe' or not SELF_SYNC):
                continue
            s = self._semof(k)
            self.streams[eng].append(lambda e, s=s, v=v: e.wait_ge(s, v))
            self.ninst += 1
            h = self.hist.get((k, v))
            if h:
                for kk, vv in h.items():
                    if vv > seen.get(kk, 0):
                        seen[kk] = vv
            if v > seen.get(k, 0):
                seen[k] = v

    def _deps(self, reads, writes):
        toks = []
        for b in reads:
            st = self.buf.get(b)
            if st and st[0]:
                toks.append(st[0])
        for b in writes:
            st = self.buf.get(b)
            if st:
                if st[0]:
                    toks.append(st[0])
                toks.extend(st[1].items())
        return toks

    def _record(self, tok, reads, writes):
        for b in reads:
            st = self.buf.setdefault(b, [None, {}])
            if tok[1] > st[1].get(tok[0], 0):
                st[1][tok[0]] = tok[1]
        for b in writes:
            self.buf[b] = [tok, {}]

    def op(self, eng, fns, reads=(), writes=()):
        if not isinstance(fns, (list, tuple)):
            fns = [fns]
        self._wait(eng, self._deps(reads, writes))
        self.count[eng] += 1
        tok = (eng, self.count[eng])
        sem = self.sem[eng]
        for f in fns[:-1]:
            self.streams[eng].append(f)
        last = fns[-1]
        self.streams[eng].append(lambda e, f=last, s=sem: f(e).then_inc(s, 1))
        self.ninst += len(fns)
        self.hist[tok] = dict(self.seen[eng])
        self._record(tok, reads, writes)
        return tok

    def dma(self, queue, chan, out, in_, reads=(), writes=(), **kw):
        if chan == 'const':
            self.nconst = getattr(self, 'nconst', 0) + 1
            chan = 'const%d' % self.nconst
        if chan not in self.dsem:
            self.dsem[chan] = [self.stack.enter_context(self.nc.semaphore('d_' + str(chan))), 0]
        self._wait(queue, self._deps(reads, writes))
        ds = self.dsem[chan]
        ds[1] += 16
        tok = (('dma', chan), ds[1])
        s = ds[0]
        self.streams[queue].append(lambda e, s=s, o=out, i=in_, kw=kw: e.dma_start(
            out=(o() if isinstance(o, FunctionType) else o), in_=(i() if isinstance(i, FunctionType) else i), **kw).then_inc(s, 16))
        self.ninst += 1
        self.hist[tok] = dict(self.seen[queue])
        self._record(tok, reads, writes)
        return tok

    def barrier(self):
        toks = []
        for b, st in self.buf.items():
            if st[0]:
                toks.append(st[0])
            toks.extend(st[1].items())
        for eng in ENG:
            self._wait(eng, toks)

    def finish(self, eng='sp'):
        toks = []
        for b, st in self.buf.items():
            if st[0]:
                toks.append(st[0])
            toks.extend(st[1].items())
        self._wait(eng, toks)

    def emit(self):
        nc = self.nc
        with nc.Block() as block:
            @block.tensor
            def _(e):
                for f in self.streams['pe']:
                    f(e)

            @block.scalar
            def _(e):
                for f in self.streams['act']:
                    f(e)

            @block.vector
            def _(e):
                for f in self.streams['dve']:
                    f(e)

            @block.gpsimd
            def _(e):
                for f in self.streams['pool']:
                    f(e)

            @block.sync
            def _(e):
                for f in self.streams['sp']:
                    f(e)


class Builder:
    def __init__(self, cfg):
        self.cfg = cfg
        self.nc = bass.Bass("TRN2", target_bir_lowering=False)
        self.stack = contextlib.ExitStack()
        self.sc = Sched(self.nc, self.stack)
        self.din = {}
        self.psn = 0
        self.uid = 0

    def inp(self, name, shape, dtype=F32):
        t = self.nc.dram_tensor(name, list(shape), dtype, kind="ExternalInput").ap()
        self.din[name] = t
        return t

    def sb(self, name, shape, dtype, st=None):
        self.uid += 1
        return (st or self.stack).enter_context(self.nc.sbuf_tensor('sb%d_%s' % (self.uid, name), list(shape), dtype))

    def bank(self):
        rot = getattr(self, 'rot', None) or list(range(8))
        i = rot[self.psn % len(rot)]
        self.psn += 1
        return ('ps', i), self.ps[i]

    def setup_common(self):
        nc = self.nc
        self.ps = [self.stack.enter_context(nc.psum_tensor('ps%d' % i, [P, 512], F32)) for i in range(8)]
        self.ident = self.sb('ident', [P, P], F32)
        self.ones = self.sb('ones', [P, P], F32)
        d_ident = self.inp('ident', [P, P])
        self.sc.dma('sp', 'const', self.ident[:], d_ident[:, :], writes=['ident'])
        self.sc.op('dve', lambda e: e.memset(self.ones[:], 1.0), writes=['ones'])

    def build_mod(self):
        sc = self.sc
        cT = self.inp('cT', [P, DC])
        ada_w = self.inp('ada_w', [4, D, 6 * D])
        ada_bT = self.inp('ada_bT', [P, 4, 48])
        norm_gT = self.inp('norm_gT', [P, 4, 2, DC])
        self.cond = self.sb('cond', [P, DC], F32)
        self.modT = self.sb('modT', [P, 4, 48], F32)
        self.adab = self.sb('adab', [P, 4, 48], F32)
        self.ng = self.sb('ng', [P, 4, 2, DC], F32)
        self.gs = self.sb('gs', [P, 4, 2, DC], F32)
        craw = self.sb('craw', [P, DC], F32)
        sig = self.sb('csig', [P, DC], F32)
        sc.dma('sp', 'const', craw[:], cT[:, :], writes=['craw'])
        sc.dma('sp', 'const', self.adab[:], ada_bT[:, :, :], writes=['adab'])
        sc.dma('sp', 'const', self.ng[:], norm_gT[:, :, :, :], writes=['ng'])
        sc.op('act', lambda e: e.activation(out=sig[:], in_=craw[:], func=AF.Sigmoid), reads=['craw'], writes=['csig'])
        sc.op('dve', lambda e: e.tensor_tensor(out=self.cond[:], in0=craw[:], in1=sig[:], op=ALU.mult),
              reads=['craw', 'csig'], writes=['cond'])
        st = contextlib.ExitStack()
        wsl = [self.sb('adaw%d' % i, [P, DC, 1024], F32, st) for i in range(2)]
        n = 0
        for i in range(4):
            pname, pt = self.bank()
            for k in range(6):
                slot = n % 2
                n += 1
                for c in range(DC):
                    sc.dma('sp', 'adaw%d' % slot, wsl[slot][:, c, :],
                           ada_w[i, c * P:(c + 1) * P, k * 1024:(k + 1) * 1024], writes=['adaw%d' % slot])
                fns = []
                for nn in range(8):
                    for c in range(DC):
                        fns.append(lambda e, pt=pt, slot=slot, nn=nn, c=c, k=k:
                                   e.matmul(pt[:, k * 8 + nn:k * 8 + nn + 1], lhsT=wsl[slot][:, c, nn * P:(nn + 1) * P],
                                            rhs=self.cond[:, c:c + 1], start=(c == 0), stop=(c == DC - 1)))
                sc.op('pe', fns, reads=['adaw%d' % slot, 'cond'], writes=[pname])
            sc.op('dve', lambda e, pt=pt, i=i: e.tensor_tensor(out=self.modT[:, i, :], in0=pt[:, 0:48], in1=self.adab[:, i, :], op=ALU.add),
                  reads=[pname, 'adab'], writes=[('modT', i)])
            for j, k in ((0, 1), (1, 4)):
                sc.op('dve', lambda e, i=i, j=j, k=k: e.scalar_tensor_tensor(
                    out=self.gs[:, i, j, :], in0=self.modT[:, i, k * 8:(k + 1) * 8], scalar=1.0, in1=self.ng[:, i, j, :],
                    op0=ALU.add, op1=ALU.mult), reads=[('modT', i), 'ng'], writes=[('modT', i)])
        sc.barrier()
        st.close()

    def modv(self, i, k, c):
        return self.modT[:, i, k * 8 + c:k * 8 + c + 1]

    def alloc_stream(self):
        self.xT_d = self.nc.dram_tensor('xT_scratch', [D, S], F32).ap()
        self.xh_d = self.nc.dram_tensor('xh_scratch', [D, S // 2], F32).ap()
        self.rd_mode = 'full'
        self.wr_mode = 'full'
        self.ntok = S
        self.sc.streams['sp'].append(lambda e: setattr(self, 'rv', e.partition_id() // 4))
        d_rankf = self.inp('rankf', [P, 1])
        self.rankf = self.sb('rankf', [P, 1], F32)
        self.sc.dma('sp', 'const', self.rankf[:], d_rankf[:, :], writes=['rankf'])
        self.xin = self.inp('x', [S, D])
        self.xblk = [self.sb('xblk%d' % i, [P, DC, 512], F32) for i in range(2)]
        self.sq = self.sb('sq', [P, 512], F32)
        self.rstd = self.sb('rstd', [P, 512], F32)
        self.tmp32 = [self.sb('tmp32_%d' % i, [P, 512], F32) for i in range(3)]
        self.xn = 0

    def xd(self, c, blk, write):
        mode = self.wr_mode if write else self.rd_mode
        if mode == 'full':
            return self.xT_d[c * P:(c + 1) * P, blk * 512:(blk + 1) * 512], ('xTd', c, blk)
        if mode == 'half':
            return self.xh_d[c * P:(c + 1) * P, blk * 512:(blk + 1) * 512], ('xh', c, blk)
        if mode == 'dynfull':
            return self.xs_d[c * P:(c + 1) * P, (blk + 1) * 512:(blk + 2) * 512], 'xs'
        raise ValueError(mode)

    def load_xT(self, blk, from_input):
        sc = self.sc
        slot = self.xn % 2
        self.xn += 1
        name = 'xblk%d' % slot
        t = self.xblk[slot]
        if from_input:
            for j in range(4):
                sc.dma('sp', 'xtok', self.xtok[:, j, :], self.xin[blk * 512 + j * P: blk * 512 + (j + 1) * P, :],
                       writes=[('xtok', j)])
            for c in range(DC):
                pname, pt = self.bank()
                fns = [lambda e, pt=pt, j=j, c=c: e.transpose(out=pt[:, j * P:(j + 1) * P], in_=self.xtok[:, j, c * P:(c + 1) * P],
                                                              identity=self.ident[:]) for j in range(4)]
                sc.op('pe', fns, reads=[('xtok', j) for j in range(4)] + ['ident'], writes=[pname])
                eng = 'act' if c % 2 == 0 else 'dve'
                if eng == 'act':
                    sc.op('act', lambda e, pt=pt, t=t, c=c: e.copy(out=t[:, c, :], in_=pt[:]), reads=[pname], writes=[(name, c)])
                else:
                    sc.op('dve', lambda e, pt=pt, t=t, c=c: e.tensor_copy(out=t[:, c, :], in_=pt[:]), reads=[pname], writes=[(name, c)])
        else:
            for c in range(DC):
                ap, tn = self.xd(c, blk, False)
                sc.dma('sp', name, t[:, c, :], ap, reads=[tn], writes=[(name, c)])
        return name, t

    def store_xT(self, blk, name, t, c):
        ap, tn = self.xd(c, blk, True)
        self.sc.dma('sp', 'st_' + name, ap, t[:, c, :], reads=[(name, c)], writes=[tn])

    def norm_mod(self, li, j, name, t, hT, hname, hoff, h32=None):
        sc = self.sc
        pname, pt = self.bank()
        for c in range(DC):
            sc.op('act', lambda e, c=c: e.activation(out=self.sq[:], in_=t[:, c, :], func=AF.Square), reads=[(name, c)], writes=['sq'])
            sc.op('pe', lambda e, c=c, pt=pt: e.matmul(pt[:], lhsT=self.ones[:], rhs=self.sq[:], start=(c == 0), stop=(c == DC - 1)),
                  reads=['sq', 'ones'], writes=[pname])
        sc.op('act', lambda e, pt=pt: e.activation(out=self.rstd[:], in_=pt[:], func=AF.Sqrt, scale=1.0 / D, bias=self.epsb[:]),
              reads=[pname, 'epsb'], writes=['rstd'])
        sc.op('dve', lambda e: e.reciprocal(out=self.rstd[:], in_=self.rstd[:]), reads=['rstd'], writes=['rstd'])
        kshift = 0 if j == 0 else 3
        for c in range(DC):
            tm = self.tmp32[c % 2]
            tn = 'tmp32_%d' % (c % 2)
            sc.op('dve', lambda e, c=c, tm=tm: e.tensor_tensor(out=tm[:], in0=t[:, c, :], in1=self.rstd[:], op=ALU.mult),
                  reads=[(name, c), 'rstd'], writes=[tn])
            if h32 is not None:
                sc.op('pool', lambda e, c=c, tm=tm: e.tensor_scalar(out=h32[:, c, :], in0=tm[:], scalar1=self.gs[:, li, j, c:c + 1],
                                                                   scalar2=self.modv(li, kshift, c), op0=ALU.mult, op1=ALU.add),
                      reads=[tn, ('modT', li)], writes=[('h32', c)])
            sc.op('dve', lambda e, c=c, tm=tm: e.tensor_scalar(out=hT[:, c, hoff:hoff + 512], in0=tm[:], scalar1=self.gs[:, li, j, c:c + 1],
                                                              scalar2=self.modv(li, kshift, c), op0=ALU.mult, op1=ALU.add),
                  reads=[tn, ('modT', li)], writes=[(hname, c, hoff)])

    def load_w_cast(self, chan, dst, src, rows_split, writes):
        n = dst.shape[-1]
        step = 2048
        for a in range(0, n, step):
            b = min(n, a + step)
            self.sc.dma('pool', chan, dst[:, a:b], src[:, a:b], writes=writes)

    def conv_setup(self):
        self.d_conv_w_in = self.inp('conv_w_in', [2, D, 3 * D])
        self.d_conv_w_out = self.inp('conv_w_out', [2, D, D])
        d_cw = self.inp('conv_wT', [P, 2, 3, DC])
        self.cwT = self.sb('cwT', [P, 2, 3, DC], F32)
        self.sc.dma('sp', 'const', self.cwT[:], d_cw[:, :, :, :], writes=['cwT'])

    def conv_mixer(self, li, j, first):
        sc = self.sc
        st = contextlib.ExitStack()
        nc = self.nc
        win = self.sb('cw_in', [P, DC, 3 * D], BF16, st)
        wout = self.sb('cw_out', [P, DC, D], BF16, st)
        hT = self.sb('hTc', [P, DC, 512], BF16, st)
        bT = self.sb('bTc', [P, DC, 512], F32, st)
        uT = self.sb('uTc', [P, DC, 514], F32, st)
        zT = self.sb('zTc', [P, DC, 512], BF16, st)
        if first:
            self.xtok = self.sb('xtok', [P, 4, D], F32, st)
        L = 'L%d' % li
        for c in range(DC):
            self.load_w_cast('cwin', win[:, c, :], self.d_conv_w_in[j, c * P:(c + 1) * P, :], None, writes=[(L, 'cwin')])
            self.load_w_cast('cwout', wout[:, c, :], self.d_conv_w_out[j, c * P:(c + 1) * P, :], None, writes=[(L, 'cwout')])
        sc.op('pool', lambda e: e.memset(uT[:, :, 0:2], 0.0), writes=[(L, 'uT', c) for c in range(DC)])
        split = (self.rd_mode == 'dynfull')
        for blk in ([-1] if split else []) + list(range(self.ntok // 512)):
            name, t = self.load_xT(blk, first)
            self.norm_mod(li, 0, name, t, hT, (L, 'hT'), 0)
            hreads = [((L, 'hT'), c, 0) for c in range(DC)] + [(L, 'cwin')]
            for c in range(DC):
                pts = []
                for kind in ((1, 2) if blk < 0 else (0, 1, 2)):
                    pname, pt = self.bank()
                    fns = [lambda e, pt=pt, k=k, kind=kind, c=c: e.matmul(
                        pt[:], lhsT=win[:, k, kind * D + c * P: kind * D + (c + 1) * P], rhs=hT[:, k, :],
                        start=(k == 0), stop=(k == DC - 1)) for k in range(DC)]
                    sc.op('pe', fns, reads=hreads, writes=[pname])
                    pts.append((pname, pt))
                if blk < 0:
                    (pcn, pc), (pvn, pv) = pts
                else:
                    (pbn, pb), (pcn, pc), (pvn, pv) = pts
                    sc.op('act', lambda e, pb=pb, c=c: e.activation(out=bT[:, c, :], in_=pb[:], func=AF.Copy), reads=[pbn], writes=[(L, 'bT', c)])
                tm = self.tmp32[2]
                sc.op('act', lambda e, pc=pc, tm=tm: e.activation(out=tm[:], in_=pc[:], func=AF.Copy), reads=[pcn], writes=['tmp32_2'])
                sc.op('dve', lambda e, pv=pv, tm=tm, c=c: e.tensor_tensor(out=uT[:, c, 2:514], in0=pv[:], in1=tm[:], op=ALU.mult),
                      reads=[pvn, 'tmp32_2'], writes=[(L, 'uT', c)])
            if blk < 0:
                for c in range(DC):
                    sc.op('dve', lambda e, c=c: e.tensor_scalar(out=uT[:, c, 0:2], in0=uT[:, c, 512:514], scalar1=self.rankf[:, 0:1], scalar2=None, op0=ALU.mult),
                          reads=[(L, 'uT', c), 'rankf'], writes=[(L, 'uT', c)])
                continue
            for c in range(DC):
                tm = self.tmp32[c % 2]
                tn = 'tmp32_%d' % (c % 2)
                sc.op('dve', lambda e, c=c, tm=tm: e.tensor_scalar(out=tm[:], in0=uT[:, c, 2:514], scalar1=self.cwT[:, j, 2, c:c + 1],
                                                                  scalar2=None, op0=ALU.mult), reads=[(L, 'uT', c), 'cwT'], writes=[tn])
                sc.op('dve', lambda e, c=c, tm=tm: e.scalar_tensor_tensor(out=tm[:], in0=uT[:, c, 1:513], scalar=self.cwT[:, j, 1, c:c + 1],
                                                                         in1=tm[:], op0=ALU.mult, op1=ALU.add),
                      reads=[(L, 'uT', c), tn], writes=[tn])
                sc.op('dve', lambda e, c=c, tm=tm: e.scalar_tensor_tensor(out=tm[:], in0=uT[:, c, 0:512], scalar=self.cwT[:, j, 0, c:c + 1],
                                                                         in1=tm[:], op0=ALU.mult, op1=ALU.add),
                      reads=[(L, 'uT', c), tn], writes=[tn])
                sc.op('dve', lambda e, c=c, tm=tm: e.tensor_tensor(out=zT[:, c, :], in0=tm[:], in1=bT[:, c, :], op=ALU.mult),
                      reads=[tn, (L, 'bT', c)], writes=[(L, 'zT', c)])
                sc.op('pool', lambda e, c=c: e.tensor_copy(out=uT[:, c, 0:2], in_=uT[:, c, 512:514]),
                      reads=[(L, 'uT', c)], writes=[(L, 'uT', c)])
            zreads = [(L, 'zT', c) for c in range(DC)] + [(L, 'cwout')]
            for c in range(DC):
                pname, pt = self.bank()
                fns = [lambda e, pt=pt, k=k, c=c: e.matmul(pt[:], lhsT=wout[:, k, c * P:(c + 1) * P], rhs=zT[:, k, :],
                                                           start=(k == 0), stop=(k == DC - 1)) for k in range(DC)]
                sc.op('pe', fns, reads=zreads, writes=[pname])
                sc.op('dve', lambda e, pt=pt, c=c, t=t: e.scalar_tensor_tensor(out=t[:, c, :], in0=pt[:], scalar=self.modv(li, 2, c),
                                                                              in1=t[:, c, :], op0=ALU.mult, op1=ALU.add),
                      reads=[pname, (name, c), ('modT', li)], writes=[(name, c)])
                self.store_xT(blk, name, t, c)
        sc.barrier()
        st.close()

    def ffn_setup(self):
        self.d_ffn_gu = self.inp('ffn_gu_r', [2, FC, P, DC * 256])
        self.d_ffn_dn = self.inp('ffn_dn_r', [2, DC, P, FC * P])
        self.d_moe_gu = self.inp('moe_gu_r', [2, NE, FC, P, DC * 256])
        self.d_moe_dn = self.inp('moe_dn_r', [2, NE, DC, P, FC * P])
        d_rw = self.inp('moe_rwT', [P, 2, DC, NE])
        d_rb = self.inp('moe_rbB', [P, 2, NE])
        d_sel = self.inp('sel8', [NE, NE, P])
        self.rw = self.sb('rw', [P, 2, DC, NE], F32)
        self.rb = self.sb('rb', [P, 2, NE], F32)
        self.sel = self.sb('sel', [NE, NE, P], F32)
        self.sc.dma('sp', 'const', self.rw[:], d_rw[:, :, :, :], writes=['rw'])
        self.sc.dma('sp', 'const', self.rb[:], d_rb[:, :, :], writes=['rb'])
        self.sc.dma('sp', 'const', self.sel[:], d_sel[:, :, :], writes=['sel'])

    def router(self, li, L, h32, half, gT, sm):
        sc = self.sc
        jl = li // 2
        lg, ex, gt, m8, sc1 = sm
        prn, pr = self.bank()
        h32r = [('h32', c) for c in range(DC)]
        for tt in range(4):
            fns = [lambda e, pr=pr, tt=tt, c=c: e.matmul(pr[:, tt * 8:(tt + 1) * 8], lhsT=h32[:, c, tt * P:(tt + 1) * P],
                                                        rhs=self.rw[:, jl, c, :], start=(c == 0), stop=(c == DC - 1))
                   for c in range(DC)]
            sc.op('pe', fns, reads=h32r + ['rw'], writes=[prn])
        ptn, ptT = self.bank()
        for tt in range(4):
            R = [(L, 'rt')]
            sc.op('dve', lambda e, tt=tt, pr=pr: e.tensor_tensor(out=lg[:, 0:8], in0=pr[:, tt * 8:(tt + 1) * 8], in1=self.rb[:, jl, :], op=ALU.add),
                  reads=[prn, 'rb'], writes=R)
            sc.op('dve', lambda e: e.tensor_reduce(out=sc1[:, 0:1], in_=lg[:, 0:8], axis=AX.X, op=ALU.max), reads=R, writes=R)
            sc.op('dve', lambda e: e.tensor_scalar(out=sc1[:, 0:1], in0=sc1[:, 0:1], scalar1=-1.0, scalar2=None, op0=ALU.mult), reads=R, writes=R)
            sc.op('act', lambda e: e.activation(out=ex[:, 0:8], in_=lg[:, 0:8], func=AF.Exp, bias=sc1[:, 0:1], scale=1.0), reads=R, writes=R)
            sc.op('dve', lambda e: e.max(out=m8[:, 0:8], in_=ex[:, 0:8]), reads=R, writes=R)
            sc.op('dve', lambda e: e.tensor_tensor(out=sc1[:, 1:2], in0=m8[:, 0:1], in1=m8[:, 1:2], op=ALU.add), reads=R, writes=R)
            sc.op('dve', lambda e: e.reciprocal(out=sc1[:, 1:2], in_=sc1[:, 1:2]), reads=R, writes=R)
            sc.op('dve', lambda e: e.tensor_scalar(out=gt[:, 0:8], in0=ex[:, 0:8], scalar1=m8[:, 1:2], scalar2=None, op0=ALU.is_ge), reads=R, writes=R)
            sc.op('dve', lambda e: e.tensor_tensor(out=gt[:, 0:8], in0=gt[:, 0:8], in1=ex[:, 0:8], op=ALU.mult), reads=R, writes=R)
            sc.op('dve', lambda e: e.tensor_scalar(out=gt[:, 0:8], in0=gt[:, 0:8], scalar1=sc1[:, 1:2], scalar2=None, op0=ALU.mult), reads=R, writes=R)
            sc.op('pe', lambda e, tt=tt, ptT=ptT: e.transpose(out=ptT[0:8, tt * P:(tt + 1) * P], in_=gt[:, 0:8], identity=self.ident[:]),
                  reads=R + ['ident'], writes=[ptn])
        sc.op('act', lambda e, ptT=ptT, half=half: e.activation(out=gT[0:8, half * 512:(half + 1) * 512], in_=ptT[0:8, :], func=AF.Copy),
              reads=[ptn], writes=[(L, 'gT', half)])

    def ffn(self, li, moe):
        sc = self.sc
        st = contextlib.ExitStack()
        L = 'F%d' % li
        TB = 1024
        hT = self.sb('hTf', [P, DC, TB], BF16, st)
        actT = self.sb('actT', [P, FC, TB], BF16, st)
        wgu = [self.sb('wgu%d' % i, [P, DC * 256], BF16, st) for i in range(3)]
        wd = [self.sb('wd%d' % i, [P, FC * P], BF16, st) for i in range(3)]
        xs = [self.sb('xs%d' % i, [P, 512], F32, st) for i in range(2)]
        if moe:
            h32 = self.sb('h32', [P, DC, 512], F32, st)
            yacc = self.sb('yacc', [P, DC, TB], F32, st)
            Gb = [self.sb('Gb%d' % i, [P, TB], F32, st) for i in range(2)]
            gT = self.sb('gT', [NE, TB], F32, st)
            sm = [self.sb('rsm%d' % i, [P, 8], F32, st) for i in range(5)]
        jl = li // 2
        nw = 0
        nd = 0
        nx = 0
        ng = 0
        for sbi in range(self.ntok // TB):
            for half in range(2):
                blk = sbi * 2 + half
                name, t = self.load_xT(blk, False)
                self.norm_mod(li, 1, name, t, hT, (L, 'hT'), half * 512, h32=(h32 if moe else None))
                if moe:
                    self.router(li, L, h32, half, gT, sm)
            for ex in (range(NE) if moe else [None]):
                if moe:
                    gsl = ng % 2
                    ng += 1
                    gbn = (L, 'Gb', gsl)
                    for half in range(2):
                        pn, pt = self.bank()
                        sc.op('pe', lambda e, pt=pt, ex=ex, half=half: e.matmul(pt[:], lhsT=self.sel[0:8, ex, :], rhs=gT[0:8, half * 512:(half + 1) * 512],
                                                                               start=True, stop=True),
                              reads=['sel', (L, 'gT', half)], writes=[pn])
                        sc.op('act', lambda e, pt=pt, gsl=gsl, half=half: e.activation(out=Gb[gsl][:, half * 512:(half + 1) * 512], in_=pt[:], func=AF.Copy),
                              reads=[pn], writes=[(gbn, half)])
                for f in range(FC):
                    slot = nw % 3
                    nw += 1
                    wn = (L, 'wgu', slot)
                    src = self.d_moe_gu[jl, ex, f, :, :] if moe else self.d_ffn_gu[jl, f, :, :]
                    self.load_w_cast('wgu%d' % slot, wgu[slot][:, :], src, None, writes=[wn])
                    for half in range(2):
                        hreads = [((L, 'hT'), c, half * 512) for c in range(DC)] + [wn]
                        pgn, pg = self.bank()
                        pun, pu = self.bank()
                        for (pn, pt, off) in ((pgn, pg, 0), (pun, pu, 128)):
                            fns = [lambda e, pt=pt, k=k, off=off, slot=slot, half=half: e.matmul(
                                pt[:], lhsT=wgu[slot][:, k * 256 + off: k * 256 + off + 128], rhs=hT[:, k, half * 512:(half + 1) * 512],
                                start=(k == 0), stop=(k == DC - 1)) for k in range(DC)]
                            sc.op('pe', fns, reads=hreads, writes=[pn])
                        tm = self.tmp32[2]
                        sc.op('act', lambda e, pg=pg, tm=tm: e.activation(out=tm[:], in_=pg[:], func=AF.Silu), reads=[pgn], writes=['tmp32_2'])
                        sc.op('dve', lambda e, pu=pu, tm=tm, f=f, half=half: e.tensor_tensor(
                            out=actT[:, f, half * 512:(half + 1) * 512], in0=pu[:], in1=tm[:], op=ALU.mult),
                            reads=[pun, 'tmp32_2'], writes=[(L, 'act', f, half)])
                for c in range(DC):
                    slot = nd % 3
                    nd += 1
                    wn = (L, 'wd', slot)
                    src = self.d_moe_dn[jl, ex, c, :, :] if moe else self.d_ffn_dn[jl, c, :, :]
                    self.load_w_cast('wd%d' % slot, wd[slot][:, :], src, None, writes=[wn])
                    for half in range(2):
                        blk = sbi * 2 + half
                        areads = [(L, 'act', f, half) for f in range(FC)] + [wn]
                        pn, pt = self.bank()
                        fns = [lambda e, pt=pt, f=f, slot=slot, half=half: e.matmul(
                            pt[:], lhsT=wd[slot][:, f * P:(f + 1) * P], rhs=actT[:, f, half * 512:(half + 1) * 512],
                            start=(f == 0), stop=(f == FC - 1)) for f in range(FC)]
                        sc.op('pe', fns, reads=areads, writes=[pn])
                        if not moe:
                            self.resid(li, 5, xs, nx, c, blk, pn, pt, None, None)
                            nx += 1
                        else:
                            yn = (L, 'yacc', c, half)
                            ysl = yacc[:, c, half * 512:(half + 1) * 512]
                            gsl_ap = Gb[gsl][:, half * 512:(half + 1) * 512]
                            if ex == 0:
                                sc.op('dve', lambda e, pt=pt, ysl=ysl, g=gsl_ap: e.tensor_tensor(out=ysl, in0=pt[:], in1=g, op=ALU.mult),
                                      reads=[pn, (gbn, half)], writes=[yn])
                            else:
                                tm = self.tmp32[c % 2]
                                tn = 'tmp32_%d' % (c % 2)
                                sc.op('dve', lambda e, pt=pt, tm=tm, g=gsl_ap: e.tensor_tensor(out=tm[:], in0=pt[:], in1=g, op=ALU.mult),
                                      reads=[pn, (gbn, half)], writes=[tn])
                                sc.op('pool', lambda e, tm=tm, ysl=ysl: e.tensor_tensor(out=ysl, in0=ysl, in1=tm[:], op=ALU.add),
                                      reads=[tn, yn], writes=[yn])
            if moe:
                for c in range(DC):
                    for half in range(2):
                        blk = sbi * 2 + half
                        self.resid(li, 5, xs, nx, c, blk, (L, 'yacc', c, half), None, yacc[:, c, half * 512:(half + 1) * 512], None)
                        nx += 1
        sc.barrier()
        st.close()

    def resid(self, li, k, xs, nx, c, blk, srcname, pt, src_ap, _):
        sc = self.sc
        xsl = nx % 2
        xn = 'xs%d' % xsl
        xt = xs[xsl]
        src = pt[:] if pt is not None else src_ap
        rap, rtn = self.xd(c, blk, False)
        wap, wtn = self.xd(c, blk, True)
        sc.dma('sp', xn, xt[:], rap, reads=[rtn], writes=[xn])
        sc.op('dve', lambda e, src=src, xt=xt, c=c: e.scalar_tensor_tensor(
            out=xt[:], in0=src, scalar=self.modv(li, k, c), in1=xt[:], op0=ALU.mult, op1=ALU.add),
            reads=[srcname, xn, ('modT', li)], writes=[xn])
        sc.dma('sp', 'st_' + xn, wap, xt[:], reads=[xn], writes=[wtn])

    def attn_setup(self):
        self.d_awq = self.inp('attn_wq_perm', [D, 1024])
        self.d_awk = self.inp('attn_wk', [D, 256])
        self.d_awv = self.inp('attn_wv', [D, 256])
        self.d_awqi = self.inp('attn_wqi', [D, 512])
        self.d_awki2 = self.inp('attn_wki2', [D, 128])
        self.d_awwi = self.inp('attn_wwi', [D, 8])
        self.d_awout = self.inp('attn_wout_r', [64, 16, D])
        self.d_gainT = self.inp('attn_gainT', [P, 2])
        self.d_biasT = self.inp('attn_biasT', [P, 32, P])
        self.d_b31 = self.inp('attn_b31B', [P, 16])
        self.d_cmask = self.inp('cmask', [P, P])
        self.d_bones = self.inp('blockones', [P, P])
        self.d_selrow = self.inp('selrow', [65, 64])

    def head_norm(self, pn, pt, gain_ap, out_ap, tagw):
        sc = self.sc
        qs = self.tmp32[2]
        sc.op('act', lambda e, pt=pt, qs=qs: e.activation(out=qs[:], in_=pt[:], func=AF.Copy), reads=[pn], writes=['tmp32_2'])
        sc.op('act', lambda e, qs=qs: e.activation(out=self.sq[:], in_=qs[:], func=AF.Square), reads=['tmp32_2'], writes=['sq'])
        p2n, p2 = self.bank()
        sc.op('pe', lambda e, p2=p2: e.matmul(p2[:], lhsT=self.bones[:], rhs=self.sq[:], start=True, stop=True), reads=['sq', 'bones'], writes=[p2n])
        sc.op('act', lambda e, p2=p2: e.activation(out=self.rstd[:], in_=p2[:], func=AF.Sqrt, scale=1.0 / 64, bias=self.epsb[:]),
              reads=[p2n, 'epsb'], writes=['rstd'])
        sc.op('dve', lambda e: e.reciprocal(out=self.rstd[:], in_=self.rstd[:]), reads=['rstd'], writes=['rstd'])
        sc.op('dve', lambda e, qs=qs, g=gain_ap, o=out_ap: e.scalar_tensor_tensor(out=o, in0=qs[:], scalar=g, in1=self.rstd[:], op0=ALU.mult, op1=ALU.mult),
              reads=['tmp32_2', 'rstd', 'again'], writes=tagw)

    def attn_mixer(self, li):
        sc = self.sc
        st = contextlib.ExitStack()
        L = 'A'
        NI = 24
        KT = self.sb('KT', [P, 2, S], BF16, st)
        VA = self.sb('VA', [P, 32, 4, 65], BF16, st)
        KI = self.sb('KI', [P, S], BF16, st)
        hT = self.sb('hTa', [P, DC, 512], BF16, st)
        wA = self.sb('wA', [P, DC * 1024], BF16, st)
        QT = self.sb('QT', [P, 8, 512], BF16, st)
        QI = self.sb('QI', [P, 4, 512], BF16, st)
        WI = self.sb('WI', [P, 4, 8], F32, st)
        score = self.sb('score', [P, S], F32, st)
        nmq = self.sb('nmq', [P, S], BF16, st)
        nmT = [self.sb('nmT%d' % i, [P, 32, P], BF16, st) for i in range(2)]
        pexp = [self.sb('pexp%d' % i, [P, 512], BF16, st) for i in range(3)]
        OTn = self.sb('OTn', [64, 16, 512], BF16, st)
        osb = self.sb('osb', [65, 512], F32, st)
        rbc = self.sb('rbc', [64, 512], F32, st)
        Rt = self.tmp32[0:2]
        biasS = self.sb('biasS', [P, 32, P], BF16, st)
        self.bones = self.sb('bones', [P, P], F32, st)
        identb = self.sb('identb', [P, P], BF16, st)
        cmask = self.sb('cmaskS', [P, P], F32, st)
        selrow = self.sb('selrowS', [65, 64], F32, st)
        again = self.sb('again', [P, 2], F32, st)
        b31 = self.sb('b31', [P, 16], F32, st)
        bs = [self.sb('bsm%d' % i, [P, 1], F32, st) for i in range(6)]
        sc.dma('sp', 'const', self.bones[:], self.d_bones[:, :], writes=['bones'])
        sc.dma('sp', 'const', cmask[:], self.d_cmask[:, :], writes=['cmask'])
        sc.dma('sp', 'const', selrow[:], self.d_selrow[:, :], writes=['selrow'])
        sc.dma('sp', 'const', again[:], self.d_gainT[:, :], writes=['again'])
        sc.dma('sp', 'const', b31[:], self.d_b31[:, :], writes=['b31'])
        sc.op('dve', lambda e: e.tensor_copy(out=identb[:], in_=self.ident[:]), reads=['ident'], writes=['identb'])
        sc.op('dve', lambda e: e.tensor_scalar(out=again[:, 0:1], in0=again[:, 0:1], scalar1=0.125, scalar2=None, op0=ALU.mult),
              reads=['again'], writes=['again'])
        sc.op('pool', lambda e: e.memset(VA[:, :, :, 64:65], 1.0), writes=[(L, 'VAones')])
        for half in range(4):
            sc.dma('sp', 'bstage', score[:, 0:1024].rearrange('p (a b) -> p a b', b=P), self.d_biasT[:, half * 8:(half + 1) * 8, :],
                   writes=[(L, 'bstage')])
            for i in range(8):
                kh = half * 8 + i
                h = kh % 16
                sc.op('dve', lambda e, i=i, kh=kh, h=h: e.tensor_scalar(out=biasS[:, kh, :], in0=score[:, i * P:(i + 1) * P], scalar1=b31[:, h:h + 1],
                                                                      scalar2=None, op0=ALU.subtract), reads=[(L, 'bstage'), 'b31'], writes=[(L, 'biasS')])
        wk = wA[:, 0:DC * 256].rearrange('p (c n) -> p c n', c=DC)
        wv = wA[:, DC * 256:DC * 512].rearrange('p (c n) -> p c n', c=DC)
        wki = wA[:, DC * 512:DC * 640].rearrange('p (c n) -> p c n', c=DC)
        for c in range(DC):
            sc.dma('pool', 'wA', wk[:, c, :], self.d_awk[c * P:(c + 1) * P, :], writes=[(L, 'wA')])
            sc.dma('pool', 'wA', wv[:, c, :], self.d_awv[c * P:(c + 1) * P, :], writes=[(L, 'wA')])
            sc.dma('pool', 'wA', wki[:, c, :], self.d_awki2[c * P:(c + 1) * P, :], writes=[(L, 'wA')])
        for blk in range(NBLK):
            name, t = self.load_xT(blk, False)
            self.norm_mod(li, 0, name, t, hT, (L, 'hT'), 0)
            hreads = [((L, 'hT'), c, 0) for c in range(DC)] + [(L, 'wA')]
            for m in range(2):
                pn, pt = self.bank()
                fns = [lambda e, pt=pt, k=k, m=m: e.matmul(pt[:], lhsT=wk[:, k, m * P:(m + 1) * P], rhs=hT[:, k, :], start=(k == 0), stop=(k == DC - 1))
                       for k in range(DC)]
                sc.op('pe', fns, reads=hreads, writes=[pn])
                self.head_norm(pn, pt, again[:, 1:2], KT[:, m, blk * 512:(blk + 1) * 512], [(L, 'KT', m, blk)])
            pn, pt = self.bank()
            fns = [lambda e, pt=pt, k=k: e.matmul(pt[:], lhsT=wki[:, k, :], rhs=hT[:, k, :], start=(k == 0), stop=(k == DC - 1)) for k in range(DC)]
            sc.op('pe', fns, reads=hreads, writes=[pn])
            sc.op('act', lambda e, pt=pt, blk=blk: e.activation(out=KI[:, blk * 512:(blk + 1) * 512], in_=pt[:], func=AF.Copy), reads=[pn], writes=[(L, 'KI', blk)])
            for tt in range(4):
                pn, pt = self.bank()
                fns = [lambda e, pt=pt, k=k, tt=tt: e.matmul(pt[:, 0:256], lhsT=hT[:, k, tt * P:(tt + 1) * P], rhs=wv[:, k, :], start=(k == 0), stop=(k == DC - 1))
                       for k in range(DC)]
                sc.op('pe', fns, reads=hreads, writes=[pn])
                sc.op('dve', lambda e, pt=pt, blk=blk, tt=tt: e.tensor_copy(out=VA[:, blk * 4 + tt, :, 0:64], in_=pt[:, 0:256].rearrange('p (a b) -> p a b', b=64)),
                      reads=[pn], writes=[(L, 'VA', blk * 4 + tt)])
        if self.cfg.get('astage', 9) < 1:
            sc.barrier(); st.close(); return
        wq = wA[:, :].rearrange('p (c n) -> p c n', c=DC)
        wqi = wA[:, 0:DC * 512].rearrange('p (c n) -> p c n', c=DC)
        wwi = wA[:, DC * 512:DC * 520].rearrange('p (c n) -> p c n', c=DC)
        wout = wA[0:64, 0:16 * 512].rearrange('p (h n) -> p h n', h=16)
        self.abank = [6, 7]
        self.nab = 0
        self.rot = [0, 1, 2, 3, 4, 5]
        nmn = 0
        npx = 0
        nrt = 0
        for blk in range(self.cfg.get('anblk', NBLK)):
            for c in range(DC):
                sc.dma('pool', 'wA', wq[:, c, :], self.d_awq[c * P:(c + 1) * P, :], writes=[(L, 'wA')])
            name, t = self.load_xT(blk, False)
            self.norm_mod(li, 0, name, t, hT, (L, 'hT'), 0)
            hreads = [((L, 'hT'), c, 0) for c in range(DC)]
            for m in range(8):
                pn, pt = self.bank()
                fns = [lambda e, pt=pt, k=k, m=m: e.matmul(pt[:], lhsT=wq[:, k, m * P:(m + 1) * P], rhs=hT[:, k, :], start=(k == 0), stop=(k == DC - 1))
                       for k in range(DC)]
                sc.op('pe', fns, reads=hreads + [(L, 'wA')], writes=[pn])
                self.head_norm(pn, pt, again[:, 0:1], QT[:, m, :], [(L, 'QT', m)])
            for c in range(DC):
                sc.dma('pool', 'wA', wqi[:, c, :], self.d_awqi[c * P:(c + 1) * P, :], writes=[(L, 'wA')])
                sc.dma('pool', 'wA', wwi[:, c, :], self.d_awwi[c * P:(c + 1) * P, :], writes=[(L, 'wA')])
            for m in range(4):
                pn, pt = self.bank()
                fns = [lambda e, pt=pt, k=k, m=m: e.matmul(pt[:], lhsT=wqi[:, k, m * P:(m + 1) * P], rhs=hT[:, k, :], start=(k == 0), stop=(k == DC - 1))
                       for k in range(DC)]
                sc.op('pe', fns, reads=hreads + [(L, 'wA')], writes=[pn])
                sc.op('act', lambda e, pt=pt, m=m: e.activation(out=QI[:, m, :], in_=pt[:], func=AF.Copy), reads=[pn], writes=[(L, 'QI', m)])
            pn, pt = self.bank()
            for qb in range(4):
                fns = [lambda e, pt=pt, k=k, qb=qb: e.matmul(pt[:, qb * 8:(qb + 1) * 8], lhsT=hT[:, k, qb * P:(qb + 1) * P], rhs=wwi[:, k, :],
                                                            start=(k == 0), stop=(k == DC - 1)) for k in range(DC)]
                sc.op('pe', fns, reads=hreads + [(L, 'wA')], writes=[pn])
            sc.op('dve', lambda e, pt=pt: e.tensor_scalar(out=WI[:, :, :], in0=pt[:, 0:32].rearrange('p (a b) -> p a b', b=8), scalar1=0.04419417382415922,
                                                        scalar2=None, op0=ALU.mult), reads=[pn], writes=[(L, 'WI')])
            def idx_bis(qb):
                gq = blk * 4 + qb
                nk = gq + 1
                W = nk * P
                nonlocal nrt
                for k0 in range(0, W, 512):
                    n = min(512, W - k0)
                    for ih in range(8):
                        b0 = (ih % 2) * 64
                        pn, pt = self.bank()
                        sc.op('pe', lambda e, pt=pt, ih=ih, b0=b0, qb=qb, k0=k0, n=n: e.matmul(
                            pt[:, 0:n], lhsT=QI[b0:b0 + 64, ih // 2, qb * P:(qb + 1) * P], rhs=KI[b0:b0 + 64, k0:k0 + n], start=True, stop=True),
                            reads=[(L, 'QI', ih // 2)] + [(L, 'KI', kb) for kb in range(k0 // 512, (k0 + n + 511) // 512)], writes=[pn])
                        rs = nrt % 2
                        nrt += 1
                        rn = 'tmp32_%d' % rs
                        sc.op('act', lambda e, pt=pt, rs=rs, n=n: e.activation(out=Rt[rs][:, 0:n], in_=pt[:, 0:n], func=AF.Relu), reads=[pn], writes=[rn])
                        if ih == 0:
                            sc.op('dve', lambda e, rs=rs, n=n, k0=k0, qb=qb: e.tensor_scalar(out=score[:, k0:k0 + n], in0=Rt[rs][:, 0:n], scalar1=WI[:, qb, 0:1],
                                                                                      scalar2=None, op0=ALU.mult), reads=[rn, (L, 'WI')], writes=[(L, 'score')])
                        else:
                            sc.op('dve', lambda e, rs=rs, n=n, k0=k0, qb=qb, ih=ih: e.scalar_tensor_tensor(
                                out=score[:, k0:k0 + n], in0=Rt[rs][:, 0:n], scalar=WI[:, qb, ih:ih + 1], in1=score[:, k0:k0 + n], op0=ALU.mult, op1=ALU.add),
                                reads=[rn, (L, 'WI'), (L, 'score')], writes=[(L, 'score')])
                SR = [(L, 'score'), (L, 'bis')]
                mx, lo, w0, mid, cnt, ff = bs
                if nk >= 3:
                    sc.op('dve', lambda e, W=W: e.tensor_reduce(out=mx[:], in_=score[:, 0:W], axis=AX.X, op=ALU.max), reads=SR, writes=[(L, 'bis')])
                    sc.op('dve', lambda e, W=W: e.tensor_reduce(out=lo[:], in_=score[:, 0:W], axis=AX.X, op=ALU.min), reads=SR, writes=[(L, 'bis')])
                    sc.op('dve', lambda e: e.tensor_tensor(out=w0[:], in0=mx[:], in1=lo[:], op=ALU.subtract), reads=SR, writes=[(L, 'bis')])
                else:
                    sc.op('dve', lambda e: e.memset(lo[:], -1e29), reads=SR, writes=[(L, 'bis')])
                sc.op('dve', lambda e, W=W: e.tensor_tensor(out=score[:, W - P:W], in0=score[:, W - P:W], in1=cmask[:], op=ALU.add),
                      reads=SR + ['cmask'], writes=SR)
                if nk >= 3:
                    for it in range(NI):
                        cst = 2.0 ** (-(it + 1))
                        sc.op('dve', lambda e, cst=cst: e.scalar_tensor_tensor(out=mid[:], in0=w0[:], scalar=cst, in1=lo[:], op0=ALU.mult, op1=ALU.add),
                              reads=SR, writes=[(L, 'bis')])
                        sc.op('dve', lambda e, W=W: e.tensor_scalar(out=nmq[:, 0:W], in0=score[:, 0:W], scalar1=mid[:, 0:1], scalar2=0.0, op0=ALU.is_ge,
                                                                   op1=ALU.add, accum_out=cnt[:, 0:1]), reads=SR + [(L, 'nmq')], writes=[(L, 'bis'), (L, 'nmq')])
                        sc.op('dve', lambda e, cst=cst: e.tensor_scalar(out=ff[:], in0=cnt[:], scalar1=256.0, scalar2=cst, op0=ALU.is_ge, op1=ALU.mult),
                              reads=SR, writes=[(L, 'bis')])
                        sc.op('dve', lambda e: e.scalar_tensor_tensor(out=lo[:], in0=ff[:], scalar=w0[:, 0:1], in1=lo[:], op0=ALU.mult, op1=ALU.add),
                              reads=SR, writes=[(L, 'bis')])
                sc.op('dve', lambda e, W=W: e.tensor_scalar(out=nmq[:, 0:W], in0=score[:, 0:W], scalar1=lo[:, 0:1], scalar2=-30000.0, op0=ALU.is_lt, op1=ALU.mult),
                      reads=SR + [(L, 'nmq')], writes=[(L, 'nmq')])
            def tr(qb):
                gq = blk * 4 + qb
                nk = gq + 1
                W = nk * P
                ms = gq % 2
                mn_ = (L, 'nmT', ms)
                for j0 in range(0, nk, 8):
                    jn = min(8, nk - j0)
                    pn, pt = self.bank()
                    ptb = pt[:].bitcast(BF16)
                    fns = [lambda e, ptb=ptb, j=j, j0=j0: e.transpose(out=ptb[:, (j - j0) * P:(j - j0 + 1) * P], in_=nmq[:, j * P:(j + 1) * P], identity=identb[:])
                           for j in range(j0, j0 + jn)]
                    sc.op('pe', fns, reads=[(L, 'nmq'), 'identb'], writes=[pn])
                    sc.op('act', lambda e, ptb=ptb, ms=ms, j0=j0, jn=jn: e.activation(out=nmT[ms][:, j0:j0 + jn, :], in_=ptb[:, 0:jn * P].rearrange('p (a b) -> p a b', b=P),
                                                                                func=AF.Copy), reads=[pn], writes=[(mn_, j0)])
            def att(qb):
                gq = blk * 4 + qb
                nk = gq + 1
                W = nk * P
                nonlocal npx
                ms = gq % 2
                mn_ = (L, 'nmT', ms)
                for kvh in range(4 if self.cfg.get('astage', 9) >= 4 else 0):
                    ab = self.abank[self.nab % 2]
                    self.nab += 1
                    pon = ('ps', ab)
                    po = self.ps[ab]
                    b0 = (kvh % 2) * 64
                    stb = {}

                    def emit_st(j, kvh=kvh, b0=b0, stb=stb):
                        pn, pt = self.bank()
                        stb[j] = (pn, pt)
                        near = (nk - 1 - j) if (nk - 1 - j) < 2 else None
                        fns = []
                        p3 = pt[:, :].rearrange('p (g q) -> p g q', q=P)
                        fns.append(lambda e, p3=p3, j=j, b0=b0, kvh=kvh, qb=qb: e.matmul(
                            p3, lhsT=KT[b0:b0 + 64, kvh // 2, j * P:(j + 1) * P],
                            rhs=QT[b0:b0 + 64, (kvh // 2) * 4:(kvh // 2) * 4 + 4, qb * P:(qb + 1) * P], start=True, stop=False))
                        if near is not None:
                            fns.append(lambda e, p3=p3, kvh=kvh, near=near: e.matmul(
                                p3, lhsT=identb[:], rhs=biasS[:, near * 16 + kvh * 4:near * 16 + kvh * 4 + 4, :], start=False, stop=False))
                        fns.append(lambda e, p3=p3, j=j, ms=ms: e.matmul(
                            p3, lhsT=identb[:], rhs=nmT[ms][:, j, :].unsqueeze(1).to_broadcast([P, 4, P]), start=False, stop=True))
                        sc.op('pe', fns, reads=[(L, 'KT', kvh // 2, j // 4), (mn_, (j // 8) * 8), 'identb', (L, 'biasS')] +
                              [(L, 'QT', (kvh // 2) * 4 + g) for g in range(4)], writes=[pn])
                    emit_st(0)
                    for j in range(nk):
                        if j + 1 < nk:
                            emit_st(j + 1)
                        pn, pt = stb[j]
                        px = npx % 3
                        npx += 1
                        pxn = 'pexp%d' % px
                        sc.op('act', lambda e, pt=pt, px=px: e.activation(out=pexp[px][:], in_=pt[:], func=AF.Exp), reads=[pn], writes=[pxn])
                        sc.op('pe', lambda e, po=po, px=px, j=j, kvh=kvh, nk=nk: e.matmul(po[0:65, :], lhsT=VA[:, j, kvh, :], rhs=pexp[px][:],
                                                                                     start=(j == 0), stop=(j == nk - 1)),
                              reads=[pxn, (L, 'VA', j), (L, 'VAones')], writes=[pon])
                    sc.op('act', lambda e, po=po: e.activation(out=osb[:], in_=po[0:65, :], func=AF.Copy), reads=[pon], writes=['osb'])
                    sc.op('act', lambda e: e.activation(out=osb[64:65, :], in_=osb[64:65, :], func=AF.Ln), reads=['osb'], writes=['osb'])
                    sc.op('act', lambda e: e.activation(out=osb[64:65, :], in_=osb[64:65, :], func=AF.Exp, scale=-1.0), reads=['osb'], writes=['osb'])
                    pn, pt = self.bank()
                    sc.op('pe', lambda e, pt=pt: e.matmul(pt[0:64, :], lhsT=selrow[:], rhs=osb[:], start=True, stop=True), reads=['osb', 'selrow'], writes=[pn])
                    sc.op('act', lambda e, pt=pt: e.activation(out=rbc[0:64, :], in_=pt[0:64, :], func=AF.Copy), reads=[pn], writes=['rbc'])
                    sc.op('pool', lambda e, kvh=kvh, qb=qb: e.tensor_tensor(out=OTn[:, kvh * 4:(kvh + 1) * 4, qb * P:(qb + 1) * P],
                                                                        in0=osb[0:64, :].rearrange('p (a b) -> p a b', b=P),
                                                                        in1=rbc[0:64, :].rearrange('p (a b) -> p a b', b=P), op=ALU.mult),
                          reads=['osb', 'rbc'], writes=[(L, 'OTn', kvh, qb)])
            nq = 4 if self.cfg.get('astage', 9) >= 2 else 0
            if nq:
                idx_bis(0)
                tr(0)
            for qb in range(nq):
                if qb + 1 < nq:
                    idx_bis(qb + 1)
                att(qb)
                if qb + 1 < nq:
                    tr(qb + 1)
            if self.cfg.get('astage', 9) < 5:
                continue
            oreads = [(L, 'OTn', kvh, qb) for kvh in range(4) for qb in range(4)] + [(L, 'wA')]
            for c in range(DC):
                if c % 4 == 0:
                    for h in range(16):
                        sc.dma('pool', 'wA', wout[:, h, :], self.d_awout[:, h, (c // 4) * 512:(c // 4 + 1) * 512], writes=[(L, 'wA')])
                pn, pt = self.bank()
                fns = [lambda e, pt=pt, h=h, c=c: e.matmul(pt[:], lhsT=wout[:, h, (c % 4) * P:(c % 4 + 1) * P], rhs=OTn[:, h, :], start=(h == 0), stop=(h == 15))
                       for h in range(16)]
                sc.op('pe', fns, reads=oreads, writes=[pn])
                sc.op('dve', lambda e, pt=pt, c=c, t=t: e.scalar_tensor_tensor(out=t[:, c, :], in0=pt[:], scalar=self.modv(li, 2, c), in1=t[:, c, :],
                                                                              op0=ALU.mult, op1=ALU.add), reads=[pn, (name, c), ('modT', li)], writes=[(name, c)])
                self.store_xT(blk, name, t, c)
        self.rot = list(range(8))
        sc.barrier()
        st.close()

    def ssm_setup(self):
        self.d_lamT = self.inp('ssm_lamT', [P, 32, 2])
        self.d_lstepT = self.inp('ssm_lstepT', [P, 32])
        self.d_Bblk = self.inp('ssm_Bblk', [32, P, 256])
        self.d_Cblk = self.inp('ssm_Cblk', [32, P, 256])
        self.d_dT = self.inp('ssm_dT', [P, DC])
        self.d_glu = self.inp('ssm_glu_r', [DC, P, DC * 256])

    def ssm_mixer(self, li):
        import math
        sc = self.sc
        st = contextlib.ExitStack()
        L = 'S'
        LC = 128
        I32 = mybir.dt.int32
        Ec = self.sb('Ec', [P, 32, LC], F32, st)
        En = self.sb('En', [P, 32, LC], F32, st)
        Bb = self.sb('Bb', [P, 32, 256], BF16, st)
        Cb = self.sb('Cb', [P, 32, 256], BF16, st)
        sm = {n: self.sb('ss_' + n, [P, 32], F32, st) for n in
              ['lr', 'li', 'stp', 'th', 'rr', 'u', 'f', 'g', 'sn', 'cs', 'x', 'y', 'den', 'cr', 'ci', 'cL', 'nL', 'ire', 'iim', 'e1c', 'e1n', 't1', 't2']}
        ni = self.sb('ss_ni', [P, 32], I32, st)
        lam = self.sb('ss_lam', [P, 32, 2], F32, st)
        dT = self.sb('ss_dT', [P, DC], F32, st)
        cs1 = self.sb('ss_c1', [P, 4], F32, st)
        SM = [(L, 'sm')]

        def dv(fn, extra_r=(), extra_w=()):
            sc.op('dve', fn, reads=SM + list(extra_r), writes=SM + list(extra_w))

        def ac(fn):
            sc.op('act', fn, reads=SM, writes=SM)
        sc.dma('sp', 'const', lam[:], self.d_lamT[:, :, :], writes=SM)
        sc.dma('sp', 'const', sm['stp'][:], self.d_lstepT[:, :], writes=SM)
        sc.dma('sp', 'const', dT[:], self.d_dT[:, :], writes=[(L, 'dT')])
        for k in range(32):
            sc.dma('pool', 'Bb', Bb[:, k, :], self.d_Bblk[k, :, :], writes=[(L, 'Bb')])
        dv(lambda e: e.tensor_scalar(out=sm['lr'][:], in0=lam[:, :, 0], scalar1=-1e-4, scalar2=None, op0=ALU.min))
        dv(lambda e: e.tensor_copy(out=sm['li'][:], in_=lam[:, :, 1]))
        ac(lambda e: e.activation(out=sm['stp'][:], in_=sm['stp'][:], func=AF.Exp))
        dv(lambda e: e.tensor_tensor(out=sm['th'][:], in0=sm['li'][:], in1=sm['stp'][:], op=ALU.mult))
        dv(lambda e: e.tensor_tensor(out=sm['rr'][:], in0=sm['lr'][:], in1=sm['stp'][:], op=ALU.mult))
        ac(lambda e: e.activation(out=sm['rr'][:], in_=sm['rr'][:], func=AF.Exp))

        def sincos(dst, off):
            dv(lambda e: e.tensor_scalar(out=sm['u'][:], in0=sm['th'][:], scalar1=1.0 / (2 * math.pi), scalar2=off, op0=ALU.mult, op1=ALU.add))
            dv(lambda e: e.tensor_copy(out=ni[:], in_=sm['u'][:]))
            dv(lambda e: e.tensor_copy(out=sm['f'][:], in_=ni[:]))
            dv(lambda e: e.tensor_tensor(out=sm['f'][:], in0=sm['u'][:], in1=sm['f'][:], op=ALU.subtract))
            dv(lambda e: e.tensor_scalar(out=sm['g'][:], in0=sm['f'][:], scalar1=0.5, scalar2=None, op0=ALU.is_gt))
            dv(lambda e: e.tensor_tensor(out=sm['f'][:], in0=sm['f'][:], in1=sm['g'][:], op=ALU.subtract))
            dv(lambda e: e.tensor_scalar(out=sm['g'][:], in0=sm['f'][:], scalar1=-0.5, scalar2=None, op0=ALU.is_lt))
            dv(lambda e: e.tensor_tensor(out=sm['f'][:], in0=sm['f'][:], in1=sm['g'][:], op=ALU.add))
            ac(lambda e, dst=dst: e.activation(out=sm[dst][:], in_=sm['f'][:], func=AF.Sin, scale=-2 * math.pi))
        sincos('sn', 64.5)
        sincos('cs', 64.75)
        dv(lambda e: e.tensor_tensor(out=sm['x'][:], in0=sm['rr'][:], in1=sm['cs'][:], op=ALU.mult))
        dv(lambda e: e.tensor_scalar(out=sm['x'][:], in0=sm['x'][:], scalar1=-1.0, scalar2=None, op0=ALU.add))
        dv(lambda e: e.tensor_tensor(out=sm['y'][:], in0=sm['rr'][:], in1=sm['sn'][:], op=ALU.mult))
        dv(lambda e: e.tensor_tensor(out=sm['den'][:], in0=sm['lr'][:], in1=sm['lr'][:], op=ALU.mult))
        dv(lambda e: e.tensor_tensor(out=sm['t1'][:], in0=sm['li'][:], in1=sm['li'][:], op=ALU.mult))
        dv(lambda e: e.tensor_tensor(out=sm['den'][:], in0=sm['den'][:], in1=sm['t1'][:], op=ALU.add))
        dv(lambda e: e.reciprocal(out=sm['den'][:], in_=sm['den'][:]))
        dv(lambda e: e.tensor_tensor(out=sm['cr'][:], in0=sm['x'][:], in1=sm['lr'][:], op=ALU.mult))
        dv(lambda e: e.tensor_tensor(out=sm['t1'][:], in0=sm['y'][:], in1=sm['li'][:], op=ALU.mult))
        dv(lambda e: e.tensor_tensor(out=sm['cr'][:], in0=sm['cr'][:], in1=sm['t1'][:], op=ALU.add))
        dv(lambda e: e.tensor_tensor(out=sm['cr'][:], in0=sm['cr'][:], in1=sm['den'][:], op=ALU.mult))
        dv(lambda e: e.tensor_tensor(out=sm['ci'][:], in0=sm['y'][:], in1=sm['lr'][:], op=ALU.mult))
        dv(lambda e: e.tensor_tensor(out=sm['t1'][:], in0=sm['x'][:], in1=sm['li'][:], op=ALU.mult))
        dv(lambda e: e.tensor_tensor(out=sm['ci'][:], in0=sm['ci'][:], in1=sm['t1'][:], op=ALU.subtract))
        dv(lambda e: e.tensor_tensor(out=sm['ci'][:], in0=sm['ci'][:], in1=sm['den'][:], op=ALU.mult))
        st2 = contextlib.ExitStack()
        Tc = self.sb('Tc', [P, LC, 32], F32, st2)
        Tn = self.sb('Tn', [P, LC, 32], F32, st2)
        U1 = self.sb('U1', [P, LC // 2, 32], F32, st2)
        U2 = self.sb('U2', [P, LC // 2, 32], F32, st2)
        cst1 = self.sb('tp1s', [P, 512], F32, st2)
        cst2 = self.sb('tp2s', [P, 512], F32, st2)
        dv(lambda e: e.tensor_copy(out=sm['e1c'][:], in_=sm['cs'][:]))
        dv(lambda e: e.tensor_scalar(out=sm['e1n'][:], in0=sm['sn'][:], scalar1=-1.0, scalar2=None, op0=ALU.mult))
        dv(lambda e: e.memset(Tc[:, 0, :], 1.0), extra_w=[(L, 'T')])
        dv(lambda e: e.memset(Tn[:, 0, :], 0.0), extra_w=[(L, 'T')])
        TT = [(L, 'T')]
        n = 1
        while n < LC:
            ecb = sm['e1c'][:, :].unsqueeze(1).to_broadcast([P, n, 32])
            enb = sm['e1n'][:, :].unsqueeze(1).to_broadcast([P, n, 32])
            dv(lambda e, n=n, ecb=ecb: e.tensor_tensor(out=Tc[:, n:2 * n, :], in0=Tc[:, 0:n, :], in1=ecb, op=ALU.mult), TT, TT)
            dv(lambda e, n=n, enb=enb: e.tensor_tensor(out=U1[:, 0:n, :], in0=Tn[:, 0:n, :], in1=enb, op=ALU.mult), TT, TT)
            dv(lambda e, n=n: e.tensor_tensor(out=Tc[:, n:2 * n, :], in0=Tc[:, n:2 * n, :], in1=U1[:, 0:n, :], op=ALU.subtract), TT, TT)
            dv(lambda e, n=n, enb=enb: e.tensor_tensor(out=Tn[:, n:2 * n, :], in0=Tc[:, 0:n, :], in1=enb, op=ALU.mult), TT, TT)
            dv(lambda e, n=n, ecb=ecb: e.tensor_tensor(out=U2[:, 0:n, :], in0=Tn[:, 0:n, :], in1=ecb, op=ALU.mult), TT, TT)
            dv(lambda e, n=n: e.tensor_tensor(out=Tn[:, n:2 * n, :], in0=Tn[:, n:2 * n, :], in1=U2[:, 0:n, :], op=ALU.add), TT, TT)
            dv(lambda e: e.tensor_tensor(out=sm['t1'][:], in0=sm['e1c'][:], in1=sm['e1c'][:], op=ALU.mult))
            dv(lambda e: e.tensor_tensor(out=sm['t2'][:], in0=sm['e1n'][:], in1=sm['e1n'][:], op=ALU.mult))
            dv(lambda e: e.tensor_tensor(out=sm['e1n'][:], in0=sm['e1c'][:], in1=sm['e1n'][:], op=ALU.mult))
            dv(lambda e: e.tensor_scalar(out=sm['e1n'][:], in0=sm['e1n'][:], scalar1=2.0, scalar2=None, op0=ALU.mult))
            dv(lambda e: e.tensor_tensor(out=sm['e1c'][:], in0=sm['t1'][:], in1=sm['t2'][:], op=ALU.subtract))
            n *= 2
        dv(lambda e: e.tensor_copy(out=sm['cL'][:], in_=sm['e1c'][:]))
        dv(lambda e: e.tensor_copy(out=sm['nL'][:], in_=sm['e1n'][:]))
        dv(lambda e: e.memset(sm['ire'][:], 0.0))
        dv(lambda e: e.memset(sm['iim'][:], 0.0))
        dv(lambda e: e.tensor_copy(out=Ec[:, :, :], in_=Tc[:, :, :].rearrange('p t k -> p k t')), TT, [(L, 'E')])
        dv(lambda e: e.tensor_copy(out=En[:, :, :], in_=Tn[:, :, :].rearrange('p t k -> p k t')), TT, [(L, 'E')])
        for k in range(32):
            sc.dma('sp', 'cstage', cst1[:, 0:256], self.d_Cblk[k, :, :], writes=['cst1'])
            crk = sm['cr'][:, k:k + 1]
            cik = sm['ci'][:, k:k + 1]
            sc.op('dve', lambda e, cik=cik: e.tensor_scalar(out=cst2[:, 0:128], in0=cst1[:, 128:256], scalar1=cik, scalar2=None, op0=ALU.mult),
                  reads=['cst1'] + SM, writes=['cst2'])
            sc.op('dve', lambda e, k=k, crk=crk: e.scalar_tensor_tensor(out=Cb[:, k, 0:128], in0=cst1[:, 0:128], scalar=crk, in1=cst2[:, 0:128], op0=ALU.mult, op1=ALU.subtract),
                  reads=['cst1', 'cst2'] + SM, writes=[(L, 'Cb')])
            sc.op('dve', lambda e, crk=crk: e.tensor_scalar(out=cst2[:, 128:256], in0=cst1[:, 128:256], scalar1=crk, scalar2=None, op0=ALU.mult),
                  reads=['cst1'] + SM, writes=['cst2'])
            sc.op('dve', lambda e, k=k, cik=cik: e.scalar_tensor_tensor(out=Cb[:, k, 128:256], in0=cst1[:, 0:128], scalar=cik, in1=cst2[:, 128:256], op0=ALU.mult, op1=ALU.add),
                  reads=['cst1', 'cst2'] + SM, writes=[(L, 'Cb')])
        if self.cfg.get('sdebug'):
            dbg = self.nc.dram_tensor('dbg', [P, 7 * 32 + 512 + 512], F32, kind="ExternalOutput").ap()
            for i, nme in enumerate(['sn', 'cs', 'rr', 'cr', 'ci', 'cL', 'nL']):
                sc.dma('sp', 'const', dbg[:, i * 32:(i + 1) * 32], sm[nme][:], reads=SM, writes=[('dbg', i)])
            sc.dma('sp', 'const', dbg[:, 224:224 + 256], Ec[:, 0:2, :], reads=[(L, 'E')], writes=[('dbg', 10)])
            sc.dma('sp', 'const', dbg[:, 224 + 256:224 + 512], En[:, 0:2, :], reads=[(L, 'E')], writes=[('dbg', 11)])
            sc.dma('sp', 'const', dbg[:, 224 + 512:224 + 768], Cb[:, 0, :], reads=[(L, 'Cb')], writes=[('dbg', 12)], allow_dtype=True) if False else None
        sc.barrier()
        st2.close()
        hT = self.sb('hTs', [P, DC, 512], BF16, st)
        h32 = self.sb('h32s', [P, DC, 512], F32, st)
        GT = self.sb('GTs', [P, DC, 512], BF16, st)
        Sb = self.sb('Sb', [P, 4, 2, 512], BF16, st)
        wgl = [self.sb('wgl%d' % i, [P, DC * 256], BF16, st) for i in range(2)]
        bre2 = [self.sb('bre%d' % i, [P, 512], F32, st) for i in range(2)]
        bim2 = [self.sb('bim%d' % i, [P, 512], F32, st) for i in range(2)]
        wre2 = [self.sb('wre%d' % i, [P, 512], F32, st) for i in range(2)]
        wim2 = [self.sb('wim%d' % i, [P, 512], F32, st) for i in range(2)]
        xrs2 = [self.sb('xrs%d' % i, [P, 512], F32, st) for i in range(2)]
        xis2 = [self.sb('xis%d' % i, [P, 512], F32, st) for i in range(2)]
        tq = self.sb('tq', [P, 512], F32, st)
        tq2 = self.sb('tq2', [P, 512], F32, st)
        tp1 = self.sb('tp1', [P, 512], F32, st)
        tp2 = self.sb('tp2', [P, 512], F32, st)
        nw = 0
        for blk in range(self.cfg.get('snblk', NBLK)):
            name, t = self.load_xT(blk, False)
            self.norm_mod(li, 0, name, t, hT, (L, 'hT'), 0, h32=h32)
            for j in range(DC):
                for kk in range(4):
                    k = 4 * j + kk
                    pxr_n, pxr = self.bank()
                    pxi_n, pxi = self.bank()
                    hr = [((L, 'hT'), j, 0), (L, 'Bb')]
                    sc.op('pe', lambda e, pxr=pxr, k=k, j=j: e.matmul(pxr[:], lhsT=Bb[:, k, 0:128], rhs=hT[:, j, :], start=True, stop=True), reads=hr, writes=[pxr_n])
                    sc.op('pe', lambda e, pxi=pxi, k=k, j=j: e.matmul(pxi[:], lhsT=Bb[:, k, 128:256], rhs=hT[:, j, :], start=True, stop=True), reads=hr, writes=[pxi_n])
                    cb = Ec[:, k, :].unsqueeze(1).to_broadcast([P, 4, LC])
                    nb = En[:, k, :].unsqueeze(1).to_broadcast([P, 4, LC])

                    def v3(ap):
                        return ap.rearrange('p (a b) -> p a b', b=LC)
                    ER = [(L, 'E')]
                    sl = k % 2
                    bre, bim, wre, wim, xrs, xis = bre2[sl], bim2[sl], wre2[sl], wim2[sl], xrs2[sl], xis2[sl]
                    BRE, BIM, WRE, WIM, XRS, XIS = ('bre%d' % sl), ('bim%d' % sl), ('wre%d' % sl), ('wim%d' % sl), ('xrs%d' % sl), ('xis%d' % sl)
                    sc.op('act', lambda e, pxr=pxr, xrs=xrs: e.activation(out=xrs[:], in_=pxr[:], func=AF.Copy), reads=[pxr_n], writes=[XRS])
                    sc.op('act', lambda e, pxi=pxi, xis=xis: e.activation(out=xis[:], in_=pxi[:], func=AF.Copy), reads=[pxi_n], writes=[XIS])
                    sc.op('dve', lambda e, xrs=xrs, cb=cb, bre=bre: e.tensor_tensor(out=v3(bre[:, :]), in0=v3(xrs[:, :]), in1=cb, op=ALU.mult), reads=[XRS] + ER, writes=[BRE])
                    sc.op('dve', lambda e, xis=xis, nb=nb: e.tensor_tensor(out=v3(tq[:, :]), in0=v3(xis[:, :]), in1=nb, op=ALU.mult), reads=[XIS] + ER, writes=['tq'])
                    sc.op('dve', lambda e, bre=bre: e.tensor_tensor(out=bre[:], in0=bre[:], in1=tq[:], op=ALU.subtract), reads=[BRE, 'tq'], writes=[BRE])
                    sc.op('pool', lambda e, xis=xis, cb=cb, bim=bim: e.tensor_tensor(out=v3(bim[:, :]), in0=v3(xis[:, :]), in1=cb, op=ALU.mult), reads=[XIS] + ER, writes=[BIM])
                    sc.op('pool', lambda e, xrs=xrs, nb=nb: e.tensor_tensor(out=v3(tq2[:, :]), in0=v3(xrs[:, :]), in1=nb, op=ALU.mult), reads=[XRS] + ER, writes=['tq2'])
                    sc.op('pool', lambda e, bim=bim: e.tensor_tensor(out=bim[:], in0=bim[:], in1=tq2[:], op=ALU.add), reads=[BIM, 'tq2'], writes=[BIM])
                    rb = sm['rr'][:, k:k + 1].to_broadcast([P, LC])
                    cLk = sm['cL'][:, k:k + 1]
                    nLk = sm['nL'][:, k:k + 1]
                    irek = sm['ire'][:, k:k + 1]
                    iimk = sm['iim'][:, k:k + 1]
                    CR = [(L, 'carry', k)]
                    for ch in range(4):
                        lo_, hi_ = ch * LC, (ch + 1) * LC
                        sc.op('dve', lambda e, rb=rb, irek=irek, lo_=lo_, hi_=hi_, wre=wre, bre=bre: e.tensor_tensor_scan(out=wre[:, lo_:hi_], data0=rb, data1=bre[:, lo_:hi_], initial=irek,
                                                                                                 op0=ALU.mult, op1=ALU.add), reads=[BRE] + CR + SM, writes=[WRE])
                        sc.op('dve', lambda e, rb=rb, iimk=iimk, lo_=lo_, hi_=hi_, wim=wim, bim=bim: e.tensor_tensor_scan(out=wim[:, lo_:hi_], data0=rb, data1=bim[:, lo_:hi_], initial=iimk,
                                                                                                 op0=ALU.mult, op1=ALU.add), reads=[BIM] + CR + SM, writes=[WIM])
                        wrl = wre[:, hi_ - 1:hi_]
                        wil = wim[:, hi_ - 1:hi_]
                        sc.op('dve', lambda e, nLk=nLk, wil=wil: e.tensor_tensor(out=cs1[:, 0:1], in0=wil, in1=nLk, op=ALU.mult), reads=[WIM] + SM, writes=['cs1'])
                        sc.op('dve', lambda e, cLk=cLk, wrl=wrl, irek=irek: e.scalar_tensor_tensor(out=irek, in0=wrl, scalar=cLk, in1=cs1[:, 0:1], op0=ALU.mult, op1=ALU.add),
                              reads=[WRE, 'cs1'] + SM, writes=CR)
                        sc.op('dve', lambda e, nLk=nLk, wrl=wrl: e.tensor_tensor(out=cs1[:, 1:2], in0=wrl, in1=nLk, op=ALU.mult), reads=[WRE] + SM, writes=['cs1'])
                        sc.op('dve', lambda e, cLk=cLk, wil=wil, iimk=iimk: e.scalar_tensor_tensor(out=iimk, in0=wil, scalar=cLk, in1=cs1[:, 1:2], op0=ALU.mult, op1=ALU.subtract),
                              reads=[WIM, 'cs1'] + SM, writes=CR)
                    if self.cfg.get('sdebug') and blk == 0 and k == 1:
                        dbg2 = self.nc.dram_tensor('dbg2', [P, 4, 512], F32, kind="ExternalOutput").ap()
                        sc.dma('sp', 'const', dbg2[:, 0, :], bre[:], reads=['bre'], writes=[('dbg2', 0)])
                        sc.dma('sp', 'const', dbg2[:, 1, :], bim[:], reads=['bim'], writes=[('dbg2', 1)])
                        sc.dma('sp', 'const', dbg2[:, 2, :], wre[:], reads=['wre'], writes=[('dbg2', 2)])
                        sc.dma('sp', 'const', dbg2[:, 3, :], wim[:], reads=['wim'], writes=[('dbg2', 3)])
                    sc.op('pool', lambda e, cb=cb, wre=wre: e.tensor_tensor(out=v3(tp1[:, :]), in0=v3(wre[:, :]), in1=cb, op=ALU.mult), reads=[WRE] + ER, writes=['tp1'])
                    sc.op('pool', lambda e, nb=nb, wim=wim: e.tensor_tensor(out=v3(tp2[:, :]), in0=v3(wim[:, :]), in1=nb, op=ALU.mult), reads=[WIM] + ER, writes=['tp2'])
                    sc.op('pool', lambda e, kk=kk: e.tensor_tensor(out=Sb[:, kk, 0, :], in0=tp1[:], in1=tp2[:], op=ALU.add), reads=['tp1', 'tp2'], writes=[(L, 'Sb', kk)])
                    sc.op('pool', lambda e, nb=nb, wre=wre: e.tensor_tensor(out=v3(tp1[:, :]), in0=v3(wre[:, :]), in1=nb, op=ALU.mult), reads=[WRE] + ER, writes=['tp1'])
                    sc.op('pool', lambda e, cb=cb, wim=wim: e.tensor_tensor(out=v3(tp2[:, :]), in0=v3(wim[:, :]), in1=cb, op=ALU.mult), reads=[WIM] + ER, writes=['tp2'])
                    sc.op('pool', lambda e, kk=kk: e.tensor_tensor(out=Sb[:, kk, 1, :], in0=tp1[:], in1=tp2[:], op=ALU.subtract), reads=['tp1', 'tp2'], writes=[(L, 'Sb', kk)])
                if self.cfg.get('sdebug') and blk == 0 and j == 0:
                    dbg3 = self.nc.dram_tensor('dbg3', [P, 2, 512], F32, kind="ExternalOutput").ap()
                    sc.dma('pool', 'dbg3', dbg3[:, 0, :], Sb[:, 1, 0, :], reads=[(L, 'Sb', 1)], writes=[('dbg3', 0)])
                    sc.dma('pool', 'dbg3', dbg3[:, 1, :], Sb[:, 1, 1, :], reads=[(L, 'Sb', 1)], writes=[('dbg3', 1)])
                pyn, py = self.bank()
                fns = []
                for kk in range(4):
                    k = 4 * j + kk
                    fns.append(lambda e, py=py, k=k, kk=kk: e.matmul(py[:], lhsT=Cb[:, k, 0:128], rhs=Sb[:, kk, 0, :], start=(kk == 0), stop=False))
                    fns.append(lambda e, py=py, k=k, kk=kk: e.matmul(py[:], lhsT=Cb[:, k, 128:256], rhs=Sb[:, kk, 1, :], start=False, stop=(kk == 3)))
                sc.op('pe', fns, reads=[(L, 'Sb', kk) for kk in range(4)] + [(L, 'Cb')], writes=[pyn])
                if self.cfg.get('sdebug') == 2 and blk == 0 and j == 0:
                    sc.op('act', lambda e, py=py: e.activation(out=bre[:], in_=py[:], func=AF.Copy), reads=[pyn, 'bre'], writes=['bre'])
                    sc.dma('sp', 'const', dbg2[:, 4, :], bre[:], reads=['bre'], writes=[('dbg2', 4)])
                z = self.tmp32[0]
                w_ = self.tmp32[1]
                sc.op('dve', lambda e, py=py, j=j, z=z: e.scalar_tensor_tensor(out=z[:], in0=h32[:, j, :], scalar=dT[:, j:j + 1], in1=py[:], op0=ALU.mult, op1=ALU.add),
                      reads=[pyn, ('h32', j), (L, 'dT')], writes=['tmp32_0'])
                if self.cfg.get('sdebug') and blk == 0 and j == 0:
                    dbg4 = self.nc.dram_tensor('dbg4', [P, 3, 512], F32, kind="ExternalOutput").ap()
                    sc.dma('sp', 'const', dbg4[:, 0, :], z[:], reads=['tmp32_0'], writes=[('dbg4', 0)])
                    sc.dma('pool', 'dbg4b', dbg4[:, 1, 0:256], Cb[:, 1, :], reads=[(L, 'Cb')], writes=[('dbg4', 1)])
                    sc.dma('pool', 'dbg4b', dbg4[:, 1, 256:512], Cb[:, 1, :], reads=[(L, 'Cb')], writes=[('dbg4', 3)])
                sc.op('act', lambda e, z=z, w_=w_: e.activation(out=w_[:], in_=z[:], func=AF.Square), reads=['tmp32_0'], writes=['tmp32_1'])
                sc.op('dve', lambda e, w_=w_: e.tensor_scalar(out=w_[:], in0=w_[:], scalar1=0.044715, scalar2=1.0, op0=ALU.mult, op1=ALU.add), reads=['tmp32_1'], writes=['tmp32_1'])
                sc.op('dve', lambda e, z=z, w_=w_: e.tensor_tensor(out=w_[:], in0=w_[:], in1=z[:], op=ALU.mult), reads=['tmp32_0', 'tmp32_1'], writes=['tmp32_1'])
                sc.op('act', lambda e, w_=w_: e.activation(out=w_[:], in_=w_[:], func=AF.Sigmoid, scale=1.5957691216057308), reads=['tmp32_1'], writes=['tmp32_1'])
                sc.op('dve', lambda e, z=z, w_=w_, j=j: e.tensor_tensor(out=GT[:, j, :], in0=z[:], in1=w_[:], op=ALU.mult), reads=['tmp32_0', 'tmp32_1'], writes=[(L, 'GT', j)])
            if self.cfg.get('sdebug') and blk == 0:
                sc.dma('pool', 'dbg4b', dbg4[:, 2, :], GT[:, 0, :], reads=[(L, 'GT', 0)], writes=[('dbg4', 2)])
            greads = [(L, 'GT', j) for j in range(DC)]
            for c in range(DC):
                slot = nw % 2
                nw += 1
                wn = (L, 'wgl', slot)
                self.load_w_cast('wgl%d' % slot, wgl[slot][:, :], self.d_glu[c, :, :], None, writes=[wn])
                pln, pl = self.bank()
                pgn, pg = self.bank()
                for (pn, pt, off) in ((pln, pl, 0), (pgn, pg, 128)):
                    fns = [lambda e, pt=pt, kq=kq, off=off, slot=slot: e.matmul(pt[:], lhsT=wgl[slot][:, kq * 256 + off: kq * 256 + off + 128], rhs=GT[:, kq, :],
                                                                              start=(kq == 0), stop=(kq == DC - 1)) for kq in range(DC)]
                    sc.op('pe', fns, reads=greads + [wn], writes=[pn])
                tm = self.tmp32[2]
                sc.op('act', lambda e, pg=pg, tm=tm: e.activation(out=tm[:], in_=pg[:], func=AF.Sigmoid), reads=[pgn], writes=['tmp32_2'])
                sc.op('dve', lambda e, pl=pl, tm=tm: e.tensor_tensor(out=tm[:], in0=pl[:], in1=tm[:], op=ALU.mult), reads=[pln, 'tmp32_2'], writes=['tmp32_2'])
                sc.op('dve', lambda e, tm=tm, c=c, t=t: e.scalar_tensor_tensor(out=t[:, c, :], in0=tm[:], scalar=self.modv(li, 2, c), in1=t[:, c, :], op0=ALU.mult, op1=ALU.add),
                      reads=['tmp32_2', (name, c), ('modT', li)], writes=[(name, c)])
                self.store_xT(blk, name, t, c)
        sc.barrier()
        st.close()

    def epilogue(self):
        sc = self.sc
        self.out = self.nc.dram_tensor('out', [self.ntok, D], F32, kind="ExternalOutput").ap()
        otok = [self.sb('otok%d' % i, [P, D], F32) for i in range(2)]
        n = 0
        for blk in range(self.ntok // 512):
            name, t = self.load_xT(blk, False)
            for j in range(4):
                slot = n % 2
                n += 1
                on = 'otok%d' % slot
                for hh in range(2):
                    pname, pt = self.bank()
                    fns = [lambda e, pt=pt, j=j, c=c, hh=hh, t=t: e.transpose(out=pt[:, (c - hh * 4) * P:(c - hh * 4 + 1) * P],
                                                                      in_=t[:, c, j * P:(j + 1) * P], identity=self.ident[:])
                           for c in range(hh * 4, hh * 4 + 4)]
                    sc.op('pe', fns, reads=[(name, c) for c in range(DC)] + ['ident'], writes=[pname])
                    if hh == 0:
                        sc.op('act', lambda e, pt=pt, slot=slot: e.activation(out=otok[slot][:, 0:512], in_=pt[:], func=AF.Copy),
                              reads=[pname], writes=[(on, 0)])
                    else:
                        sc.op('dve', lambda e, pt=pt, slot=slot: e.tensor_copy(out=otok[slot][:, 512:1024], in_=pt[:]),
                              reads=[pname], writes=[(on, 1)])
                sc.dma('sp', 'st_' + on, self.out[blk * 512 + j * P: blk * 512 + (j + 1) * P, :], otok[slot][:],
                       reads=[(on, 0), (on, 1)], writes=[('out', blk, j)])

    def build(self):
        cfg = self.cfg
        self.setup_common()
        self.epsb = self.sb('epsb', [P, 1], F32)
        self.sc.op('dve', lambda e: e.memset(self.epsb[:], EPS), writes=['epsb'])
        self.build_mod()
        self.alloc_stream()
        self.conv_setup()
        self.ffn_setup()
        self.attn_setup()
        self.ssm_setup()
        steps = cfg.get('steps', ['m0', 'f0', 'm1', 'f1', 'm2', 'f2', 'm3', 'f3'])
        first = True
        if steps[0][0] != 'm' or int(steps[0][1]) % 3 != 0:
            st = contextlib.ExitStack()
            self.xtok = self.sb('xtok', [P, 4, D], F32, st)
            for blk in range(NBLK):
                name, t = self.load_xT(blk, True)
                for c in range(DC):
                    self.store_xT(blk, name, t, c)
            self.sc.barrier()
            st.close()
            first = False
        tail_split = cfg.get('tail_split', False)
        for s in steps:
            li = int(s[1])
            if tail_split and s == 'm3':
                self.sc.barrier()
                self.xs_d = self.nc.dram_tensor('xs_scratch', [D, S // 2 + 512], F32).ap()
                self.sc.dma('sp', 'xstage', self.xs_d[:, 512:512 + S // 2], (lambda: self.xT_d[:, bass.ds(self.rv * 2048, 2048)]), writes=['xs'])
                self.sc.dma('sp', 'xstage', self.xs_d[:, 0:512], (lambda: self.xT_d[:, bass.ds(self.rv * 1536, 512)]), writes=['xs'])
                self.rd_mode, self.wr_mode, self.ntok = 'dynfull', 'half', S // 2
            if tail_split and s == 'f3':
                self.rd_mode, self.wr_mode, self.ntok = 'half', 'half', S // 2
            if s[0] == 'm':
                if li % 3 == 0:
                    self.conv_mixer(li, li // 3, first)
                elif li % 3 == 1:
                    self.attn_mixer(li)
                else:
                    self.ssm_mixer(li)
                first = False
            else:
                self.ffn(li, li % 2 == 1)
        self.epilogue()
        self.sc.finish('sp')
        self.sc.emit()
        return self.nc


def host_layout(inputs, b):
    f = np.float32
    m = {}
    m['ident'] = np.eye(P, dtype=f)
    m['x'] = np.ascontiguousarray(inputs['x'][b])
    m['cT'] = np.ascontiguousarray(inputs['c'][b].reshape(DC, P).T)
    m['ada_w'] = inputs['ada_w']
    m['ada_bT'] = np.ascontiguousarray(inputs['ada_b'].reshape(4, 48, P).transpose(2, 0, 1))
    m['norm_gT'] = np.ascontiguousarray(inputs['norm_g'].reshape(4, 2, DC, P).transpose(3, 0, 1, 2))
    m['conv_w_in'] = inputs['conv_w_in']
    m['conv_w_out'] = inputs['conv_w_out']
    m['conv_wT'] = np.ascontiguousarray(inputs['conv_w'].reshape(2, 3, DC, P).transpose(3, 0, 1, 2))
    m['rankf'] = np.zeros((P, 1), dtype=f)
    return m


def rel_bucket_np(dist):
    import math
    d = np.maximum(dist, 1).astype(np.float32)
    large = 16 + (np.log(d / np.float32(16)) / np.float32(math.log(128 / 16)) * np.float32(16)).astype(np.int32)
    large = np.minimum(large, 31)
    return np.where(dist < 16, dist, large)


def ssm_layout(inputs):
    f = np.float32
    m = {}
    lre = inputs['ssm_lambda_re'][0]
    lim = inputs['ssm_lambda_im'][0]
    lamT = np.empty((P, 32, 2), dtype=f)
    lst = np.empty((P, 32), dtype=f)
    Bb = np.zeros((32, P, 2, P), dtype=f)
    Cb = np.zeros((32, P, 2, P), dtype=f)
    bre = inputs['ssm_b_re'][0]
    bim = inputs['ssm_b_im'][0]
    cre = inputs['ssm_c_re'][0]
    cim = inputs['ssm_c_im'][0]
    for k in range(32):
        for gg in range(2):
            g = 2 * k + gg
            lamT[gg * 64:(gg + 1) * 64, k, 0] = lre[g]
            lamT[gg * 64:(gg + 1) * 64, k, 1] = lim[g]
            lst[gg * 64:(gg + 1) * 64, k] = inputs['ssm_log_step'][0, g]
            r0 = (g % 8) * 16
            Bb[k, r0:r0 + 16, 0, gg * 64:(gg + 1) * 64] = bre[g].T
            Bb[k, r0:r0 + 16, 1, gg * 64:(gg + 1) * 64] = bim[g].T
            Cb[k, gg * 64:(gg + 1) * 64, 0, r0:r0 + 16] = cre[g].T
            Cb[k, gg * 64:(gg + 1) * 64, 1, r0:r0 + 16] = cim[g].T
    m['ssm_lamT'] = lamT
    m['ssm_lstepT'] = lst
    m['ssm_Bblk'] = Bb.reshape(32, P, 256)
    m['ssm_Cblk'] = Cb.reshape(32, P, 256)
    m['ssm_dT'] = np.ascontiguousarray(inputs['ssm_d'][0].reshape(DC, P).T)
    w = inputs['ssm_w_glu'][0]
    lin = w[:, :D].reshape(DC, P, DC, P)
    gate = w[:, D:].reshape(DC, P, DC, P)
    r = np.empty((DC, P, DC, 2, P), dtype=f)
    r[:, :, :, 0, :] = lin.transpose(2, 1, 0, 3)
    r[:, :, :, 1, :] = gate.transpose(2, 1, 0, 3)
    m['ssm_glu_r'] = r.reshape(DC, P, DC * 256)
    return m


def attn_layout(inputs):
    f = np.float32
    m = {}
    w = inputs['attn_w_in'][0]
    wq = w[:, 0:1024].reshape(D, 16, 64)
    order = []
    for pair in range(2):
        for g in range(4):
            order += [(2 * pair) * 4 + g, (2 * pair + 1) * 4 + g]
    m['attn_wq_perm'] = np.ascontiguousarray(wq[:, order, :]).reshape(D, 1024)
    m['attn_wk'] = np.ascontiguousarray(w[:, 1024:1280])
    m['attn_wv'] = np.ascontiguousarray(w[:, 1280:1536])
    m['attn_wqi'] = np.ascontiguousarray(w[:, 1536:2048])
    ki = w[:, 2048:2112]
    m['attn_wki2'] = np.ascontiguousarray(np.concatenate([ki, ki], axis=1))
    m['attn_wwi'] = np.ascontiguousarray(w[:, 2112:2120])
    m['attn_wout_r'] = np.ascontiguousarray(inputs['attn_w_out'][0].reshape(16, 64, D).transpose(1, 0, 2))
    qg = inputs['attn_q_gain'][0]
    kg = inputs['attn_k_gain'][0]
    m['attn_gainT'] = np.ascontiguousarray(np.stack([np.tile(qg, 2), np.tile(kg, 2)], axis=1))
    rb = inputs['rel_bias']
    sl = np.arange(P)[:, None]
    tl = np.arange(P)[None, :]
    bt = np.empty((P, 32, P), dtype=f)
    for kind in range(2):
        dist = np.maximum(tl - sl + kind * P, 0)
        bk = rel_bucket_np(dist)
        for h in range(16):
            bt[:, kind * 16 + h, :] = rb[bk, h]
    m['attn_biasT'] = bt
    m['attn_b31B'] = np.ascontiguousarray(np.broadcast_to(rb[31][None, :], (P, 16)))
    cm = np.zeros((P, P), dtype=f)
    cm[np.arange(P)[None, :] > np.arange(P)[:, None]] = -1e30
    m['cmask'] = cm
    bo = np.zeros((P, P), dtype=f)
    bo[0:64, 0:64] = 1.0
    bo[64:128, 64:128] = 1.0
    m['blockones'] = bo
    sr = np.zeros((65, 64), dtype=f)
    sr[64, :] = 1.0
    m['selrow'] = sr
    return m


_shared_cache = {}


def shared_layout(inputs):
    f = np.float32
    m = {}
    gu = inputs['ffn_w_gu']
    g = gu[:, :, :DFF].reshape(2, DC, P, FC, P)
    u = gu[:, :, DFF:].reshape(2, DC, P, FC, P)
    gu_r = np.stack([g, u], axis=4)
    m['ffn_gu_r'] = np.ascontiguousarray(gu_r.transpose(0, 3, 2, 1, 4, 5)).reshape(2, FC, P, DC * 256)
    dn = inputs['ffn_w_down'].reshape(2, FC, P, DC, P)
    m['ffn_dn_r'] = np.ascontiguousarray(dn.transpose(0, 3, 2, 1, 4)).reshape(2, DC, P, FC * P)
    gu = inputs['moe_w_gu']
    g = gu[:, :, :, :DFF].reshape(2, NE, DC, P, FC, P)
    u = gu[:, :, :, DFF:].reshape(2, NE, DC, P, FC, P)
    r = np.empty((2, NE, FC, P, DC, 2, P), dtype=f)
    r[:, :, :, :, :, 0, :] = g.transpose(0, 1, 4, 3, 2, 5)
    r[:, :, :, :, :, 1, :] = u.transpose(0, 1, 4, 3, 2, 5)
    m['moe_gu_r'] = r.reshape(2, NE, FC, P, DC * 256)
    dn = inputs['moe_w_down'].reshape(2, NE, FC, P, DC, P)
    m['moe_dn_r'] = np.ascontiguousarray(dn.transpose(0, 1, 4, 3, 2, 5)).reshape(2, NE, DC, P, FC * P)
    m['moe_rwT'] = np.ascontiguousarray(inputs['moe_router_w'].reshape(2, DC, P, NE).transpose(2, 0, 1, 3))
    m['moe_rbB'] = np.ascontiguousarray(np.broadcast_to(inputs['moe_router_b'][None], (P, 2, NE)))
    m.update(attn_layout(inputs))
    m.update(ssm_layout(inputs))
    sel = np.zeros((NE, NE, P), dtype=f)
    for e in range(NE):
        sel[e, e, :] = 1.0
    m['sel8'] = sel
    return m


def kernel(**inputs):
    inputs = {k: np.asarray(v) for k, v in inputs.items()}
    b = Builder({'tail_split': True})
    nc = b.build()
    shared = shared_layout(inputs)
    in_maps = []
    for core in range(8):
        m = host_layout(inputs, core % 4)
        m['rankf'] = np.full((P, 1), float(core // 4), dtype=np.float32)
        m.update(shared)
        in_maps.append({k: m[k] for k in b.din})
    res = run_bass_kernel_spmd(nc, in_maps, core_ids=list(range(8)))
    out = np.stack([np.concatenate([res.results[i]['out'], res.results[i + 4]['out']], axis=0) for i in range(4)], axis=0)
    return out.astype(np.float32)
```

```python
import contextlib
from types import FunctionType
import numpy as np
import concourse.bass as bass
import concourse.mybir as mybir
from concourse.bass_utils import run_bass_kernel_spmd

F32 = mybir.dt.float32
BF16 = mybir.dt.bfloat16
AF = mybir.ActivationFunctionType
ALU = mybir.AluOpType
AX = mybir.AxisListType

S = 4096
D = 1024
DC = 8
P = 128
DFF = 2816
FC = 22
NE = 8
EPS = 1e-6
NBLK = S // 512

ENG = ['pe', 'act', 'dve', 'pool', 'sp']
SELF_SYNC = True


class Sched:
    def __init__(self, nc, stack):
        self.nc = nc
        self.stack = stack
        self.streams = {e: [] for e in ENG}
        self.sem = {e: stack.enter_context(nc.semaphore('s_' + e)) for e in ENG}
        self.count = {e: 0 for e in ENG}
        self.dsem = {}
        self.seen = {e: {} for e in ENG}
        self.hist = {}
        self.buf = {}
        self.ninst = 0

    def _semof(self, key):
        if isinstance(key, tuple):
            return self.dsem[key[1]][0]
        return self.sem[key]

    def _wait(self, eng, toks):
        best = {}
        for (k, v) in toks:
            if v > best.get(k, 0):
                best[k] = v
        seen = self.seen[eng]
        for k, v in best.items():
            if seen.get(k, 0) >= v:
                continue
            if k == eng and (eng == 'pe' or not SELF_SYNC):
                continue
            s = self._semof(k)
            self.streams[eng].append(lambda e, s=s, v=v: e.wait_ge(s, v))
            self.ninst += 1
            h = self.hist.get((k, v))
            if h:
                for kk, vv in h.items():
                    if vv > seen.get(kk, 0):
                        seen[kk] = vv
            if v > seen.get(k, 0):
                seen[k] = v

    def _deps(self, reads, writes):
        toks = []
        for b in reads:
            st = self.buf.get(b)
            if st and st[0]:
                toks.append(st[0])
        for b in writes:
            st = self.buf.get(b)
            if st:
                if st[0]:
                    toks.append(st[0])
                toks.extend(st[1].items())
        return toks

    def _record(self, tok, reads, writes):
        for b in reads:
            st = self.buf.setdefault(b, [None, {}])
            if tok[1] > st[1].get(tok[0], 0):
                st[1][tok[0]] = tok[1]
        for b in writes:
            self.buf[b] = [tok, {}]

    def op(self, eng, fns, reads=(), writes=()):
        if not isinstance(fns, (list, tuple)):
            fns = [fns]
        self._wait(eng, self._deps(reads, writes))
        self.count[eng] += 1
        tok = (eng, self.count[eng])
        sem = self.sem[eng]
        for f in fns[:-1]:
            self.streams[eng].append(f)
        last = fns[-1]
        self.streams[eng].append(lambda e, f=last, s=sem: f(e).then_inc(s, 1))
        self.ninst += len(fns)
        self.hist[tok] = dict(self.seen[eng])
        self._record(tok, reads, writes)
        return tok

    def dma(self, queue, chan, out, in_, reads=(), writes=(), **kw):
        if chan == 'const':
            self.nconst = getattr(self, 'nconst', 0) + 1
            chan = 'const%d' % self.nconst
        if chan not in self.dsem:
            self.dsem[chan] = [self.stack.enter_context(self.nc.semaphore('d_' + str(chan))), 0]
        self._wait(queue, self._deps(reads, writes))
        ds = self.dsem[chan]
        ds[1] += 16
        tok = (('dma', chan), ds[1])
        s = ds[0]
        self.streams[queue].append(lambda e, s=s, o=out, i=in_, kw=kw: e.dma_start(
            out=(o() if isinstance(o, FunctionType) else o), in_=(i() if isinstance(i, FunctionType) else i), **kw).then_inc(s, 16))
        self.ninst += 1
        self.hist[tok] = dict(self.seen[queue])
        self._record(tok, reads, writes)
        return tok

    def barrier(self):
        toks = []
        for b, st in self.buf.items():
            if st[0]:
                toks.append(st[0])
            toks.extend(st[1].items())
        for eng in ENG:
            self._wait(eng, toks)

    def finish(self, eng='sp'):
        toks = []
        for b, st in self.buf.items():
            if st[0]:
                toks.append(st[0])
            toks.extend(st[1].items())
        self._wait(eng, toks)

    def emit(self):
        nc = self.nc
        with nc.Block() as block:
            @block.tensor
            def _(e):
                for f in self.streams['pe']:
                    f(e)

            @block.scalar
            def _(e):
                for f in self.streams['act']:
                    f(e)

            @block.vector
            def _(e):
                for f in self.streams['dve']:
                    f(e)

            @block.gpsimd
            def _(e):
                for f in self.streams['pool']:
                    f(e)

            @block.sync
            def _(e):
                for f in self.streams['sp']:
                    f(e)


class Builder:
    def __init__(self, cfg):
        self.cfg = cfg
        self.nc = bass.Bass("TRN2", target_bir_lowering=False)
        self.stack = contextlib.ExitStack()
        self.sc = Sched(self.nc, self.stack)
        self.din = {}
        self.psn = 0
        self.uid = 0

    def inp(self, name, shape, dtype=F32):
        t = self.nc.dram_tensor(name, list(shape), dtype, kind="ExternalInput").ap()
        self.din[name] = t
        return t

    def sb(self, name, shape, dtype, st=None):
        self.uid += 1
        return (st or self.stack).enter_context(self.nc.sbuf_tensor('sb%d_%s' % (self.uid, name), list(shape), dtype))

    def bank(self):
        rot = getattr(self, 'rot', None) or list(range(8))
        i = rot[self.psn % len(rot)]
        self.psn += 1
        return ('ps', i), self.ps[i]

    def setup_common(self):
        nc = self.nc
        self.ps = [self.stack.enter_context(nc.psum_tensor('ps%d' % i, [P, 512], F32)) for i in range(8)]
        self.ident = self.sb('ident', [P, P], F32)
        self.ones = self.sb('ones', [P, P], F32)
        d_ident = self.inp('ident', [P, P])
        self.sc.dma('sp', 'const', self.ident[:], d_ident[:, :], writes=['ident'])
        self.sc.op('dve', lambda e: e.memset(self.ones[:], 1.0), writes=['ones'])

    def build_mod(self):
        sc = self.sc
        cT = self.inp('cT', [P, DC])
        ada_w = self.inp('ada_w', [4, D, 6 * D])
        ada_bT = self.inp('ada_bT', [P, 4, 48])
        norm_gT = self.inp('norm_gT', [P, 4, 2, DC])
        self.cond = self.sb('cond', [P, DC], F32)
        self.modT = self.sb('modT', [P, 4, 48], F32)
        self.adab = self.sb('adab', [P, 4, 48], F32)
        self.ng = self.sb('ng', [P, 4, 2, DC], F32)
        self.gs = self.sb('gs', [P, 4, 2, DC], F32)
        craw = self.sb('craw', [P, DC], F32)
        sig = self.sb('csig', [P, DC], F32)
        sc.dma('sp', 'const', craw[:], cT[:, :], writes=['craw'])
        sc.dma('sp', 'const', self.adab[:], ada_bT[:, :, :], writes=['adab'])
        sc.dma('sp', 'const', self.ng[:], norm_gT[:, :, :, :], writes=['ng'])
        sc.op('act', lambda e: e.activation(out=sig[:], in_=craw[:], func=AF.Sigmoid), reads=['craw'], writes=['csig'])
        sc.op('dve', lambda e: e.tensor_tensor(out=self.cond[:], in0=craw[:], in1=sig[:], op=ALU.mult),
              reads=['craw', 'csig'], writes=['cond'])
        st = contextlib.ExitStack()
        wsl = [self.sb('adaw%d' % i, [P, DC, 1024], F32, st) for i in range(2)]
        n = 0
        for i in range(4):
            pname, pt = self.bank()
            for k in range(6):
                slot = n % 2
                n += 1
                for c in range(DC):
                    sc.dma('sp', 'adaw%d' % slot, wsl[slot][:, c, :],
                           ada_w[i, c * P:(c + 1) * P, k * 1024:(k + 1) * 1024], writes=['adaw%d' % slot])
                fns = []
                for nn in range(8):
                    for c in range(DC):
                        fns.append(lambda e, pt=pt, slot=slot, nn=nn, c=c, k=k:
                                   e.matmul(pt[:, k * 8 + nn:k * 8 + nn + 1], lhsT=wsl[slot][:, c, nn * P:(nn + 1) * P],
                                            rhs=self.cond[:, c:c + 1], start=(c == 0), stop=(c == DC - 1)))
                sc.op('pe', fns, reads=['adaw%d' % slot, 'cond'], writes=[pname])
            sc.op('dve', lambda e, pt=pt, i=i: e.tensor_tensor(out=self.modT[:, i, :], in0=pt[:, 0:48], in1=self.adab[:, i, :], op=ALU.add),
                  reads=[pname, 'adab'], writes=[('modT', i)])
            for j, k in ((0, 1), (1, 4)):
                sc.op('dve', lambda e, i=i, j=j, k=k: e.scalar_tensor_tensor(
                    out=self.gs[:, i, j, :], in0=self.modT[:, i, k * 8:(k + 1) * 8], scalar=1.0, in1=self.ng[:, i, j, :],
                    op0=ALU.add, op1=ALU.mult), reads=[('modT', i), 'ng'], writes=[('modT', i)])
        sc.barrier()
        st.close()

    def modv(self, i, k, c):
        return self.modT[:, i, k * 8 + c:k * 8 + c + 1]

    def alloc_stream(self):
        self.xT_d = self.nc.dram_tensor('xT_scratch', [D, S], F32).ap()
        self.xh_d = self.nc.dram_tensor('xh_scratch', [D, S // 2], F32).ap()
        self.rd_mode = 'full'
        self.wr_mode = 'full'
        self.ntok = S
        self.sc.streams['sp'].append(lambda e: setattr(self, 'rv', e.partition_id() // 4))
        d_rankf = self.inp('rankf', [P, 1])
        self.rankf = self.sb('rankf', [P, 1], F32)
        self.sc.dma('sp', 'const', self.rankf[:], d_rankf[:, :], writes=['rankf'])
        self.xin = self.inp('x', [S, D])
        self.xblk = [self.sb('xblk%d' % i, [P, DC, 512], F32) for i in range(2)]
        self.sq = self.sb('sq', [P, 512], F32)
        self.rstd = self.sb('rstd', [P, 512], F32)
        self.tmp32 = [self.sb('tmp32_%d' % i, [P, 512], F32) for i in range(3)]
        self.xn = 0

    def xd(self, c, blk, write):
        mode = self.wr_mode if write else self.rd_mode
        if mode == 'full':
            return self.xT_d[c * P:(c + 1) * P, blk * 512:(blk + 1) * 512], ('xTd', c, blk)
        if mode == 'half':
            return self.xh_d[c * P:(c + 1) * P, blk * 512:(blk + 1) * 512], ('xh', c, blk)
        if mode == 'dynfull':
            return self.xs_d[c * P:(c + 1) * P, (blk + 1) * 512:(blk + 2) * 512], 'xs'
        raise ValueError(mode)

    def load_xT(self, blk, from_input):
        sc = self.sc
        slot = self.xn % 2
        self.xn += 1
        name = 'xblk%d' % slot
        t = self.xblk[slot]
        if from_input:
            for j in range(4):
                sc.dma('sp', 'xtok', self.xtok[:, j, :], self.xin[blk * 512 + j * P: blk * 512 + (j + 1) * P, :],
                       writes=[('xtok', j)])
            for c in range(DC):
                pname, pt = self.bank()
                fns = [lambda e, pt=pt, j=j, c=c: e.transpose(out=pt[:, j * P:(j + 1) * P], in_=self.xtok[:, j, c * P:(c + 1) * P],
                                                              identity=self.ident[:]) for j in range(4)]
                sc.op('pe', fns, reads=[('xtok', j) for j in range(4)] + ['ident'], writes=[pname])
                eng = 'act' if c % 2 == 0 else 'dve'
                if eng == 'act':
                    sc.op('act', lambda e, pt=pt, t=t, c=c: e.copy(out=t[:, c, :], in_=pt[:]), reads=[pname], writes=[(name, c)])
                else:
                    sc.op('dve', lambda e, pt=pt, t=t, c=c: e.tensor_copy(out=t[:, c, :], in_=pt[:]), reads=[pname], writes=[(name, c)])
        else:
            for c in range(DC):
                ap, tn = self.xd(c, blk, False)
                sc.dma('sp', name, t[:, c, :], ap, reads=[tn], writes=[(name, c)])
        return name, t

    def store_xT(self, blk, name, t, c):
        ap, tn = self.xd(c, blk, True)
        self.sc.dma('sp', 'st_' + name, ap, t[:, c, :], reads=[(name, c)], writes=[tn])

    def norm_mod(self, li, j, name, t, hT, hname, hoff, h32=None):
        sc = self.sc
        pname, pt = self.bank()
        for c in range(DC):
            sc.op('act', lambda e, c=c: e.activation(out=self.sq[:], in_=t[:, c, :], func=AF.Square), reads=[(name, c)], writes=['sq'])
            sc.op('pe', lambda e, c=c, pt=pt: e.matmul(pt[:], lhsT=self.ones[:], rhs=self.sq[:], start=(c == 0), stop=(c == DC - 1)),
                  reads=['sq', 'ones'], writes=[pname])
        sc.op('act', lambda e, pt=pt: e.activation(out=self.rstd[:], in_=pt[:], func=AF.Sqrt, scale=1.0 / D, bias=self.epsb[:]),
              reads=[pname, 'epsb'], writes=['rstd'])
        sc.op('dve', lambda e: e.reciprocal(out=self.rstd[:], in_=self.rstd[:]), reads=['rstd'], writes=['rstd'])
        kshift = 0 if j == 0 else 3
        for c in range(DC):
            tm = self.tmp32[c % 2]
            tn = 'tmp32_%d' % (c % 2)
            sc.op('dve', lambda e, c=c, tm=tm: e.tensor_tensor(out=tm[:], in0=t[:, c, :], in1=self.rstd[:], op=ALU.mult),
                  reads=[(name, c), 'rstd'], writes=[tn])
            if h32 is not None:
                sc.op('pool', lambda e, c=c, tm=tm: e.tensor_scalar(out=h32[:, c, :], in0=tm[:], scalar1=self.gs[:, li, j, c:c + 1],
                                                                   scalar2=self.modv(li, kshift, c), op0=ALU.mult, op1=ALU.add),
                      reads=[tn, ('modT', li)], writes=[('h32', c)])
            sc.op('dve', lambda e, c=c, tm=tm: e.tensor_scalar(out=hT[:, c, hoff:hoff + 512], in0=tm[:], scalar1=self.gs[:, li, j, c:c + 1],
                                                              scalar2=self.modv(li, kshift, c), op0=ALU.mult, op1=ALU.add),
                  reads=[tn, ('modT', li)], writes=[(hname, c, hoff)])

    def load_w_cast(self, chan, dst, src, rows_split, writes):
        n = dst.shape[-1]
        step = 2048
        for a in range(0, n, step):
            b = min(n, a + step)
            self.sc.dma('pool', chan, dst[:, a:b], src[:, a:b], writes=writes)

    def conv_setup(self):
        self.d_conv_w_in = self.inp('conv_w_in', [2, D, 3 * D])
        self.d_conv_w_out = self.inp('conv_w_out', [2, D, D])
        d_cw = self.inp('conv_wT', [P, 2, 3, DC])
        self.cwT = self.sb('cwT', [P, 2, 3, DC], F32)
        self.sc.dma('sp', 'const', self.cwT[:], d_cw[:, :, :, :], writes=['cwT'])

    def conv_mixer(self, li, j, first):
        sc = self.sc
        st = contextlib.ExitStack()
        nc = self.nc
        win = self.sb('cw_in', [P, DC, 3 * D], BF16, st)
        wout = self.sb('cw_out', [P, DC, D], BF16, st)
        hT = self.sb('hTc', [P, DC, 512], BF16, st)
        bT = self.sb('bTc', [P, DC, 512], F32, st)
        uT = self.sb('uTc', [P, DC, 514], F32, st)
        zT = self.sb('zTc', [P, DC, 512], BF16, st)
        if first:
            self.xtok = self.sb('xtok', [P, 4, D], F32, st)
        L = 'L%d' % li
        for c in range(DC):
            self.load_w_cast('cwin', win[:, c, :], self.d_conv_w_in[j, c * P:(c + 1) * P, :], None, writes=[(L, 'cwin')])
            self.load_w_cast('cwout', wout[:, c, :], self.d_conv_w_out[j, c * P:(c + 1) * P, :], None, writes=[(L, 'cwout')])
        sc.op('pool', lambda e: e.memset(uT[:, :, 0:2], 0.0), writes=[(L, 'uT', c) for c in range(DC)])
        split = (self.rd_mode == 'dynfull')
        for blk in ([-1] if split else []) + list(range(self.ntok // 512)):
            name, t = self.load_xT(blk, first)
            self.norm_mod(li, 0, name, t, hT, (L, 'hT'), 0)
            hreads = [((L, 'hT'), c, 0) for c in range(DC)] + [(L, 'cwin')]
            for c in range(DC):
                pts = []
                for kind in ((1, 2) if blk < 0 else (0, 1, 2)):
                    pname, pt = self.bank()
                    fns = [lambda e, pt=pt, k=k, kind=kind, c=c: e.matmul(
                        pt[:], lhsT=win[:, k, kind * D + c * P: kind * D + (c + 1) * P], rhs=hT[:, k, :],
                        start=(k == 0), stop=(k == DC - 1)) for k in range(DC)]
                    sc.op('pe', fns, reads=hreads, writes=[pname])
                    pts.append((pname, pt))
                if blk < 0:
                    (pcn, pc), (pvn, pv) = pts
                else:
                    (pbn, pb), (pcn, pc), (pvn, pv) = pts
                    sc.op('act', lambda e, pb=pb, c=c: e.activation(out=bT[:, c, :], in_=pb[:], func=AF.Copy), reads=[pbn], writes=[(L, 'bT', c)])
                tm = self.tmp32[2]
                sc.op('act', lambda e, pc=pc, tm=tm: e.activation(out=tm[:], in_=pc[:], func=AF.Copy), reads=[pcn], writes=['tmp32_2'])
                sc.op('dve', lambda e, pv=pv, tm=tm, c=c: e.tensor_tensor(out=uT[:, c, 2:514], in0=pv[:], in1=tm[:], op=ALU.mult),
                      reads=[pvn, 'tmp32_2'], writes=[(L, 'uT', c)])
            if blk < 0:
                for c in range(DC):
                    sc.op('dve', lambda e, c=c: e.tensor_scalar(out=uT[:, c, 0:2], in0=uT[:, c, 512:514], scalar1=self.rankf[:, 0:1], scalar2=None, op0=ALU.mult),
                          reads=[(L, 'uT', c), 'rankf'], writes=[(L, 'uT', c)])
                continue
            for c in range(DC):
                tm = self.tmp32[c % 2]
                tn = 'tmp32_%d' % (c % 2)
                sc.op('dve', lambda e, c=c, tm=tm: e.tensor_scalar(out=tm[:], in0=uT[:, c, 2:514], scalar1=self.cwT[:, j, 2, c:c + 1],
                                                                  scalar2=None, op0=ALU.mult), reads=[(L, 'uT', c), 'cwT'], writes=[tn])
                sc.op('dve', lambda e, c=c, tm=tm: e.scalar_tensor_tensor(out=tm[:], in0=uT[:, c, 1:513], scalar=self.cwT[:, j, 1, c:c + 1],
                                                                         in1=tm[:], op0=ALU.mult, op1=ALU.add),
                      reads=[(L, 'uT', c), tn], writes=[tn])
                sc.op('dve', lambda e, c=c, tm=tm: e.scalar_tensor_tensor(out=tm[:], in0=uT[:, c, 0:512], scalar=self.cwT[:, j, 0, c:c + 1],
                                                                         in1=tm[:], op0=ALU.mult, op1=ALU.add),
                      reads=[(L, 'uT', c), tn], writes=[tn])
                sc.op('dve', lambda e, c=c, tm=tm: e.tensor_tensor(out=zT[:, c, :], in0=tm[:], in1=bT[:, c, :], op=ALU.mult),
                      reads=[tn, (L, 'bT', c)], writes=[(L, 'zT', c)])
                sc.op('pool', lambda e, c=c: e.tensor_copy(out=uT[:, c, 0:2], in_=uT[:, c, 512:514]),
                      reads=[(L, 'uT', c)], writes=[(L, 'uT', c)])
            zreads = [(L, 'zT', c) for c in range(DC)] + [(L, 'cwout')]
            for c in range(DC):
                pname, pt = self.bank()
                fns = [lambda e, pt=pt, k=k, c=c: e.matmul(pt[:], lhsT=wout[:, k, c * P:(c + 1) * P], rhs=zT[:, k, :],
                                                           start=(k == 0), stop=(k == DC - 1)) for k in range(DC)]
                sc.op('pe', fns, reads=zreads, writes=[pname])
                sc.op('dve', lambda e, pt=pt, c=c, t=t: e.scalar_tensor_tensor(out=t[:, c, :], in0=pt[:], scalar=self.modv(li, 2, c),
                                                                              in1=t[:, c, :], op0=ALU.mult, op1=ALU.add),
                      reads=[pname, (name, c), ('modT', li)], writes=[(name, c)])
                self.store_xT(blk, name, t, c)
        sc.barrier()
        st.close()

    def ffn_setup(self):
        self.d_ffn_gu = self.inp('ffn_gu_r', [2, FC, P, DC * 256])
        self.d_ffn_dn = self.inp('ffn_dn_r', [2, DC, P, FC * P])
        self.d_moe_gu = self.inp('moe_gu_r', [2, NE, FC, P, DC * 256])
        self.d_moe_dn = self.inp('moe_dn_r', [2, NE, DC, P, FC * P])
        d_rw = self.inp('moe_rwT', [P, 2, DC, NE])
        d_rb = self.inp('moe_rbB', [P, 2, NE])
        d_sel = self.inp('sel8', [NE, NE, P])
        self.rw = self.sb('rw', [P, 2, DC, NE], F32)
        self.rb = self.sb('rb', [P, 2, NE], F32)
        self.sel = self.sb('sel', [NE, NE, P], F32)
        self.sc.dma('sp', 'const', self.rw[:], d_rw[:, :, :, :], writes=['rw'])
        self.sc.dma('sp', 'const', self.rb[:], d_rb[:, :, :], writes=['rb'])
        self.sc.dma('sp', 'const', self.sel[:], d_sel[:, :, :], writes=['sel'])

    def router(self, li, L, h32, half, gT, sm):
        sc = self.sc
        jl = li // 2
        lg, ex, gt, m8, sc1 = sm
        prn, pr = self.bank()
        h32r = [('h32', c) for c in range(DC)]
        for tt in range(4):
            fns = [lambda e, pr=pr, tt=tt, c=c: e.matmul(pr[:, tt * 8:(tt + 1) * 8], lhsT=h32[:, c, tt * P:(tt + 1) * P],
                                                        rhs=self.rw[:, jl, c, :], start=(c == 0), stop=(c == DC - 1))
                   for c in range(DC)]
            sc.op('pe', fns, reads=h32r + ['rw'], writes=[prn])
        ptn, ptT = self.bank()
        for tt in range(4):
            R = [(L, 'rt')]
            sc.op('dve', lambda e, tt=tt, pr=pr: e.tensor_tensor(out=lg[:, 0:8], in0=pr[:, tt * 8:(tt + 1) * 8], in1=self.rb[:, jl, :], op=ALU.add),
                  reads=[prn, 'rb'], writes=R)
            sc.op('dve', lambda e: e.tensor_reduce(out=sc1[:, 0:1], in_=lg[:, 0:8], axis=AX.X, op=ALU.max), reads=R, writes=R)
            sc.op('dve', lambda e: e.tensor_scalar(out=sc1[:, 0:1], in0=sc1[:, 0:1], scalar1=-1.0, scalar2=None, op0=ALU.mult), reads=R, writes=R)
            sc.op('act', lambda e: e.activation(out=ex[:, 0:8], in_=lg[:, 0:8], func=AF.Exp, bias=sc1[:, 0:1], scale=1.0), reads=R, writes=R)
            sc.op('dve', lambda e: e.max(out=m8[:, 0:8], in_=ex[:, 0:8]), reads=R, writes=R)
            sc.op('dve', lambda e: e.tensor_tensor(out=sc1[:, 1:2], in0=m8[:, 0:1], in1=m8[:, 1:2], op=ALU.add), reads=R, writes=R)
            sc.op('dve', lambda e: e.reciprocal(out=sc1[:, 1:2], in_=sc1[:, 1:2]), reads=R, writes=R)
            sc.op('dve', lambda e: e.tensor_scalar(out=gt[:, 0:8], in0=ex[:, 0:8], scalar1=m8[:, 1:2], scalar2=None, op0=ALU.is_ge), reads=R, writes=R)
            sc.op('dve', lambda e: e.tensor_tensor(out=gt[:, 0:8], in0=gt[:, 0:8], in1=ex[:, 0:8], op=ALU.mult), reads=R, writes=R)
            sc.op('dve', lambda e: e.tensor_scalar(out=gt[:, 0:8], in0=gt[:, 0:8], scalar1=sc1[:, 1:2], scalar2=None, op0=ALU.mult), reads=R, writes=R)
            sc.op('pe', lambda e, tt=tt, ptT=ptT: e.transpose(out=ptT[0:8, tt * P:(tt + 1) * P], in_=gt[:, 0:8], identity=self.ident[:]),
                  reads=R + ['ident'], writes=[ptn])
        sc.op('act', lambda e, ptT=ptT, half=half: e.activation(out=gT[0:8, half * 512:(half + 1) * 512], in_=ptT[0:8, :], func=AF.Copy),
              reads=[ptn], writes=[(L, 'gT', half)])

    def ffn(self, li, moe):
        sc = self.sc
        st = contextlib.ExitStack()
        L = 'F%d' % li
        TB = 1024
        hT = self.sb('hTf', [P, DC, TB], BF16, st)
        actT = self.sb('actT', [P, FC, TB], BF16, st)
        wgu = [self.sb('wgu%d' % i, [P, DC * 256], BF16, st) for i in range(3)]
        wd = [self.sb('wd%d' % i, [P, FC * P], BF16, st) for i in range(3)]
        xs = [self.sb('xs%d' % i, [P, 512], F32, st) for i in range(2)]
        if moe:
            h32 = self.sb('h32', [P, DC, 512], F32, st)
            yacc = self.sb('yacc', [P, DC, TB], F32, st)
            Gb = [self.sb('Gb%d' % i, [P, TB], F32, st) for i in range(2)]
            gT = self.sb('gT', [NE, TB], F32, st)
            sm = [self.sb('rsm%d' % i, [P, 8], F32, st) for i in range(5)]
        jl = li // 2
        nw = 0
        nd = 0
        nx = 0
        ng = 0
        for sbi in range(self.ntok // TB):
            for half in range(2):
                blk = sbi * 2 + half
                name, t = self.load_xT(blk, False)
                self.norm_mod(li, 1, name, t, hT, (L, 'hT'), half * 512, h32=(h32 if moe else None))
                if moe:
                    self.router(li, L, h32, half, gT, sm)
            for ex in (range(NE) if moe else [None]):
                if moe:
                    gsl = ng % 2
                    ng += 1
                    gbn = (L, 'Gb', gsl)
                    for half in range(2):
                        pn, pt = self.bank()
                        sc.op('pe', lambda e, pt=pt, ex=ex, half=half: e.matmul(pt[:], lhsT=self.sel[0:8, ex, :], rhs=gT[0:8, half * 512:(half + 1) * 512],
                                                                               start=True, stop=True),
                              reads=['sel', (L, 'gT', half)], writes=[pn])
                        sc.op('act', lambda e, pt=pt, gsl=gsl, half=half: e.activation(out=Gb[gsl][:, half * 512:(half + 1) * 512], in_=pt[:], func=AF.Copy),
                              reads=[pn], writes=[(gbn, half)])
                for f in range(FC):
                    slot = nw % 3
                    nw += 1
                    wn = (L, 'wgu', slot)
                    src = self.d_moe_gu[jl, ex, f, :, :] if moe else self.d_ffn_gu[jl, f, :, :]
                    self.load_w_cast('wgu%d' % slot, wgu[slot][:, :], src, None, writes=[wn])
                    for half in range(2):
                        hreads = [((L, 'hT'), c, half * 512) for c in range(DC)] + [wn]
                        pgn, pg = self.bank()
                        pun, pu = self.bank()
                        for (pn, pt, off) in ((pgn, pg, 0), (pun, pu, 128)):
                            fns = [lambda e, pt=pt, k=k, off=off, slot=slot, half=half: e.matmul(
                                pt[:], lhsT=wgu[slot][:, k * 256 + off: k * 256 + off + 128], rhs=hT[:, k, half * 512:(half + 1) * 512],
                                start=(k == 0), stop=(k == DC - 1)) for k in range(DC)]
                            sc.op('pe', fns, reads=hreads, writes=[pn])
                        tm = self.tmp32[2]
                        sc.op('act', lambda e, pg=pg, tm=tm: e.activation(out=tm[:], in_=pg[:], func=AF.Silu), reads=[pgn], writes=['tmp32_2'])
                        sc.op('dve', lambda e, pu=pu, tm=tm, f=f, half=half: e.tensor_tensor(
                            out=actT[:, f, half * 512:(half + 1) * 512], in0=pu[:], in1=tm[:], op=ALU.mult),
                            reads=[pun, 'tmp32_2'], writes=[(L, 'act', f, half)])
                for c in range(DC):
                    slot = nd % 3
                    nd += 1
                    wn = (L, 'wd', slot)
                    src = self.d_moe_dn[jl, ex, c, :, :] if moe else self.d_ffn_dn[jl, c, :, :]
                    self.load_w_cast('wd%d' % slot, wd[slot][:, :], src, None, writes=[wn])
                    for half in range(2):
                        blk = sbi * 2 + half
                        areads = [(L, 'act', f, half) for f in range(FC)] + [wn]
                        pn, pt = self.bank()
                        fns = [lambda e, pt=pt, f=f, slot=slot, half=half: e.matmul(
                            pt[:], lhsT=wd[slot][:, f * P:(f + 1) * P], rhs=actT[:, f, half * 512:(half + 1) * 512],
                            start=(f == 0), stop=(f == FC - 1)) for f in range(FC)]
                        sc.op('pe', fns, reads=areads, writes=[pn])
                        if not moe:
                            self.resid(li, 5, xs, nx, c, blk, pn, pt, None, None)
                            nx += 1
                        else:
                            yn = (L, 'yacc', c, half)
                            ysl = yacc[:, c, half * 512:(half + 1) * 512]
                            gsl_ap = Gb[gsl][:, half * 512:(half + 1) * 512]
                            if ex == 0:
                                sc.op('dve', lambda e, pt=pt, ysl=ysl, g=gsl_ap: e.tensor_tensor(out=ysl, in0=pt[:], in1=g, op=ALU.mult),
                                      reads=[pn, (gbn, half)], writes=[yn])
                            else:
                                tm = self.tmp32[c % 2]
                                tn = 'tmp32_%d' % (c % 2)
                                sc.op('dve', lambda e, pt=pt, tm=tm, g=gsl_ap: e.tensor_tensor(out=tm[:], in0=pt[:], in1=g, op=ALU.mult),
                                      reads=[pn, (gbn, half)], writes=[tn])
                                sc.op('pool', lambda e, tm=tm, ysl=ysl: e.tensor_tensor(out=ysl, in0=ysl, in1=tm[:], op=ALU.add),
                                      reads=[tn, yn], writes=[yn])
            if moe:
                for c in range(DC):
                    for half in range(2):
                        blk = sbi * 2 + half
                        self.resid(li, 5, xs, nx, c, blk, (L, 'yacc', c, half), None, yacc[:, c, half * 512:(half + 1) * 512], None)
                        nx += 1
        sc.barrier()
        st.close()

    def resid(self, li, k, xs, nx, c, blk, srcname, pt, src_ap, _):
        sc = self.sc
        xsl = nx % 2
        xn = 'xs%d' % xsl
        xt = xs[xsl]
        src = pt[:] if pt is not None else src_ap
        rap, rtn = self.xd(c, blk, False)
        wap, wtn = self.xd(c, blk, True)
        sc.dma('sp', xn, xt[:], rap, reads=[rtn], writes=[xn])
        sc.op('dve', lambda e, src=src, xt=xt, c=c: e.scalar_tensor_tensor(
            out=xt[:], in0=src, scalar=self.modv(li, k, c), in1=xt[:], op0=ALU.mult, op1=ALU.add),
            reads=[srcname, xn, ('modT', li)], writes=[xn])
        sc.dma('sp', 'st_' + xn, wap, xt[:], reads=[xn], writes=[wtn])

    def attn_setup(self):
        self.d_awq = self.inp('attn_wq_perm', [D, 1024])
        self.d_awk = self.inp('attn_wk', [D, 256])
        self.d_awv = self.inp('attn_wv', [D, 256])
        self.d_awqi = self.inp('attn_wqi', [D, 512])
        self.d_awki2 = self.inp('attn_wki2', [D, 128])
        self.d_awwi = self.inp('attn_wwi', [D, 8])
        self.d_awout = self.inp('attn_wout_r', [64, 16, D])
        self.d_gainT = self.inp('attn_gainT', [P, 2])
        self.d_biasT = self.inp('attn_biasT', [P, 32, P])
        self.d_b31 = self.inp('attn_b31B', [P, 16])
        self.d_cmask = self.inp('cmask', [P, P])
        self.d_bones = self.inp('blockones', [P, P])
        self.d_selrow = self.inp('selrow', [65, 64])

    def head_norm(self, pn, pt, gain_ap, out_ap, tagw):
        sc = self.sc
        qs = self.tmp32[2]
        sc.op('act', lambda e, pt=pt, qs=qs: e.activation(out=qs[:], in_=pt[:], func=AF.Copy), reads=[pn], writes=['tmp32_2'])
        sc.op('act', lambda e, qs=qs: e.activation(out=self.sq[:], in_=qs[:], func=AF.Square), reads=['tmp32_2'], writes=['sq'])
        p2n, p2 = self.bank()
        sc.op('pe', lambda e, p2=p2: e.matmul(p2[:], lhsT=self.bones[:], rhs=self.sq[:], start=True, stop=True), reads=['sq', 'bones'], writes=[p2n])
        sc.op('act', lambda e, p2=p2: e.activation(out=self.rstd[:], in_=p2[:], func=AF.Sqrt, scale=1.0 / 64, bias=self.epsb[:]),
              reads=[p2n, 'epsb'], writes=['rstd'])
        sc.op('dve', lambda e: e.reciprocal(out=self.rstd[:], in_=self.rstd[:]), reads=['rstd'], writes=['rstd'])
        sc.op('dve', lambda e, qs=qs, g=gain_ap, o=out_ap: e.scalar_tensor_tensor(out=o, in0=qs[:], scalar=g, in1=self.rstd[:], op0=ALU.mult, op1=ALU.mult),
              reads=['tmp32_2', 'rstd', 'again'], writes=tagw)

    def attn_mixer(self, li):
        sc = self.sc
        st = contextlib.ExitStack()
        L = 'A'
        NI = 24
        KT = self.sb('KT', [P, 2, S], BF16, st)
        VA = self.sb('VA', [P, 32, 4, 65], BF16, st)
        KI = self.sb('KI', [P, S], BF16, st)
        hT = self.sb('hTa', [P, DC, 512], BF16, st)
        wA = self.sb('wA', [P, DC * 1024], BF16, st)
        QT = self.sb('QT', [P, 8, 512], BF16, st)
        QI = self.sb('QI', [P, 4, 512], BF16, st)
        WI = self.sb('WI', [P, 4, 8], F32, st)
        score = self.sb('score', [P, S], F32, st)
        nmq = self.sb('nmq', [P, S], BF16, st)
        nmT = [self.sb('nmT%d' % i, [P, 32, P], BF16, st) for i in range(2)]
        pexp = [self.sb('pexp%d' % i, [P, 512], BF16, st) for i in range(3)]
        OTn = self.sb('OTn', [64, 16, 512], BF16, st)
        osb = self.sb('osb', [65, 512], F32, st)
        rbc = self.sb('rbc', [64, 512], F32, st)
        Rt = self.tmp32[0:2]
        biasS = self.sb('biasS', [P, 32, P], BF16, st)
        self.bones = self.sb('bones', [P, P], F32, st)
        identb = self.sb('identb', [P, P], BF16, st)
        cmask = self.sb('cmaskS', [P, P], F32, st)
        selrow = self.sb('selrowS', [65, 64], F32, st)
        again = self.sb('again', [P, 2], F32, st)
        b31 = self.sb('b31', [P, 16], F32, st)
        bs = [self.sb('bsm%d' % i, [P, 1], F32, st) for i in range(6)]
        sc.dma('sp', 'const', self.bones[:], self.d_bones[:, :], writes=['bones'])
        sc.dma('sp', 'const', cmask[:], self.d_cmask[:, :], writes=['cmask'])
        sc.dma('sp', 'const', selrow[:], self.d_selrow[:, :], writes=['selrow'])
        sc.dma('sp', 'const', again[:], self.d_gainT[:, :], writes=['again'])
        sc.dma('sp', 'const', b31[:], self.d_b31[:, :], writes=['b31'])
        sc.op('dve', lambda e: e.tensor_copy(out=identb[:], in_=self.ident[:]), reads=['ident'], writes=['identb'])
        sc.op('dve', lambda e: e.tensor_scalar(out=again[:, 0:1], in0=again[:, 0:1], scalar1=0.125, scalar2=None, op0=ALU.mult),
              reads=['again'], writes=['again'])
        sc.op('pool', lambda e: e.memset(VA[:, :, :, 64:65], 1.0), writes=[(L, 'VAones')])
        for half in range(4):
            sc.dma('sp', 'bstage', score[:, 0:1024].rearrange('p (a b) -> p a b', b=P), self.d_biasT[:, half * 8:(half + 1) * 8, :],
                   writes=[(L, 'bstage')])
            for i in range(8):
                kh = half * 8 + i
                h = kh % 16
                sc.op('dve', lambda e, i=i, kh=kh, h=h: e.tensor_scalar(out=biasS[:, kh, :], in0=score[:, i * P:(i + 1) * P], scalar1=b31[:, h:h + 1],
                                                                      scalar2=None, op0=ALU.subtract), reads=[(L, 'bstage'), 'b31'], writes=[(L, 'biasS')])
        wk = wA[:, 0:DC * 256].rearrange('p (c n) -> p c n', c=DC)
        wv = wA[:, DC * 256:DC * 512].rearrange('p (c n) -> p c n', c=DC)
        wki = wA[:, DC * 512:DC * 640].rearrange('p (c n) -> p c n', c=DC)
        for c in range(DC):
            sc.dma('pool', 'wA', wk[:, c, :], self.d_awk[c * P:(c + 1) * P, :], writes=[(L, 'wA')])
            sc.dma('pool', 'wA', wv[:, c, :], self.d_awv[c * P:(c + 1) * P, :], writes=[(L, 'wA')])
            sc.dma('pool', 'wA', wki[:, c, :], self.d_awki2[c * P:(c + 1) * P, :], writes=[(L, 'wA')])
        for blk in range(NBLK):
            name, t = self.load_xT(blk, False)
            self.norm_mod(li, 0, name, t, hT, (L, 'hT'), 0)
            hreads = [((L, 'hT'), c, 0) for c in range(DC)] + [(L, 'wA')]
            for m in range(2):
                pn, pt = self.bank()
                fns = [lambda e, pt=pt, k=k, m=m: e.matmul(pt[:], lhsT=wk[:, k, m * P:(m + 1) * P], rhs=hT[:, k, :], start=(k == 0), stop=(k == DC - 1))
                       for k in range(DC)]
                sc.op('pe', fns, reads=hreads, writes=[pn])
                self.head_norm(pn, pt, again[:, 1:2], KT[:, m, blk * 512:(blk + 1) * 512], [(L, 'KT', m, blk)])
            pn, pt = self.bank()
            fns = [lambda e, pt=pt, k=k: e.matmul(pt[:], lhsT=wki[:, k, :], rhs=hT[:, k, :], start=(k == 0), stop=(k == DC - 1)) for k in range(DC)]
            sc.op('pe', fns, reads=hreads, writes=[pn])
            sc.op('act', lambda e, pt=pt, blk=blk: e.activation(out=KI[:, blk * 512:(blk + 1) * 512], in_=pt[:], func=AF.Copy), reads=[pn], writes=[(L, 'KI', blk)])
            for tt in range(4):
                pn, pt = self.bank()
                fns = [lambda e, pt=pt, k=k, tt=tt: e.matmul(pt[:, 0:256], lhsT=hT[:, k, tt * P:(tt + 1) * P], rhs=wv[:, k, :], start=(k == 0), stop=(k == DC - 1))
                       for k in range(DC)]
                sc.op('pe', fns, reads=hreads, writes=[pn])
                sc.op('dve', lambda e, pt=pt, blk=blk, tt=tt: e.tensor_copy(out=VA[:, blk * 4 + tt, :, 0:64], in_=pt[:, 0:256].rearrange('p (a b) -> p a b', b=64)),
                      reads=[pn], writes=[(L, 'VA', blk * 4 + tt)])
        if self.cfg.get('astage', 9) < 1:
            sc.barrier(); st.close(); return
        wq = wA[:, :].rearrange('p (c n) -> p c n', c=DC)
        wqi = wA[:, 0:DC * 512].rearrange('p (c n) -> p c n', c=DC)
        wwi = wA[:, DC * 512:DC * 520].rearrange('p (c n) -> p c n', c=DC)
        wout = wA[0:64, 0:16 * 512].rearrange('p (h n) -> p h n', h=16)
        self.abank = [6, 7]
        self.nab = 0
        self.rot = [0, 1, 2, 3, 4, 5]
        nmn = 0
        npx = 0
        nrt = 0
        for blk in range(self.cfg.get('anblk', NBLK)):
            for c in range(DC):
                sc.dma('pool', 'wA', wq[:, c, :], self.d_awq[c * P:(c + 1) * P, :], writes=[(L, 'wA')])
            name, t = self.load_xT(blk, False)
            self.norm_mod(li, 0, name, t, hT, (L, 'hT'), 0)
            hreads = [((L, 'hT'), c, 0) for c in range(DC)]
            for m in range(8):
                pn, pt = self.bank()
                fns = [lambda e, pt=pt, k=k, m=m: e.matmul(pt[:], lhsT=wq[:, k, m * P:(m + 1) * P], rhs=hT[:, k, :], start=(k == 0), stop=(k == DC - 1))
                       for k in range(DC)]
                sc.op('pe', fns, reads=hreads + [(L, 'wA')], writes=[pn])
                self.head_norm(pn, pt, again[:, 0:1], QT[:, m, :], [(L, 'QT', m)])
            for c in range(DC):
                sc.dma('pool', 'wA', wqi[:, c, :], self.d_awqi[c * P:(c + 1) * P, :], writes=[(L, 'wA')])
                sc.dma('pool', 'wA', wwi[:, c, :], self.d_awwi[c * P:(c + 1) * P, :], writes=[(L, 'wA')])
            for m in range(4):
                pn, pt = self.bank()
                fns = [lambda e, pt=pt, k=k, m=m: e.matmul(pt[:], lhsT=wqi[:, k, m * P:(m + 1) * P], rhs=hT[:, k, :], start=(k == 0), stop=(k == DC - 1))
                       for k in range(DC)]
                sc.op('pe', fns, reads=hreads + [(L, 'wA')], writes=[pn])
                sc.op('act', lambda e, pt=pt, m=m: e.activation(out=QI[:, m, :], in_=pt[:], func=AF.Copy), reads=[pn], writes=[(L, 'QI', m)])
            pn, pt = self.bank()
            for qb in range(4):
                fns = [lambda e, pt=pt, k=k, qb=qb: e.matmul(pt[:, qb * 8:(qb + 1) * 8], lhsT=hT[:, k, qb * P:(qb + 1) * P], rhs=wwi[:, k, :],
                                                            start=(k == 0), stop=(k == DC - 1)) for k in range(DC)]
                sc.op('pe', fns, reads=hreads + [(L, 'wA')], writes=[pn])
            sc.op('dve', lambda e, pt=pt: e.tensor_scalar(out=WI[:, :, :], in0=pt[:, 0:32].rearrange('p (a b) -> p a b', b=8), scalar1=0.04419417382415922,
                                                        scalar2=None, op0=ALU.mult), reads=[pn], writes=[(L, 'WI')])
            def idx_bis(qb):
                gq = blk * 4 + qb
                nk = gq + 1
                W = nk * P
                nonlocal nrt
                for k0 in range(0, W, 512):
                    n = min(512, W - k0)
                    for ih in range(8):
                        b0 = (ih % 2) * 64
                        pn, pt = self.bank()
                        sc.op('pe', lambda e, pt=pt, ih=ih, b0=b0, qb=qb, k0=k0, n=n: e.matmul(
                            pt[:, 0:n], lhsT=QI[b0:b0 + 64, ih // 2, qb * P:(qb + 1) * P], rhs=KI[b0:b0 + 64, k0:k0 + n], start=True, stop=True),
                            reads=[(L, 'QI', ih // 2)] + [(L, 'KI', kb) for kb in range(k0 // 512, (k0 + n + 511) // 512)], writes=[pn])
                        rs = nrt % 2
                        nrt += 1
                        rn = 'tmp32_%d' % rs
                        sc.op('act', lambda e, pt=pt, rs=rs, n=n: e.activation(out=Rt[rs][:, 0:n], in_=pt[:, 0:n], func=AF.Relu), reads=[pn], writes=[rn])
                        if ih == 0:
                            sc.op('dve', lambda e, rs=rs, n=n, k0=k0, qb=qb: e.tensor_scalar(out=score[:, k0:k0 + n], in0=Rt[rs][:, 0:n], scalar1=WI[:, qb, 0:1],
                                                                                      scalar2=None, op0=ALU.mult), reads=[rn, (L, 'WI')], writes=[(L, 'score')])
                        else:
                            sc.op('dve', lambda e, rs=rs, n=n, k0=k0, qb=qb, ih=ih: e.scalar_tensor_tensor(
                                out=score[:, k0:k0 + n], in0=Rt[rs][:, 0:n], scalar=WI[:, qb, ih:ih + 1], in1=score[:, k0:k0 + n], op0=ALU.mult, op1=ALU.add),
                                reads=[rn, (L, 'WI'), (L, 'score')], writes=[(L, 'score')])
                SR = [(L, 'score'), (L, 'bis')]
                mx, lo, w0, mid, cnt, ff = bs
                if nk >= 3:
                    sc.op('dve', lambda e, W=W: e.tensor_reduce(out=mx[:], in_=score[:, 0:W], axis=AX.X, op=ALU.max), reads=SR, writes=[(L, 'bis')])
                    sc.op('dve', lambda e, W=W: e.tensor_reduce(out=lo[:], in_=score[:, 0:W], axis=AX.X, op=ALU.min), reads=SR, writes=[(L, 'bis')])
                    sc.op('dve', lambda e: e.tensor_tensor(out=w0[:], in0=mx[:], in1=lo[:], op=ALU.subtract), reads=SR, writes=[(L, 'bis')])
                else:
                    sc.op('dve', lambda e: e.memset(lo[:], -1e29), reads=SR, writes=[(L, 'bis')])
                sc.op('dve', lambda e, W=W: e.tensor_tensor(out=score[:, W - P:W], in0=score[:, W - P:W], in1=cmask[:], op=ALU.add),
                      reads=SR + ['cmask'], writes=SR)
                if nk >= 3:
                    for it in range(NI):
                        cst = 2.0 ** (-(it + 1))
                        sc.op('dve', lambda e, cst=cst: e.scalar_tensor_tensor(out=mid[:], in0=w0[:], scalar=cst, in1=lo[:], op0=ALU.mult, op1=ALU.add),
                              reads=SR, writes=[(L, 'bis')])
                        sc.op('dve', lambda e, W=W: e.tensor_scalar(out=nmq[:, 0:W], in0=score[:, 0:W], scalar1=mid[:, 0:1], scalar2=0.0, op0=ALU.is_ge,
                                                                   op1=ALU.add, accum_out=cnt[:, 0:1]), reads=SR + [(L, 'nmq')], writes=[(L, 'bis'), (L, 'nmq')])
                        sc.op('dve', lambda e, cst=cst: e.tensor_scalar(out=ff[:], in0=cnt[:], scalar1=256.0, scalar2=cst, op0=ALU.is_ge, op1=ALU.mult),
                              reads=SR, writes=[(L, 'bis')])
                        sc.op('dve', lambda e: e.scalar_tensor_tensor(out=lo[:], in0=ff[:], scalar=w0[:, 0:1], in1=lo[:], op0=ALU.mult, op1=ALU.add),
                              reads=SR, writes=[(L, 'bis')])
                sc.op('dve', lambda e, W=W: e.tensor_scalar(out=nmq[:, 0:W], in0=score[:, 0:W], scalar1=lo[:, 0:1], scalar2=-30000.0, op0=ALU.is_lt, op1=ALU.mult),
                      reads=SR + [(L, 'nmq')], writes=[(L, 'nmq')])
            def tr(qb):
                gq = blk * 4 + qb
                nk = gq + 1
                W = nk * P
                ms = gq % 2
                mn_ = (L, 'nmT', ms)
                for j0 in range(0, nk, 8):
                    jn = min(8, nk - j0)
                    pn, pt = self.bank()
                    ptb = pt[:].bitcast(BF16)
                    fns = [lambda e, ptb=ptb, j=j, j0=j0: e.transpose(out=ptb[:, (j - j0) * P:(j - j0 + 1) * P], in_=nmq[:, j * P:(j + 1) * P], identity=identb[:])
                           for j in range(j0, j0 + jn)]
                    sc.op('pe', fns, reads=[(L, 'nmq'), 'identb'], writes=[pn])
                    sc.op('act', lambda e, ptb=ptb, ms=ms, j0=j0, jn=jn: e.activation(out=nmT[ms][:, j0:j0 + jn, :], in_=ptb[:, 0:jn * P].rearrange('p (a b) -> p a b', b=P),
                                                                                func=AF.Copy), reads=[pn], writes=[(mn_, j0)])
            def att(qb):
                gq = blk * 4 + qb
                nk = gq + 1
                W = nk * P
                nonlocal npx
                ms = gq % 2
                mn_ = (L, 'nmT', ms)
                for kvh in range(4 if self.cfg.get('astage', 9) >= 4 else 0):
                    ab = self.abank[self.nab % 2]
                    self.nab += 1
                    pon = ('ps', ab)
                    po = self.ps[ab]
                    b0 = (kvh % 2) * 64
                    stb = {}

                    def emit_st(j, kvh=kvh, b0=b0, stb=stb):
                        pn, pt = self.bank()
                        stb[j] = (pn, pt)
                        near = (nk - 1 - j) if (nk - 1 - j) < 2 else None
                        fns = []
                        p3 = pt[:, :].rearrange('p (g q) -> p g q', q=P)
                        fns.append(lambda e, p3=p3, j=j, b0=b0, kvh=kvh, qb=qb: e.matmul(
                            p3, lhsT=KT[b0:b0 + 64, kvh // 2, j * P:(j + 1) * P],
                            rhs=QT[b0:b0 + 64, (kvh // 2) * 4:(kvh // 2) * 4 + 4, qb * P:(qb + 1) * P], start=True, stop=False))
                        if near is not None:
                            fns.append(lambda e, p3=p3, kvh=kvh, near=near: e.matmul(
                                p3, lhsT=identb[:], rhs=biasS[:, near * 16 + kvh * 4:near * 16 + kvh * 4 + 4, :], start=False, stop=False))
                        fns.append(lambda e, p3=p3, j=j, ms=ms: e.matmul(
                            p3, lhsT=identb[:], rhs=nmT[ms][:, j, :].unsqueeze(1).to_broadcast([P, 4, P]), start=False, stop=True))
                        sc.op('pe', fns, reads=[(L, 'KT', kvh // 2, j // 4), (mn_, (j // 8) * 8), 'identb', (L, 'biasS')] +
                              [(L, 'QT', (kvh // 2) * 4 + g) for g in range(4)], writes=[pn])
                    emit_st(0)
                    for j in range(nk):
                        if j + 1 < nk:
                            emit_st(j + 1)
                        pn, pt = stb[j]
                        px = npx % 3
                        npx += 1
                        pxn = 'pexp%d' % px
                        sc.op('act', lambda e, pt=pt, px=px: e.activation(out=pexp[px][:], in_=pt[:], func=AF.Exp), reads=[pn], writes=[pxn])
                        sc.op('pe', lambda e, po=po, px=px, j=j, kvh=kvh, nk=nk: e.matmul(po[0:65, :], lhsT=VA[:, j, kvh, :], rhs=pexp[px][:],
                                                                                     start=(j == 0), stop=(j == nk - 1)),
                              reads=[pxn, (L, 'VA', j), (L, 'VAones')], writes=[pon])
                    sc.op('act', lambda e, po=po: e.activation(out=osb[:], in_=po[0:65, :], func=AF.Copy), reads=[pon], writes=['osb'])
                    sc.op('act', lambda e: e.activation(out=osb[64:65, :], in_=osb[64:65, :], func=AF.Ln), reads=['osb'], writes=['osb'])
                    sc.op('act', lambda e: e.activation(out=osb[64:65, :], in_=osb[64:65, :], func=AF.Exp, scale=-1.0), reads=['osb'], writes=['osb'])
                    pn, pt = self.bank()
                    sc.op('pe', lambda e, pt=pt: e.matmul(pt[0:64, :], lhsT=selrow[:], rhs=osb[:], start=True, stop=True), reads=['osb', 'selrow'], writes=[pn])
                    sc.op('act', lambda e, pt=pt: e.activation(out=rbc[0:64, :], in_=pt[0:64, :], func=AF.Copy), reads=[pn], writes=['rbc'])
                    sc.op('pool', lambda e, kvh=kvh, qb=qb: e.tensor_tensor(out=OTn[:, kvh * 4:(kvh + 1) * 4, qb * P:(qb + 1) * P],
                                                                        in0=osb[0:64, :].rearrange('p (a b) -> p a b', b=P),
                                                                        in1=rbc[0:64, :].rearrange('p (a b) -> p a b', b=P), op=ALU.mult),
                          reads=['osb', 'rbc'], writes=[(L, 'OTn', kvh, qb)])
            nq = 4 if self.cfg.get('astage', 9) >= 2 else 0
            if nq:
                idx_bis(0)
                tr(0)
            for qb in range(nq):
                if qb + 1 < nq:
                    idx_bis(qb + 1)
                att(qb)
                if qb + 1 < nq:
                    tr(qb + 1)
            if self.cfg.get('astage', 9) < 5:
                continue
            oreads = [(L, 'OTn', kvh, qb) for kvh in range(4) for qb in range(4)] + [(L, 'wA')]
            for c in range(DC):
                if c % 4 == 0:
                    for h in range(16):
                        sc.dma('pool', 'wA', wout[:, h, :], self.d_awout[:, h, (c // 4) * 512:(c // 4 + 1) * 512], writes=[(L, 'wA')])
                pn, pt = self.bank()
                fns = [lambda e, pt=pt, h=h, c=c: e.matmul(pt[:], lhsT=wout[:, h, (c % 4) * P:(c % 4 + 1) * P], rhs=OTn[:, h, :], start=(h == 0), stop=(h == 15))
                       for h in range(16)]
                sc.op('pe', fns, reads=oreads, writes=[pn])
                sc.op('dve', lambda e, pt=pt, c=c, t=t: e.scalar_tensor_tensor(out=t[:, c, :], in0=pt[:], scalar=self.modv(li, 2, c), in1=t[:, c, :],
                                                                              op0=ALU.mult, op1=ALU.add), reads=[pn, (name, c), ('modT', li)], writes=[(name, c)])
                self.store_xT(blk, name, t, c)
        self.rot = list(range(8))
        sc.barrier()
        st.close()

    def ssm_setup(self):
        self.d_lamT = self.inp('ssm_lamT', [P, 32, 2])
        self.d_lstepT = self.inp('ssm_lstepT', [P, 32])
        self.d_Bblk = self.inp('ssm_Bblk', [32, P, 256])
        self.d_Cblk = self.inp('ssm_Cblk', [32, P, 256])
        self.d_dT = self.inp('ssm_dT', [P, DC])
        self.d_glu = self.inp('ssm_glu_r', [DC, P, DC * 256])

    def ssm_mixer(self, li):
        import math
        sc = self.sc
        st = contextlib.ExitStack()
        L = 'S'
        LC = 128
        I32 = mybir.dt.int32
        Ec = self.sb('Ec', [P, 32, LC], F32, st)
        En = self.sb('En', [P, 32, LC], F32, st)
        Bb = self.sb('Bb', [P, 32, 256], BF16, st)
        Cb = self.sb('Cb', [P, 32, 256], BF16, st)
        sm = {n: self.sb('ss_' + n, [P, 32], F32, st) for n in
              ['lr', 'li', 'stp', 'th', 'rr', 'u', 'f', 'g', 'sn', 'cs', 'x', 'y', 'den', 'cr', 'ci', 'cL', 'nL', 'ire', 'iim', 'e1c', 'e1n', 't1', 't2']}
        ni = self.sb('ss_ni', [P, 32], I32, st)
        lam = self.sb('ss_lam', [P, 32, 2], F32, st)
        dT = self.sb('ss_dT', [P, DC], F32, st)
        cs1 = self.sb('ss_c1', [P, 4], F32, st)
        SM = [(L, 'sm')]

        def dv(fn, extra_r=(), extra_w=()):
            sc.op('dve', fn, reads=SM + list(extra_r), writes=SM + list(extra_w))

        def ac(fn):
            sc.op('act', fn, reads=SM, writes=SM)
        sc.dma('sp', 'const', lam[:], self.d_lamT[:, :, :], writes=SM)
        sc.dma('sp', 'const', sm['stp'][:], self.d_lstepT[:, :], writes=SM)
        sc.dma('sp', 'const', dT[:], self.d_dT[:, :], writes=[(L, 'dT')])
        for k in range(32):
            sc.dma('pool', 'Bb', Bb[:, k, :], self.d_Bblk[k, :, :], writes=[(L, 'Bb')])
        dv(lambda e: e.tensor_scalar(out=sm['lr'][:], in0=lam[:, :, 0], scalar1=-1e-4, scalar2=None, op0=ALU.min))
        dv(lambda e: e.tensor_copy(out=sm['li'][:], in_=lam[:, :, 1]))
        ac(lambda e: e.activation(out=sm['stp'][:], in_=sm['stp'][:], func=AF.Exp))
        dv(lambda e: e.tensor_tensor(out=sm['th'][:], in0=sm['li'][:], in1=sm['stp'][:], op=ALU.mult))
        dv(lambda e: e.tensor_tensor(out=sm['rr'][:], in0=sm['lr'][:], in1=sm['stp'][:], op=ALU.mult))
        ac(lambda e: e.activation(out=sm['rr'][:], in_=sm['rr'][:], func=AF.Exp))

        def sincos(dst, off):
            dv(lambda e: e.tensor_scalar(out=sm['u'][:], in0=sm['th'][:], scalar1=1.0 / (2 * math.pi), scalar2=off, op0=ALU.mult, op1=ALU.add))
            dv(lambda e: e.tensor_copy(out=ni[:], in_=sm['u'][:]))
            dv(lambda e: e.tensor_copy(out=sm['f'][:], in_=ni[:]))
            dv(lambda e: e.tensor_tensor(out=sm['f'][:], in0=sm['u'][:], in1=sm['f'][:], op=ALU.subtract))
            dv(lambda e: e.tensor_scalar(out=sm['g'][:], in0=sm['f'][:], scalar1=0.5, scalar2=None, op0=ALU.is_gt))
            dv(lambda e: e.tensor_tensor(out=sm['f'][:], in0=sm['f'][:], in1=sm['g'][:], op=ALU.subtract))
            dv(lambda e: e.tensor_scalar(out=sm['g'][:], in0=sm['f'][:], scalar1=-0.5, scalar2=None, op0=ALU.is_lt))
            dv(lambda e: e.tensor_tensor(out=sm['f'][:], in0=sm['f'][:], in1=sm['g'][:], op=ALU.add))
            ac(lambda e, dst=dst: e.activation(out=sm[dst][:], in_=sm['f'][:], func=AF.Sin, scale=-2 * math.pi))
        sincos('sn', 64.5)
        sincos('cs', 64.75)
        dv(lambda e: e.tensor_tensor(out=sm['x'][:], in0=sm['rr'][:], in1=sm['cs'][:], op=ALU.mult))
        dv(lambda e: e.tensor_scalar(out=sm['x'][:], in0=sm['x'][:], scalar1=-1.0, scalar2=None, op0=ALU.add))
        dv(lambda e: e.tensor_tensor(out=sm['y'][:], in0=sm['rr'][:], in1=sm['sn'][:], op=ALU.mult))
        dv(lambda e: e.tensor_tensor(out=sm['den'][:], in0=sm['lr'][:], in1=sm['lr'][:], op=ALU.mult))
        dv(lambda e: e.tensor_tensor(out=sm['t1'][:], in0=sm['li'][:], in1=sm['li'][:], op=ALU.mult))
        dv(lambda e: e.tensor_tensor(out=sm['den'][:], in0=sm['den'][:], in1=sm['t1'][:], op=ALU.add))
        dv(lambda e: e.reciprocal(out=sm['den'][:], in_=sm['den'][:]))
        dv(lambda e: e.tensor_tensor(out=sm['cr'][:], in0=sm['x'][:], in1=sm['lr'][:], op=ALU.mult))
        dv(lambda e: e.tensor_tensor(out=sm['t1'][:], in0=sm['y'][:], in1=sm['li'][:], op=ALU.mult))
        dv(lambda e: e.tensor_tensor(out=sm['cr'][:], in0=sm['cr'][:], in1=sm['t1'][:], op=ALU.add))
        dv(lambda e: e.tensor_tensor(out=sm['cr'][:], in0=sm['cr'][:], in1=sm['den'][:], op=ALU.mult))
        dv(lambda e: e.tensor_tensor(out=sm['ci'][:], in0=sm['y'][:], in1=sm['lr'][:], op=ALU.mult))
        dv(lambda e: e.tensor_tensor(out=sm['t1'][:], in0=sm['x'][:], in1=sm['li'][:], op=ALU.mult))
        dv(lambda e: e.tensor_tensor(out=sm['ci'][:], in0=sm['ci'][:], in1=sm['t1'][:], op=ALU.subtract))
        dv(lambda e: e.tensor_tensor(out=sm['ci'][:], in0=sm['ci'][:], in1=sm['den'][:], op=ALU.mult))
        st2 = contextlib.ExitStack()
        Tc = self.sb('Tc', [P, LC, 32], F32, st2)
        Tn = self.sb('Tn', [P, LC, 32], F32, st2)
        U1 = self.sb('U1', [P, LC // 2, 32], F32, st2)
        U2 = self.sb('U2', [P, LC // 2, 32], F32, st2)
        cst1 = self.sb('tp1s', [P, 512], F32, st2)
        cst2 = self.sb('tp2s', [P, 512], F32, st2)
        dv(lambda e: e.tensor_copy(out=sm['e1c'][:], in_=sm['cs'][:]))
        dv(lambda e: e.tensor_scalar(out=sm['e1n'][:], in0=sm['sn'][:], scalar1=-1.0, scalar2=None, op0=ALU.mult))
        dv(lambda e: e.memset(Tc[:, 0, :], 1.0), extra_w=[(L, 'T')])
        dv(lambda e: e.memset(Tn[:, 0, :], 0.0), extra_w=[(L, 'T')])
        TT = [(L, 'T')]
        n = 1
        while n < LC:
            ecb = sm['e1c'][:, :].unsqueeze(1).to_broadcast([P, n, 32])
            enb = sm['e1n'][:, :].unsqueeze(1).to_broadcast([P, n, 32])
            dv(lambda e, n=n, ecb=ecb: e.tensor_tensor(out=Tc[:, n:2 * n, :], in0=Tc[:, 0:n, :], in1=ecb, op=ALU.mult), TT, TT)
            dv(lambda e, n=n, enb=enb: e.tensor_tensor(out=U1[:, 0:n, :], in0=Tn[:, 0:n, :], in1=enb, op=ALU.mult), TT, TT)
            dv(lambda e, n=n: e.tensor_tensor(out=Tc[:, n:2 * n, :], in0=Tc[:, n:2 * n, :], in1=U1[:, 0:n, :], op=ALU.subtract), TT, TT)
            dv(lambda e, n=n, enb=enb: e.tensor_tensor(out=Tn[:, n:2 * n, :], in0=Tc[:, 0:n, :], in1=enb, op=ALU.mult), TT, TT)
            dv(lambda e, n=n, ecb=ecb: e.tensor_tensor(out=U2[:, 0:n, :], in0=Tn[:, 0:n, :], in1=ecb, op=ALU.mult), TT, TT)
            dv(lambda e, n=n: e.tensor_tensor(out=Tn[:, n:2 * n, :], in0=Tn[:, n:2 * n, :], in1=U2[:, 0:n, :], op=ALU.add), TT, TT)
            dv(lambda e: e.tensor_tensor(out=sm['t1'][:], in0=sm['e1c'][:], in1=sm['e1c'][:], op=ALU.mult))
            dv(lambda e: e.tensor_tensor(out=sm['t2'][:], in0=sm['e1n'][:], in1=sm['e1n'][:], op=ALU.mult))
            dv(lambda e: e.tensor_tensor(out=sm['e1n'][:], in0=sm['e1c'][:], in1=sm['e1n'][:], op=ALU.mult))
            dv(lambda e: e.tensor_scalar(out=sm['e1n'][:], in0=sm['e1n'][:], scalar1=2.0, scalar2=None, op0=ALU.mult))
            dv(lambda e: e.tensor_tensor(out=sm['e1c'][:], in0=sm['t1'][:], in1=sm['t2'][:], op=ALU.subtract))
            n *= 2
        dv(lambda e: e.tensor_copy(out=sm['cL'][:], in_=sm['e1c'][:]))
        dv(lambda e: e.tensor_copy(out=sm['nL'][:], in_=sm['e1n'][:]))
        dv(lambda e: e.memset(sm['ire'][:], 0.0))
        dv(lambda e: e.memset(sm['iim'][:], 0.0))
        dv(lambda e: e.tensor_copy(out=Ec[:, :, :], in_=Tc[:, :, :].rearrange('p t k -> p k t')), TT, [(L, 'E')])
        dv(lambda e: e.tensor_copy(out=En[:, :, :], in_=Tn[:, :, :].rearrange('p t k -> p k t')), TT, [(L, 'E')])
        for k in range(32):
            sc.dma('sp', 'cstage', cst1[:, 0:256], self.d_Cblk[k, :, :], writes=['cst1'])
            crk = sm['cr'][:, k:k + 1]
            cik = sm['ci'][:, k:k + 1]
            sc.op('dve', lambda e, cik=cik: e.tensor_scalar(out=cst2[:, 0:128], in0=cst1[:, 128:256], scalar1=cik, scalar2=None, op0=ALU.mult),
                  reads=['cst1'] + SM, writes=['cst2'])
            sc.op('dve', lambda e, k=k, crk=crk: e.scalar_tensor_tensor(out=Cb[:, k, 0:128], in0=cst1[:, 0:128], scalar=crk, in1=cst2[:, 0:128], op0=ALU.mult, op1=ALU.subtract),
                  reads=['cst1', 'cst2'] + SM, writes=[(L, 'Cb')])
            sc.op('dve', lambda e, crk=crk: e.tensor_scalar(out=cst2[:, 128:256], in0=cst1[:, 128:256], scalar1=crk, scalar2=None, op0=ALU.mult),
                  reads=['cst1'] + SM, writes=['cst2'])
            sc.op('dve', lambda e, k=k, cik=cik: e.scalar_tensor_tensor(out=Cb[:, k, 128:256], in0=cst1[:, 0:128], scalar=cik, in1=cst2[:, 128:256], op0=ALU.mult, op1=ALU.add),
                  reads=['cst1', 'cst2'] + SM, writes=[(L, 'Cb')])
        if self.cfg.get('sdebug'):
            dbg = self.nc.dram_tensor('dbg', [P, 7 * 32 + 512 + 512], F32, kind="ExternalOutput").ap()
            for i, nme in enumerate(['sn', 'cs', 'rr', 'cr', 'ci', 'cL', 'nL']):
                sc.dma('sp', 'const', dbg[:, i * 32:(i + 1) * 32], sm[nme][:], reads=SM, writes=[('dbg', i)])
            sc.dma('sp', 'const', dbg[:, 224:224 + 256], Ec[:, 0:2, :], reads=[(L, 'E')], writes=[('dbg', 10)])
            sc.dma('sp', 'const', dbg[:, 224 + 256:224 + 512], En[:, 0:2, :], reads=[(L, 'E')], writes=[('dbg', 11)])
            sc.dma('sp', 'const', dbg[:, 224 + 512:224 + 768], Cb[:, 0, :], reads=[(L, 'Cb')], writes=[('dbg', 12)], allow_dtype=True) if False else None
        sc.barrier()
        st2.close()
        hT = self.sb('hTs', [P, DC, 512], BF16, st)
        h32 = self.sb('h32s', [P, DC, 512], F32, st)
        GT = self.sb('GTs', [P, DC, 512], BF16, st)
        Sb = self.sb('Sb', [P, 4, 2, 512], BF16, st)
        wgl = [self.sb('wgl%d' % i, [P, DC * 256], BF16, st) for i in range(2)]
        bre2 = [self.sb('bre%d' % i, [P, 512], F32, st) for i in range(2)]
        bim2 = [self.sb('bim%d' % i, [P, 512], F32, st) for i in range(2)]
        wre2 = [self.sb('wre%d' % i, [P, 512], F32, st) for i in range(2)]
        wim2 = [self.sb('wim%d' % i, [P, 512], F32, st) for i in range(2)]
        xrs2 = [self.sb('xrs%d' % i, [P, 512], F32, st) for i in range(2)]
        xis2 = [self.sb('xis%d' % i, [P, 512], F32, st) for i in range(2)]
        tq = self.sb('tq', [P, 512], F32, st)
        tq2 = self.sb('tq2', [P, 512], F32, st)
        tp1 = self.sb('tp1', [P, 512], F32, st)
        tp2 = self.sb('tp2', [P, 512], F32, st)
        nw = 0
        for blk in range(self.cfg.get('snblk', NBLK)):
            name, t = self.load_xT(blk, False)
            self.norm_mod(li, 0, name, t, hT, (L, 'hT'), 0, h32=h32)
            for j in range(DC):
                def v3(ap):
                    return ap.rearrange('p (a b) -> p a b', b=LC)
                ER = [(L, 'E')]
                for pair in range(2):
                    tl = []
                    for kk in (2 * pair, 2 * pair + 1):
                        k = 4 * j + kk
                        sl = k % 2
                        T = dict(k=k, kk=kk, sl=sl, bre=bre2[sl], bim=bim2[sl], wre=wre2[sl], wim=wim2[sl], xrs=xrs2[sl], xis=xis2[sl],
                                 BRE='bre%d' % sl, BIM='bim%d' % sl, WRE='wre%d' % sl, WIM='wim%d' % sl, XRS='xrs%d' % sl, XIS='xis%d' % sl, CS=('cs1', sl),
                                 cb=Ec[:, k, :].unsqueeze(1).to_broadcast([P, 4, LC]), nb=En[:, k, :].unsqueeze(1).to_broadcast([P, 4, LC]),
                                 rb=sm['rr'][:, k:k + 1].to_broadcast([P, LC]), cLk=sm['cL'][:, k:k + 1], nLk=sm['nL'][:, k:k + 1],
                                 irek=sm['ire'][:, k:k + 1], iimk=sm['iim'][:, k:k + 1], CR=[(L, 'carry', k)],
                                 c0=cs1[:, 2 * sl:2 * sl + 1], c1=cs1[:, 2 * sl + 1:2 * sl + 2])
                        tl.append(T)
                        pxr_n, pxr = self.bank()
                        pxi_n, pxi = self.bank()
                        hr = [((L, 'hT'), j, 0), (L, 'Bb')]
                        sc.op('pe', lambda e, pxr=pxr, k=k, j=j: e.matmul(pxr[:], lhsT=Bb[:, k, 0:128], rhs=hT[:, j, :], start=True, stop=True), reads=hr, writes=[pxr_n])
                        sc.op('pe', lambda e, pxi=pxi, k=k, j=j: e.matmul(pxi[:], lhsT=Bb[:, k, 128:256], rhs=hT[:, j, :], start=True, stop=True), reads=hr, writes=[pxi_n])
                        sc.op('act', lambda e, pxr=pxr, T=T: e.activation(out=T['xrs'][:], in_=pxr[:], func=AF.Copy), reads=[pxr_n], writes=[T['XRS']])
                        sc.op('act', lambda e, pxi=pxi, T=T: e.activation(out=T['xis'][:], in_=pxi[:], func=AF.Copy), reads=[pxi_n], writes=[T['XIS']])
                    for T in tl:
                        sc.op('dve', lambda e, T=T: e.tensor_tensor(out=v3(T['bre'][:, :]), in0=v3(T['xrs'][:, :]), in1=T['cb'], op=ALU.mult), reads=[T['XRS']] + ER, writes=[T['BRE']])
                        sc.op('dve', lambda e, T=T: e.tensor_tensor(out=v3(tq[:, :]), in0=v3(T['xis'][:, :]), in1=T['nb'], op=ALU.mult), reads=[T['XIS']] + ER, writes=['tq'])
                        sc.op('dve', lambda e, T=T: e.tensor_tensor(out=T['bre'][:], in0=T['bre'][:], in1=tq[:], op=ALU.subtract), reads=[T['BRE'], 'tq'], writes=[T['BRE']])
                        sc.op('pool', lambda e, T=T: e.tensor_tensor(out=v3(T['bim'][:, :]), in0=v3(T['xis'][:, :]), in1=T['cb'], op=ALU.mult), reads=[T['XIS']] + ER, writes=[T['BIM']])
                        sc.op('pool', lambda e, T=T: e.tensor_tensor(out=v3(tq2[:, :]), in0=v3(T['xrs'][:, :]), in1=T['nb'], op=ALU.mult), reads=[T['XRS']] + ER, writes=['tq2'])
                        sc.op('pool', lambda e, T=T: e.tensor_tensor(out=T['bim'][:], in0=T['bim'][:], in1=tq2[:], op=ALU.add), reads=[T['BIM'], 'tq2'], writes=[T['BIM']])
                    for ch in range(4):
                        lo_, hi_ = ch * LC, (ch + 1) * LC
                        for T in tl:
                            sc.op('dve', lambda e, T=T, lo_=lo_, hi_=hi_: e.tensor_tensor_scan(out=T['wre'][:, lo_:hi_], data0=T['rb'], data1=T['bre'][:, lo_:hi_], initial=T['irek'],
                                                                                          op0=ALU.mult, op1=ALU.add), reads=[T['BRE']] + T['CR'] + SM, writes=[T['WRE']])
                        for T in tl:
                            sc.op('dve', lambda e, T=T, lo_=lo_, hi_=hi_: e.tensor_tensor_scan(out=T['wim'][:, lo_:hi_], data0=T['rb'], data1=T['bim'][:, lo_:hi_], initial=T['iimk'],
                                                                                          op0=ALU.mult, op1=ALU.add), reads=[T['BIM']] + T['CR'] + SM, writes=[T['WIM']])
                        for T in tl:
                            sc.op('dve', lambda e, T=T, hi_=hi_: e.tensor_tensor(out=T['c0'], in0=T['wim'][:, hi_ - 1:hi_], in1=T['nLk'], op=ALU.mult), reads=[T['WIM']] + SM, writes=[T['CS']])
                        for T in tl:
                            sc.op('dve', lambda e, T=T, hi_=hi_: e.scalar_tensor_tensor(out=T['irek'], in0=T['wre'][:, hi_ - 1:hi_], scalar=T['cLk'], in1=T['c0'], op0=ALU.mult, op1=ALU.add),
                                  reads=[T['WRE'], T['CS']] + SM, writes=T['CR'])
                        for T in tl:
                            sc.op('dve', lambda e, T=T, hi_=hi_: e.tensor_tensor(out=T['c1'], in0=T['wre'][:, hi_ - 1:hi_], in1=T['nLk'], op=ALU.mult), reads=[T['WRE']] + SM, writes=[T['CS']])
                        for T in tl:
                            sc.op('dve', lambda e, T=T, hi_=hi_: e.scalar_tensor_tensor(out=T['iimk'], in0=T['wim'][:, hi_ - 1:hi_], scalar=T['cLk'], in1=T['c1'], op0=ALU.mult, op1=ALU.subtract),
                                  reads=[T['WIM'], T['CS']] + SM, writes=T['CR'])
                    for T in tl:
                        kk = T['kk']
                        sc.op('pool', lambda e, T=T: e.tensor_tensor(out=v3(tp1[:, :]), in0=v3(T['wre'][:, :]), in1=T['cb'], op=ALU.mult), reads=[T['WRE']] + ER, writes=['tp1'])
                        sc.op('pool', lambda e, T=T: e.tensor_tensor(out=v3(tp2[:, :]), in0=v3(T['wim'][:, :]), in1=T['nb'], op=ALU.mult), reads=[T['WIM']] + ER, writes=['tp2'])
                        sc.op('pool', lambda e, kk=kk: e.tensor_tensor(out=Sb[:, kk, 0, :], in0=tp1[:], in1=tp2[:], op=ALU.add), reads=['tp1', 'tp2'], writes=[(L, 'Sb', kk)])
                        sc.op('pool', lambda e, T=T: e.tensor_tensor(out=v3(tp1[:, :]), in0=v3(T['wre'][:, :]), in1=T['nb'], op=ALU.mult), reads=[T['WRE']] + ER, writes=['tp1'])
                        sc.op('pool', lambda e, T=T: e.tensor_tensor(out=v3(tp2[:, :]), in0=v3(T['wim'][:, :]), in1=T['cb'], op=ALU.mult), reads=[T['WIM']] + ER, writes=['tp2'])
                        sc.op('pool', lambda e, kk=kk: e.tensor_tensor(out=Sb[:, kk, 1, :], in0=tp1[:], in1=tp2[:], op=ALU.subtract), reads=['tp1', 'tp2'], writes=[(L, 'Sb', kk)])
                pyn, py = self.bank()
                fns = []
                for kk in range(4):
                    k = 4 * j + kk
                    fns.append(lambda e, py=py, k=k, kk=kk: e.matmul(py[:], lhsT=Cb[:, k, 0:128], rhs=Sb[:, kk, 0, :], start=(kk == 0), stop=False))
                    fns.append(lambda e, py=py, k=k, kk=kk: e.matmul(py[:], lhsT=Cb[:, k, 128:256], rhs=Sb[:, kk, 1, :], start=False, stop=(kk == 3)))
                sc.op('pe', fns, reads=[(L, 'Sb', kk) for kk in range(4)] + [(L, 'Cb')], writes=[pyn])
                if self.cfg.get('sdebug') == 2 and blk == 0 and j == 0:
                    sc.op('act', lambda e, py=py: e.activation(out=bre[:], in_=py[:], func=AF.Copy), reads=[pyn, 'bre'], writes=['bre'])
                    sc.dma('sp', 'const', dbg2[:, 4, :], bre[:], reads=['bre'], writes=[('dbg2', 4)])
                z = self.tmp32[0]
                w_ = self.tmp32[1]
                sc.op('dve', lambda e, py=py, j=j, z=z: e.scalar_tensor_tensor(out=z[:], in0=h32[:, j, :], scalar=dT[:, j:j + 1], in1=py[:], op0=ALU.mult, op1=ALU.add),
                      reads=[pyn, ('h32', j), (L, 'dT')], writes=['tmp32_0'])
                if self.cfg.get('sdebug') and blk == 0 and j == 0:
                    dbg4 = self.nc.dram_tensor('dbg4', [P, 3, 512], F32, kind="ExternalOutput").ap()
                    sc.dma('sp', 'const', dbg4[:, 0, :], z[:], reads=['tmp32_0'], writes=[('dbg4', 0)])
                    sc.dma('pool', 'dbg4b', dbg4[:, 1, 0:256], Cb[:, 1, :], reads=[(L, 'Cb')], writes=[('dbg4', 1)])
                    sc.dma('pool', 'dbg4b', dbg4[:, 1, 256:512], Cb[:, 1, :], reads=[(L, 'Cb')], writes=[('dbg4', 3)])
                sc.op('act', lambda e, z=z, w_=w_: e.activation(out=w_[:], in_=z[:], func=AF.Square), reads=['tmp32_0'], writes=['tmp32_1'])
                sc.op('dve', lambda e, w_=w_: e.tensor_scalar(out=w_[:], in0=w_[:], scalar1=0.044715, scalar2=1.0, op0=ALU.mult, op1=ALU.add), reads=['tmp32_1'], writes=['tmp32_1'])
                sc.op('dve', lambda e, z=z, w_=w_: e.tensor_tensor(out=w_[:], in0=w_[:], in1=z[:], op=ALU.mult), reads=['tmp32_0', 'tmp32_1'], writes=['tmp32_1'])
                sc.op('act', lambda e, w_=w_: e.activation(out=w_[:], in_=w_[:], func=AF.Sigmoid, scale=1.5957691216057308), reads=['tmp32_1'], writes=['tmp32_1'])
                sc.op('dve', lambda e, z=z, w_=w_, j=j: e.tensor_tensor(out=GT[:, j, :], in0=z[:], in1=w_[:], op=ALU.mult), reads=['tmp32_0', 'tmp32_1'], writes=[(L, 'GT', j)])
            if self.cfg.get('sdebug') and blk == 0:
                sc.dma('pool', 'dbg4b', dbg4[:, 2, :], GT[:, 0, :], reads=[(L, 'GT', 0)], writes=[('dbg4', 2)])
            greads = [(L, 'GT', j) for j in range(DC)]
            for c in range(DC):
                slot = nw % 2
                nw += 1
                wn = (L, 'wgl', slot)
                self.load_w_cast('wgl%d' % slot, wgl[slot][:, :], self.d_glu[c, :, :], None, writes=[wn])
                pln, pl = self.bank()
                pgn, pg = self.bank()
                for (pn, pt, off) in ((pln, pl, 0), (pgn, pg, 128)):
                    fns = [lambda e, pt=pt, kq=kq, off=off, slot=slot: e.matmul(pt[:], lhsT=wgl[slot][:, kq * 256 + off: kq * 256 + off + 128], rhs=GT[:, kq, :],
                                                                              start=(kq == 0), stop=(kq == DC - 1)) for kq in range(DC)]
                    sc.op('pe', fns, reads=greads + [wn], writes=[pn])
                tm = self.tmp32[2]
                sc.op('act', lambda e, pg=pg, tm=tm: e.activation(out=tm[:], in_=pg[:], func=AF.Sigmoid), reads=[pgn], writes=['tmp32_2'])
                sc.op('dve', lambda e, pl=pl, tm=tm: e.tensor_tensor(out=tm[:], in0=pl[:], in1=tm[:], op=ALU.mult), reads=[pln, 'tmp32_2'], writes=['tmp32_2'])
                sc.op('dve', lambda e, tm=tm, c=c, t=t: e.scalar_tensor_tensor(out=t[:, c, :], in0=tm[:], scalar=self.modv(li, 2, c), in1=t[:, c, :], op0=ALU.mult, op1=ALU.add),
                      reads=['tmp32_2', (name, c), ('modT', li)], writes=[(name, c)])
                self.store_xT(blk, name, t, c)
        sc.barrier()
        st.close()

    def epilogue(self):
        sc = self.sc
        self.out = self.nc.dram_tensor('out', [self.ntok, D], F32, kind="ExternalOutput").ap()
        otok = [self.sb('otok%d' % i, [P, D], F32) for i in range(2)]
        n = 0
        for blk in range(self.ntok // 512):
            name, t = self.load_xT(blk, False)
            for j in range(4):
                slot = n % 2
                n += 1
                on = 'otok%d' % slot
                for hh in range(2):
                    pname, pt = self.bank()
                    fns = [lambda e, pt=pt, j=j, c=c, hh=hh, t=t: e.transpose(out=pt[:, (c - hh * 4) * P:(c - hh * 4 + 1) * P],
                                                                      in_=t[:, c, j * P:(j + 1) * P], identity=self.ident[:])
                           for c in range(hh * 4, hh * 4 + 4)]
                    sc.op('pe', fns, reads=[(name, c) for c in range(DC)] + ['ident'], writes=[pname])
                    if hh == 0:
                        sc.op('act', lambda e, pt=pt, slot=slot: e.activation(out=otok[slot][:, 0:512], in_=pt[:], func=AF.Copy),
                              reads=[pname], writes=[(on, 0)])
                    else:
                        sc.op('dve', lambda e, pt=pt, slot=slot: e.tensor_copy(out=otok[slot][:, 512:1024], in_=pt[:]),
                              reads=[pname], writes=[(on, 1)])
                sc.dma('sp', 'st_' + on, self.out[blk * 512 + j * P: blk * 512 + (j + 1) * P, :], otok[slot][:],
                       reads=[(on, 0), (on, 1)], writes=[('out', blk, j)])

    def build(self):
        cfg = self.cfg
        self.setup_common()
        self.epsb = self.sb('epsb', [P, 1], F32)
        self.sc.op('dve', lambda e: e.memset(self.epsb[:], EPS), writes=['epsb'])
        self.build_mod()
        self.alloc_stream()
        self.conv_setup()
        self.ffn_setup()
        self.attn_setup()
        self.ssm_setup()
        steps = cfg.get('steps', ['m0', 'f0', 'm1', 'f1', 'm2', 'f2', 'm3', 'f3'])
        first = True
        if steps[0][0] != 'm' or int(steps[0][1]) % 3 != 0:
            st = contextlib.ExitStack()
            self.xtok = self.sb('xtok', [P, 4, D], F32, st)
            for blk in range(NBLK):
                name, t = self.load_xT(blk, True)
                for c in range(DC):
                    self.store_xT(blk, name, t, c)
            self.sc.barrier()
            st.close()
            first = False
        tail_split = cfg.get('tail_split', False)
        for s in steps:
            li = int(s[1])
            if tail_split and s == 'm3':
                self.sc.barrier()
                self.xs_d = self.nc.dram_tensor('xs_scratch', [D, S // 2 + 512], F32).ap()
                self.sc.dma('sp', 'xstage', self.xs_d[:, 512:512 + S // 2], (lambda: self.xT_d[:, bass.ds(self.rv * 2048, 2048)]), writes=['xs'])
                self.sc.dma('sp', 'xstage', self.xs_d[:, 0:512], (lambda: self.xT_d[:, bass.ds(self.rv * 1536, 512)]), writes=['xs'])
                self.rd_mode, self.wr_mode, self.ntok = 'dynfull', 'half', S // 2
            if tail_split and s == 'f3':
                self.rd_mode, self.wr_mode, self.ntok = 'half', 'half', S // 2
            if s[0] == 'm':
                if li % 3 == 0:
                    self.conv_mixer(li, li // 3, first)
                elif li % 3 == 1:
                    self.attn_mixer(li)
                else:
                    self.ssm_mixer(li)
                first = False
            else:
                self.ffn(li, li % 2 == 1)
        self.epilogue()
        self.sc.finish('sp')
        self.sc.emit()
        return self.nc


def host_layout(inputs, b):
    f = np.float32
    m = {}
    m['ident'] = np.eye(P, dtype=f)
    m['x'] = np.ascontiguousarray(inputs['x'][b])
    m['cT'] = np.ascontiguousarray(inputs['c'][b].reshape(DC, P).T)
    m['ada_w'] = inputs['ada_w']
    m['ada_bT'] = np.ascontiguousarray(inputs['ada_b'].reshape(4, 48, P).transpose(2, 0, 1))
    m['norm_gT'] = np.ascontiguousarray(inputs['norm_g'].reshape(4, 2, DC, P).transpose(3, 0, 1, 2))
    m['conv_w_in'] = inputs['conv_w_in']
    m['conv_w_out'] = inputs['conv_w_out']
    m['conv_wT'] = np.ascontiguousarray(inputs['conv_w'].reshape(2, 3, DC, P).transpose(3, 0, 1, 2))
    m['rankf'] = np.zeros((P, 1), dtype=f)
    return m


def rel_bucket_np(dist):
    import math
    d = np.maximum(dist, 1).astype(np.float32)
    large = 16 + (np.log(d / np.float32(16)) / np.float32(math.log(128 / 16)) * np.float32(16)).astype(np.int32)
    large = np.minimum(large, 31)
    return np.where(dist < 16, dist, large)


def ssm_layout(inputs):
    f = np.float32
    m = {}
    lre = inputs['ssm_lambda_re'][0]
    lim = inputs['ssm_lambda_im'][0]
    lamT = np.empty((P, 32, 2), dtype=f)
    lst = np.empty((P, 32), dtype=f)
    Bb = np.zeros((32, P, 2, P), dtype=f)
    Cb = np.zeros((32, P, 2, P), dtype=f)
    bre = inputs['ssm_b_re'][0]
    bim = inputs['ssm_b_im'][0]
    cre = inputs['ssm_c_re'][0]
    cim = inputs['ssm_c_im'][0]
    for k in range(32):
        for gg in range(2):
            g = 2 * k + gg
            lamT[gg * 64:(gg + 1) * 64, k, 0] = lre[g]
            lamT[gg * 64:(gg + 1) * 64, k, 1] = lim[g]
            lst[gg * 64:(gg + 1) * 64, k] = inputs['ssm_log_step'][0, g]
            r0 = (g % 8) * 16
            Bb[k, r0:r0 + 16, 0, gg * 64:(gg + 1) * 64] = bre[g].T
            Bb[k, r0:r0 + 16, 1, gg * 64:(gg + 1) * 64] = bim[g].T
            Cb[k, gg * 64:(gg + 1) * 64, 0, r0:r0 + 16] = cre[g].T
            Cb[k, gg * 64:(gg + 1) * 64, 1, r0:r0 + 16] = cim[g].T
    m['ssm_lamT'] = lamT
    m['ssm_lstepT'] = lst
    m['ssm_Bblk'] = Bb.reshape(32, P, 256)
    m['ssm_Cblk'] = Cb.reshape(32, P, 256)
    m['ssm_dT'] = np.ascontiguousarray(inputs['ssm_d'][0].reshape(DC, P).T)
    w = inputs['ssm_w_glu'][0]
    lin = w[:, :D].reshape(DC, P, DC, P)
    gate = w[:, D:].reshape(DC, P, DC, P)
    r = np.empty((DC, P, DC, 2, P), dtype=f)
    r[:, :, :, 0, :] = lin.transpose(2, 1, 0, 3)
    r[:, :, :, 1, :] = gate.transpose(2, 1, 0, 3)
    m['ssm_glu_r'] = r.reshape(DC, P, DC * 256)
    return m


def attn_layout(inputs):
    f = np.float32
    m = {}
    w = inputs['attn_w_in'][0]
    wq = w[:, 0:1024].reshape(D, 16, 64)
    order = []
    for pair in range(2):
        for g in range(4):
            order += [(2 * pair) * 4 + g, (2 * pair + 1) * 4 + g]
    m['attn_wq_perm'] = np.ascontiguousarray(wq[:, order, :]).reshape(D, 1024)
    m['attn_wk'] = np.ascontiguousarray(w[:, 1024:1280])
    m['attn_wv'] = np.ascontiguousarray(w[:, 1280:1536])
    m['attn_wqi'] = np.ascontiguousarray(w[:, 1536:2048])
    ki = w[:, 2048:2112]
    m['attn_wki2'] = np.ascontiguousarray(np.concatenate([ki, ki], axis=1))
    m['attn_wwi'] = np.ascontiguousarray(w[:, 2112:2120])
    m['attn_wout_r'] = np.ascontiguousarray(inputs['attn_w_out'][0].reshape(16, 64, D).transpose(1, 0, 2))
    qg = inputs['attn_q_gain'][0]
    kg = inputs['attn_k_gain'][0]
    m['attn_gainT'] = np.ascontiguousarray(np.stack([np.tile(qg, 2), np.tile(kg, 2)], axis=1))
    rb = inputs['rel_bias']
    sl = np.arange(P)[:, None]
    tl = np.arange(P)[None, :]
    bt = np.empty((P, 32, P), dtype=f)
    for kind in range(2):
        dist = np.maximum(tl - sl + kind * P, 0)
        bk = rel_bucket_np(dist)
        for h in range(16):
            bt[:, kind * 16 + h, :] = rb[bk, h]
    m['attn_biasT'] = bt
    m['attn_b31B'] = np.ascontiguousarray(np.broadcast_to(rb[31][None, :], (P, 16)))
    cm = np.zeros((P, P), dtype=f)
    cm[np.arange(P)[None, :] > np.arange(P)[:, None]] = -1e30
    m['cmask'] = cm
    bo = np.zeros((P, P), dtype=f)
    bo[0:64, 0:64] = 1.0
    bo[64:128, 64:128] = 1.0
    m['blockones'] = bo
    sr = np.zeros((65, 64), dtype=f)
    sr[64, :] = 1.0
    m['selrow'] = sr
    return m


_shared_cache = {}


def shared_layout(inputs):
    f = np.float32
    m = {}
    gu = inputs['ffn_w_gu']
    g = gu[:, :, :DFF].reshape(2, DC, P, FC, P)
    u = gu[:, :, DFF:].reshape(2, DC, P, FC, P)
    gu_r = np.stack([g, u], axis=4)
    m['ffn_gu_r'] = np.ascontiguousarray(gu_r.transpose(0, 3, 2, 1, 4, 5)).reshape(2, FC, P, DC * 256)
    dn = inputs['ffn_w_down'].reshape(2, FC, P, DC, P)
    m['ffn_dn_r'] = np.ascontiguousarray(dn.transpose(0, 3, 2, 1, 4)).reshape(2, DC, P, FC * P)
    gu = inputs['moe_w_gu']
    g = gu[:, :, :, :DFF].reshape(2, NE, DC, P, FC, P)
    u = gu[:, :, :, DFF:].reshape(2, NE, DC, P, FC, P)
    r = np.empty((2, NE, FC, P, DC, 2, P), dtype=f)
    r[:, :, :, :, :, 0, :] = g.transpose(0, 1, 4, 3, 2, 5)
    r[:, :, :, :, :, 1, :] = u.transpose(0, 1, 4, 3, 2, 5)
    m['moe_gu_r'] = r.reshape(2, NE, FC, P, DC * 256)
    dn = inputs['moe_w_down'].reshape(2, NE, FC, P, DC, P)
    m['moe_dn_r'] = np.ascontiguousarray(dn.transpose(0, 1, 4, 3, 2, 5)).reshape(2, NE, DC, P, FC * P)
    m['moe_rwT'] = np.ascontiguousarray(inputs['moe_router_w'].reshape(2, DC, P, NE).transpose(2, 0, 1, 3))
    m['moe_rbB'] = np.ascontiguousarray(np.broadcast_to(inputs['moe_router_b'][None], (P, 2, NE)))
    m.update(attn_layout(inputs))
    m.update(ssm_layout(inputs))
    sel = np.zeros((NE, NE, P), dtype=f)
    for e in range(NE):
        sel[e, e, :] = 1.0
    m['sel8'] = sel
    return m


def kernel(**inputs):
    inputs = {k: np.asarray(v) for k, v in inputs.items()}
    b = Builder({'tail_split': True})
    nc = b.build()
    shared = shared_layout(inputs)
    in_maps = []
    for core in range(8):
        m = host_layout(inputs, core % 4)
        m['rankf'] = np.full((P, 1), float(core // 4), dtype=np.float32)
        m.update(shared)
        in_maps.append({k: m[k] for k in b.din})
    res = run_bass_kernel_spmd(nc, in_maps, core_ids=list(range(8)))
    out = np.stack([np.concatenate([res.results[i]['out'], res.results[i + 4]['out']], axis=0) for i in range(4)], axis=0)
    return out.astype(np.float32)
```

```python
import contextlib
from types import FunctionType
import numpy as np
import concourse.bass as bass
import concourse.mybir as mybir
from concourse.bass_utils import run_bass_kernel_spmd

F32 = mybir.dt.float32
BF16 = mybir.dt.bfloat16
AF = mybir.ActivationFunctionType
ALU = mybir.AluOpType
AX = mybir.AxisListType

S = 4096
D = 1024
DC = 8
P = 128
DFF = 2816
FC = 22
NE = 8
EPS = 1e-6
NBLK = S // 512

ENG = ['pe', 'act', 'dve', 'pool', 'sp']
SELF_SYNC = True


class Sched:
    def __init__(self, nc, stack):
        self.nc = nc
        self.stack = stack
        self.streams = {e: [] for e in ENG}
        self.sem = {e: stack.enter_context(nc.semaphore('s_' + e)) for e in ENG}
        self.count = {e: 0 for e in ENG}
        self.dsem = {}
        self.seen = {e: {} for e in ENG}
        self.hist = {}
        self.buf = {}
        self.ninst = 0

    def _semof(self, key):
        if isinstance(key, tuple):
            return self.dsem[key[1]][0]
        return self.sem[key]

    def _wait(self, eng, toks):
        best = {}
        for (k, v) in toks:
            if v > best.get(k, 0):
                best[k] = v
        seen = self.seen[eng]
        for k, v in best.items():
            if seen.get(k, 0) >= v:
                continue
            if k == eng and (eng == 'pe' or not SELF_SYNC):
                continue
            s = self._semof(k)
            self.streams[eng].append(lambda e, s=s, v=v: e.wait_ge(s, v))
            self.ninst += 1
            h = self.hist.get((k, v))
            if h:
                for kk, vv in h.items():
                    if vv > seen.get(kk, 0):
                        seen[kk] = vv
            if v > seen.get(k, 0):
                seen[k] = v

    def _deps(self, reads, writes):
        toks = []
        for b in reads:
            st = self.buf.get(b)
            if st and st[0]:
                toks.append(st[0])
        for b in writes:
            st = self.buf.get(b)
            if st:
                if st[0]:
                    toks.append(st[0])
                toks.extend(st[1].items())
        return toks

    def _record(self, tok, reads, writes):
        for b in reads:
            st = self.buf.setdefault(b, [None, {}])
            if tok[1] > st[1].get(tok[0], 0):
                st[1][tok[0]] = tok[1]
        for b in writes:
            self.buf[b] = [tok, {}]

    def op(self, eng, fns, reads=(), writes=()):
        if not isinstance(fns, (list, tuple)):
            fns = [fns]
        self._wait(eng, self._deps(reads, writes))
        self.count[eng] += 1
        tok = (eng, self.count[eng])
        sem = self.sem[eng]
        for f in fns[:-1]:
            self.streams[eng].append(f)
        last = fns[-1]
        self.streams[eng].append(lambda e, f=last, s=sem: f(e).then_inc(s, 1))
        self.ninst += len(fns)
        self.hist[tok] = dict(self.seen[eng])
        self._record(tok, reads, writes)
        return tok

    def dma(self, queue, chan, out, in_, reads=(), writes=(), **kw):
        if chan == 'const':
            self.nconst = getattr(self, 'nconst', 0) + 1
            chan = 'const%d' % self.nconst
        if chan not in self.dsem:
            self.dsem[chan] = [self.stack.enter_context(self.nc.semaphore('d_' + str(chan))), 0]
        self._wait(queue, self._deps(reads, writes))
        ds = self.dsem[chan]
        ds[1] += 16
        tok = (('dma', chan), ds[1])
        s = ds[0]
        self.streams[queue].append(lambda e, s=s, o=out, i=in_, kw=kw: e.dma_start(
            out=(o() if isinstance(o, FunctionType) else o), in_=(i() if isinstance(i, FunctionType) else i), **kw).then_inc(s, 16))
        self.ninst += 1
        self.hist[tok] = dict(self.seen[queue])
        self._record(tok, reads, writes)
        return tok

    def barrier(self):
        toks = []
        for b, st in self.buf.items():
            if st[0]:
                toks.append(st[0])
            toks.extend(st[1].items())
        for eng in ENG:
            self._wait(eng, toks)

    def finish(self, eng='sp'):
        toks = []
        for b, st in self.buf.items():
            if st[0]:
                toks.append(st[0])
            toks.extend(st[1].items())
        self._wait(eng, toks)

    def emit(self):
        nc = self.nc
        with nc.Block() as block:
            @block.tensor
            def _(e):
                for f in self.streams['pe']:
                    f(e)

            @block.scalar
            def _(e):
                for f in self.streams['act']:
                    f(e)

            @block.vector
            def _(e):
                for f in self.streams['dve']:
                    f(e)

            @block.gpsimd
            def _(e):
                for f in self.streams['pool']:
                    f(e)

            @block.sync
            def _(e):
                for f in self.streams['sp']:
                    f(e)


class Builder:
    def __init__(self, cfg):
        self.cfg = cfg
        self.nc = bass.Bass("TRN2", target_bir_lowering=False)
        self.stack = contextlib.ExitStack()
        self.sc = Sched(self.nc, self.stack)
        self.din = {}
        self.psn = 0
        self.uid = 0

    def inp(self, name, shape, dtype=F32):
        t = self.nc.dram_tensor(name, list(shape), dtype, kind="ExternalInput").ap()
        self.din[name] = t
        return t

    def sb(self, name, shape, dtype, st=None):
        self.uid += 1
        return (st or self.stack).enter_context(self.nc.sbuf_tensor('sb%d_%s' % (self.uid, name), list(shape), dtype))

    def bank(self):
        rot = getattr(self, 'rot', None) or list(range(8))
        i = rot[self.psn % len(rot)]
        self.psn += 1
        return ('ps', i), self.ps[i]

    def setup_common(self):
        nc = self.nc
        self.ps = [self.stack.enter_context(nc.psum_tensor('ps%d' % i, [P, 512], F32)) for i in range(8)]
        self.ident = self.sb('ident', [P, P], F32)
        self.ones = self.sb('ones', [P, P], F32)
        d_ident = self.inp('ident', [P, P])
        self.sc.dma('sp', 'const', self.ident[:], d_ident[:, :], writes=['ident'])
        self.sc.op('dve', lambda e: e.memset(self.ones[:], 1.0), writes=['ones'])

    def build_mod(self):
        sc = self.sc
        cT = self.inp('cT', [P, DC])
        ada_w = self.inp('ada_w', [4, D, 6 * D])
        ada_bT = self.inp('ada_bT', [P, 4, 48])
        norm_gT = self.inp('norm_gT', [P, 4, 2, DC])
        self.cond = self.sb('cond', [P, DC], F32)
        self.modT = self.sb('modT', [P, 4, 48], F32)
        self.adab = self.sb('adab', [P, 4, 48], F32)
        self.ng = self.sb('ng', [P, 4, 2, DC], F32)
        self.gs = self.sb('gs', [P, 4, 2, DC], F32)
        craw = self.sb('craw', [P, DC], F32)
        sig = self.sb('csig', [P, DC], F32)
        sc.dma('sp', 'const', craw[:], cT[:, :], writes=['craw'])
        sc.dma('sp', 'const', self.adab[:], ada_bT[:, :, :], writes=['adab'])
        sc.dma('sp', 'const', self.ng[:], norm_gT[:, :, :, :], writes=['ng'])
        sc.op('act', lambda e: e.activation(out=sig[:], in_=craw[:], func=AF.Sigmoid), reads=['craw'], writes=['csig'])
        sc.op('dve', lambda e: e.tensor_tensor(out=self.cond[:], in0=craw[:], in1=sig[:], op=ALU.mult),
              reads=['craw', 'csig'], writes=['cond'])
        st = contextlib.ExitStack()
        wsl = [self.sb('adaw%d' % i, [P, DC, 1024], F32, st) for i in range(2)]
        n = 0
        for i in range(4):
            pname, pt = self.bank()
            for k in range(6):
                slot = n % 2
                n += 1
                for c in range(DC):
                    sc.dma('sp', 'adaw%d' % slot, wsl[slot][:, c, :],
                           ada_w[i, c * P:(c + 1) * P, k * 1024:(k + 1) * 1024], writes=['adaw%d' % slot])
                fns = []
                for nn in range(8):
                    for c in range(DC):
                        fns.append(lambda e, pt=pt, slot=slot, nn=nn, c=c, k=k:
                                   e.matmul(pt[:, k * 8 + nn:k * 8 + nn + 1], lhsT=wsl[slot][:, c, nn * P:(nn + 1) * P],
                                            rhs=self.cond[:, c:c + 1], start=(c == 0), stop=(c == DC - 1)))
                sc.op('pe', fns, reads=['adaw%d' % slot, 'cond'], writes=[pname])
            sc.op('dve', lambda e, pt=pt, i=i: e.tensor_tensor(out=self.modT[:, i, :], in0=pt[:, 0:48], in1=self.adab[:, i, :], op=ALU.add),
                  reads=[pname, 'adab'], writes=[('modT', i)])
            for j, k in ((0, 1), (1, 4)):
                sc.op('dve', lambda e, i=i, j=j, k=k: e.scalar_tensor_tensor(
                    out=self.gs[:, i, j, :], in0=self.modT[:, i, k * 8:(k + 1) * 8], scalar=1.0, in1=self.ng[:, i, j, :],
                    op0=ALU.add, op1=ALU.mult), reads=[('modT', i), 'ng'], writes=[('modT', i)])
        sc.barrier()
        st.close()

    def modv(self, i, k, c):
        return self.modT[:, i, k * 8 + c:k * 8 + c + 1]

    def alloc_stream(self):
        self.xT_d = self.nc.dram_tensor('xT_scratch', [D, S], F32).ap()
        self.xh_d = self.nc.dram_tensor('xh_scratch', [D, S // 2], F32).ap()
        self.rd_mode = 'full'
        self.wr_mode = 'full'
        self.ntok = S
        self.sc.streams['sp'].append(lambda e: setattr(self, 'rv', e.partition_id() // 4))
        d_rankf = self.inp('rankf', [P, 1])
        self.rankf = self.sb('rankf', [P, 1], F32)
        self.sc.dma('sp', 'const', self.rankf[:], d_rankf[:, :], writes=['rankf'])
        self.xin = self.inp('x', [S, D])
        self.xblk = [self.sb('xblk%d' % i, [P, DC, 512], F32) for i in range(2)]
        self.sq = self.sb('sq', [P, 512], F32)
        self.rstd = self.sb('rstd', [P, 512], F32)
        self.tmp32 = [self.sb('tmp32_%d' % i, [P, 512], F32) for i in range(3)]
        self.xn = 0

    def xd(self, c, blk, write):
        mode = self.wr_mode if write else self.rd_mode
        if mode == 'full':
            return self.xT_d[c * P:(c + 1) * P, blk * 512:(blk + 1) * 512], ('xTd', c, blk)
        if mode == 'half':
            return self.xh_d[c * P:(c + 1) * P, blk * 512:(blk + 1) * 512], ('xh', c, blk)
        if mode == 'dynfull':
            return self.xs_d[c * P:(c + 1) * P, (blk + 1) * 512:(blk + 2) * 512], 'xs'
        raise ValueError(mode)

    def load_xT(self, blk, from_input):
        sc = self.sc
        slot = self.xn % 2
        self.xn += 1
        name = 'xblk%d' % slot
        t = self.xblk[slot]
        if from_input:
            for j in range(4):
                sc.dma('sp', 'xtok', self.xtok[:, j, :], self.xin[blk * 512 + j * P: blk * 512 + (j + 1) * P, :],
                       writes=[('xtok', j)])
            for c in range(DC):
                pname, pt = self.bank()
                fns = [lambda e, pt=pt, j=j, c=c: e.transpose(out=pt[:, j * P:(j + 1) * P], in_=self.xtok[:, j, c * P:(c + 1) * P],
                                                              identity=self.ident[:]) for j in range(4)]
                sc.op('pe', fns, reads=[('xtok', j) for j in range(4)] + ['ident'], writes=[pname])
                eng = 'act' if c % 2 == 0 else 'dve'
                if eng == 'act':
                    sc.op('act', lambda e, pt=pt, t=t, c=c: e.copy(out=t[:, c, :], in_=pt[:]), reads=[pname], writes=[(name, c)])
                else:
                    sc.op('dve', lambda e, pt=pt, t=t, c=c: e.tensor_copy(out=t[:, c, :], in_=pt[:]), reads=[pname], writes=[(name, c)])
        else:
            for c in range(DC):
                ap, tn = self.xd(c, blk, False)
                sc.dma('sp', name, t[:, c, :], ap, reads=[tn], writes=[(name, c)])
        return name, t

    def store_xT(self, blk, name, t, c):
        ap, tn = self.xd(c, blk, True)
        self.sc.dma('sp', 'st_' + name, ap, t[:, c, :], reads=[(name, c)], writes=[tn])

    def norm_mod(self, li, j, name, t, hT, hname, hoff, h32=None):
        sc = self.sc
        pname, pt = self.bank()
        for c in range(DC):
            sc.op('act', lambda e, c=c: e.activation(out=self.sq[:], in_=t[:, c, :], func=AF.Square), reads=[(name, c)], writes=['sq'])
            sc.op('pe', lambda e, c=c, pt=pt: e.matmul(pt[:], lhsT=self.ones[:], rhs=self.sq[:], start=(c == 0), stop=(c == DC - 1)),
                  reads=['sq', 'ones'], writes=[pname])
        sc.op('act', lambda e, pt=pt: e.activation(out=self.rstd[:], in_=pt[:], func=AF.Sqrt, scale=1.0 / D, bias=self.epsb[:]),
              reads=[pname, 'epsb'], writes=['rstd'])
        sc.op('dve', lambda e: e.reciprocal(out=self.rstd[:], in_=self.rstd[:]), reads=['rstd'], writes=['rstd'])
        kshift = 0 if j == 0 else 3
        for c in range(DC):
            tm = self.tmp32[c % 2]
            tn = 'tmp32_%d' % (c % 2)
            sc.op('dve', lambda e, c=c, tm=tm: e.tensor_tensor(out=tm[:], in0=t[:, c, :], in1=self.rstd[:], op=ALU.mult),
                  reads=[(name, c), 'rstd'], writes=[tn])
            if h32 is not None:
                sc.op('pool', lambda e, c=c, tm=tm: e.tensor_scalar(out=h32[:, c, :], in0=tm[:], scalar1=self.gs[:, li, j, c:c + 1],
                                                                   scalar2=self.modv(li, kshift, c), op0=ALU.mult, op1=ALU.add),
                      reads=[tn, ('modT', li)], writes=[('h32', c)])
            sc.op('dve', lambda e, c=c, tm=tm: e.tensor_scalar(out=hT[:, c, hoff:hoff + 512], in0=tm[:], scalar1=self.gs[:, li, j, c:c + 1],
                                                              scalar2=self.modv(li, kshift, c), op0=ALU.mult, op1=ALU.add),
                  reads=[tn, ('modT', li)], writes=[(hname, c, hoff)])

    def load_w_cast(self, chan, dst, src, rows_split, writes):
        n = dst.shape[-1]
        step = 2048
        for a in range(0, n, step):
            b = min(n, a + step)
            self.sc.dma('pool', chan, dst[:, a:b], src[:, a:b], writes=writes)

    def conv_setup(self):
        self.d_conv_w_in = self.inp('conv_w_in', [2, D, 3 * D])
        self.d_conv_w_out = self.inp('conv_w_out', [2, D, D])
        d_cw = self.inp('conv_wT', [P, 2, 3, DC])
        self.cwT = self.sb('cwT', [P, 2, 3, DC], F32)
        self.sc.dma('sp', 'const', self.cwT[:], d_cw[:, :, :, :], writes=['cwT'])

    def conv_mixer(self, li, j, first):
        sc = self.sc
        st = contextlib.ExitStack()
        nc = self.nc
        win = self.sb('cw_in', [P, DC, 3 * D], BF16, st)
        wout = self.sb('cw_out', [P, DC, D], BF16, st)
        hT = self.sb('hTc', [P, DC, 512], BF16, st)
        bT = self.sb('bTc', [P, DC, 512], F32, st)
        uT = self.sb('uTc', [P, DC, 514], F32, st)
        zT = self.sb('zTc', [P, DC, 512], BF16, st)
        if first:
            self.xtok = self.sb('xtok', [P, 4, D], F32, st)
        L = 'L%d' % li
        for c in range(DC):
            self.load_w_cast('cwin', win[:, c, :], self.d_conv_w_in[j, c * P:(c + 1) * P, :], None, writes=[(L, 'cwin')])
            self.load_w_cast('cwout', wout[:, c, :], self.d_conv_w_out[j, c * P:(c + 1) * P, :], None, writes=[(L, 'cwout')])
        sc.op('pool', lambda e: e.memset(uT[:, :, 0:2], 0.0), writes=[(L, 'uT', c) for c in range(DC)])
        split = (self.rd_mode == 'dynfull')
        for blk in ([-1] if split else []) + list(range(self.ntok // 512)):
            name, t = self.load_xT(blk, first)
            self.norm_mod(li, 0, name, t, hT, (L, 'hT'), 0)
            hreads = [((L, 'hT'), c, 0) for c in range(DC)] + [(L, 'cwin')]
            for c in range(DC):
                pts = []
                for kind in ((1, 2) if blk < 0 else (0, 1, 2)):
                    pname, pt = self.bank()
                    fns = [lambda e, pt=pt, k=k, kind=kind, c=c: e.matmul(
                        pt[:], lhsT=win[:, k, kind * D + c * P: kind * D + (c + 1) * P], rhs=hT[:, k, :],
                        start=(k == 0), stop=(k == DC - 1)) for k in range(DC)]
                    sc.op('pe', fns, reads=hreads, writes=[pname])
                    pts.append((pname, pt))
                if blk < 0:
                    (pcn, pc), (pvn, pv) = pts
                else:
                    (pbn, pb), (pcn, pc), (pvn, pv) = pts
                    sc.op('act', lambda e, pb=pb, c=c: e.activation(out=bT[:, c, :], in_=pb[:], func=AF.Copy), reads=[pbn], writes=[(L, 'bT', c)])
                tm = self.tmp32[2]
                sc.op('act', lambda e, pc=pc, tm=tm: e.activation(out=tm[:], in_=pc[:], func=AF.Copy), reads=[pcn], writes=['tmp32_2'])
                sc.op('dve', lambda e, pv=pv, tm=tm, c=c: e.tensor_tensor(out=uT[:, c, 2:514], in0=pv[:], in1=tm[:], op=ALU.mult),
                      reads=[pvn, 'tmp32_2'], writes=[(L, 'uT', c)])
            if blk < 0:
                for c in range(DC):
                    sc.op('dve', lambda e, c=c: e.tensor_scalar(out=uT[:, c, 0:2], in0=uT[:, c, 512:514], scalar1=self.rankf[:, 0:1], scalar2=None, op0=ALU.mult),
                          reads=[(L, 'uT', c), 'rankf'], writes=[(L, 'uT', c)])
                continue
            for c in range(DC):
                tm = self.tmp32[c % 2]
                tn = 'tmp32_%d' % (c % 2)
                sc.op('dve', lambda e, c=c, tm=tm: e.tensor_scalar(out=tm[:], in0=uT[:, c, 2:514], scalar1=self.cwT[:, j, 2, c:c + 1],
                                                                  scalar2=None, op0=ALU.mult), reads=[(L, 'uT', c), 'cwT'], writes=[tn])
                sc.op('dve', lambda e, c=c, tm=tm: e.scalar_tensor_tensor(out=tm[:], in0=uT[:, c, 1:513], scalar=self.cwT[:, j, 1, c:c + 1],
                                                                         in1=tm[:], op0=ALU.mult, op1=ALU.add),
                      reads=[(L, 'uT', c), tn], writes=[tn])
                sc.op('dve', lambda e, c=c, tm=tm: e.scalar_tensor_tensor(out=tm[:], in0=uT[:, c, 0:512], scalar=self.cwT[:, j, 0, c:c + 1],
                                                                         in1=tm[:], op0=ALU.mult, op1=ALU.add),
                      reads=[(L, 'uT', c), tn], writes=[tn])
                sc.op('dve', lambda e, c=c, tm=tm: e.tensor_tensor(out=zT[:, c, :], in0=tm[:], in1=bT[:, c, :], op=ALU.mult),
                      reads=[tn, (L, 'bT', c)], writes=[(L, 'zT', c)])
                sc.op('pool', lambda e, c=c: e.tensor_copy(out=uT[:, c, 0:2], in_=uT[:, c, 512:514]),
                      reads=[(L, 'uT', c)], writes=[(L, 'uT', c)])
            zreads = [(L, 'zT', c) for c in range(DC)] + [(L, 'cwout')]
            for c in range(DC):
                pname, pt = self.bank()
                fns = [lambda e, pt=pt, k=k, c=c: e.matmul(pt[:], lhsT=wout[:, k, c * P:(c + 1) * P], rhs=zT[:, k, :],
                                                           start=(k == 0), stop=(k == DC - 1)) for k in range(DC)]
                sc.op('pe', fns, reads=zreads, writes=[pname])
                sc.op('dve', lambda e, pt=pt, c=c, t=t: e.scalar_tensor_tensor(out=t[:, c, :], in0=pt[:], scalar=self.modv(li, 2, c),
                                                                              in1=t[:, c, :], op0=ALU.mult, op1=ALU.add),
                      reads=[pname, (name, c), ('modT', li)], writes=[(name, c)])
                self.store_xT(blk, name, t, c)
        sc.barrier()
        st.close()

    def ffn_setup(self):
        self.d_ffn_gu = self.inp('ffn_gu_r', [2, FC, P, DC * 256])
        self.d_ffn_dn = self.inp('ffn_dn_r', [2, DC, P, FC * P])
        self.d_moe_gu = self.inp('moe_gu_r', [2, NE, FC, P, DC * 256])
        self.d_moe_dn = self.inp('moe_dn_r', [2, NE, DC, P, FC * P])
        d_rw = self.inp('moe_rwT', [P, 2, DC, NE])
        d_rb = self.inp('moe_rbB', [P, 2, NE])
        d_sel = self.inp('sel8', [NE, NE, P])
        self.rw = self.sb('rw', [P, 2, DC, NE], F32)
        self.rb = self.sb('rb', [P, 2, NE], F32)
        self.sel = self.sb('sel', [NE, NE, P], F32)
        self.sc.dma('sp', 'const', self.rw[:], d_rw[:, :, :, :], writes=['rw'])
        self.sc.dma('sp', 'const', self.rb[:], d_rb[:, :, :], writes=['rb'])
        self.sc.dma('sp', 'const', self.sel[:], d_sel[:, :, :], writes=['sel'])

    def router(self, li, L, h32, half, gT, sm):
        sc = self.sc
        jl = li // 2
        lg, ex, gt, m8, sc1 = sm
        prn, pr = self.bank()
        h32r = [('h32', c) for c in range(DC)]
        for tt in range(4):
            fns = [lambda e, pr=pr, tt=tt, c=c: e.matmul(pr[:, tt * 8:(tt + 1) * 8], lhsT=h32[:, c, tt * P:(tt + 1) * P],
                                                        rhs=self.rw[:, jl, c, :], start=(c == 0), stop=(c == DC - 1))
                   for c in range(DC)]
            sc.op('pe', fns, reads=h32r + ['rw'], writes=[prn])
        ptn, ptT = self.bank()
        for tt in range(4):
            R = [(L, 'rt')]
            sc.op('dve', lambda e, tt=tt, pr=pr: e.tensor_tensor(out=lg[:, 0:8], in0=pr[:, tt * 8:(tt + 1) * 8], in1=self.rb[:, jl, :], op=ALU.add),
                  reads=[prn, 'rb'], writes=R)
            sc.op('dve', lambda e: e.tensor_reduce(out=sc1[:, 0:1], in_=lg[:, 0:8], axis=AX.X, op=ALU.max), reads=R, writes=R)
            sc.op('dve', lambda e: e.tensor_scalar(out=sc1[:, 0:1], in0=sc1[:, 0:1], scalar1=-1.0, scalar2=None, op0=ALU.mult), reads=R, writes=R)
            sc.op('act', lambda e: e.activation(out=ex[:, 0:8], in_=lg[:, 0:8], func=AF.Exp, bias=sc1[:, 0:1], scale=1.0), reads=R, writes=R)
            sc.op('dve', lambda e: e.max(out=m8[:, 0:8], in_=ex[:, 0:8]), reads=R, writes=R)
            sc.op('dve', lambda e: e.tensor_tensor(out=sc1[:, 1:2], in0=m8[:, 0:1], in1=m8[:, 1:2], op=ALU.add), reads=R, writes=R)
            sc.op('dve', lambda e: e.reciprocal(out=sc1[:, 1:2], in_=sc1[:, 1:2]), reads=R, writes=R)
            sc.op('dve', lambda e: e.tensor_scalar(out=gt[:, 0:8], in0=ex[:, 0:8], scalar1=m8[:, 1:2], scalar2=None, op0=ALU.is_ge), reads=R, writes=R)
            sc.op('dve', lambda e: e.tensor_tensor(out=gt[:, 0:8], in0=gt[:, 0:8], in1=ex[:, 0:8], op=ALU.mult), reads=R, writes=R)
            sc.op('dve', lambda e: e.tensor_scalar(out=gt[:, 0:8], in0=gt[:, 0:8], scalar1=sc1[:, 1:2], scalar2=None, op0=ALU.mult), reads=R, writes=R)
            sc.op('pe', lambda e, tt=tt, ptT=ptT: e.transpose(out=ptT[0:8, tt * P:(tt + 1) * P], in_=gt[:, 0:8], identity=self.ident[:]),
                  reads=R + ['ident'], writes=[ptn])
        sc.op('act', lambda e, ptT=ptT, half=half: e.activation(out=gT[0:8, half * 512:(half + 1) * 512], in_=ptT[0:8, :], func=AF.Copy),
              reads=[ptn], writes=[(L, 'gT', half)])

    def ffn(self, li, moe):
        sc = self.sc
        st = contextlib.ExitStack()
        L = 'F%d' % li
        TB = 1024
        hT = self.sb('hTf', [P, DC, TB], BF16, st)
        actT = self.sb('actT', [P, FC, TB], BF16, st)
        wgu = [self.sb('wgu%d' % i, [P, DC * 256], BF16, st) for i in range(3)]
        wd = [self.sb('wd%d' % i, [P, FC * P], BF16, st) for i in range(3)]
        xs = [self.sb('xs%d' % i, [P, 512], F32, st) for i in range(2)]
        if moe:
            h32 = self.sb('h32', [P, DC, 512], F32, st)
            yacc = self.sb('yacc', [P, DC, TB], F32, st)
            Gb = [self.sb('Gb%d' % i, [P, TB], F32, st) for i in range(2)]
            gT = self.sb('gT', [NE, TB], F32, st)
            sm = [self.sb('rsm%d' % i, [P, 8], F32, st) for i in range(5)]
        jl = li // 2
        nw = 0
        nd = 0
        nx = 0
        ng = 0
        for sbi in range(self.ntok // TB):
            for half in range(2):
                blk = sbi * 2 + half
                name, t = self.load_xT(blk, False)
                self.norm_mod(li, 1, name, t, hT, (L, 'hT'), half * 512, h32=(h32 if moe else None))
                if moe:
                    self.router(li, L, h32, half, gT, sm)
            for ex in (range(NE) if moe else [None]):
                if moe:
                    gsl = ng % 2
                    ng += 1
                    gbn = (L, 'Gb', gsl)
                    for half in range(2):
                        pn, pt = self.bank()
                        sc.op('pe', lambda e, pt=pt, ex=ex, half=half: e.matmul(pt[:], lhsT=self.sel[0:8, ex, :], rhs=gT[0:8, half * 512:(half + 1) * 512],
                                                                               start=True, stop=True),
                              reads=['sel', (L, 'gT', half)], writes=[pn])
                        sc.op('act', lambda e, pt=pt, gsl=gsl, half=half: e.activation(out=Gb[gsl][:, half * 512:(half + 1) * 512], in_=pt[:], func=AF.Copy),
                              reads=[pn], writes=[(gbn, half)])
                for f in range(FC):
                    slot = nw % 3
                    nw += 1
                    wn = (L, 'wgu', slot)
                    src = self.d_moe_gu[jl, ex, f, :, :] if moe else self.d_ffn_gu[jl, f, :, :]
                    self.load_w_cast('wgu%d' % slot, wgu[slot][:, :], src, None, writes=[wn])
                    for half in range(2):
                        hreads = [((L, 'hT'), c, half * 512) for c in range(DC)] + [wn]
                        pgn, pg = self.bank()
                        pun, pu = self.bank()
                        for (pn, pt, off) in ((pgn, pg, 0), (pun, pu, 128)):
                            fns = [lambda e, pt=pt, k=k, off=off, slot=slot, half=half: e.matmul(
                                pt[:], lhsT=wgu[slot][:, k * 256 + off: k * 256 + off + 128], rhs=hT[:, k, half * 512:(half + 1) * 512],
                                start=(k == 0), stop=(k == DC - 1)) for k in range(DC)]
                            sc.op('pe', fns, reads=hreads, writes=[pn])
                        tm = self.tmp32[2]
                        sc.op('act', lambda e, pg=pg, tm=tm: e.activation(out=tm[:], in_=pg[:], func=AF.Silu), reads=[pgn], writes=['tmp32_2'])
                        sc.op('dve', lambda e, pu=pu, tm=tm, f=f, half=half: e.tensor_tensor(
                            out=actT[:, f, half * 512:(half + 1) * 512], in0=pu[:], in1=tm[:], op=ALU.mult),
                            reads=[pun, 'tmp32_2'], writes=[(L, 'act', f, half)])
                for c in range(DC):
                    slot = nd % 3
                    nd += 1
                    wn = (L, 'wd', slot)
                    src = self.d_moe_dn[jl, ex, c, :, :] if moe else self.d_ffn_dn[jl, c, :, :]
                    self.load_w_cast('wd%d' % slot, wd[slot][:, :], src, None, writes=[wn])
                    for half in range(2):
                        blk = sbi * 2 + half
                        areads = [(L, 'act', f, half) for f in range(FC)] + [wn]
                        pn, pt = self.bank()
                        fns = [lambda e, pt=pt, f=f, slot=slot, half=half: e.matmul(
                            pt[:], lhsT=wd[slot][:, f * P:(f + 1) * P], rhs=actT[:, f, half * 512:(half + 1) * 512],
                            start=(f == 0), stop=(f == FC - 1)) for f in range(FC)]
                        sc.op('pe', fns, reads=areads, writes=[pn])
                        if not moe:
                            self.resid(li, 5, xs, nx, c, blk, pn, pt, None, None)
                            nx += 1
                        else:
                            yn = (L, 'yacc', c, half)
                            ysl = yacc[:, c, half * 512:(half + 1) * 512]
                            gsl_ap = Gb[gsl][:, half * 512:(half + 1) * 512]
                            if ex == 0:
                                sc.op('dve', lambda e, pt=pt, ysl=ysl, g=gsl_ap: e.tensor_tensor(out=ysl, in0=pt[:], in1=g, op=ALU.mult),
                                      reads=[pn, (gbn, half)], writes=[yn])
                            else:
                                tm = self.tmp32[c % 2]
                                tn = 'tmp32_%d' % (c % 2)
                                sc.op('dve', lambda e, pt=pt, tm=tm, g=gsl_ap: e.tensor_tensor(out=tm[:], in0=pt[:], in1=g, op=ALU.mult),
                                      reads=[pn, (gbn, half)], writes=[tn])
                                sc.op('dve', lambda e, tm=tm, ysl=ysl: e.tensor_tensor(out=ysl, in0=ysl, in1=tm[:], op=ALU.add),
                                      reads=[tn, yn], writes=[yn])
            if moe:
                for c in range(DC):
                    for half in range(2):
                        blk = sbi * 2 + half
                        self.resid(li, 5, xs, nx, c, blk, (L, 'yacc', c, half), None, yacc[:, c, half * 512:(half + 1) * 512], None)
                        nx += 1
        sc.barrier()
        st.close()

    def resid(self, li, k, xs, nx, c, blk, srcname, pt, src_ap, _):
        sc = self.sc
        xsl = nx % 2
        xn = 'xs%d' % xsl
        xt = xs[xsl]
        src = pt[:] if pt is not None else src_ap
        rap, rtn = self.xd(c, blk, False)
        wap, wtn = self.xd(c, blk, True)
        sc.dma('sp', xn, xt[:], rap, reads=[rtn], writes=[xn])
        sc.op('dve', lambda e, src=src, xt=xt, c=c: e.scalar_tensor_tensor(
            out=xt[:], in0=src, scalar=self.modv(li, k, c), in1=xt[:], op0=ALU.mult, op1=ALU.add),
            reads=[srcname, xn, ('modT', li)], writes=[xn])
        sc.dma('sp', 'st_' + xn, wap, xt[:], reads=[xn], writes=[wtn])

    def attn_setup(self):
        self.d_awq = self.inp('attn_wq_perm', [D, 1024])
        self.d_awk = self.inp('attn_wk', [D, 256])
        self.d_awv = self.inp('attn_wv', [D, 256])
        self.d_awqi = self.inp('attn_wqi', [D, 512])
        self.d_awki2 = self.inp('attn_wki2', [D, 128])
        self.d_awwi = self.inp('attn_wwi', [D, 8])
        self.d_awout = self.inp('attn_wout_r', [64, 16, D])
        self.d_gainT = self.inp('attn_gainT', [P, 2])
        self.d_biasT = self.inp('attn_biasT', [P, 32, P])
        self.d_b31 = self.inp('attn_b31B', [P, 16])
        self.d_cmask = self.inp('cmask', [P, P])
        self.d_bones = self.inp('blockones', [P, P])
        self.d_selrow = self.inp('selrow', [65, 64])

    def head_norm(self, pn, pt, gain_ap, out_ap, tagw):
        sc = self.sc
        qs = self.tmp32[2]
        sc.op('act', lambda e, pt=pt, qs=qs: e.activation(out=qs[:], in_=pt[:], func=AF.Copy), reads=[pn], writes=['tmp32_2'])
        sc.op('act', lambda e, qs=qs: e.activation(out=self.sq[:], in_=qs[:], func=AF.Square), reads=['tmp32_2'], writes=['sq'])
        p2n, p2 = self.bank()
        sc.op('pe', lambda e, p2=p2: e.matmul(p2[:], lhsT=self.bones[:], rhs=self.sq[:], start=True, stop=True), reads=['sq', 'bones'], writes=[p2n])
        sc.op('act', lambda e, p2=p2: e.activation(out=self.rstd[:], in_=p2[:], func=AF.Sqrt, scale=1.0 / 64, bias=self.epsb[:]),
              reads=[p2n, 'epsb'], writes=['rstd'])
        sc.op('dve', lambda e: e.reciprocal(out=self.rstd[:], in_=self.rstd[:]), reads=['rstd'], writes=['rstd'])
        sc.op('dve', lambda e, qs=qs, g=gain_ap, o=out_ap: e.scalar_tensor_tensor(out=o, in0=qs[:], scalar=g, in1=self.rstd[:], op0=ALU.mult, op1=ALU.mult),
              reads=['tmp32_2', 'rstd', 'again'], writes=tagw)

    def attn_mixer(self, li):
        sc = self.sc
        st = contextlib.ExitStack()
        L = 'A'
        NI = 24
        KT = self.sb('KT', [P, 2, S], BF16, st)
        VA = self.sb('VA', [P, 32, 4, 65], BF16, st)
        KI = self.sb('KI', [P, S], BF16, st)
        hT = self.sb('hTa', [P, DC, 512], BF16, st)
        wA = self.sb('wA', [P, DC * 1024], BF16, st)
        QT = self.sb('QT', [P, 8, 512], BF16, st)
        QI = self.sb('QI', [P, 4, 512], BF16, st)
        WI = self.sb('WI', [P, 4, 8], F32, st)
        score = self.sb('score', [P, S], F32, st)
        nmq = self.sb('nmq', [P, S], BF16, st)
        nmT = [self.sb('nmT%d' % i, [P, 32, P], BF16, st) for i in range(2)]
        pexp = [self.sb('pexp%d' % i, [P, 512], BF16, st) for i in range(3)]
        OTn = self.sb('OTn', [64, 16, 512], BF16, st)
        osb = self.sb('osb', [65, 512], F32, st)
        rbc = self.sb('rbc', [64, 512], F32, st)
        Rt = self.tmp32[0:2]
        biasS = self.sb('biasS', [P, 32, P], BF16, st)
        self.bones = self.sb('bones', [P, P], F32, st)
        identb = self.sb('identb', [P, P], BF16, st)
        cmask = self.sb('cmaskS', [P, P], F32, st)
        selrow = self.sb('selrowS', [65, 64], F32, st)
        again = self.sb('again', [P, 2], F32, st)
        b31 = self.sb('b31', [P, 16], F32, st)
        bs = [self.sb('bsm%d' % i, [P, 1], F32, st) for i in range(6)]
        sc.dma('sp', 'const', self.bones[:], self.d_bones[:, :], writes=['bones'])
        sc.dma('sp', 'const', cmask[:], self.d_cmask[:, :], writes=['cmask'])
        sc.dma('sp', 'const', selrow[:], self.d_selrow[:, :], writes=['selrow'])
        sc.dma('sp', 'const', again[:], self.d_gainT[:, :], writes=['again'])
        sc.dma('sp', 'const', b31[:], self.d_b31[:, :], writes=['b31'])
        sc.op('dve', lambda e: e.tensor_copy(out=identb[:], in_=self.ident[:]), reads=['ident'], writes=['identb'])
        sc.op('dve', lambda e: e.tensor_scalar(out=again[:, 0:1], in0=again[:, 0:1], scalar1=0.125, scalar2=None, op0=ALU.mult),
              reads=['again'], writes=['again'])
        sc.op('pool', lambda e: e.memset(VA[:, :, :, 64:65], 1.0), writes=[(L, 'VAones')])
        for half in range(4):
            sc.dma('sp', 'bstage', score[:, 0:1024].rearrange('p (a b) -> p a b', b=P), self.d_biasT[:, half * 8:(half + 1) * 8, :],
                   writes=[(L, 'bstage')])
            for i in range(8):
                kh = half * 8 + i
                h = kh % 16
                sc.op('dve', lambda e, i=i, kh=kh, h=h: e.tensor_scalar(out=biasS[:, kh, :], in0=score[:, i * P:(i + 1) * P], scalar1=b31[:, h:h + 1],
                                                                      scalar2=None, op0=ALU.subtract), reads=[(L, 'bstage'), 'b31'], writes=[(L, 'biasS')])
        wk = wA[:, 0:DC * 256].rearrange('p (c n) -> p c n', c=DC)
        wv = wA[:, DC * 256:DC * 512].rearrange('p (c n) -> p c n', c=DC)
        wki = wA[:, DC * 512:DC * 640].rearrange('p (c n) -> p c n', c=DC)
        for c in range(DC):
            sc.dma('pool', 'wA', wk[:, c, :], self.d_awk[c * P:(c + 1) * P, :], writes=[(L, 'wA')])
            sc.dma('pool', 'wA', wv[:, c, :], self.d_awv[c * P:(c + 1) * P, :], writes=[(L, 'wA')])
            sc.dma('pool', 'wA', wki[:, c, :], self.d_awki2[c * P:(c + 1) * P, :], writes=[(L, 'wA')])
        for blk in range(NBLK):
            name, t = self.load_xT(blk, False)
            self.norm_mod(li, 0, name, t, hT, (L, 'hT'), 0)
            hreads = [((L, 'hT'), c, 0) for c in range(DC)] + [(L, 'wA')]
            for m in range(2):
                pn, pt = self.bank()
                fns = [lambda e, pt=pt, k=k, m=m: e.matmul(pt[:], lhsT=wk[:, k, m * P:(m + 1) * P], rhs=hT[:, k, :], start=(k == 0), stop=(k == DC - 1))
                       for k in range(DC)]
                sc.op('pe', fns, reads=hreads, writes=[pn])
                self.head_norm(pn, pt, again[:, 1:2], KT[:, m, blk * 512:(blk + 1) * 512], [(L, 'KT', m, blk)])
            pn, pt = self.bank()
            fns = [lambda e, pt=pt, k=k: e.matmul(pt[:], lhsT=wki[:, k, :], rhs=hT[:, k, :], start=(k == 0), stop=(k == DC - 1)) for k in range(DC)]
            sc.op('pe', fns, reads=hreads, writes=[pn])
            sc.op('act', lambda e, pt=pt, blk=blk: e.activation(out=KI[:, blk * 512:(blk + 1) * 512], in_=pt[:], func=AF.Copy), reads=[pn], writes=[(L, 'KI', blk)])
            for tt in range(4):
                pn, pt = self.bank()
                fns = [lambda e, pt=pt, k=k, tt=tt: e.matmul(pt[:, 0:256], lhsT=hT[:, k, tt * P:(tt + 1) * P], rhs=wv[:, k, :], start=(k == 0), stop=(k == DC - 1))
                       for k in range(DC)]
                sc.op('pe', fns, reads=hreads, writes=[pn])
                sc.op('dve', lambda e, pt=pt, blk=blk, tt=tt: e.tensor_copy(out=VA[:, blk * 4 + tt, :, 0:64], in_=pt[:, 0:256].rearrange('p (a b) -> p a b', b=64)),
                      reads=[pn], writes=[(L, 'VA', blk * 4 + tt)])
        if self.cfg.get('astage', 9) < 1:
            sc.barrier(); st.close(); return
        wq = wA[:, :].rearrange('p (c n) -> p c n', c=DC)
        wqi = wA[:, 0:DC * 512].rearrange('p (c n) -> p c n', c=DC)
        wwi = wA[:, DC * 512:DC * 520].rearrange('p (c n) -> p c n', c=DC)
        wout = wA[0:64, 0:16 * 512].rearrange('p (h n) -> p h n', h=16)
        self.abank = [6, 7]
        self.nab = 0
        self.rot = [0, 1, 2, 3, 4, 5]
        nmn = 0
        npx = 0
        nrt = 0
        for blk in range(self.cfg.get('anblk', NBLK)):
            for c in range(DC):
                sc.dma('pool', 'wA', wq[:, c, :], self.d_awq[c * P:(c + 1) * P, :], writes=[(L, 'wA')])
            name, t = self.load_xT(blk, False)
            self.norm_mod(li, 0, name, t, hT, (L, 'hT'), 0)
            hreads = [((L, 'hT'), c, 0) for c in range(DC)]
            for m in range(8):
                pn, pt = self.bank()
                fns = [lambda e, pt=pt, k=k, m=m: e.matmul(pt[:], lhsT=wq[:, k, m * P:(m + 1) * P], rhs=hT[:, k, :], start=(k == 0), stop=(k == DC - 1))
                       for k in range(DC)]
                sc.op('pe', fns, reads=hreads + [(L, 'wA')], writes=[pn])
                self.head_norm(pn, pt, again[:, 0:1], QT[:, m, :], [(L, 'QT', m)])
            for c in range(DC):
                sc.dma('pool', 'wA', wqi[:, c, :], self.d_awqi[c * P:(c + 1) * P, :], writes=[(L, 'wA')])
                sc.dma('pool', 'wA', wwi[:, c, :], self.d_awwi[c * P:(c + 1) * P, :], writes=[(L, 'wA')])
            for m in range(4):
                pn, pt = self.bank()
                fns = [lambda e, pt=pt, k=k, m=m: e.matmul(pt[:], lhsT=wqi[:, k, m * P:(m + 1) * P], rhs=hT[:, k, :], start=(k == 0), stop=(k == DC - 1))
                       for k in range(DC)]
                sc.op('pe', fns, reads=hreads + [(L, 'wA')], writes=[pn])
                sc.op('act', lambda e, pt=pt, m=m: e.activation(out=QI[:, m, :], in_=pt[:], func=AF.Copy), reads=[pn], writes=[(L, 'QI', m)])
            pn, pt = self.bank()
            for qb in range(4):
                fns = [lambda e, pt=pt, k=k, qb=qb: e.matmul(pt[:, qb * 8:(qb + 1) * 8], lhsT=hT[:, k, qb * P:(qb + 1) * P], rhs=wwi[:, k, :],
                                                            start=(k == 0), stop=(k == DC - 1)) for k in range(DC)]
                sc.op('pe', fns, reads=hreads + [(L, 'wA')], writes=[pn])
            sc.op('dve', lambda e, pt=pt: e.tensor_scalar(out=WI[:, :, :], in0=pt[:, 0:32].rearrange('p (a b) -> p a b', b=8), scalar1=0.04419417382415922,
                                                        scalar2=None, op0=ALU.mult), reads=[pn], writes=[(L, 'WI')])
            def idx_bis(qb):
                gq = blk * 4 + qb
                nk = gq + 1
                W = nk * P
                nonlocal nrt
                for k0 in range(0, W, 512):
                    n = min(512, W - k0)
                    for ih in range(8):
                        b0 = (ih % 2) * 64
                        pn, pt = self.bank()
                        sc.op('pe', lambda e, pt=pt, ih=ih, b0=b0, qb=qb, k0=k0, n=n: e.matmul(
                            pt[:, 0:n], lhsT=QI[b0:b0 + 64, ih // 2, qb * P:(qb + 1) * P], rhs=KI[b0:b0 + 64, k0:k0 + n], start=True, stop=True),
                            reads=[(L, 'QI', ih // 2)] + [(L, 'KI', kb) for kb in range(k0 // 512, (k0 + n + 511) // 512)], writes=[pn])
                        rs = nrt % 2
                        nrt += 1
                        rn = 'tmp32_%d' % rs
                        sc.op('act', lambda e, pt=pt, rs=rs, n=n: e.activation(out=Rt[rs][:, 0:n], in_=pt[:, 0:n], func=AF.Relu), reads=[pn], writes=[rn])
                        if ih == 0:
                            sc.op('dve', lambda e, rs=rs, n=n, k0=k0, qb=qb: e.tensor_scalar(out=score[:, k0:k0 + n], in0=Rt[rs][:, 0:n], scalar1=WI[:, qb, 0:1],
                                                                                      scalar2=None, op0=ALU.mult), reads=[rn, (L, 'WI')], writes=[(L, 'score')])
                        else:
                            sc.op('dve', lambda e, rs=rs, n=n, k0=k0, qb=qb, ih=ih: e.scalar_tensor_tensor(
                                out=score[:, k0:k0 + n], in0=Rt[rs][:, 0:n], scalar=WI[:, qb, ih:ih + 1], in1=score[:, k0:k0 + n], op0=ALU.mult, op1=ALU.add),
                                reads=[rn, (L, 'WI'), (L, 'score')], writes=[(L, 'score')])
                SR = [(L, 'score'), (L, 'bis')]
                mx, lo, w0, mid, cnt, ff = bs
                if nk >= 3:
                    sc.op('dve', lambda e, W=W: e.tensor_reduce(out=mx[:], in_=score[:, 0:W], axis=AX.X, op=ALU.max), reads=SR, writes=[(L, 'bis')])
                    sc.op('dve', lambda e, W=W: e.tensor_reduce(out=lo[:], in_=score[:, 0:W], axis=AX.X, op=ALU.min), reads=SR, writes=[(L, 'bis')])
                    sc.op('dve', lambda e: e.tensor_tensor(out=w0[:], in0=mx[:], in1=lo[:], op=ALU.subtract), reads=SR, writes=[(L, 'bis')])
                else:
                    sc.op('dve', lambda e: e.memset(lo[:], -1e29), reads=SR, writes=[(L, 'bis')])
                sc.op('dve', lambda e, W=W: e.tensor_tensor(out=score[:, W - P:W], in0=score[:, W - P:W], in1=cmask[:], op=ALU.add),
                      reads=SR + ['cmask'], writes=SR)
                if nk >= 3:
                    for it in range(NI):
                        cst = 2.0 ** (-(it + 1))
                        sc.op('dve', lambda e, cst=cst: e.scalar_tensor_tensor(out=mid[:], in0=w0[:], scalar=cst, in1=lo[:], op0=ALU.mult, op1=ALU.add),
                              reads=SR, writes=[(L, 'bis')])
                        sc.op('dve', lambda e, W=W: e.tensor_scalar(out=nmq[:, 0:W], in0=score[:, 0:W], scalar1=mid[:, 0:1], scalar2=0.0, op0=ALU.is_ge,
                                                                   op1=ALU.add, accum_out=cnt[:, 0:1]), reads=SR + [(L, 'nmq')], writes=[(L, 'bis'), (L, 'nmq')])
                        sc.op('dve', lambda e, cst=cst: e.tensor_scalar(out=ff[:], in0=cnt[:], scalar1=256.0, scalar2=cst, op0=ALU.is_ge, op1=ALU.mult),
                              reads=SR, writes=[(L, 'bis')])
                        sc.op('dve', lambda e: e.scalar_tensor_tensor(out=lo[:], in0=ff[:], scalar=w0[:, 0:1], in1=lo[:], op0=ALU.mult, op1=ALU.add),
                              reads=SR, writes=[(L, 'bis')])
                sc.op('dve', lambda e, W=W: e.tensor_scalar(out=nmq[:, 0:W], in0=score[:, 0:W], scalar1=lo[:, 0:1], scalar2=-30000.0, op0=ALU.is_lt, op1=ALU.mult),
                      reads=SR + [(L, 'nmq')], writes=[(L, 'nmq')])
            def tr(qb):
                gq = blk * 4 + qb
                nk = gq + 1
                W = nk * P
                ms = gq % 2
                mn_ = (L, 'nmT', ms)
                for j0 in range(0, nk, 8):
                    jn = min(8, nk - j0)
                    pn, pt = self.bank()
                    ptb = pt[:].bitcast(BF16)
                    fns = [lambda e, ptb=ptb, j=j, j0=j0: e.transpose(out=ptb[:, (j - j0) * P:(j - j0 + 1) * P], in_=nmq[:, j * P:(j + 1) * P], identity=identb[:])
                           for j in range(j0, j0 + jn)]
                    sc.op('pe', fns, reads=[(L, 'nmq'), 'identb'], writes=[pn])
                    sc.op('act', lambda e, ptb=ptb, ms=ms, j0=j0, jn=jn: e.activation(out=nmT[ms][:, j0:j0 + jn, :], in_=ptb[:, 0:jn * P].rearrange('p (a b) -> p a b', b=P),
                                                                                func=AF.Copy), reads=[pn], writes=[(mn_, j0)])
            def att(qb):
                gq = blk * 4 + qb
                nk = gq + 1
                W = nk * P
                nonlocal npx
                ms = gq % 2
                mn_ = (L, 'nmT', ms)
                for kvh in range(4 if self.cfg.get('astage', 9) >= 4 else 0):
                    ab = self.abank[self.nab % 2]
                    self.nab += 1
                    pon = ('ps', ab)
                    po = self.ps[ab]
                    b0 = (kvh % 2) * 64
                    stb = {}

                    def emit_st(j, kvh=kvh, b0=b0, stb=stb):
                        pn, pt = self.bank()
                        stb[j] = (pn, pt)
                        near = (nk - 1 - j) if (nk - 1 - j) < 2 else None
                        fns = []
                        p3 = pt[:, :].rearrange('p (g q) -> p g q', q=P)
                        fns.append(lambda e, p3=p3, j=j, b0=b0, kvh=kvh, qb=qb: e.matmul(
                            p3, lhsT=KT[b0:b0 + 64, kvh // 2, j * P:(j + 1) * P],
                            rhs=QT[b0:b0 + 64, (kvh // 2) * 4:(kvh // 2) * 4 + 4, qb * P:(qb + 1) * P], start=True, stop=False))
                        if near is not None:
                            fns.append(lambda e, p3=p3, kvh=kvh, near=near: e.matmul(
                                p3, lhsT=identb[:], rhs=biasS[:, near * 16 + kvh * 4:near * 16 + kvh * 4 + 4, :], start=False, stop=False))
                        fns.append(lambda e, p3=p3, j=j, ms=ms: e.matmul(
                            p3, lhsT=identb[:], rhs=nmT[ms][:, j, :].unsqueeze(1).to_broadcast([P, 4, P]), start=False, stop=True))
                        sc.op('pe', fns, reads=[(L, 'KT', kvh // 2, j // 4), (mn_, (j // 8) * 8), 'identb', (L, 'biasS')] +
                              [(L, 'QT', (kvh // 2) * 4 + g) for g in range(4)], writes=[pn])
                    emit_st(0)
                    for j in range(nk):
                        if j + 1 < nk:
                            emit_st(j + 1)
                        pn, pt = stb[j]
                        px = npx % 3
                        npx += 1
                        pxn = 'pexp%d' % px
                        sc.op('act', lambda e, pt=pt, px=px: e.activation(out=pexp[px][:], in_=pt[:], func=AF.Exp), reads=[pn], writes=[pxn])
                        sc.op('pe', lambda e, po=po, px=px, j=j, kvh=kvh, nk=nk: e.matmul(po[0:65, :], lhsT=VA[:, j, kvh, :], rhs=pexp[px][:],
                                                                                     start=(j == 0), stop=(j == nk - 1)),
                              reads=[pxn, (L, 'VA', j), (L, 'VAones')], writes=[pon])
                    sc.op('act', lambda e, po=po: e.activation(out=osb[:], in_=po[0:65, :], func=AF.Copy), reads=[pon], writes=['osb'])
                    sc.op('act', lambda e: e.activation(out=osb[64:65, :], in_=osb[64:65, :], func=AF.Ln), reads=['osb'], writes=['osb'])
                    sc.op('act', lambda e: e.activation(out=osb[64:65, :], in_=osb[64:65, :], func=AF.Exp, scale=-1.0), reads=['osb'], writes=['osb'])
                    pn, pt = self.bank()
                    sc.op('pe', lambda e, pt=pt: e.matmul(pt[0:64, :], lhsT=selrow[:], rhs=osb[:], start=True, stop=True), reads=['osb', 'selrow'], writes=[pn])
                    sc.op('act', lambda e, pt=pt: e.activation(out=rbc[0:64, :], in_=pt[0:64, :], func=AF.Copy), reads=[pn], writes=['rbc'])
                    sc.op('pool', lambda e, kvh=kvh, qb=qb: e.tensor_tensor(out=OTn[:, kvh * 4:(kvh + 1) * 4, qb * P:(qb + 1) * P],
                                                                        in0=osb[0:64, :].rearrange('p (a b) -> p a b', b=P),
                                                                        in1=rbc[0:64, :].rearrange('p (a b) -> p a b', b=P), op=ALU.mult),
                          reads=['osb', 'rbc'], writes=[(L, 'OTn', kvh, qb)])
            nq = 4 if self.cfg.get('astage', 9) >= 2 else 0
            if nq:
                idx_bis(0)
                tr(0)
            for qb in range(nq):
                if qb + 1 < nq:
                    idx_bis(qb + 1)
                att(qb)
                if qb + 1 < nq:
                    tr(qb + 1)
            if self.cfg.get('astage', 9) < 5:
                continue
            oreads = [(L, 'OTn', kvh, qb) for kvh in range(4) for qb in range(4)] + [(L, 'wA')]
            for c in range(DC):
                if c % 4 == 0:
                    for h in range(16):
                        sc.dma('pool', 'wA', wout[:, h, :], self.d_awout[:, h, (c // 4) * 512:(c // 4 + 1) * 512], writes=[(L, 'wA')])
                pn, pt = self.bank()
                fns = [lambda e, pt=pt, h=h, c=c: e.matmul(pt[:], lhsT=wout[:, h, (c % 4) * P:(c % 4 + 1) * P], rhs=OTn[:, h, :], start=(h == 0), stop=(h == 15))
                       for h in range(16)]
                sc.op('pe', fns, reads=oreads, writes=[pn])
                sc.op('dve', lambda e, pt=pt, c=c, t=t: e.scalar_tensor_tensor(out=t[:, c, :], in0=pt[:], scalar=self.modv(li, 2, c), in1=t[:, c, :],
                                                                              op0=ALU.mult, op1=ALU.add), reads=[pn, (name, c), ('modT', li)], writes=[(name, c)])
                self.store_xT(blk, name, t, c)
        self.rot = list(range(8))
        sc.barrier()
        st.close()

    def ssm_setup(self):
        self.d_lamT = self.inp('ssm_lamT', [P, 32, 2])
        self.d_lstepT = self.inp('ssm_lstepT', [P, 32])
        self.d_Bblk = self.inp('ssm_Bblk', [32, P, 256])
        self.d_Cblk = self.inp('ssm_Cblk', [32, P, 256])
        self.d_dT = self.inp('ssm_dT', [P, DC])
        self.d_glu = self.inp('ssm_glu_r', [DC, P, DC * 256])

    def ssm_mixer(self, li):
        import math
        sc = self.sc
        st = contextlib.ExitStack()
        L = 'S'
        LC = 128
        I32 = mybir.dt.int32
        Ec = self.sb('Ec', [P, 32, LC], F32, st)
        En = self.sb('En', [P, 32, LC], F32, st)
        Bb = self.sb('Bb', [P, 32, 256], BF16, st)
        Cb = self.sb('Cb', [P, 32, 256], BF16, st)
        sm = {n: self.sb('ss_' + n, [P, 32], F32, st) for n in
              ['lr', 'li', 'stp', 'th', 'rr', 'u', 'f', 'g', 'sn', 'cs', 'x', 'y', 'den', 'cr', 'ci', 'cL', 'nL', 'ire', 'iim', 'e1c', 'e1n', 't1', 't2']}
        ni = self.sb('ss_ni', [P, 32], I32, st)
        lam = self.sb('ss_lam', [P, 32, 2], F32, st)
        dT = self.sb('ss_dT', [P, DC], F32, st)
        cs1 = self.sb('ss_c1', [P, 4], F32, st)
        SM = [(L, 'sm')]

        def dv(fn, extra_r=(), extra_w=()):
            sc.op('dve', fn, reads=SM + list(extra_r), writes=SM + list(extra_w))

        def ac(fn):
            sc.op('act', fn, reads=SM, writes=SM)
        sc.dma('sp', 'const', lam[:], self.d_lamT[:, :, :], writes=SM)
        sc.dma('sp', 'const', sm['stp'][:], self.d_lstepT[:, :], writes=SM)
        sc.dma('sp', 'const', dT[:], self.d_dT[:, :], writes=[(L, 'dT')])
        for k in range(32):
            sc.dma('pool', 'Bb', Bb[:, k, :], self.d_Bblk[k, :, :], writes=[(L, 'Bb')])
        dv(lambda e: e.tensor_scalar(out=sm['lr'][:], in0=lam[:, :, 0], scalar1=-1e-4, scalar2=None, op0=ALU.min))
        dv(lambda e: e.tensor_copy(out=sm['li'][:], in_=lam[:, :, 1]))
        ac(lambda e: e.activation(out=sm['stp'][:], in_=sm['stp'][:], func=AF.Exp))
        dv(lambda e: e.tensor_tensor(out=sm['th'][:], in0=sm['li'][:], in1=sm['stp'][:], op=ALU.mult))
        dv(lambda e: e.tensor_tensor(out=sm['rr'][:], in0=sm['lr'][:], in1=sm['stp'][:], op=ALU.mult))
        ac(lambda e: e.activation(out=sm['rr'][:], in_=sm['rr'][:], func=AF.Exp))

        def sincos(dst, off):
            dv(lambda e: e.tensor_scalar(out=sm['u'][:], in0=sm['th'][:], scalar1=1.0 / (2 * math.pi), scalar2=off, op0=ALU.mult, op1=ALU.add))
            dv(lambda e: e.tensor_copy(out=ni[:], in_=sm['u'][:]))
            dv(lambda e: e.tensor_copy(out=sm['f'][:], in_=ni[:]))
            dv(lambda e: e.tensor_tensor(out=sm['f'][:], in0=sm['u'][:], in1=sm['f'][:], op=ALU.subtract))
            dv(lambda e: e.tensor_scalar(out=sm['g'][:], in0=sm['f'][:], scalar1=0.5, scalar2=None, op0=ALU.is_gt))
            dv(lambda e: e.tensor_tensor(out=sm['f'][:], in0=sm['f'][:], in1=sm['g'][:], op=ALU.subtract))
            dv(lambda e: e.tensor_scalar(out=sm['g'][:], in0=sm['f'][:], scalar1=-0.5, scalar2=None, op0=ALU.is_lt))
            dv(lambda e: e.tensor_tensor(out=sm['f'][:], in0=sm['f'][:], in1=sm['g'][:], op=ALU.add))
            ac(lambda e, dst=dst: e.activation(out=sm[dst][:], in_=sm['f'][:], func=AF.Sin, scale=-2 * math.pi))
        sincos('sn', 64.5)
        sincos('cs', 64.75)
        dv(lambda e: e.tensor_tensor(out=sm['x'][:], in0=sm['rr'][:], in1=sm['cs'][:], op=ALU.mult))
        dv(lambda e: e.tensor_scalar(out=sm['x'][:], in0=sm['x'][:], scalar1=-1.0, scalar2=None, op0=ALU.add))
        dv(lambda e: e.tensor_tensor(out=sm['y'][:], in0=sm['rr'][:], in1=sm['sn'][:], op=ALU.mult))
        dv(lambda e: e.tensor_tensor(out=sm['den'][:], in0=sm['lr'][:], in1=sm['lr'][:], op=ALU.mult))
        dv(lambda e: e.tensor_tensor(out=sm['t1'][:], in0=sm['li'][:], in1=sm['li'][:], op=ALU.mult))
        dv(lambda e: e.tensor_tensor(out=sm['den'][:], in0=sm['den'][:], in1=sm['t1'][:], op=ALU.add))
        dv(lambda e: e.reciprocal(out=sm['den'][:], in_=sm['den'][:]))
        dv(lambda e: e.tensor_tensor(out=sm['cr'][:], in0=sm['x'][:], in1=sm['lr'][:], op=ALU.mult))
        dv(lambda e: e.tensor_tensor(out=sm['t1'][:], in0=sm['y'][:], in1=sm['li'][:], op=ALU.mult))
        dv(lambda e: e.tensor_tensor(out=sm['cr'][:], in0=sm['cr'][:], in1=sm['t1'][:], op=ALU.add))
        dv(lambda e: e.tensor_tensor(out=sm['cr'][:], in0=sm['cr'][:], in1=sm['den'][:], op=ALU.mult))
        dv(lambda e: e.tensor_tensor(out=sm['ci'][:], in0=sm['y'][:], in1=sm['lr'][:], op=ALU.mult))
        dv(lambda e: e.tensor_tensor(out=sm['t1'][:], in0=sm['x'][:], in1=sm['li'][:], op=ALU.mult))
        dv(lambda e: e.tensor_tensor(out=sm['ci'][:], in0=sm['ci'][:], in1=sm['t1'][:], op=ALU.subtract))
        dv(lambda e: e.tensor_tensor(out=sm['ci'][:], in0=sm['ci'][:], in1=sm['den'][:], op=ALU.mult))
        st2 = contextlib.ExitStack()
        Tc = self.sb('Tc', [P, LC, 32], F32, st2)
        Tn = self.sb('Tn', [P, LC, 32], F32, st2)
        U1 = self.sb('U1', [P, LC // 2, 32], F32, st2)
        U2 = self.sb('U2', [P, LC // 2, 32], F32, st2)
        cst1 = self.sb('tp1s', [P, 512], F32, st2)
        cst2 = self.sb('tp2s', [P, 512], F32, st2)
        dv(lambda e: e.tensor_copy(out=sm['e1c'][:], in_=sm['cs'][:]))
        dv(lambda e: e.tensor_scalar(out=sm['e1n'][:], in0=sm['sn'][:], scalar1=-1.0, scalar2=None, op0=ALU.mult))
        dv(lambda e: e.memset(Tc[:, 0, :], 1.0), extra_w=[(L, 'T')])
        dv(lambda e: e.memset(Tn[:, 0, :], 0.0), extra_w=[(L, 'T')])
        TT = [(L, 'T')]
        n = 1
        while n < LC:
            ecb = sm['e1c'][:, :].unsqueeze(1).to_broadcast([P, n, 32])
            enb = sm['e1n'][:, :].unsqueeze(1).to_broadcast([P, n, 32])
            dv(lambda e, n=n, ecb=ecb: e.tensor_tensor(out=Tc[:, n:2 * n, :], in0=Tc[:, 0:n, :], in1=ecb, op=ALU.mult), TT, TT)
            dv(lambda e, n=n, enb=enb: e.tensor_tensor(out=U1[:, 0:n, :], in0=Tn[:, 0:n, :], in1=enb, op=ALU.mult), TT, TT)
            dv(lambda e, n=n: e.tensor_tensor(out=Tc[:, n:2 * n, :], in0=Tc[:, n:2 * n, :], in1=U1[:, 0:n, :], op=ALU.subtract), TT, TT)
            dv(lambda e, n=n, enb=enb: e.tensor_tensor(out=Tn[:, n:2 * n, :], in0=Tc[:, 0:n, :], in1=enb, op=ALU.mult), TT, TT)
            dv(lambda e, n=n, ecb=ecb: e.tensor_tensor(out=U2[:, 0:n, :], in0=Tn[:, 0:n, :], in1=ecb, op=ALU.mult), TT, TT)
            dv(lambda e, n=n: e.tensor_tensor(out=Tn[:, n:2 * n, :], in0=Tn[:, n:2 * n, :], in1=U2[:, 0:n, :], op=ALU.add), TT, TT)
            dv(lambda e: e.tensor_tensor(out=sm['t1'][:], in0=sm['e1c'][:], in1=sm['e1c'][:], op=ALU.mult))
            dv(lambda e: e.tensor_tensor(out=sm['t2'][:], in0=sm['e1n'][:], in1=sm['e1n'][:], op=ALU.mult))
            dv(lambda e: e.tensor_tensor(out=sm['e1n'][:], in0=sm['e1c'][:], in1=sm['e1n'][:], op=ALU.mult))
            dv(lambda e: e.tensor_scalar(out=sm['e1n'][:], in0=sm['e1n'][:], scalar1=2.0, scalar2=None, op0=ALU.mult))
            dv(lambda e: e.tensor_tensor(out=sm['e1c'][:], in0=sm['t1'][:], in1=sm['t2'][:], op=ALU.subtract))
            n *= 2
        dv(lambda e: e.tensor_copy(out=sm['cL'][:], in_=sm['e1c'][:]))
        dv(lambda e: e.tensor_copy(out=sm['nL'][:], in_=sm['e1n'][:]))
        dv(lambda e: e.memset(sm['ire'][:], 0.0))
        dv(lambda e: e.memset(sm['iim'][:], 0.0))
        dv(lambda e: e.tensor_copy(out=Ec[:, :, :], in_=Tc[:, :, :].rearrange('p t k -> p k t')), TT, [(L, 'E')])
        dv(lambda e: e.tensor_copy(out=En[:, :, :], in_=Tn[:, :, :].rearrange('p t k -> p k t')), TT, [(L, 'E')])
        for k in range(32):
            sc.dma('sp', 'cstage', cst1[:, 0:256], self.d_Cblk[k, :, :], writes=['cst1'])
            crk = sm['cr'][:, k:k + 1]
            cik = sm['ci'][:, k:k + 1]
            sc.op('dve', lambda e, cik=cik: e.tensor_scalar(out=cst2[:, 0:128], in0=cst1[:, 128:256], scalar1=cik, scalar2=None, op0=ALU.mult),
                  reads=['cst1'] + SM, writes=['cst2'])
            sc.op('dve', lambda e, k=k, crk=crk: e.scalar_tensor_tensor(out=Cb[:, k, 0:128], in0=cst1[:, 0:128], scalar=crk, in1=cst2[:, 0:128], op0=ALU.mult, op1=ALU.subtract),
                  reads=['cst1', 'cst2'] + SM, writes=[(L, 'Cb')])
            sc.op('dve', lambda e, crk=crk: e.tensor_scalar(out=cst2[:, 128:256], in0=cst1[:, 128:256], scalar1=crk, scalar2=None, op0=ALU.mult),
                  reads=['cst1'] + SM, writes=['cst2'])
            sc.op('dve', lambda e, k=k, cik=cik: e.scalar_tensor_tensor(out=Cb[:, k, 128:256], in0=cst1[:, 0:128], scalar=cik, in1=cst2[:, 128:256], op0=ALU.mult, op1=ALU.add),
                  reads=['cst1', 'cst2'] + SM, writes=[(L, 'Cb')])
        if self.cfg.get('sdebug'):
            dbg = self.nc.dram_tensor('dbg', [P, 7 * 32 + 512 + 512], F32, kind="ExternalOutput").ap()
            for i, nme in enumerate(['sn', 'cs', 'rr', 'cr', 'ci', 'cL', 'nL']):
                sc.dma('sp', 'const', dbg[:, i * 32:(i + 1) * 32], sm[nme][:], reads=SM, writes=[('dbg', i)])
            sc.dma('sp', 'const', dbg[:, 224:224 + 256], Ec[:, 0:2, :], reads=[(L, 'E')], writes=[('dbg', 10)])
            sc.dma('sp', 'const', dbg[:, 224 + 256:224 + 512], En[:, 0:2, :], reads=[(L, 'E')], writes=[('dbg', 11)])
            sc.dma('sp', 'const', dbg[:, 224 + 512:224 + 768], Cb[:, 0, :], reads=[(L, 'Cb')], writes=[('dbg', 12)], allow_dtype=True) if False else None
        sc.barrier()
        st2.close()
        hT = self.sb('hTs', [P, DC, 512], BF16, st)
        h32 = self.sb('h32s', [P, DC, 512], F32, st)
        GT = self.sb('GTs', [P, DC, 512], BF16, st)
        Sb = self.sb('Sb', [P, 4, 2, 512], BF16, st)
        wgl = [self.sb('wgl%d' % i, [P, DC * 256], BF16, st) for i in range(2)]
        bre2 = [self.sb('bre%d' % i, [P, 512], F32, st) for i in range(2)]
        bim2 = [self.sb('bim%d' % i, [P, 512], F32, st) for i in range(2)]
        wre2 = [self.sb('wre%d' % i, [P, 512], F32, st) for i in range(2)]
        wim2 = [self.sb('wim%d' % i, [P, 512], F32, st) for i in range(2)]
        xrs2 = [self.sb('xrs%d' % i, [P, 512], F32, st) for i in range(2)]
        xis2 = [self.sb('xis%d' % i, [P, 512], F32, st) for i in range(2)]
        tq = self.sb('tq', [P, 512], F32, st)
        tq2 = self.sb('tq2', [P, 512], F32, st)
        tp1 = self.sb('tp1', [P, 512], F32, st)
        tp2 = self.sb('tp2', [P, 512], F32, st)
        nw = 0
        for blk in range(self.cfg.get('snblk', NBLK)):
            name, t = self.load_xT(blk, False)
            self.norm_mod(li, 0, name, t, hT, (L, 'hT'), 0, h32=h32)
            for j in range(DC):
                def v3(ap):
                    return ap.rearrange('p (a b) -> p a b', b=LC)
                ER = [(L, 'E')]
                for pair in range(2):
                    tl = []
                    for kk in (2 * pair, 2 * pair + 1):
                        k = 4 * j + kk
                        sl = k % 2
                        T = dict(k=k, kk=kk, sl=sl, bre=bre2[sl], bim=bim2[sl], wre=wre2[sl], wim=wim2[sl], xrs=xrs2[sl], xis=xis2[sl],
                                 BRE='bre%d' % sl, BIM='bim%d' % sl, WRE='wre%d' % sl, WIM='wim%d' % sl, XRS='xrs%d' % sl, XIS='xis%d' % sl, CS=('cs1', sl),
                                 cb=Ec[:, k, :].unsqueeze(1).to_broadcast([P, 4, LC]), nb=En[:, k, :].unsqueeze(1).to_broadcast([P, 4, LC]),
                                 rb=sm['rr'][:, k:k + 1].to_broadcast([P, LC]), cLk=sm['cL'][:, k:k + 1], nLk=sm['nL'][:, k:k + 1],
                                 irek=sm['ire'][:, k:k + 1], iimk=sm['iim'][:, k:k + 1], CR=[(L, 'carry', k)],
                                 c0=cs1[:, 2 * sl:2 * sl + 1], c1=cs1[:, 2 * sl + 1:2 * sl + 2])
                        tl.append(T)
                        pxr_n, pxr = self.bank()
                        pxi_n, pxi = self.bank()
                        hr = [((L, 'hT'), j, 0), (L, 'Bb')]
                        sc.op('pe', lambda e, pxr=pxr, k=k, j=j: e.matmul(pxr[:], lhsT=Bb[:, k, 0:128], rhs=hT[:, j, :], start=True, stop=True), reads=hr, writes=[pxr_n])
                        sc.op('pe', lambda e, pxi=pxi, k=k, j=j: e.matmul(pxi[:], lhsT=Bb[:, k, 128:256], rhs=hT[:, j, :], start=True, stop=True), reads=hr, writes=[pxi_n])
                        sc.op('act', lambda e, pxr=pxr, T=T: e.activation(out=T['xrs'][:], in_=pxr[:], func=AF.Copy), reads=[pxr_n], writes=[T['XRS']])
                        sc.op('act', lambda e, pxi=pxi, T=T: e.activation(out=T['xis'][:], in_=pxi[:], func=AF.Copy), reads=[pxi_n], writes=[T['XIS']])
                    for T in tl:
                        sc.op('dve', lambda e, T=T: e.tensor_tensor(out=v3(T['bre'][:, :]), in0=v3(T['xrs'][:, :]), in1=T['cb'], op=ALU.mult), reads=[T['XRS']] + ER, writes=[T['BRE']])
                        sc.op('dve', lambda e, T=T: e.tensor_tensor(out=v3(tq[:, :]), in0=v3(T['xis'][:, :]), in1=T['nb'], op=ALU.mult), reads=[T['XIS']] + ER, writes=['tq'])
                        sc.op('dve', lambda e, T=T: e.tensor_tensor(out=T['bre'][:], in0=T['bre'][:], in1=tq[:], op=ALU.subtract), reads=[T['BRE'], 'tq'], writes=[T['BRE']])
                        sc.op('pool', lambda e, T=T: e.tensor_tensor(out=v3(T['bim'][:, :]), in0=v3(T['xis'][:, :]), in1=T['cb'], op=ALU.mult), reads=[T['XIS']] + ER, writes=[T['BIM']])
                        sc.op('pool', lambda e, T=T: e.tensor_tensor(out=v3(tq2[:, :]), in0=v3(T['xrs'][:, :]), in1=T['nb'], op=ALU.mult), reads=[T['XRS']] + ER, writes=['tq2'])
                        sc.op('pool', lambda e, T=T: e.tensor_tensor(out=T['bim'][:], in0=T['bim'][:], in1=tq2[:], op=ALU.add), reads=[T['BIM'], 'tq2'], writes=[T['BIM']])
                    for ch in range(4):
                        lo_, hi_ = ch * LC, (ch + 1) * LC
                        for T in tl:
                            sc.op('dve', lambda e, T=T, lo_=lo_, hi_=hi_: e.tensor_tensor_scan(out=T['wre'][:, lo_:hi_], data0=T['rb'], data1=T['bre'][:, lo_:hi_], initial=T['irek'],
                                                                                          op0=ALU.mult, op1=ALU.add), reads=[T['BRE']] + T['CR'] + SM, writes=[T['WRE']])
                        for T in tl:
                            sc.op('dve', lambda e, T=T, lo_=lo_, hi_=hi_: e.tensor_tensor_scan(out=T['wim'][:, lo_:hi_], data0=T['rb'], data1=T['bim'][:, lo_:hi_], initial=T['iimk'],
                                                                                          op0=ALU.mult, op1=ALU.add), reads=[T['BIM']] + T['CR'] + SM, writes=[T['WIM']])
                        for T in tl:
                            sc.op('dve', lambda e, T=T, hi_=hi_: e.tensor_tensor(out=T['c0'], in0=T['wim'][:, hi_ - 1:hi_], in1=T['nLk'], op=ALU.mult), reads=[T['WIM']] + SM, writes=[T['CS']])
                        for T in tl:
                            sc.op('dve', lambda e, T=T, hi_=hi_: e.scalar_tensor_tensor(out=T['irek'], in0=T['wre'][:, hi_ - 1:hi_], scalar=T['cLk'], in1=T['c0'], op0=ALU.mult, op1=ALU.add),
                                  reads=[T['WRE'], T['CS']] + SM, writes=T['CR'])
                        for T in tl:
                            sc.op('dve', lambda e, T=T, hi_=hi_: e.tensor_tensor(out=T['c1'], in0=T['wre'][:, hi_ - 1:hi_], in1=T['nLk'], op=ALU.mult), reads=[T['WRE']] + SM, writes=[T['CS']])
                        for T in tl:
                            sc.op('dve', lambda e, T=T, hi_=hi_: e.scalar_tensor_tensor(out=T['iimk'], in0=T['wim'][:, hi_ - 1:hi_], scalar=T['cLk'], in1=T['c1'], op0=ALU.mult, op1=ALU.subtract),
                                  reads=[T['WIM'], T['CS']] + SM, writes=T['CR'])
                    for T in tl:
                        kk = T['kk']
                        sc.op('pool', lambda e, T=T: e.tensor_tensor(out=v3(tp1[:, :]), in0=v3(T['wre'][:, :]), in1=T['cb'], op=ALU.mult), reads=[T['WRE']] + ER, writes=['tp1'])
                        sc.op('pool', lambda e, T=T: e.tensor_tensor(out=v3(tp2[:, :]), in0=v3(T['wim'][:, :]), in1=T['nb'], op=ALU.mult), reads=[T['WIM']] + ER, writes=['tp2'])
                        sc.op('pool', lambda e, kk=kk: e.tensor_tensor(out=Sb[:, kk, 0, :], in0=tp1[:], in1=tp2[:], op=ALU.add), reads=['tp1', 'tp2'], writes=[(L, 'Sb', kk)])
                        sc.op('pool', lambda e, T=T: e.tensor_tensor(out=v3(tp1[:, :]), in0=v3(T['wre'][:, :]), in1=T['nb'], op=ALU.mult), reads=[T['WRE']] + ER, writes=['tp1'])
                        sc.op('pool', lambda e, T=T: e.tensor_tensor(out=v3(tp2[:, :]), in0=v3(T['wim'][:, :]), in1=T['cb'], op=ALU.mult), reads=[T['WIM']] + ER, writes=['tp2'])
                        sc.op('pool', lambda e, kk=kk: e.tensor_tensor(out=Sb[:, kk, 1, :], in0=tp1[:], in1=tp2[:], op=ALU.subtract), reads=['tp1', 'tp2'], writes=[(L, 'Sb', kk)])
                pyn, py = self.bank()
                fns = []
                for kk in range(4):
                    k = 4 * j + kk
                    fns.append(lambda e, py=py, k=k, kk=kk: e.matmul(py[:], lhsT=Cb[:, k, 0:128], rhs=Sb[:, kk, 0, :], start=(kk == 0), stop=False))
                    fns.append(lambda e, py=py, k=k, kk=kk: e.matmul(py[:], lhsT=Cb[:, k, 128:256], rhs=Sb[:, kk, 1, :], start=False, stop=(kk == 3)))
                sc.op('pe', fns, reads=[(L, 'Sb', kk) for kk in range(4)] + [(L, 'Cb')], writes=[pyn])
                if self.cfg.get('sdebug') == 2 and blk == 0 and j == 0:
                    sc.op('act', lambda e, py=py: e.activation(out=bre[:], in_=py[:], func=AF.Copy), reads=[pyn, 'bre'], writes=['bre'])
                    sc.dma('sp', 'const', dbg2[:, 4, :], bre[:], reads=['bre'], writes=[('dbg2', 4)])
                z = self.tmp32[0]
                w_ = self.tmp32[1]
                sc.op('dve', lambda e, py=py, j=j, z=z: e.scalar_tensor_tensor(out=z[:], in0=h32[:, j, :], scalar=dT[:, j:j + 1], in1=py[:], op0=ALU.mult, op1=ALU.add),
                      reads=[pyn, ('h32', j), (L, 'dT')], writes=['tmp32_0'])
                if self.cfg.get('sdebug') and blk == 0 and j == 0:
                    dbg4 = self.nc.dram_tensor('dbg4', [P, 3, 512], F32, kind="ExternalOutput").ap()
                    sc.dma('sp', 'const', dbg4[:, 0, :], z[:], reads=['tmp32_0'], writes=[('dbg4', 0)])
                    sc.dma('pool', 'dbg4b', dbg4[:, 1, 0:256], Cb[:, 1, :], reads=[(L, 'Cb')], writes=[('dbg4', 1)])
                    sc.dma('pool', 'dbg4b', dbg4[:, 1, 256:512], Cb[:, 1, :], reads=[(L, 'Cb')], writes=[('dbg4', 3)])
                sc.op('act', lambda e, z=z, w_=w_: e.activation(out=w_[:], in_=z[:], func=AF.Square), reads=['tmp32_0'], writes=['tmp32_1'])
                sc.op('dve', lambda e, w_=w_: e.tensor_scalar(out=w_[:], in0=w_[:], scalar1=0.044715, scalar2=1.0, op0=ALU.mult, op1=ALU.add), reads=['tmp32_1'], writes=['tmp32_1'])
                sc.op('dve', lambda e, z=z, w_=w_: e.tensor_tensor(out=w_[:], in0=w_[:], in1=z[:], op=ALU.mult), reads=['tmp32_0', 'tmp32_1'], writes=['tmp32_1'])
                sc.op('act', lambda e, w_=w_: e.activation(out=w_[:], in_=w_[:], func=AF.Sigmoid, scale=1.5957691216057308), reads=['tmp32_1'], writes=['tmp32_1'])
                sc.op('dve', lambda e, z=z, w_=w_, j=j: e.tensor_tensor(out=GT[:, j, :], in0=z[:], in1=w_[:], op=ALU.mult), reads=['tmp32_0', 'tmp32_1'], writes=[(L, 'GT', j)])
            if self.cfg.get('sdebug') and blk == 0:
                sc.dma('pool', 'dbg4b', dbg4[:, 2, :], GT[:, 0, :], reads=[(L, 'GT', 0)], writes=[('dbg4', 2)])
            greads = [(L, 'GT', j) for j in range(DC)]
            for c in range(DC):
                slot = nw % 2
                nw += 1
                wn = (L, 'wgl', slot)
                self.load_w_cast('wgl%d' % slot, wgl[slot][:, :], self.d_glu[c, :, :], None, writes=[wn])
                pln, pl = self.bank()
                pgn, pg = self.bank()
                for (pn, pt, off) in ((pln, pl, 0), (pgn, pg, 128)):
                    fns = [lambda e, pt=pt, kq=kq, off=off, slot=slot: e.matmul(pt[:], lhsT=wgl[slot][:, kq * 256 + off: kq * 256 + off + 128], rhs=GT[:, kq, :],
                                                                              start=(kq == 0), stop=(kq == DC - 1)) for kq in range(DC)]
                    sc.op('pe', fns, reads=greads + [wn], writes=[pn])
                tm = self.tmp32[2]
                sc.op('act', lambda e, pg=pg, tm=tm: e.activation(out=tm[:], in_=pg[:], func=AF.Sigmoid), reads=[pgn], writes=['tmp32_2'])
                sc.op('dve', lambda e, pl=pl, tm=tm: e.tensor_tensor(out=tm[:], in0=pl[:], in1=tm[:], op=ALU.mult), reads=[pln, 'tmp32_2'], writes=['tmp32_2'])
                sc.op('dve', lambda e, tm=tm, c=c, t=t: e.scalar_tensor_tensor(out=t[:, c, :], in0=tm[:], scalar=self.modv(li, 2, c), in1=t[:, c, :], op0=ALU.mult, op1=ALU.add),
                      reads=['tmp32_2', (name, c), ('modT', li)], writes=[(name, c)])
                self.store_xT(blk, name, t, c)
        sc.barrier()
        st.close()

    def epilogue(self):
        sc = self.sc
        self.out = self.nc.dram_tensor('out', [self.ntok, D], F32, kind="ExternalOutput").ap()
        otok = [self.sb('otok%d' % i, [P, D], F32) for i in range(2)]
        n = 0
        for blk in range(self.ntok // 512):
            name, t = self.load_xT(blk, False)
            for j in range(4):
                slot = n % 2
                n += 1
                on = 'otok%d' % slot
                for hh in range(2):
                    pname, pt = self.bank()
                    fns = [lambda e, pt=pt, j=j, c=c, hh=hh, t=t: e.transpose(out=pt[:, (c - hh * 4) * P:(c - hh * 4 + 1) * P],
                                                                      in_=t[:, c, j * P:(j + 1) * P], identity=self.ident[:])
                           for c in range(hh * 4, hh * 4 + 4)]
                    sc.op('pe', fns, reads=[(name, c) for c in range(DC)] + ['ident'], writes=[pname])
                    if hh == 0:
                        sc.op('act', lambda e, pt=pt, slot=slot: e.activation(out=otok[slot][:, 0:512], in_=pt[:], func=AF.Copy),
                              reads=[pname], writes=[(on, 0)])
                    else:
                        sc.op('dve', lambda e, pt=pt, slot=slot: e.tensor_copy(out=otok[slot][:, 512:1024], in_=pt[:]),
                              reads=[pname], writes=[(on, 1)])
                sc.dma('sp', 'st_' + on, self.out[blk * 512 + j * P: blk * 512 + (j + 1) * P, :], otok[slot][:],
                       reads=[(on, 0), (on, 1)], writes=[('out', blk, j)])

    def build(self):
        cfg = self.cfg
        self.setup_common()
        self.epsb = self.sb('epsb', [P, 1], F32)
        self.sc.op('dve', lambda e: e.memset(self.epsb[:], EPS), writes=['epsb'])
        self.build_mod()
        self.alloc_stream()
        self.conv_setup()
        self.ffn_setup()
        self.attn_setup()
        self.ssm_setup()
        steps = cfg.get('steps', ['m0', 'f0', 'm1', 'f1', 'm2', 'f2', 'm3', 'f3'])
        first = True
        if steps[0][0] != 'm' or int(steps[0][1]) % 3 != 0:
            st = contextlib.ExitStack()
            self.xtok = self.sb('xtok', [P, 4, D], F32, st)
            for blk in range(NBLK):
                name, t = self.load_xT(blk, True)
                for c in range(DC):
                    self.store_xT(blk, name, t, c)
            self.sc.barrier()
            st.close()
            first = False
        tail_split = cfg.get('tail_split', False)
        for s in steps:
            li = int(s[1])
            if tail_split and s == 'm3':
                self.sc.barrier()
                self.xs_d = self.nc.dram_tensor('xs_scratch', [D, S // 2 + 512], F32).ap()
                self.sc.dma('sp', 'xstage', self.xs_d[:, 512:512 + S // 2], (lambda: self.xT_d[:, bass.ds(self.rv * 2048, 2048)]), writes=['xs'])
                self.sc.dma('sp', 'xstage', self.xs_d[:, 0:512], (lambda: self.xT_d[:, bass.ds(self.rv * 1536, 512)]), writes=['xs'])
                self.rd_mode, self.wr_mode, self.ntok = 'dynfull', 'half', S // 2
            if tail_split and s == 'f3':
                self.rd_mode, self.wr_mode, self.ntok = 'half', 'half', S // 2
            if s[0] == 'm':
                if li % 3 == 0:
                    self.conv_mixer(li, li // 3, first)
                elif li % 3 == 1:
                    self.attn_mixer(li)
                else:
                    self.ssm_mixer(li)
                first = False
            else:
                self.ffn(li, li % 2 == 1)
        self.epilogue()
        self.sc.finish('sp')
        self.sc.emit()
        return self.nc


def host_layout(inputs, b):
    f = np.float32
    m = {}
    m['ident'] = np.eye(P, dtype=f)
    m['x'] = np.ascontiguousarray(inputs['x'][b])
    m['cT'] = np.ascontiguousarray(inputs['c'][b].reshape(DC, P).T)
    m['ada_w'] = inputs['ada_w']
    m['ada_bT'] = np.ascontiguousarray(inputs['ada_b'].reshape(4, 48, P).transpose(2, 0, 1))
    m['norm_gT'] = np.ascontiguousarray(inputs['norm_g'].reshape(4, 2, DC, P).transpose(3, 0, 1, 2))
    m['conv_w_in'] = inputs['conv_w_in']
    m['conv_w_out'] = inputs['conv_w_out']
    m['conv_wT'] = np.ascontiguousarray(inputs['conv_w'].reshape(2, 3, DC, P).transpose(3, 0, 1, 2))
    m['rankf'] = np.zeros((P, 1), dtype=f)
    return m


def rel_bucket_np(dist):
    import math
    d = np.maximum(dist, 1).astype(np.float32)
    large = 16 + (np.log(d / np.float32(16)) / np.float32(math.log(128 / 16)) * np.float32(16)).astype(np.int32)
    large = np.minimum(large, 31)
    return np.where(dist < 16, dist, large)


def ssm_layout(inputs):
    f = np.float32
    m = {}
    lre = inputs['ssm_lambda_re'][0]
    lim = inputs['ssm_lambda_im'][0]
    lamT = np.empty((P, 32, 2), dtype=f)
    lst = np.empty((P, 32), dtype=f)
    Bb = np.zeros((32, P, 2, P), dtype=f)
    Cb = np.zeros((32, P, 2, P), dtype=f)
    bre = inputs['ssm_b_re'][0]
    bim = inputs['ssm_b_im'][0]
    cre = inputs['ssm_c_re'][0]
    cim = inputs['ssm_c_im'][0]
    for k in range(32):
        for gg in range(2):
            g = 2 * k + gg
            lamT[gg * 64:(gg + 1) * 64, k, 0] = lre[g]
            lamT[gg * 64:(gg + 1) * 64, k, 1] = lim[g]
            lst[gg * 64:(gg + 1) * 64, k] = inputs['ssm_log_step'][0, g]
            r0 = (g % 8) * 16
            Bb[k, r0:r0 + 16, 0, gg * 64:(gg + 1) * 64] = bre[g].T
            Bb[k, r0:r0 + 16, 1, gg * 64:(gg + 1) * 64] = bim[g].T
            Cb[k, gg * 64:(gg + 1) * 64, 0, r0:r0 + 16] = cre[g].T
            Cb[k, gg * 64:(gg + 1) * 64, 1, r0:r0 + 16] = cim[g].T
    m['ssm_lamT'] = lamT
    m['ssm_lstepT'] = lst
    m['ssm_Bblk'] = Bb.reshape(32, P, 256)
    m['ssm_Cblk'] = Cb.reshape(32, P, 256)
    m['ssm_dT'] = np.ascontiguousarray(inputs['ssm_d'][0].reshape(DC, P).T)
    w = inputs['ssm_w_glu'][0]
    lin = w[:, :D].reshape(DC, P, DC, P)
    gate = w[:, D:].reshape(DC, P, DC, P)
    r = np.empty((DC, P, DC, 2, P), dtype=f)
    r[:, :, :, 0, :] = lin.transpose(2, 1, 0, 3)
    r[:, :, :, 1, :] = gate.transpose(2, 1, 0, 3)
    m['ssm_glu_r'] = r.reshape(DC, P, DC * 256)
    return m


def attn_layout(inputs):
    f = np.float32
    m = {}
    w = inputs['attn_w_in'][0]
    wq = w[:, 0:1024].reshape(D, 16, 64)
    order = []
    for pair in range(2):
        for g in range(4):
            order += [(2 * pair) * 4 + g, (2 * pair + 1) * 4 + g]
    m['attn_wq_perm'] = np.ascontiguousarray(wq[:, order, :]).reshape(D, 1024)
    m['attn_wk'] = np.ascontiguousarray(w[:, 1024:1280])
    m['attn_wv'] = np.ascontiguousarray(w[:, 1280:1536])
    m['attn_wqi'] = np.ascontiguousarray(w[:, 1536:2048])
    ki = w[:, 2048:2112]
    m['attn_wki2'] = np.ascontiguousarray(np.concatenate([ki, ki], axis=1))
    m['attn_wwi'] = np.ascontiguousarray(w[:, 2112:2120])
    m['attn_wout_r'] = np.ascontiguousarray(inputs['attn_w_out'][0].reshape(16, 64, D).transpose(1, 0, 2))
    qg = inputs['attn_q_gain'][0]
    kg = inputs['attn_k_gain'][0]
    m['attn_gainT'] = np.ascontiguousarray(np.stack([np.tile(qg, 2), np.tile(kg, 2)], axis=1))
    rb = inputs['rel_bias']
    sl = np.arange(P)[:, None]
    tl = np.arange(P)[None, :]
    bt = np.empty((P, 32, P), dtype=f)
    for kind in range(2):
        dist = np.maximum(tl - sl + kind * P, 0)
        bk = rel_bucket_np(dist)
        for h in range(16):
            bt[:, kind * 16 + h, :] = rb[bk, h]
    m['attn_biasT'] = bt
    m['attn_b31B'] = np.ascontiguousarray(np.broadcast_to(rb[31][None, :], (P, 16)))
    cm = np.zeros((P, P), dtype=f)
    cm[np.arange(P)[None, :] > np.arange(P)[:, None]] = -1e30
    m['cmask'] = cm
    bo = np.zeros((P, P), dtype=f)
    bo[0:64, 0:64] = 1.0
    bo[64:128, 64:128] = 1.0
    m['blockones'] = bo
    sr = np.zeros((65, 64), dtype=f)
    sr[64, :] = 1.0
    m['selrow'] = sr
    return m


_shared_cache = {}


def shared_layout(inputs):
    f = np.float32
    m = {}
    gu = inputs['ffn_w_gu']
    g = gu[:, :, :DFF].reshape(2, DC, P, FC, P)
    u = gu[:, :, DFF:].reshape(2, DC, P, FC, P)
    gu_r = np.stack([g, u], axis=4)
    m['ffn_gu_r'] = np.ascontiguousarray(gu_r.transpose(0, 3, 2, 1, 4, 5)).reshape(2, FC, P, DC * 256)
    dn = inputs['ffn_w_down'].reshape(2, FC, P, DC, P)
    m['ffn_dn_r'] = np.ascontiguousarray(dn.transpose(0, 3, 2, 1, 4)).reshape(2, DC, P, FC * P)
    gu = inputs['moe_w_gu']
    g = gu[:, :, :, :DFF].reshape(2, NE, DC, P, FC, P)
    u = gu[:, :, :, DFF:].reshape(2, NE, DC, P, FC, P)
    r = np.empty((2, NE, FC, P, DC, 2, P), dtype=f)
    r[:, :, :, :, :, 0, :] = g.transpose(0, 1, 4, 3, 2, 5)
    r[:, :, :, :, :, 1, :] = u.transpose(0, 1, 4, 3, 2, 5)
    m['moe_gu_r'] = r.reshape(2, NE, FC, P, DC * 256)
    dn = inputs['moe_w_down'].reshape(2, NE, FC, P, DC, P)
    m['moe_dn_r'] = np.ascontiguousarray(dn.transpose(0, 1, 4, 3, 2, 5)).reshape(2, NE, DC, P, FC * P)
    m['moe_rwT'] = np.ascontiguousarray(inputs['moe_router_w'].reshape(2, DC, P, NE).transpose(2, 0, 1, 3))
    m['moe_rbB'] = np.ascontiguousarray(np.broadcast_to(inputs['moe_router_b'][None], (P, 2, NE)))
    m.update(attn_layout(inputs))
    m.update(ssm_layout(inputs))
    sel = np.zeros((NE, NE, P), dtype=f)
    for e in range(NE):
        sel[e, e, :] = 1.0
    m['sel8'] = sel
    return m


def kernel(**inputs):
    inputs = {k: np.asarray(v) for k, v in inputs.items()}
    b = Builder({'tail_split': True})
    nc = b.build()
    shared = shared_layout(inputs)
    in_maps = []
    for core in range(8):
        m = host_layout(inputs, core % 4)
        m['rankf'] = np.full((P, 1), float(core // 4), dtype=np.float32)
        m.update(shared)
        in_maps.append({k: m[k] for k in b.din})
    res = run_bass_kernel_spmd(nc, in_maps, core_ids=list(range(8)))
    out = np.stack([np.concatenate([res.results[i]['out'], res.results[i + 4]['out']], axis=0) for i in range(4)], axis=0)
    return out.astype(np.float32)
```

```python
import contextlib
from types import FunctionType
import numpy as np
import concourse.bass as bass
import concourse.mybir as mybir
from concourse.bass_utils import run_bass_kernel_spmd

F32 = mybir.dt.float32
BF16 = mybir.dt.bfloat16
AF = mybir.ActivationFunctionType
ALU = mybir.AluOpType
AX = mybir.AxisListType

S = 4096
D = 1024
DC = 8
P = 128
DFF = 2816
FC = 22
NE = 8
EPS = 1e-6
NBLK = S // 512

ENG = ['pe', 'act', 'dve', 'pool', 'sp']
SELF_SYNC = True


class Sched:
    def __init__(self, nc, stack):
        self.nc = nc
        self.stack = stack
        self.streams = {e: [] for e in ENG}
        self.sem = {e: stack.enter_context(nc.semaphore('s_' + e)) for e in ENG}
        self.count = {e: 0 for e in ENG}
        self.dsem = {}
        self.seen = {e: {} for e in ENG}
        self.hist = {}
        self.buf = {}
        self.ninst = 0

    def _semof(self, key):
        if isinstance(key, tuple):
            return self.dsem[key[1]][0]
        return self.sem[key]

    def _wait(self, eng, toks):
        best = {}
        for (k, v) in toks:
            if v > best.get(k, 0):
                best[k] = v
        seen = self.seen[eng]
        for k, v in best.items():
            if seen.get(k, 0) >= v:
                continue
            if k == eng and (eng == 'pe' or not SELF_SYNC):
                continue
            s = self._semof(k)
            self.streams[eng].append(lambda e, s=s, v=v: e.wait_ge(s, v))
            self.ninst += 1
            h = self.hist.get((k, v))
            if h:
                for kk, vv in h.items():
                    if vv > seen.get(kk, 0):
                        seen[kk] = vv
            if v > seen.get(k, 0):
                seen[k] = v

    def _deps(self, reads, writes):
        toks = []
        for b in reads:
            st = self.buf.get(b)
            if st and st[0]:
                toks.append(st[0])
        for b in writes:
            st = self.buf.get(b)
            if st:
                if st[0]:
                    toks.append(st[0])
                toks.extend(st[1].items())
        return toks

    def _record(self, tok, reads, writes):
        for b in reads:
            st = self.buf.setdefault(b, [None, {}])
            if tok[1] > st[1].get(tok[0], 0):
                st[1][tok[0]] = tok[1]
        for b in writes:
            self.buf[b] = [tok, {}]

    def op(self, eng, fns, reads=(), writes=()):
        if not isinstance(fns, (list, tuple)):
            fns = [fns]
        self._wait(eng, self._deps(reads, writes))
        self.count[eng] += 1
        tok = (eng, self.count[eng])
        sem = self.sem[eng]
        for f in fns[:-1]:
            self.streams[eng].append(f)
        last = fns[-1]
        self.streams[eng].append(lambda e, f=last, s=sem: f(e).then_inc(s, 1))
        self.ninst += len(fns)
        self.hist[tok] = dict(self.seen[eng])
        self._record(tok, reads, writes)
        return tok

    def dma(self, queue, chan, out, in_, reads=(), writes=(), **kw):
        if chan == 'const':
            self.nconst = getattr(self, 'nconst', 0) + 1
            chan = 'const%d' % self.nconst
        if chan not in self.dsem:
            self.dsem[chan] = [self.stack.enter_context(self.nc.semaphore('d_' + str(chan))), 0]
        self._wait(queue, self._deps(reads, writes))
        ds = self.dsem[chan]
        ds[1] += 16
        tok = (('dma', chan), ds[1])
        s = ds[0]
        self.streams[queue].append(lambda e, s=s, o=out, i=in_, kw=kw: e.dma_start(
            out=(o() if isinstance(o, FunctionType) else o), in_=(i() if isinstance(i, FunctionType) else i), **kw).then_inc(s, 16))
        self.ninst += 1
        self.hist[tok] = dict(self.seen[queue])
        self._record(tok, reads, writes)
        return tok

    def barrier(self):
        toks = []
        for b, st in self.buf.items():
            if st[0]:
                toks.append(st[0])
            toks.extend(st[1].items())
        for eng in ENG:
            self._wait(eng, toks)

    def finish(self, eng='sp'):
        toks = []
        for b, st in self.buf.items():
            if st[0]:
                toks.append(st[0])
            toks.extend(st[1].items())
        self._wait(eng, toks)

    def emit(self):
        nc = self.nc
        with nc.Block() as block:
            @block.tensor
            def _(e):
                for f in self.streams['pe']:
                    f(e)

            @block.scalar
            def _(e):
                for f in self.streams['act']:
                    f(e)

            @block.vector
            def _(e):
                for f in self.streams['dve']:
                    f(e)

            @block.gpsimd
            def _(e):
                for f in self.streams['pool']:
                    f(e)

            @block.sync
            def _(e):
                for f in self.streams['sp']:
                    f(e)


class Builder:
    def __init__(self, cfg):
        self.cfg = cfg
        self.nc = bass.Bass("TRN2", target_bir_lowering=False)
        self.stack = contextlib.ExitStack()
        self.sc = Sched(self.nc, self.stack)
        self.din = {}
        self.psn = 0
        self.uid = 0

    def inp(self, name, shape, dtype=F32):
        t = self.nc.dram_tensor(name, list(shape), dtype, kind="ExternalInput").ap()
        self.din[name] = t
        return t

    def sb(self, name, shape, dtype, st=None):
        self.uid += 1
        return (st or self.stack).enter_context(self.nc.sbuf_tensor('sb%d_%s' % (self.uid, name), list(shape), dtype))

    def bank(self):
        rot = getattr(self, 'rot', None) or list(range(8))
        i = rot[self.psn % len(rot)]
        self.psn += 1
        return ('ps', i), self.ps[i]

    def setup_common(self):
        nc = self.nc
        self.ps = [self.stack.enter_context(nc.psum_tensor('ps%d' % i, [P, 512], F32)) for i in range(8)]
        self.ident = self.sb('ident', [P, P], F32)
        self.ones = self.sb('ones', [P, P], F32)
        d_ident = self.inp('ident', [P, P])
        self.sc.dma('sp', 'const', self.ident[:], d_ident[:, :], writes=['ident'])
        self.sc.op('dve', lambda e: e.memset(self.ones[:], 1.0), writes=['ones'])

    def build_mod(self):
        sc = self.sc
        cT = self.inp('cT', [P, DC])
        ada_w = self.inp('ada_w', [4, D, 6 * D])
        ada_bT = self.inp('ada_bT', [P, 4, 48])
        norm_gT = self.inp('norm_gT', [P, 4, 2, DC])
        self.cond = self.sb('cond', [P, DC], F32)
        self.modT = self.sb('modT', [P, 4, 48], F32)
        self.adab = self.sb('adab', [P, 4, 48], F32)
        self.ng = self.sb('ng', [P, 4, 2, DC], F32)
        self.gs = self.sb('gs', [P, 4, 2, DC], F32)
        craw = self.sb('craw', [P, DC], F32)
        sig = self.sb('csig', [P, DC], F32)
        sc.dma('sp', 'const', craw[:], cT[:, :], writes=['craw'])
        sc.dma('sp', 'const', self.adab[:], ada_bT[:, :, :], writes=['adab'])
        sc.dma('sp', 'const', self.ng[:], norm_gT[:, :, :, :], writes=['ng'])
        sc.op('act', lambda e: e.activation(out=sig[:], in_=craw[:], func=AF.Sigmoid), reads=['craw'], writes=['csig'])
        sc.op('dve', lambda e: e.tensor_tensor(out=self.cond[:], in0=craw[:], in1=sig[:], op=ALU.mult),
              reads=['craw', 'csig'], writes=['cond'])
        st = contextlib.ExitStack()
        wsl = [self.sb('adaw%d' % i, [P, DC, 1024], F32, st) for i in range(2)]
        n = 0
        for i in range(4):
            pname, pt = self.bank()
            for k in range(6):
                slot = n % 2
                n += 1
                for c in range(DC):
                    sc.dma('sp', 'adaw%d' % slot, wsl[slot][:, c, :],
                           ada_w[i, c * P:(c + 1) * P, k * 1024:(k + 1) * 1024], writes=['adaw%d' % slot])
                fns = []
                for nn in range(8):
                    for c in range(DC):
                        fns.append(lambda e, pt=pt, slot=slot, nn=nn, c=c, k=k:
                                   e.matmul(pt[:, k * 8 + nn:k * 8 + nn + 1], lhsT=wsl[slot][:, c, nn * P:(nn + 1) * P],
                                            rhs=self.cond[:, c:c + 1], start=(c == 0), stop=(c == DC - 1)))
                sc.op('pe', fns, reads=['adaw%d' % slot, 'cond'], writes=[pname])
            sc.op('dve', lambda e, pt=pt, i=i: e.tensor_tensor(out=self.modT[:, i, :], in0=pt[:, 0:48], in1=self.adab[:, i, :], op=ALU.add),
                  reads=[pname, 'adab'], writes=[('modT', i)])
            for j, k in ((0, 1), (1, 4)):
                sc.op('dve', lambda e, i=i, j=j, k=k: e.scalar_tensor_tensor(
                    out=self.gs[:, i, j, :], in0=self.modT[:, i, k * 8:(k + 1) * 8], scalar=1.0, in1=self.ng[:, i, j, :],
                    op0=ALU.add, op1=ALU.mult), reads=[('modT', i), 'ng'], writes=[('modT', i)])
        sc.barrier()
        st.close()

    def modv(self, i, k, c):
        return self.modT[:, i, k * 8 + c:k * 8 + c + 1]

    def alloc_stream(self):
        self.xT_d = self.nc.dram_tensor('xT_scratch', [D, S], F32).ap()
        self.xh_d = self.nc.dram_tensor('xh_scratch', [D, S // 2], F32).ap()
        self.rd_mode = 'full'
        self.wr_mode = 'full'
        self.ntok = S
        self.sc.streams['sp'].append(lambda e: setattr(self, 'rv', e.partition_id() // 4))
        d_rankf = self.inp('rankf', [P, 1])
        self.rankf = self.sb('rankf', [P, 1], F32)
        self.sc.dma('sp', 'const', self.rankf[:], d_rankf[:, :], writes=['rankf'])
        self.xin = self.inp('x', [S, D])
        self.xblk = [self.sb('xblk%d' % i, [P, DC, 512], F32) for i in range(2)]
        self.sq = self.sb('sq', [P, 512], F32)
        self.rstd = self.sb('rstd', [P, 512], F32)
        self.tmp32 = [self.sb('tmp32_%d' % i, [P, 512], F32) for i in range(3)]
        self.xn = 0

    def xd(self, c, blk, write):
        mode = self.wr_mode if write else self.rd_mode
        if mode == 'full':
            return self.xT_d[c * P:(c + 1) * P, blk * 512:(blk + 1) * 512], ('xTd', c, blk)
        if mode == 'half':
            return self.xh_d[c * P:(c + 1) * P, blk * 512:(blk + 1) * 512], ('xh', c, blk)
        if mode == 'dynfull':
            return self.xs_d[c * P:(c + 1) * P, (blk + 1) * 512:(blk + 2) * 512], 'xs'
        raise ValueError(mode)

    def load_xT(self, blk, from_input):
        sc = self.sc
        slot = self.xn % 2
        self.xn += 1
        name = 'xblk%d' % slot
        t = self.xblk[slot]
        if from_input:
            for j in range(4):
                sc.dma('sp', 'xtok', self.xtok[:, j, :], self.xin[blk * 512 + j * P: blk * 512 + (j + 1) * P, :],
                       writes=[('xtok', j)])
            for c in range(DC):
                pname, pt = self.bank()
                fns = [lambda e, pt=pt, j=j, c=c: e.transpose(out=pt[:, j * P:(j + 1) * P], in_=self.xtok[:, j, c * P:(c + 1) * P],
                                                              identity=self.ident[:]) for j in range(4)]
                sc.op('pe', fns, reads=[('xtok', j) for j in range(4)] + ['ident'], writes=[pname])
                eng = 'act' if c % 2 == 0 else 'dve'
                if eng == 'act':
                    sc.op('act', lambda e, pt=pt, t=t, c=c: e.copy(out=t[:, c, :], in_=pt[:]), reads=[pname], writes=[(name, c)])
                else:
                    sc.op('dve', lambda e, pt=pt, t=t, c=c: e.tensor_copy(out=t[:, c, :], in_=pt[:]), reads=[pname], writes=[(name, c)])
        else:
            for c in range(DC):
                ap, tn = self.xd(c, blk, False)
                sc.dma('sp', name, t[:, c, :], ap, reads=[tn], writes=[(name, c)])
        return name, t

    def get_xT(self, blk, from_input, nxt=None):
        pre = getattr(self, '_pre', None)
        if pre is None:
            pre = self._pre = {}
        key = (self.rd_mode, blk)
        if key in pre:
            r = pre.pop(key)
        else:
            r = self.load_xT(blk, from_input)
        if nxt is not None:
            pre[(self.rd_mode, nxt)] = self.load_xT(nxt, from_input)
        return r

    def store_xT(self, blk, name, t, c):
        ap, tn = self.xd(c, blk, True)
        self.sc.dma('sp', 'st_' + name, ap, t[:, c, :], reads=[(name, c)], writes=[tn])

    def norm_mod(self, li, j, name, t, hT, hname, hoff, h32=None):
        sc = self.sc
        pname, pt = self.bank()
        for c in range(DC):
            sc.op('act', lambda e, c=c: e.activation(out=self.sq[:], in_=t[:, c, :], func=AF.Square), reads=[(name, c)], writes=['sq'])
            sc.op('pe', lambda e, c=c, pt=pt: e.matmul(pt[:], lhsT=self.ones[:], rhs=self.sq[:], start=(c == 0), stop=(c == DC - 1)),
                  reads=['sq', 'ones'], writes=[pname])
        sc.op('act', lambda e, pt=pt: e.activation(out=self.rstd[:], in_=pt[:], func=AF.Sqrt, scale=1.0 / D, bias=self.epsb[:]),
              reads=[pname, 'epsb'], writes=['rstd'])
        sc.op('dve', lambda e: e.reciprocal(out=self.rstd[:], in_=self.rstd[:]), reads=['rstd'], writes=['rstd'])
        kshift = 0 if j == 0 else 3
        for c in range(DC):
            tm = self.tmp32[c % 2]
            tn = 'tmp32_%d' % (c % 2)
            sc.op('dve', lambda e, c=c, tm=tm: e.tensor_tensor(out=tm[:], in0=t[:, c, :], in1=self.rstd[:], op=ALU.mult),
                  reads=[(name, c), 'rstd'], writes=[tn])
            if h32 is not None:
                sc.op('pool', lambda e, c=c, tm=tm: e.tensor_scalar(out=h32[:, c, :], in0=tm[:], scalar1=self.gs[:, li, j, c:c + 1],
                                                                   scalar2=self.modv(li, kshift, c), op0=ALU.mult, op1=ALU.add),
                      reads=[tn, ('modT', li)], writes=[('h32', c)])
            sc.op('dve', lambda e, c=c, tm=tm: e.tensor_scalar(out=hT[:, c, hoff:hoff + 512], in0=tm[:], scalar1=self.gs[:, li, j, c:c + 1],
                                                              scalar2=self.modv(li, kshift, c), op0=ALU.mult, op1=ALU.add),
                  reads=[tn, ('modT', li)], writes=[(hname, c, hoff)])

    def load_w_cast(self, chan, dst, src, rows_split, writes):
        n = dst.shape[-1]
        step = 2048
        for a in range(0, n, step):
            b = min(n, a + step)
            self.sc.dma('pool', chan, dst[:, a:b], src[:, a:b], writes=writes)

    def conv_setup(self):
        self.d_conv_w_in = self.inp('conv_w_in', [2, D, 3 * D])
        self.d_conv_w_out = self.inp('conv_w_out', [2, D, D])
        d_cw = self.inp('conv_wT', [P, 2, 3, DC])
        self.cwT = self.sb('cwT', [P, 2, 3, DC], F32)
        self.sc.dma('sp', 'const', self.cwT[:], d_cw[:, :, :, :], writes=['cwT'])

    def conv_mixer(self, li, j, first):
        sc = self.sc
        st = contextlib.ExitStack()
        nc = self.nc
        win = self.sb('cw_in', [P, DC, 3 * D], BF16, st)
        wout = self.sb('cw_out', [P, DC, D], BF16, st)
        hT = self.sb('hTc', [P, DC, 512], BF16, st)
        bT = self.sb('bTc', [P, DC, 512], F32, st)
        uT = self.sb('uTc', [P, DC, 514], F32, st)
        zT = self.sb('zTc', [P, DC, 512], BF16, st)
        if first:
            self.xtok = self.sb('xtok', [P, 4, D], F32, st)
        L = 'L%d' % li
        for c in range(DC):
            self.load_w_cast('cwin', win[:, c, :], self.d_conv_w_in[j, c * P:(c + 1) * P, :], None, writes=[(L, 'cwin')])
            self.load_w_cast('cwout', wout[:, c, :], self.d_conv_w_out[j, c * P:(c + 1) * P, :], None, writes=[(L, 'cwout')])
        sc.op('pool', lambda e: e.memset(uT[:, :, 0:2], 0.0), writes=[(L, 'uT', c) for c in range(DC)])
        split = (self.rd_mode == 'dynfull')
        blist = ([-1] if split else []) + list(range(self.ntok // 512))
        for bi, blk in enumerate(blist):
            name, t = self.get_xT(blk, first, blist[bi + 1] if bi + 1 < len(blist) else None)
            self.norm_mod(li, 0, name, t, hT, (L, 'hT'), 0)
            hreads = [((L, 'hT'), c, 0) for c in range(DC)] + [(L, 'cwin')]
            for c in range(DC):
                pts = []
                for kind in ((1, 2) if blk < 0 else (0, 1, 2)):
                    pname, pt = self.bank()
                    fns = [lambda e, pt=pt, k=k, kind=kind, c=c: e.matmul(
                        pt[:], lhsT=win[:, k, kind * D + c * P: kind * D + (c + 1) * P], rhs=hT[:, k, :],
                        start=(k == 0), stop=(k == DC - 1)) for k in range(DC)]
                    sc.op('pe', fns, reads=hreads, writes=[pname])
                    pts.append((pname, pt))
                if blk < 0:
                    (pcn, pc), (pvn, pv) = pts
                else:
                    (pbn, pb), (pcn, pc), (pvn, pv) = pts
                    sc.op('act', lambda e, pb=pb, c=c: e.activation(out=bT[:, c, :], in_=pb[:], func=AF.Copy), reads=[pbn], writes=[(L, 'bT', c)])
                tm = self.tmp32[2]
                sc.op('act', lambda e, pc=pc, tm=tm: e.activation(out=tm[:], in_=pc[:], func=AF.Copy), reads=[pcn], writes=['tmp32_2'])
                sc.op('dve', lambda e, pv=pv, tm=tm, c=c: e.tensor_tensor(out=uT[:, c, 2:514], in0=pv[:], in1=tm[:], op=ALU.mult),
                      reads=[pvn, 'tmp32_2'], writes=[(L, 'uT', c)])
            if blk < 0:
                for c in range(DC):
                    sc.op('dve', lambda e, c=c: e.tensor_scalar(out=uT[:, c, 0:2], in0=uT[:, c, 512:514], scalar1=self.rankf[:, 0:1], scalar2=None, op0=ALU.mult),
                          reads=[(L, 'uT', c), 'rankf'], writes=[(L, 'uT', c)])
                continue
            for c in range(DC):
                tm = self.tmp32[c % 2]
                tn = 'tmp32_%d' % (c % 2)
                sc.op('dve', lambda e, c=c, tm=tm: e.tensor_scalar(out=tm[:], in0=uT[:, c, 2:514], scalar1=self.cwT[:, j, 2, c:c + 1],
                                                                  scalar2=None, op0=ALU.mult), reads=[(L, 'uT', c), 'cwT'], writes=[tn])
                sc.op('dve', lambda e, c=c, tm=tm: e.scalar_tensor_tensor(out=tm[:], in0=uT[:, c, 1:513], scalar=self.cwT[:, j, 1, c:c + 1],
                                                                         in1=tm[:], op0=ALU.mult, op1=ALU.add),
                      reads=[(L, 'uT', c), tn], writes=[tn])
                sc.op('dve', lambda e, c=c, tm=tm: e.scalar_tensor_tensor(out=tm[:], in0=uT[:, c, 0:512], scalar=self.cwT[:, j, 0, c:c + 1],
                                                                         in1=tm[:], op0=ALU.mult, op1=ALU.add),
                      reads=[(L, 'uT', c), tn], writes=[tn])
                sc.op('dve', lambda e, c=c, tm=tm: e.tensor_tensor(out=zT[:, c, :], in0=tm[:], in1=bT[:, c, :], op=ALU.mult),
                      reads=[tn, (L, 'bT', c)], writes=[(L, 'zT', c)])
                sc.op('pool', lambda e, c=c: e.tensor_copy(out=uT[:, c, 0:2], in_=uT[:, c, 512:514]),
                      reads=[(L, 'uT', c)], writes=[(L, 'uT', c)])
            zreads = [(L, 'zT', c) for c in range(DC)] + [(L, 'cwout')]
            for c in range(DC):
                pname, pt = self.bank()
                fns = [lambda e, pt=pt, k=k, c=c: e.matmul(pt[:], lhsT=wout[:, k, c * P:(c + 1) * P], rhs=zT[:, k, :],
                                                           start=(k == 0), stop=(k == DC - 1)) for k in range(DC)]
                sc.op('pe', fns, reads=zreads, writes=[pname])
                sc.op('dve', lambda e, pt=pt, c=c, t=t: e.scalar_tensor_tensor(out=t[:, c, :], in0=pt[:], scalar=self.modv(li, 2, c),
                                                                              in1=t[:, c, :], op0=ALU.mult, op1=ALU.add),
                      reads=[pname, (name, c), ('modT', li)], writes=[(name, c)])
                self.store_xT(blk, name, t, c)
        sc.barrier()
        st.close()

    def ffn_setup(self):
        self.d_ffn_gu = self.inp('ffn_gu_r', [2, FC, P, DC * 256])
        self.d_ffn_dn = self.inp('ffn_dn_r', [2, DC, P, FC * P])
        self.d_moe_gu = self.inp('moe_gu_r', [2, NE, FC, P, DC * 256])
        self.d_moe_dn = self.inp('moe_dn_r', [2, NE, DC, P, FC * P])
        d_rw = self.inp('moe_rwT', [P, 2, DC, NE])
        d_rb = self.inp('moe_rbB', [P, 2, NE])
        d_sel = self.inp('sel8', [NE, NE, P])
        self.rw = self.sb('rw', [P, 2, DC, NE], F32)
        self.rb = self.sb('rb', [P, 2, NE], F32)
        self.sel = self.sb('sel', [NE, NE, P], F32)
        self.sc.dma('sp', 'const', self.rw[:], d_rw[:, :, :, :], writes=['rw'])
        self.sc.dma('sp', 'const', self.rb[:], d_rb[:, :, :], writes=['rb'])
        self.sc.dma('sp', 'const', self.sel[:], d_sel[:, :, :], writes=['sel'])

    def router(self, li, L, h32, half, gT, sm):
        sc = self.sc
        jl = li // 2
        lg, ex, gt, m8, sc1 = sm
        prn, pr = self.bank()
        h32r = [('h32', c) for c in range(DC)]
        for tt in range(4):
            fns = [lambda e, pr=pr, tt=tt, c=c: e.matmul(pr[:, tt * 8:(tt + 1) * 8], lhsT=h32[:, c, tt * P:(tt + 1) * P],
                                                        rhs=self.rw[:, jl, c, :], start=(c == 0), stop=(c == DC - 1))
                   for c in range(DC)]
            sc.op('pe', fns, reads=h32r + ['rw'], writes=[prn])
        ptn, ptT = self.bank()
        for tt in range(4):
            R = [(L, 'rt')]
            sc.op('dve', lambda e, tt=tt, pr=pr: e.tensor_tensor(out=lg[:, 0:8], in0=pr[:, tt * 8:(tt + 1) * 8], in1=self.rb[:, jl, :], op=ALU.add),
                  reads=[prn, 'rb'], writes=R)
            sc.op('dve', lambda e: e.tensor_reduce(out=sc1[:, 0:1], in_=lg[:, 0:8], axis=AX.X, op=ALU.max), reads=R, writes=R)
            sc.op('dve', lambda e: e.tensor_scalar(out=sc1[:, 0:1], in0=sc1[:, 0:1], scalar1=-1.0, scalar2=None, op0=ALU.mult), reads=R, writes=R)
            sc.op('act', lambda e: e.activation(out=ex[:, 0:8], in_=lg[:, 0:8], func=AF.Exp, bias=sc1[:, 0:1], scale=1.0), reads=R, writes=R)
            sc.op('dve', lambda e: e.max(out=m8[:, 0:8], in_=ex[:, 0:8]), reads=R, writes=R)
            sc.op('dve', lambda e: e.tensor_tensor(out=sc1[:, 1:2], in0=m8[:, 0:1], in1=m8[:, 1:2], op=ALU.add), reads=R, writes=R)
            sc.op('dve', lambda e: e.reciprocal(out=sc1[:, 1:2], in_=sc1[:, 1:2]), reads=R, writes=R)
            sc.op('dve', lambda e: e.tensor_scalar(out=gt[:, 0:8], in0=ex[:, 0:8], scalar1=m8[:, 1:2], scalar2=None, op0=ALU.is_ge), reads=R, writes=R)
            sc.op('dve', lambda e: e.tensor_tensor(out=gt[:, 0:8], in0=gt[:, 0:8], in1=ex[:, 0:8], op=ALU.mult), reads=R, writes=R)
            sc.op('dve', lambda e: e.tensor_scalar(out=gt[:, 0:8], in0=gt[:, 0:8], scalar1=sc1[:, 1:2], scalar2=None, op0=ALU.mult), reads=R, writes=R)
            sc.op('pe', lambda e, tt=tt, ptT=ptT: e.transpose(out=ptT[0:8, tt * P:(tt + 1) * P], in_=gt[:, 0:8], identity=self.ident[:]),
                  reads=R + ['ident'], writes=[ptn])
        sc.op('act', lambda e, ptT=ptT, half=half: e.activation(out=gT[0:8, half * 512:(half + 1) * 512], in_=ptT[0:8, :], func=AF.Copy),
              reads=[ptn], writes=[(L, 'gT', half)])

    def ffn(self, li, moe):
        sc = self.sc
        st = contextlib.ExitStack()
        L = 'F%d' % li
        TB = 1024
        hT = self.sb('hTf', [P, DC, TB], BF16, st)
        actT = self.sb('actT', [P, FC, TB], BF16, st)
        wgu = [self.sb('wgu%d' % i, [P, DC * 256], BF16, st) for i in range(3)]
        wd = [self.sb('wd%d' % i, [P, FC * P], BF16, st) for i in range(3)]
        xs = [self.sb('xs%d' % i, [P, 512], F32, st) for i in range(2)]
        if moe:
            h32 = self.sb('h32', [P, DC, 512], F32, st)
            yacc = self.sb('yacc', [P, DC, TB], F32, st)
            Gb = [self.sb('Gb%d' % i, [P, TB], F32, st) for i in range(2)]
            gT = self.sb('gT', [NE, TB], F32, st)
            sm = [self.sb('rsm%d' % i, [P, 8], F32, st) for i in range(5)]
        jl = li // 2
        nw = 0
        nd = 0
        nx = 0
        ng = 0
        for sbi in range(self.ntok // TB):
            for half in range(2):
                blk = sbi * 2 + half
                name, t = self.get_xT(blk, False, blk + 1 if half == 0 else None)
                self.norm_mod(li, 1, name, t, hT, (L, 'hT'), half * 512, h32=(h32 if moe else None))
                if moe:
                    self.router(li, L, h32, half, gT, sm)
            for ex in (range(NE) if moe else [None]):
                if moe:
                    gsl = ng % 2
                    ng += 1
                    gbn = (L, 'Gb', gsl)
                    for half in range(2):
                        pn, pt = self.bank()
                        sc.op('pe', lambda e, pt=pt, ex=ex, half=half: e.matmul(pt[:], lhsT=self.sel[0:8, ex, :], rhs=gT[0:8, half * 512:(half + 1) * 512],
                                                                               start=True, stop=True),
                              reads=['sel', (L, 'gT', half)], writes=[pn])
                        sc.op('act', lambda e, pt=pt, gsl=gsl, half=half: e.activation(out=Gb[gsl][:, half * 512:(half + 1) * 512], in_=pt[:], func=AF.Copy),
                              reads=[pn], writes=[(gbn, half)])
                for f in range(FC):
                    slot = nw % 3
                    nw += 1
                    wn = (L, 'wgu', slot)
                    src = self.d_moe_gu[jl, ex, f, :, :] if moe else self.d_ffn_gu[jl, f, :, :]
                    self.load_w_cast('wgu%d' % slot, wgu[slot][:, :], src, None, writes=[wn])
                    for half in range(2):
                        hreads = [((L, 'hT'), c, half * 512) for c in range(DC)] + [wn]
                        pgn, pg = self.bank()
                        pun, pu = self.bank()
                        for (pn, pt, off) in ((pgn, pg, 0), (pun, pu, 128)):
                            fns = [lambda e, pt=pt, k=k, off=off, slot=slot, half=half: e.matmul(
                                pt[:], lhsT=wgu[slot][:, k * 256 + off: k * 256 + off + 128], rhs=hT[:, k, half * 512:(half + 1) * 512],
                                start=(k == 0), stop=(k == DC - 1)) for k in range(DC)]
                            sc.op('pe', fns, reads=hreads, writes=[pn])
                        tm = self.tmp32[2]
                        sc.op('act', lambda e, pg=pg, tm=tm: e.activation(out=tm[:], in_=pg[:], func=AF.Silu), reads=[pgn], writes=['tmp32_2'])
                        sc.op('dve', lambda e, pu=pu, tm=tm, f=f, half=half: e.tensor_tensor(
                            out=actT[:, f, half * 512:(half + 1) * 512], in0=pu[:], in1=tm[:], op=ALU.mult),
                            reads=[pun, 'tmp32_2'], writes=[(L, 'act', f, half)])
                for c in range(DC):
                    slot = nd % 3
                    nd += 1
                    wn = (L, 'wd', slot)
                    src = self.d_moe_dn[jl, ex, c, :, :] if moe else self.d_ffn_dn[jl, c, :, :]
                    self.load_w_cast('wd%d' % slot, wd[slot][:, :], src, None, writes=[wn])
                    for half in range(2):
                        blk = sbi * 2 + half
                        areads = [(L, 'act', f, half) for f in range(FC)] + [wn]
                        pn, pt = self.bank()
                        fns = [lambda e, pt=pt, f=f, slot=slot, half=half: e.matmul(
                            pt[:], lhsT=wd[slot][:, f * P:(f + 1) * P], rhs=actT[:, f, half * 512:(half + 1) * 512],
                            start=(f == 0), stop=(f == FC - 1)) for f in range(FC)]
                        sc.op('pe', fns, reads=areads, writes=[pn])
                        if not moe:
                            self.resid(li, 5, xs, nx, c, blk, pn, pt, None, None)
                            nx += 1
                        else:
                            yn = (L, 'yacc', c, half)
                            ysl = yacc[:, c, half * 512:(half + 1) * 512]
                            gsl_ap = Gb[gsl][:, half * 512:(half + 1) * 512]
                            if ex == 0:
                                sc.op('dve', lambda e, pt=pt, ysl=ysl, g=gsl_ap: e.tensor_tensor(out=ysl, in0=pt[:], in1=g, op=ALU.mult),
                                      reads=[pn, (gbn, half)], writes=[yn])
                            else:
                                tm = self.tmp32[c % 2]
                                tn = 'tmp32_%d' % (c % 2)
                                sc.op('dve', lambda e, pt=pt, tm=tm, g=gsl_ap: e.tensor_tensor(out=tm[:], in0=pt[:], in1=g, op=ALU.mult),
                                      reads=[pn, (gbn, half)], writes=[tn])
                                sc.op('dve', lambda e, tm=tm, ysl=ysl: e.tensor_tensor(out=ysl, in0=ysl, in1=tm[:], op=ALU.add),
                                      reads=[tn, yn], writes=[yn])
            if moe:
                for c in range(DC):
                    for half in range(2):
                        blk = sbi * 2 + half
                        self.resid(li, 5, xs, nx, c, blk, (L, 'yacc', c, half), None, yacc[:, c, half * 512:(half + 1) * 512], None)
                        nx += 1
        sc.barrier()
        st.close()

    def resid(self, li, k, xs, nx, c, blk, srcname, pt, src_ap, _):
        sc = self.sc
        xsl = nx % 2
        xn = 'xs%d' % xsl
        xt = xs[xsl]
        src = pt[:] if pt is not None else src_ap
        rap, rtn = self.xd(c, blk, False)
        wap, wtn = self.xd(c, blk, True)
        sc.dma('sp', xn, xt[:], rap, reads=[rtn], writes=[xn])
        sc.op('dve', lambda e, src=src, xt=xt, c=c: e.scalar_tensor_tensor(
            out=xt[:], in0=src, scalar=self.modv(li, k, c), in1=xt[:], op0=ALU.mult, op1=ALU.add),
            reads=[srcname, xn, ('modT', li)], writes=[xn])
        sc.dma('sp', 'st_' + xn, wap, xt[:], reads=[xn], writes=[wtn])

    def attn_setup(self):
        self.d_awq = self.inp('attn_wq_perm', [D, 1024])
        self.d_awk = self.inp('attn_wk', [D, 256])
        self.d_awv = self.inp('attn_wv', [D, 256])
        self.d_awqi = self.inp('attn_wqi', [D, 512])
        self.d_awki2 = self.inp('attn_wki2', [D, 128])
        self.d_awwi = self.inp('attn_wwi', [D, 8])
        self.d_awout = self.inp('attn_wout_r', [64, 16, D])
        self.d_gainT = self.inp('attn_gainT', [P, 2])
        self.d_biasT = self.inp('attn_biasT', [P, 32, P])
        self.d_b31 = self.inp('attn_b31B', [P, 16])
        self.d_cmask = self.inp('cmask', [P, P])
        self.d_bones = self.inp('blockones', [P, P])
        self.d_selrow = self.inp('selrow', [65, 64])

    def head_norm(self, pn, pt, gain_ap, out_ap, tagw):
        sc = self.sc
        qs = self.tmp32[2]
        sc.op('act', lambda e, pt=pt, qs=qs: e.activation(out=qs[:], in_=pt[:], func=AF.Copy), reads=[pn], writes=['tmp32_2'])
        sc.op('act', lambda e, qs=qs: e.activation(out=self.sq[:], in_=qs[:], func=AF.Square), reads=['tmp32_2'], writes=['sq'])
        p2n, p2 = self.bank()
        sc.op('pe', lambda e, p2=p2: e.matmul(p2[:], lhsT=self.bones[:], rhs=self.sq[:], start=True, stop=True), reads=['sq', 'bones'], writes=[p2n])
        sc.op('act', lambda e, p2=p2: e.activation(out=self.rstd[:], in_=p2[:], func=AF.Sqrt, scale=1.0 / 64, bias=self.epsb[:]),
              reads=[p2n, 'epsb'], writes=['rstd'])
        sc.op('dve', lambda e: e.reciprocal(out=self.rstd[:], in_=self.rstd[:]), reads=['rstd'], writes=['rstd'])
        sc.op('dve', lambda e, qs=qs, g=gain_ap, o=out_ap: e.scalar_tensor_tensor(out=o, in0=qs[:], scalar=g, in1=self.rstd[:], op0=ALU.mult, op1=ALU.mult),
              reads=['tmp32_2', 'rstd', 'again'], writes=tagw)

    def attn_mixer(self, li):
        sc = self.sc
        st = contextlib.ExitStack()
        L = 'A'
        NI = 24
        KT = self.sb('KT', [P, 2, S], BF16, st)
        VA = self.sb('VA', [P, 32, 4, 65], BF16, st)
        KI = self.sb('KI', [P, S], BF16, st)
        hT = self.sb('hTa', [P, DC, 512], BF16, st)
        wA = self.sb('wA', [P, DC * 1024], BF16, st)
        QT = self.sb('QT', [P, 8, 512], BF16, st)
        QI = self.sb('QI', [P, 4, 512], BF16, st)
        WI = self.sb('WI', [P, 4, 8], F32, st)
        score = self.sb('score', [P, S], F32, st)
        nmq = self.sb('nmq', [P, S], BF16, st)
        nmT = [self.sb('nmT%d' % i, [P, 32, P], BF16, st) for i in range(2)]
        pexp = [self.sb('pexp%d' % i, [P, 512], BF16, st) for i in range(3)]
        OTn = self.sb('OTn', [64, 16, 512], BF16, st)
        osb = self.sb('osb', [65, 512], F32, st)
        rbc = self.sb('rbc', [64, 512], F32, st)
        Rt = self.tmp32[0:2]
        biasS = self.sb('biasS', [P, 32, P], BF16, st)
        self.bones = self.sb('bones', [P, P], F32, st)
        identb = self.sb('identb', [P, P], BF16, st)
        cmask = self.sb('cmaskS', [P, P], F32, st)
        selrow = self.sb('selrowS', [65, 64], F32, st)
        again = self.sb('again', [P, 2], F32, st)
        b31 = self.sb('b31', [P, 16], F32, st)
        bs = [self.sb('bsm%d' % i, [P, 1], F32, st) for i in range(6)]
        sc.dma('sp', 'const', self.bones[:], self.d_bones[:, :], writes=['bones'])
        sc.dma('sp', 'const', cmask[:], self.d_cmask[:, :], writes=['cmask'])
        sc.dma('sp', 'const', selrow[:], self.d_selrow[:, :], writes=['selrow'])
        sc.dma('sp', 'const', again[:], self.d_gainT[:, :], writes=['again'])
        sc.dma('sp', 'const', b31[:], self.d_b31[:, :], writes=['b31'])
        sc.op('dve', lambda e: e.tensor_copy(out=identb[:], in_=self.ident[:]), reads=['ident'], writes=['identb'])
        sc.op('dve', lambda e: e.tensor_scalar(out=again[:, 0:1], in0=again[:, 0:1], scalar1=0.125, scalar2=None, op0=ALU.mult),
              reads=['again'], writes=['again'])
        sc.op('pool', lambda e: e.memset(VA[:, :, :, 64:65], 1.0), writes=[(L, 'VAones')])
        for half in range(4):
            sc.dma('sp', 'bstage', score[:, 0:1024].rearrange('p (a b) -> p a b', b=P), self.d_biasT[:, half * 8:(half + 1) * 8, :],
                   writes=[(L, 'bstage')])
            for i in range(8):
                kh = half * 8 + i
                h = kh % 16
                sc.op('dve', lambda e, i=i, kh=kh, h=h: e.tensor_scalar(out=biasS[:, kh, :], in0=score[:, i * P:(i + 1) * P], scalar1=b31[:, h:h + 1],
                                                                      scalar2=None, op0=ALU.subtract), reads=[(L, 'bstage'), 'b31'], writes=[(L, 'biasS')])
        wk = wA[:, 0:DC * 256].rearrange('p (c n) -> p c n', c=DC)
        wv = wA[:, DC * 256:DC * 512].rearrange('p (c n) -> p c n', c=DC)
        wki = wA[:, DC * 512:DC * 640].rearrange('p (c n) -> p c n', c=DC)
        for c in range(DC):
            sc.dma('pool', 'wA', wk[:, c, :], self.d_awk[c * P:(c + 1) * P, :], writes=[(L, 'wA')])
            sc.dma('pool', 'wA', wv[:, c, :], self.d_awv[c * P:(c + 1) * P, :], writes=[(L, 'wA')])
            sc.dma('pool', 'wA', wki[:, c, :], self.d_awki2[c * P:(c + 1) * P, :], writes=[(L, 'wA')])
        for blk in range(NBLK):
            name, t = self.get_xT(blk, False, blk + 1 if blk + 1 < NBLK else None)
            self.norm_mod(li, 0, name, t, hT, (L, 'hT'), 0)
            hreads = [((L, 'hT'), c, 0) for c in range(DC)] + [(L, 'wA')]
            for m in range(2):
                pn, pt = self.bank()
                fns = [lambda e, pt=pt, k=k, m=m: e.matmul(pt[:], lhsT=wk[:, k, m * P:(m + 1) * P], rhs=hT[:, k, :], start=(k == 0), stop=(k == DC - 1))
                       for k in range(DC)]
                sc.op('pe', fns, reads=hreads, writes=[pn])
                self.head_norm(pn, pt, again[:, 1:2], KT[:, m, blk * 512:(blk + 1) * 512], [(L, 'KT', m, blk)])
            pn, pt = self.bank()
            fns = [lambda e, pt=pt, k=k: e.matmul(pt[:], lhsT=wki[:, k, :], rhs=hT[:, k, :], start=(k == 0), stop=(k == DC - 1)) for k in range(DC)]
            sc.op('pe', fns, reads=hreads, writes=[pn])
            sc.op('act', lambda e, pt=pt, blk=blk: e.activation(out=KI[:, blk * 512:(blk + 1) * 512], in_=pt[:], func=AF.Copy), reads=[pn], writes=[(L, 'KI', blk)])
            for tt in range(4):
                pn, pt = self.bank()
                fns = [lambda e, pt=pt, k=k, tt=tt: e.matmul(pt[:, 0:256], lhsT=hT[:, k, tt * P:(tt + 1) * P], rhs=wv[:, k, :], start=(k == 0), stop=(k == DC - 1))
                       for k in range(DC)]
                sc.op('pe', fns, reads=hreads, writes=[pn])
                sc.op('dve', lambda e, pt=pt, blk=blk, tt=tt: e.tensor_copy(out=VA[:, blk * 4 + tt, :, 0:64], in_=pt[:, 0:256].rearrange('p (a b) -> p a b', b=64)),
                      reads=[pn], writes=[(L, 'VA', blk * 4 + tt)])
        if self.cfg.get('astage', 9) < 1:
            sc.barrier(); st.close(); return
        wq = wA[:, :].rearrange('p (c n) -> p c n', c=DC)
        wqi = wA[:, 0:DC * 512].rearrange('p (c n) -> p c n', c=DC)
        wwi = wA[:, DC * 512:DC * 520].rearrange('p (c n) -> p c n', c=DC)
        wout = wA[0:64, 0:16 * 512].rearrange('p (h n) -> p h n', h=16)
        self.abank = [6, 7]
        self.nab = 0
        self.rot = [0, 1, 2, 3, 4, 5]
        nmn = 0
        npx = 0
        nrt = 0
        for blk in range(self.cfg.get('anblk', NBLK)):
            for c in range(DC):
                sc.dma('pool', 'wA', wq[:, c, :], self.d_awq[c * P:(c + 1) * P, :], writes=[(L, 'wA')])
            name, t = self.load_xT(blk, False)
            self.norm_mod(li, 0, name, t, hT, (L, 'hT'), 0)
            hreads = [((L, 'hT'), c, 0) for c in range(DC)]
            for m in range(8):
                pn, pt = self.bank()
                fns = [lambda e, pt=pt, k=k, m=m: e.matmul(pt[:], lhsT=wq[:, k, m * P:(m + 1) * P], rhs=hT[:, k, :], start=(k == 0), stop=(k == DC - 1))
                       for k in range(DC)]
                sc.op('pe', fns, reads=hreads + [(L, 'wA')], writes=[pn])
                self.head_norm(pn, pt, again[:, 0:1], QT[:, m, :], [(L, 'QT', m)])
            for c in range(DC):
                sc.dma('pool', 'wA', wqi[:, c, :], self.d_awqi[c * P:(c + 1) * P, :], writes=[(L, 'wA')])
                sc.dma('pool', 'wA', wwi[:, c, :], self.d_awwi[c * P:(c + 1) * P, :], writes=[(L, 'wA')])
            for m in range(4):
                pn, pt = self.bank()
                fns = [lambda e, pt=pt, k=k, m=m: e.matmul(pt[:], lhsT=wqi[:, k, m * P:(m + 1) * P], rhs=hT[:, k, :], start=(k == 0), stop=(k == DC - 1))
                       for k in range(DC)]
                sc.op('pe', fns, reads=hreads + [(L, 'wA')], writes=[pn])
                sc.op('act', lambda e, pt=pt, m=m: e.activation(out=QI[:, m, :], in_=pt[:], func=AF.Copy), reads=[pn], writes=[(L, 'QI', m)])
            pn, pt = self.bank()
            for qb in range(4):
                fns = [lambda e, pt=pt, k=k, qb=qb: e.matmul(pt[:, qb * 8:(qb + 1) * 8], lhsT=hT[:, k, qb * P:(qb + 1) * P], rhs=wwi[:, k, :],
                                                            start=(k == 0), stop=(k == DC - 1)) for k in range(DC)]
                sc.op('pe', fns, reads=hreads + [(L, 'wA')], writes=[pn])
            sc.op('dve', lambda e, pt=pt: e.tensor_scalar(out=WI[:, :, :], in0=pt[:, 0:32].rearrange('p (a b) -> p a b', b=8), scalar1=0.04419417382415922,
                                                        scalar2=None, op0=ALU.mult), reads=[pn], writes=[(L, 'WI')])
            def idx_bis(qb):
                gq = blk * 4 + qb
                nk = gq + 1
                W = nk * P
                nonlocal nrt
                for k0 in range(0, W, 512):
                    n = min(512, W - k0)
                    for ih in range(8):
                        b0 = (ih % 2) * 64
                        pn, pt = self.bank()
                        sc.op('pe', lambda e, pt=pt, ih=ih, b0=b0, qb=qb, k0=k0, n=n: e.matmul(
                            pt[:, 0:n], lhsT=QI[b0:b0 + 64, ih // 2, qb * P:(qb + 1) * P], rhs=KI[b0:b0 + 64, k0:k0 + n], start=True, stop=True),
                            reads=[(L, 'QI', ih // 2)] + [(L, 'KI', kb) for kb in range(k0 // 512, (k0 + n + 511) // 512)], writes=[pn])
                        rs = nrt % 2
                        nrt += 1
                        rn = 'tmp32_%d' % rs
                        sc.op('act', lambda e, pt=pt, rs=rs, n=n: e.activation(out=Rt[rs][:, 0:n], in_=pt[:, 0:n], func=AF.Relu), reads=[pn], writes=[rn])
                        if ih == 0:
                            sc.op('dve', lambda e, rs=rs, n=n, k0=k0, qb=qb: e.tensor_scalar(out=score[:, k0:k0 + n], in0=Rt[rs][:, 0:n], scalar1=WI[:, qb, 0:1],
                                                                                      scalar2=None, op0=ALU.mult), reads=[rn, (L, 'WI')], writes=[(L, 'score')])
                        else:
                            sc.op('dve', lambda e, rs=rs, n=n, k0=k0, qb=qb, ih=ih: e.scalar_tensor_tensor(
                                out=score[:, k0:k0 + n], in0=Rt[rs][:, 0:n], scalar=WI[:, qb, ih:ih + 1], in1=score[:, k0:k0 + n], op0=ALU.mult, op1=ALU.add),
                                reads=[rn, (L, 'WI'), (L, 'score')], writes=[(L, 'score')])
                SR = [(L, 'score'), (L, 'bis')]
                mx, lo, w0, mid, cnt, ff = bs
                if nk >= 3:
                    sc.op('dve', lambda e, W=W: e.tensor_reduce(out=mx[:], in_=score[:, 0:W], axis=AX.X, op=ALU.max), reads=SR, writes=[(L, 'bis')])
                    sc.op('dve', lambda e, W=W: e.tensor_reduce(out=lo[:], in_=score[:, 0:W], axis=AX.X, op=ALU.min), reads=SR, writes=[(L, 'bis')])
                    sc.op('dve', lambda e: e.tensor_tensor(out=w0[:], in0=mx[:], in1=lo[:], op=ALU.subtract), reads=SR, writes=[(L, 'bis')])
                else:
                    sc.op('dve', lambda e: e.memset(lo[:], -1e29), reads=SR, writes=[(L, 'bis')])
                sc.op('dve', lambda e, W=W: e.tensor_tensor(out=score[:, W - P:W], in0=score[:, W - P:W], in1=cmask[:], op=ALU.add),
                      reads=SR + ['cmask'], writes=SR)
                if nk >= 3:
                    for it in range(NI):
                        cst = 2.0 ** (-(it + 1))
                        sc.op('dve', lambda e, cst=cst: e.scalar_tensor_tensor(out=mid[:], in0=w0[:], scalar=cst, in1=lo[:], op0=ALU.mult, op1=ALU.add),
                              reads=SR, writes=[(L, 'bis')])
                        sc.op('dve', lambda e, W=W: e.tensor_scalar(out=nmq[:, 0:W], in0=score[:, 0:W], scalar1=mid[:, 0:1], scalar2=0.0, op0=ALU.is_ge,
                                                                   op1=ALU.add, accum_out=cnt[:, 0:1]), reads=SR + [(L, 'nmq')], writes=[(L, 'bis'), (L, 'nmq')])
                        sc.op('dve', lambda e, cst=cst: e.tensor_scalar(out=ff[:], in0=cnt[:], scalar1=256.0, scalar2=cst, op0=ALU.is_ge, op1=ALU.mult),
                              reads=SR, writes=[(L, 'bis')])
                        sc.op('dve', lambda e: e.scalar_tensor_tensor(out=lo[:], in0=ff[:], scalar=w0[:, 0:1], in1=lo[:], op0=ALU.mult, op1=ALU.add),
                              reads=SR, writes=[(L, 'bis')])
                sc.op('dve', lambda e, W=W: e.tensor_scalar(out=nmq[:, 0:W], in0=score[:, 0:W], scalar1=lo[:, 0:1], scalar2=-30000.0, op0=ALU.is_lt, op1=ALU.mult),
                      reads=SR + [(L, 'nmq')], writes=[(L, 'nmq')])
            def tr(qb):
                gq = blk * 4 + qb
                nk = gq + 1
                W = nk * P
                ms = gq % 2
                mn_ = (L, 'nmT', ms)
                for j0 in range(0, nk, 8):
                    jn = min(8, nk - j0)
                    pn, pt = self.bank()
                    ptb = pt[:].bitcast(BF16)
                    fns = [lambda e, ptb=ptb, j=j, j0=j0: e.transpose(out=ptb[:, (j - j0) * P:(j - j0 + 1) * P], in_=nmq[:, j * P:(j + 1) * P], identity=identb[:])
                           for j in range(j0, j0 + jn)]
                    sc.op('pe', fns, reads=[(L, 'nmq'), 'identb'], writes=[pn])
                    sc.op('act', lambda e, ptb=ptb, ms=ms, j0=j0, jn=jn: e.activation(out=nmT[ms][:, j0:j0 + jn, :], in_=ptb[:, 0:jn * P].rearrange('p (a b) -> p a b', b=P),
                                                                                func=AF.Copy), reads=[pn], writes=[(mn_, j0)])
            def att(qb):
                gq = blk * 4 + qb
                nk = gq + 1
                W = nk * P
                nonlocal npx
                ms = gq % 2
                mn_ = (L, 'nmT', ms)
                for kvh in range(4 if self.cfg.get('astage', 9) >= 4 else 0):
                    ab = self.abank[self.nab % 2]
                    self.nab += 1
                    pon = ('ps', ab)
                    po = self.ps[ab]
                    b0 = (kvh % 2) * 64
                    stb = {}

                    def emit_st(j, kvh=kvh, b0=b0, stb=stb):
                        pn, pt = self.bank()
                        stb[j] = (pn, pt)
                        near = (nk - 1 - j) if (nk - 1 - j) < 2 else None
                        fns = []
                        p3 = pt[:, :].rearrange('p (g q) -> p g q', q=P)
                        fns.append(lambda e, p3=p3, j=j, b0=b0, kvh=kvh, qb=qb: e.matmul(
                            p3, lhsT=KT[b0:b0 + 64, kvh // 2, j * P:(j + 1) * P],
                            rhs=QT[b0:b0 + 64, (kvh // 2) * 4:(kvh // 2) * 4 + 4, qb * P:(qb + 1) * P], start=True, stop=False))
                        if near is not None:
                            fns.append(lambda e, p3=p3, kvh=kvh, near=near: e.matmul(
                                p3, lhsT=identb[:], rhs=biasS[:, near * 16 + kvh * 4:near * 16 + kvh * 4 + 4, :], start=False, stop=False))
                        fns.append(lambda e, p3=p3, j=j, ms=ms: e.matmul(
                            p3, lhsT=identb[:], rhs=nmT[ms][:, j, :].unsqueeze(1).to_broadcast([P, 4, P]), start=False, stop=True))
                        sc.op('pe', fns, reads=[(L, 'KT', kvh // 2, j // 4), (mn_, (j // 8) * 8), 'identb', (L, 'biasS')] +
                              [(L, 'QT', (kvh // 2) * 4 + g) for g in range(4)], writes=[pn])
                    emit_st(0)
                    for j in range(nk):
                        if j + 1 < nk:
                            emit_st(j + 1)
                        pn, pt = stb[j]
                        px = npx % 3
                        npx += 1
                        pxn = 'pexp%d' % px
                        sc.op('act', lambda e, pt=pt, px=px: e.activation(out=pexp[px][:], in_=pt[:], func=AF.Exp), reads=[pn], writes=[pxn])
                        sc.op('pe', lambda e, po=po, px=px, j=j, kvh=kvh, nk=nk: e.matmul(po[0:65, :], lhsT=VA[:, j, kvh, :], rhs=pexp[px][:],
                                                                                     start=(j == 0), stop=(j == nk - 1)),
                              reads=[pxn, (L, 'VA', j), (L, 'VAones')], writes=[pon])
                    sc.op('act', lambda e, po=po: e.activation(out=osb[:], in_=po[0:65, :], func=AF.Copy), reads=[pon], writes=['osb'])
                    sc.op('act', lambda e: e.activation(out=osb[64:65, :], in_=osb[64:65, :], func=AF.Ln), reads=['osb'], writes=['osb'])
                    sc.op('act', lambda e: e.activation(out=osb[64:65, :], in_=osb[64:65, :], func=AF.Exp, scale=-1.0), reads=['osb'], writes=['osb'])
                    pn, pt = self.bank()
                    sc.op('pe', lambda e, pt=pt: e.matmul(pt[0:64, :], lhsT=selrow[:], rhs=osb[:], start=True, stop=True), reads=['osb', 'selrow'], writes=[pn])
                    sc.op('act', lambda e, pt=pt: e.activation(out=rbc[0:64, :], in_=pt[0:64, :], func=AF.Copy), reads=[pn], writes=['rbc'])
                    sc.op('pool', lambda e, kvh=kvh, qb=qb: e.tensor_tensor(out=OTn[:, kvh * 4:(kvh + 1) * 4, qb * P:(qb + 1) * P],
                                                                        in0=osb[0:64, :].rearrange('p (a b) -> p a b', b=P),
                                                                        in1=rbc[0:64, :].rearrange('p (a b) -> p a b', b=P), op=ALU.mult),
                          reads=['osb', 'rbc'], writes=[(L, 'OTn', kvh, qb)])
            nq = 4 if self.cfg.get('astage', 9) >= 2 else 0
            if nq:
                idx_bis(0)
                tr(0)
            for qb in range(nq):
                if qb + 1 < nq:
                    idx_bis(qb + 1)
                att(qb)
                if qb + 1 < nq:
                    tr(qb + 1)
            if self.cfg.get('astage', 9) < 5:
                continue
            oreads = [(L, 'OTn', kvh, qb) for kvh in range(4) for qb in range(4)] + [(L, 'wA')]
            for c in range(DC):
                if c % 4 == 0:
                    for h in range(16):
                        sc.dma('pool', 'wA', wout[:, h, :], self.d_awout[:, h, (c // 4) * 512:(c // 4 + 1) * 512], writes=[(L, 'wA')])
                pn, pt = self.bank()
                fns = [lambda e, pt=pt, h=h, c=c: e.matmul(pt[:], lhsT=wout[:, h, (c % 4) * P:(c % 4 + 1) * P], rhs=OTn[:, h, :], start=(h == 0), stop=(h == 15))
                       for h in range(16)]
                sc.op('pe', fns, reads=oreads, writes=[pn])
                sc.op('dve', lambda e, pt=pt, c=c, t=t: e.scalar_tensor_tensor(out=t[:, c, :], in0=pt[:], scalar=self.modv(li, 2, c), in1=t[:, c, :],
                                                                              op0=ALU.mult, op1=ALU.add), reads=[pn, (name, c), ('modT', li)], writes=[(name, c)])
                self.store_xT(blk, name, t, c)
        self.rot = list(range(8))
        sc.barrier()
        st.close()

    def ssm_setup(self):
        self.d_lamT = self.inp('ssm_lamT', [P, 32, 2])
        self.d_lstepT = self.inp('ssm_lstepT', [P, 32])
        self.d_Bblk = self.inp('ssm_Bblk', [32, P, 256])
        self.d_Cblk = self.inp('ssm_Cblk', [32, P, 256])
        self.d_dT = self.inp('ssm_dT', [P, DC])
        self.d_glu = self.inp('ssm_glu_r', [DC, P, DC * 256])

    def ssm_mixer(self, li):
        import math
        sc = self.sc
        st = contextlib.ExitStack()
        L = 'S'
        LC = 128
        I32 = mybir.dt.int32
        Ec = self.sb('Ec', [P, 32, LC], F32, st)
        En = self.sb('En', [P, 32, LC], F32, st)
        Bb = self.sb('Bb', [P, 32, 256], BF16, st)
        Cb = self.sb('Cb', [P, 32, 256], BF16, st)
        sm = {n: self.sb('ss_' + n, [P, 32], F32, st) for n in
              ['lr', 'li', 'stp', 'th', 'rr', 'u', 'f', 'g', 'sn', 'cs', 'x', 'y', 'den', 'cr', 'ci', 'cL', 'nL', 'ire', 'iim', 'e1c', 'e1n', 't1', 't2']}
        ni = self.sb('ss_ni', [P, 32], I32, st)
        lam = self.sb('ss_lam', [P, 32, 2], F32, st)
        dT = self.sb('ss_dT', [P, DC], F32, st)
        cs1 = self.sb('ss_c1', [P, 4], F32, st)
        SM = [(L, 'sm')]

        def dv(fn, extra_r=(), extra_w=()):
            sc.op('dve', fn, reads=SM + list(extra_r), writes=SM + list(extra_w))

        def ac(fn):
            sc.op('act', fn, reads=SM, writes=SM)
        sc.dma('sp', 'const', lam[:], self.d_lamT[:, :, :], writes=SM)
        sc.dma('sp', 'const', sm['stp'][:], self.d_lstepT[:, :], writes=SM)
        sc.dma('sp', 'const', dT[:], self.d_dT[:, :], writes=[(L, 'dT')])
        for k in range(32):
            sc.dma('pool', 'Bb', Bb[:, k, :], self.d_Bblk[k, :, :], writes=[(L, 'Bb')])
        dv(lambda e: e.tensor_scalar(out=sm['lr'][:], in0=lam[:, :, 0], scalar1=-1e-4, scalar2=None, op0=ALU.min))
        dv(lambda e: e.tensor_copy(out=sm['li'][:], in_=lam[:, :, 1]))
        ac(lambda e: e.activation(out=sm['stp'][:], in_=sm['stp'][:], func=AF.Exp))
        dv(lambda e: e.tensor_tensor(out=sm['th'][:], in0=sm['li'][:], in1=sm['stp'][:], op=ALU.mult))
        dv(lambda e: e.tensor_tensor(out=sm['rr'][:], in0=sm['lr'][:], in1=sm['stp'][:], op=ALU.mult))
        ac(lambda e: e.activation(out=sm['rr'][:], in_=sm['rr'][:], func=AF.Exp))

        def sincos(dst, off):
            dv(lambda e: e.tensor_scalar(out=sm['u'][:], in0=sm['th'][:], scalar1=1.0 / (2 * math.pi), scalar2=off, op0=ALU.mult, op1=ALU.add))
            dv(lambda e: e.tensor_copy(out=ni[:], in_=sm['u'][:]))
            dv(lambda e: e.tensor_copy(out=sm['f'][:], in_=ni[:]))
            dv(lambda e: e.tensor_tensor(out=sm['f'][:], in0=sm['u'][:], in1=sm['f'][:], op=ALU.subtract))
            dv(lambda e: e.tensor_scalar(out=sm['g'][:], in0=sm['f'][:], scalar1=0.5, scalar2=None, op0=ALU.is_gt))
            dv(lambda e: e.tensor_tensor(out=sm['f'][:], in0=sm['f'][:], in1=sm['g'][:], op=ALU.subtract))
            dv(lambda e: e.tensor_scalar(out=sm['g'][:], in0=sm['f'][:], scalar1=-0.5, scalar2=None, op0=ALU.is_lt))
            dv(lambda e: e.tensor_tensor(out=sm['f'][:], in0=sm['f'][:], in1=sm['g'][:], op=ALU.add))
            ac(lambda e, dst=dst: e.activation(out=sm[dst][:], in_=sm['f'][:], func=AF.Sin, scale=-2 * math.pi))
        sincos('sn', 64.5)
        sincos('cs', 64.75)
        dv(lambda e: e.tensor_tensor(out=sm['x'][:], in0=sm['rr'][:], in1=sm['cs'][:], op=ALU.mult))
        dv(lambda e: e.tensor_scalar(out=sm['x'][:], in0=sm['x'][:], scalar1=-1.0, scalar2=None, op0=ALU.add))
        dv(lambda e: e.tensor_tensor(out=sm['y'][:], in0=sm['rr'][:], in1=sm['sn'][:], op=ALU.mult))
        dv(lambda e: e.tensor_tensor(out=sm['den'][:], in0=sm['lr'][:], in1=sm['lr'][:], op=ALU.mult))
        dv(lambda e: e.tensor_tensor(out=sm['t1'][:], in0=sm['li'][:], in1=sm['li'][:], op=ALU.mult))
        dv(lambda e: e.tensor_tensor(out=sm['den'][:], in0=sm['den'][:], in1=sm['t1'][:], op=ALU.add))
        dv(lambda e: e.reciprocal(out=sm['den'][:], in_=sm['den'][:]))
        dv(lambda e: e.tensor_tensor(out=sm['cr'][:], in0=sm['x'][:], in1=sm['lr'][:], op=ALU.mult))
        dv(lambda e: e.tensor_tensor(out=sm['t1'][:], in0=sm['y'][:], in1=sm['li'][:], op=ALU.mult))
        dv(lambda e: e.tensor_tensor(out=sm['cr'][:], in0=sm['cr'][:], in1=sm['t1'][:], op=ALU.add))
        dv(lambda e: e.tensor_tensor(out=sm['cr'][:], in0=sm['cr'][:], in1=sm['den'][:], op=ALU.mult))
        dv(lambda e: e.tensor_tensor(out=sm['ci'][:], in0=sm['y'][:], in1=sm['lr'][:], op=ALU.mult))
        dv(lambda e: e.tensor_tensor(out=sm['t1'][:], in0=sm['x'][:], in1=sm['li'][:], op=ALU.mult))
        dv(lambda e: e.tensor_tensor(out=sm['ci'][:], in0=sm['ci'][:], in1=sm['t1'][:], op=ALU.subtract))
        dv(lambda e: e.tensor_tensor(out=sm['ci'][:], in0=sm['ci'][:], in1=sm['den'][:], op=ALU.mult))
        st2 = contextlib.ExitStack()
        Tc = self.sb('Tc', [P, LC, 32], F32, st2)
        Tn = self.sb('Tn', [P, LC, 32], F32, st2)
        U1 = self.sb('U1', [P, LC // 2, 32], F32, st2)
        U2 = self.sb('U2', [P, LC // 2, 32], F32, st2)
        cst1 = self.sb('tp1s', [P, 512], F32, st2)
        cst2 = self.sb('tp2s', [P, 512], F32, st2)
        dv(lambda e: e.tensor_copy(out=sm['e1c'][:], in_=sm['cs'][:]))
        dv(lambda e: e.tensor_scalar(out=sm['e1n'][:], in0=sm['sn'][:], scalar1=-1.0, scalar2=None, op0=ALU.mult))
        dv(lambda e: e.memset(Tc[:, 0, :], 1.0), extra_w=[(L, 'T')])
        dv(lambda e: e.memset(Tn[:, 0, :], 0.0), extra_w=[(L, 'T')])
        TT = [(L, 'T')]
        n = 1
        while n < LC:
            ecb = sm['e1c'][:, :].unsqueeze(1).to_broadcast([P, n, 32])
            enb = sm['e1n'][:, :].unsqueeze(1).to_broadcast([P, n, 32])
            dv(lambda e, n=n, ecb=ecb: e.tensor_tensor(out=Tc[:, n:2 * n, :], in0=Tc[:, 0:n, :], in1=ecb, op=ALU.mult), TT, TT)
            dv(lambda e, n=n, enb=enb: e.tensor_tensor(out=U1[:, 0:n, :], in0=Tn[:, 0:n, :], in1=enb, op=ALU.mult), TT, TT)
            dv(lambda e, n=n: e.tensor_tensor(out=Tc[:, n:2 * n, :], in0=Tc[:, n:2 * n, :], in1=U1[:, 0:n, :], op=ALU.subtract), TT, TT)
            dv(lambda e, n=n, enb=enb: e.tensor_tensor(out=Tn[:, n:2 * n, :], in0=Tc[:, 0:n, :], in1=enb, op=ALU.mult), TT, TT)
            dv(lambda e, n=n, ecb=ecb: e.tensor_tensor(out=U2[:, 0:n, :], in0=Tn[:, 0:n, :], in1=ecb, op=ALU.mult), TT, TT)
            dv(lambda e, n=n: e.tensor_tensor(out=Tn[:, n:2 * n, :], in0=Tn[:, n:2 * n, :], in1=U2[:, 0:n, :], op=ALU.add), TT, TT)
            dv(lambda e: e.tensor_tensor(out=sm['t1'][:], in0=sm['e1c'][:], in1=sm['e1c'][:], op=ALU.mult))
            dv(lambda e: e.tensor_tensor(out=sm['t2'][:], in0=sm['e1n'][:], in1=sm['e1n'][:], op=ALU.mult))
            dv(lambda e: e.tensor_tensor(out=sm['e1n'][:], in0=sm['e1c'][:], in1=sm['e1n'][:], op=ALU.mult))
            dv(lambda e: e.tensor_scalar(out=sm['e1n'][:], in0=sm['e1n'][:], scalar1=2.0, scalar2=None, op0=ALU.mult))
            dv(lambda e: e.tensor_tensor(out=sm['e1c'][:], in0=sm['t1'][:], in1=sm['t2'][:], op=ALU.subtract))
            n *= 2
        dv(lambda e: e.tensor_copy(out=sm['cL'][:], in_=sm['e1c'][:]))
        dv(lambda e: e.tensor_copy(out=sm['nL'][:], in_=sm['e1n'][:]))
        dv(lambda e: e.memset(sm['ire'][:], 0.0))
        dv(lambda e: e.memset(sm['iim'][:], 0.0))
        dv(lambda e: e.tensor_copy(out=Ec[:, :, :], in_=Tc[:, :, :].rearrange('p t k -> p k t')), TT, [(L, 'E')])
        dv(lambda e: e.tensor_copy(out=En[:, :, :], in_=Tn[:, :, :].rearrange('p t k -> p k t')), TT, [(L, 'E')])
        for k in range(32):
            sc.dma('sp', 'cstage', cst1[:, 0:256], self.d_Cblk[k, :, :], writes=['cst1'])
            crk = sm['cr'][:, k:k + 1]
            cik = sm['ci'][:, k:k + 1]
            sc.op('dve', lambda e, cik=cik: e.tensor_scalar(out=cst2[:, 0:128], in0=cst1[:, 128:256], scalar1=cik, scalar2=None, op0=ALU.mult),
                  reads=['cst1'] + SM, writes=['cst2'])
            sc.op('dve', lambda e, k=k, crk=crk: e.scalar_tensor_tensor(out=Cb[:, k, 0:128], in0=cst1[:, 0:128], scalar=crk, in1=cst2[:, 0:128], op0=ALU.mult, op1=ALU.subtract),
                  reads=['cst1', 'cst2'] + SM, writes=[(L, 'Cb')])
            sc.op('dve', lambda e, crk=crk: e.tensor_scalar(out=cst2[:, 128:256], in0=cst1[:, 128:256], scalar1=crk, scalar2=None, op0=ALU.mult),
                  reads=['cst1'] + SM, writes=['cst2'])
            sc.op('dve', lambda e, k=k, cik=cik: e.scalar_tensor_tensor(out=Cb[:, k, 128:256], in0=cst1[:, 0:128], scalar=cik, in1=cst2[:, 128:256], op0=ALU.mult, op1=ALU.add),
                  reads=['cst1', 'cst2'] + SM, writes=[(L, 'Cb')])
        if self.cfg.get('sdebug'):
            dbg = self.nc.dram_tensor('dbg', [P, 7 * 32 + 512 + 512], F32, kind="ExternalOutput").ap()
            for i, nme in enumerate(['sn', 'cs', 'rr', 'cr', 'ci', 'cL', 'nL']):
                sc.dma('sp', 'const', dbg[:, i * 32:(i + 1) * 32], sm[nme][:], reads=SM, writes=[('dbg', i)])
            sc.dma('sp', 'const', dbg[:, 224:224 + 256], Ec[:, 0:2, :], reads=[(L, 'E')], writes=[('dbg', 10)])
            sc.dma('sp', 'const', dbg[:, 224 + 256:224 + 512], En[:, 0:2, :], reads=[(L, 'E')], writes=[('dbg', 11)])
            sc.dma('sp', 'const', dbg[:, 224 + 512:224 + 768], Cb[:, 0, :], reads=[(L, 'Cb')], writes=[('dbg', 12)], allow_dtype=True) if False else None
        sc.barrier()
        st2.close()
        hT = self.sb('hTs', [P, DC, 512], BF16, st)
        h32 = self.sb('h32s', [P, DC, 512], F32, st)
        GT = self.sb('GTs', [P, DC, 512], BF16, st)
        Sb = self.sb('Sb', [P, 4, 2, 512], BF16, st)
        wgl = [self.sb('wgl%d' % i, [P, DC * 256], BF16, st) for i in range(2)]
        bre2 = [self.sb('bre%d' % i, [P, 512], F32, st) for i in range(2)]
        bim2 = [self.sb('bim%d' % i, [P, 512], F32, st) for i in range(2)]
        wre2 = [self.sb('wre%d' % i, [P, 512], F32, st) for i in range(2)]
        wim2 = [self.sb('wim%d' % i, [P, 512], F32, st) for i in range(2)]
        xrs2 = [self.sb('xrs%d' % i, [P, 512], F32, st) for i in range(2)]
        xis2 = [self.sb('xis%d' % i, [P, 512], F32, st) for i in range(2)]
        tq = self.sb('tq', [P, 512], F32, st)
        tq2 = self.sb('tq2', [P, 512], F32, st)
        tp1 = self.sb('tp1', [P, 512], F32, st)
        tp2 = self.sb('tp2', [P, 512], F32, st)
        nw = 0
        for blk in range(self.cfg.get('snblk', NBLK)):
            name, t = self.get_xT(blk, False)
            self.norm_mod(li, 0, name, t, hT, (L, 'hT'), 0, h32=h32)
            pre_w = {}
            for c in range(2):
                slot = nw % 2
                nw += 1
                wn = (L, 'wgl', slot)
                self.load_w_cast('wgl%d' % slot, wgl[slot][:, :], self.d_glu[c, :, :], None, writes=[wn])
                pre_w[c] = (slot, wn)
            for j in range(DC):
                def v3(ap):
                    return ap.rearrange('p (a b) -> p a b', b=LC)
                ER = [(L, 'E')]
                for pair in range(2):
                    tl = []
                    for kk in (2 * pair, 2 * pair + 1):
                        k = 4 * j + kk
                        sl = k % 2
                        T = dict(k=k, kk=kk, sl=sl, bre=bre2[sl], bim=bim2[sl], wre=wre2[sl], wim=wim2[sl], xrs=xrs2[sl], xis=xis2[sl],
                                 BRE='bre%d' % sl, BIM='bim%d' % sl, WRE='wre%d' % sl, WIM='wim%d' % sl, XRS='xrs%d' % sl, XIS='xis%d' % sl, CS=('cs1', sl),
                                 cb=Ec[:, k, :].unsqueeze(1).to_broadcast([P, 4, LC]), nb=En[:, k, :].unsqueeze(1).to_broadcast([P, 4, LC]),
                                 rb=sm['rr'][:, k:k + 1].to_broadcast([P, LC]), cLk=sm['cL'][:, k:k + 1], nLk=sm['nL'][:, k:k + 1],
                                 irek=sm['ire'][:, k:k + 1], iimk=sm['iim'][:, k:k + 1], CR=[(L, 'carry', k)],
                                 c0=cs1[:, 2 * sl:2 * sl + 1], c1=cs1[:, 2 * sl + 1:2 * sl + 2])
                        tl.append(T)
                        pxr_n, pxr = self.bank()
                        pxi_n, pxi = self.bank()
                        hr = [((L, 'hT'), j, 0), (L, 'Bb')]
                        sc.op('pe', lambda e, pxr=pxr, k=k, j=j: e.matmul(pxr[:], lhsT=Bb[:, k, 0:128], rhs=hT[:, j, :], start=True, stop=True), reads=hr, writes=[pxr_n])
                        sc.op('pe', lambda e, pxi=pxi, k=k, j=j: e.matmul(pxi[:], lhsT=Bb[:, k, 128:256], rhs=hT[:, j, :], start=True, stop=True), reads=hr, writes=[pxi_n])
                        sc.op('act', lambda e, pxr=pxr, T=T: e.activation(out=T['xrs'][:], in_=pxr[:], func=AF.Copy), reads=[pxr_n], writes=[T['XRS']])
                        sc.op('act', lambda e, pxi=pxi, T=T: e.activation(out=T['xis'][:], in_=pxi[:], func=AF.Copy), reads=[pxi_n], writes=[T['XIS']])
                    for T in tl:
                        sc.op('dve', lambda e, T=T: e.tensor_tensor(out=v3(T['bre'][:, :]), in0=v3(T['xrs'][:, :]), in1=T['cb'], op=ALU.mult), reads=[T['XRS']] + ER, writes=[T['BRE']])
                        sc.op('dve', lambda e, T=T: e.tensor_tensor(out=v3(tq[:, :]), in0=v3(T['xis'][:, :]), in1=T['nb'], op=ALU.mult), reads=[T['XIS']] + ER, writes=['tq'])
                        sc.op('dve', lambda e, T=T: e.tensor_tensor(out=T['bre'][:], in0=T['bre'][:], in1=tq[:], op=ALU.subtract), reads=[T['BRE'], 'tq'], writes=[T['BRE']])
                        sc.op('pool', lambda e, T=T: e.tensor_tensor(out=v3(T['bim'][:, :]), in0=v3(T['xis'][:, :]), in1=T['cb'], op=ALU.mult), reads=[T['XIS']] + ER, writes=[T['BIM']])
                        sc.op('pool', lambda e, T=T: e.tensor_tensor(out=v3(tq2[:, :]), in0=v3(T['xrs'][:, :]), in1=T['nb'], op=ALU.mult), reads=[T['XRS']] + ER, writes=['tq2'])
                        sc.op('pool', lambda e, T=T: e.tensor_tensor(out=T['bim'][:], in0=T['bim'][:], in1=tq2[:], op=ALU.add), reads=[T['BIM'], 'tq2'], writes=[T['BIM']])
                    for ch in range(4):
                        lo_, hi_ = ch * LC, (ch + 1) * LC
                        for T in tl:
                            sc.op('dve', lambda e, T=T, lo_=lo_, hi_=hi_: e.tensor_tensor_scan(out=T['wre'][:, lo_:hi_], data0=T['rb'], data1=T['bre'][:, lo_:hi_], initial=T['irek'],
                                                                                          op0=ALU.mult, op1=ALU.add), reads=[T['BRE']] + T['CR'] + SM, writes=[T['WRE']])
                        for T in tl:
                            sc.op('dve', lambda e, T=T, lo_=lo_, hi_=hi_: e.tensor_tensor_scan(out=T['wim'][:, lo_:hi_], data0=T['rb'], data1=T['bim'][:, lo_:hi_], initial=T['iimk'],
                                                                                          op0=ALU.mult, op1=ALU.add), reads=[T['BIM']] + T['CR'] + SM, writes=[T['WIM']])
                        for T in tl:
                            sc.op('dve', lambda e, T=T, hi_=hi_: e.tensor_tensor(out=T['c0'], in0=T['wim'][:, hi_ - 1:hi_], in1=T['nLk'], op=ALU.mult), reads=[T['WIM']] + SM, writes=[T['CS']])
                        for T in tl:
                            sc.op('dve', lambda e, T=T, hi_=hi_: e.scalar_tensor_tensor(out=T['irek'], in0=T['wre'][:, hi_ - 1:hi_], scalar=T['cLk'], in1=T['c0'], op0=ALU.mult, op1=ALU.add),
                                  reads=[T['WRE'], T['CS']] + SM, writes=T['CR'])
                        for T in tl:
                            sc.op('dve', lambda e, T=T, hi_=hi_: e.tensor_tensor(out=T['c1'], in0=T['wre'][:, hi_ - 1:hi_], in1=T['nLk'], op=ALU.mult), reads=[T['WRE']] + SM, writes=[T['CS']])
                        for T in tl:
                            sc.op('dve', lambda e, T=T, hi_=hi_: e.scalar_tensor_tensor(out=T['iimk'], in0=T['wim'][:, hi_ - 1:hi_], scalar=T['cLk'], in1=T['c1'], op0=ALU.mult, op1=ALU.subtract),
                                  reads=[T['WIM'], T['CS']] + SM, writes=T['CR'])
                    for T in tl:
                        kk = T['kk']
                        sc.op('pool', lambda e, T=T: e.tensor_tensor(out=v3(tp1[:, :]), in0=v3(T['wre'][:, :]), in1=T['cb'], op=ALU.mult), reads=[T['WRE']] + ER, writes=['tp1'])
                        sc.op('pool', lambda e, T=T: e.tensor_tensor(out=v3(tp2[:, :]), in0=v3(T['wim'][:, :]), in1=T['nb'], op=ALU.mult), reads=[T['WIM']] + ER, writes=['tp2'])
                        sc.op('pool', lambda e, kk=kk: e.tensor_tensor(out=Sb[:, kk, 0, :], in0=tp1[:], in1=tp2[:], op=ALU.add), reads=['tp1', 'tp2'], writes=[(L, 'Sb', kk)])
                        sc.op('pool', lambda e, T=T: e.tensor_tensor(out=v3(tp1[:, :]), in0=v3(T['wre'][:, :]), in1=T['nb'], op=ALU.mult), reads=[T['WRE']] + ER, writes=['tp1'])
                        sc.op('pool', lambda e, T=T: e.tensor_tensor(out=v3(tp2[:, :]), in0=v3(T['wim'][:, :]), in1=T['cb'], op=ALU.mult), reads=[T['WIM']] + ER, writes=['tp2'])
                        sc.op('pool', lambda e, kk=kk: e.tensor_tensor(out=Sb[:, kk, 1, :], in0=tp1[:], in1=tp2[:], op=ALU.subtract), reads=['tp1', 'tp2'], writes=[(L, 'Sb', kk)])
                pyn, py = self.bank()
                fns = []
                for kk in range(4):
                    k = 4 * j + kk
                    fns.append(lambda e, py=py, k=k, kk=kk: e.matmul(py[:], lhsT=Cb[:, k, 0:128], rhs=Sb[:, kk, 0, :], start=(kk == 0), stop=False))
                    fns.append(lambda e, py=py, k=k, kk=kk: e.matmul(py[:], lhsT=Cb[:, k, 128:256], rhs=Sb[:, kk, 1, :], start=False, stop=(kk == 3)))
                sc.op('pe', fns, reads=[(L, 'Sb', kk) for kk in range(4)] + [(L, 'Cb')], writes=[pyn])
                if self.cfg.get('sdebug') == 2 and blk == 0 and j == 0:
                    sc.op('act', lambda e, py=py: e.activation(out=bre[:], in_=py[:], func=AF.Copy), reads=[pyn, 'bre'], writes=['bre'])
                    sc.dma('sp', 'const', dbg2[:, 4, :], bre[:], reads=['bre'], writes=[('dbg2', 4)])
                z = self.tmp32[0]
                w_ = self.tmp32[1]
                sc.op('dve', lambda e, py=py, j=j, z=z: e.scalar_tensor_tensor(out=z[:], in0=h32[:, j, :], scalar=dT[:, j:j + 1], in1=py[:], op0=ALU.mult, op1=ALU.add),
                      reads=[pyn, ('h32', j), (L, 'dT')], writes=['tmp32_0'])
                if self.cfg.get('sdebug') and blk == 0 and j == 0:
                    dbg4 = self.nc.dram_tensor('dbg4', [P, 3, 512], F32, kind="ExternalOutput").ap()
                    sc.dma('sp', 'const', dbg4[:, 0, :], z[:], reads=['tmp32_0'], writes=[('dbg4', 0)])
                    sc.dma('pool', 'dbg4b', dbg4[:, 1, 0:256], Cb[:, 1, :], reads=[(L, 'Cb')], writes=[('dbg4', 1)])
                    sc.dma('pool', 'dbg4b', dbg4[:, 1, 256:512], Cb[:, 1, :], reads=[(L, 'Cb')], writes=[('dbg4', 3)])
                sc.op('act', lambda e, z=z, w_=w_: e.activation(out=w_[:], in_=z[:], func=AF.Square), reads=['tmp32_0'], writes=['tmp32_1'])
                sc.op('dve', lambda e, w_=w_: e.tensor_scalar(out=w_[:], in0=w_[:], scalar1=0.044715, scalar2=1.0, op0=ALU.mult, op1=ALU.add), reads=['tmp32_1'], writes=['tmp32_1'])
                sc.op('dve', lambda e, z=z, w_=w_: e.tensor_tensor(out=w_[:], in0=w_[:], in1=z[:], op=ALU.mult), reads=['tmp32_0', 'tmp32_1'], writes=['tmp32_1'])
                sc.op('act', lambda e, w_=w_: e.activation(out=w_[:], in_=w_[:], func=AF.Sigmoid, scale=1.5957691216057308), reads=['tmp32_1'], writes=['tmp32_1'])
                sc.op('dve', lambda e, z=z, w_=w_, j=j: e.tensor_tensor(out=GT[:, j, :], in0=z[:], in1=w_[:], op=ALU.mult), reads=['tmp32_0', 'tmp32_1'], writes=[(L, 'GT', j)])
            if self.cfg.get('sdebug') and blk == 0:
                sc.dma('pool', 'dbg4b', dbg4[:, 2, :], GT[:, 0, :], reads=[(L, 'GT', 0)], writes=[('dbg4', 2)])
            greads = [(L, 'GT', j) for j in range(DC)]
            for c in range(DC):
                if c in pre_w:
                    slot, wn = pre_w[c]
                else:
                    slot = nw % 2
                    nw += 1
                    wn = (L, 'wgl', slot)
                    self.load_w_cast('wgl%d' % slot, wgl[slot][:, :], self.d_glu[c, :, :], None, writes=[wn])
                pln, pl = self.bank()
                pgn, pg = self.bank()
                for (pn, pt, off) in ((pln, pl, 0), (pgn, pg, 128)):
                    fns = [lambda e, pt=pt, kq=kq, off=off, slot=slot: e.matmul(pt[:], lhsT=wgl[slot][:, kq * 256 + off: kq * 256 + off + 128], rhs=GT[:, kq, :],
                                                                              start=(kq == 0), stop=(kq == DC - 1)) for kq in range(DC)]
                    sc.op('pe', fns, reads=greads + [wn], writes=[pn])
                tm = self.tmp32[2]
                sc.op('act', lambda e, pg=pg, tm=tm: e.activation(out=tm[:], in_=pg[:], func=AF.Sigmoid), reads=[pgn], writes=['tmp32_2'])
                sc.op('dve', lambda e, pl=pl, tm=tm: e.tensor_tensor(out=tm[:], in0=pl[:], in1=tm[:], op=ALU.mult), reads=[pln, 'tmp32_2'], writes=['tmp32_2'])
                sc.op('dve', lambda e, tm=tm, c=c, t=t: e.scalar_tensor_tensor(out=t[:, c, :], in0=tm[:], scalar=self.modv(li, 2, c), in1=t[:, c, :], op0=ALU.mult, op1=ALU.add),
                      reads=['tmp32_2', (name, c), ('modT', li)], writes=[(name, c)])
                self.store_xT(blk, name, t, c)
        sc.barrier()
        st.close()

    def epilogue(self):
        sc = self.sc
        self.out = self.nc.dram_tensor('out', [self.ntok, D], F32, kind="ExternalOutput").ap()
        otok = [self.sb('otok%d' % i, [P, D], F32) for i in range(2)]
        n = 0
        for blk in range(self.ntok // 512):
            name, t = self.load_xT(blk, False)
            for j in range(4):
                slot = n % 2
                n += 1
                on = 'otok%d' % slot
                for hh in range(2):
                    pname, pt = self.bank()
                    fns = [lambda e, pt=pt, j=j, c=c, hh=hh, t=t: e.transpose(out=pt[:, (c - hh * 4) * P:(c - hh * 4 + 1) * P],
                                                                      in_=t[:, c, j * P:(j + 1) * P], identity=self.ident[:])
                           for c in range(hh * 4, hh * 4 + 4)]
                    sc.op('pe', fns, reads=[(name, c) for c in range(DC)] + ['ident'], writes=[pname])
                    if hh == 0:
                        sc.op('act', lambda e, pt=pt, slot=slot: e.activation(out=otok[slot][:, 0:512], in_=pt[:], func=AF.Copy),
                              reads=[pname], writes=[(on, 0)])
                    else:
                        sc.op('dve', lambda e, pt=pt, slot=slot: e.tensor_copy(out=otok[slot][:, 512:1024], in_=pt[:]),
                              reads=[pname], writes=[(on, 1)])
                sc.dma('sp', 'st_' + on, self.out[blk * 512 + j * P: blk * 512 + (j + 1) * P, :], otok[slot][:],
                       reads=[(on, 0), (on, 1)], writes=[('out', blk, j)])

    def build(self):
        cfg = self.cfg
        self.setup_common()
        self.epsb = self.sb('epsb', [P, 1], F32)
        self.sc.op('dve', lambda e: e.memset(self.epsb[:], EPS), writes=['epsb'])
        self.build_mod()
        self.alloc_stream()
        self.conv_setup()
        self.ffn_setup()
        self.attn_setup()
        self.ssm_setup()
        steps = cfg.get('steps', ['m0', 'f0', 'm1', 'f1', 'm2', 'f2', 'm3', 'f3'])
        first = True
        if steps[0][0] != 'm' or int(steps[0][1]) % 3 != 0:
            st = contextlib.ExitStack()
            self.xtok = self.sb('xtok', [P, 4, D], F32, st)
            for blk in range(NBLK):
                name, t = self.load_xT(blk, True)
                for c in range(DC):
                    self.store_xT(blk, name, t, c)
            self.sc.barrier()
            st.close()
            first = False
        tail_split = cfg.get('tail_split', False)
        for s in steps:
            li = int(s[1])
            if tail_split and s == 'm3':
                self.sc.barrier()
                self.xs_d = self.nc.dram_tensor('xs_scratch', [D, S // 2 + 512], F32).ap()
                self.sc.dma('sp', 'xstage', self.xs_d[:, 512:512 + S // 2], (lambda: self.xT_d[:, bass.ds(self.rv * 2048, 2048)]), writes=['xs'])
                self.sc.dma('sp', 'xstage', self.xs_d[:, 0:512], (lambda: self.xT_d[:, bass.ds(self.rv * 1536, 512)]), writes=['xs'])
                self.rd_mode, self.wr_mode, self.ntok = 'dynfull', 'half', S // 2
            if tail_split and s == 'f3':
                self.rd_mode, self.wr_mode, self.ntok = 'half', 'half', S // 2
            if s[0] == 'm':
                if li % 3 == 0:
                    self.conv_mixer(li, li // 3, first)
                elif li % 3 == 1:
                    self.attn_mixer(li)
                else:
                    self.ssm_mixer(li)
                first = False
            else:
                self.ffn(li, li % 2 == 1)
        self.epilogue()
        self.sc.finish('sp')
        self.sc.emit()
        return self.nc


def host_layout(inputs, b):
    f = np.float32
    m = {}
    m['ident'] = np.eye(P, dtype=f)
    m['x'] = np.ascontiguousarray(inputs['x'][b])
    m['cT'] = np.ascontiguousarray(inputs['c'][b].reshape(DC, P).T)
    m['ada_w'] = inputs['ada_w']
    m['ada_bT'] = np.ascontiguousarray(inputs['ada_b'].reshape(4, 48, P).transpose(2, 0, 1))
    m['norm_gT'] = np.ascontiguousarray(inputs['norm_g'].reshape(4, 2, DC, P).transpose(3, 0, 1, 2))
    m['conv_w_in'] = inputs['conv_w_in']
    m['conv_w_out'] = inputs['conv_w_out']
    m['conv_wT'] = np.ascontiguousarray(inputs['conv_w'].reshape(2, 3, DC, P).transpose(3, 0, 1, 2))
    m['rankf'] = np.zeros((P, 1), dtype=f)
    return m


def rel_bucket_np(dist):
    import math
    d = np.maximum(dist, 1).astype(np.float32)
    large = 16 + (np.log(d / np.float32(16)) / np.float32(math.log(128 / 16)) * np.float32(16)).astype(np.int32)
    large = np.minimum(large, 31)
    return np.where(dist < 16, dist, large)


def ssm_layout(inputs):
    f = np.float32
    m = {}
    lre = inputs['ssm_lambda_re'][0]
    lim = inputs['ssm_lambda_im'][0]
    lamT = np.empty((P, 32, 2), dtype=f)
    lst = np.empty((P, 32), dtype=f)
    Bb = np.zeros((32, P, 2, P), dtype=f)
    Cb = np.zeros((32, P, 2, P), dtype=f)
    bre = inputs['ssm_b_re'][0]
    bim = inputs['ssm_b_im'][0]
    cre = inputs['ssm_c_re'][0]
    cim = inputs['ssm_c_im'][0]
    for k in range(32):
        for gg in range(2):
            g = 2 * k + gg
            lamT[gg * 64:(gg + 1) * 64, k, 0] = lre[g]
            lamT[gg * 64:(gg + 1) * 64, k, 1] = lim[g]
            lst[gg * 64:(gg + 1) * 64, k] = inputs['ssm_log_step'][0, g]
            r0 = (g % 8) * 16
            Bb[k, r0:r0 + 16, 0, gg * 64:(gg + 1) * 64] = bre[g].T
            Bb[k, r0:r0 + 16, 1, gg * 64:(gg + 1) * 64] = bim[g].T
            Cb[k, gg * 64:(gg + 1) * 64, 0, r0:r0 + 16] = cre[g].T
            Cb[k, gg * 64:(gg + 1) * 64, 1, r0:r0 + 16] = cim[g].T
    m['ssm_lamT'] = lamT
    m['ssm_lstepT'] = lst
    m['ssm_Bblk'] = Bb.reshape(32, P, 256)
    m['ssm_Cblk'] = Cb.reshape(32, P, 256)
    m['ssm_dT'] = np.ascontiguousarray(inputs['ssm_d'][0].reshape(DC, P).T)
    w = inputs['ssm_w_glu'][0]
    lin = w[:, :D].reshape(DC, P, DC, P)
    gate = w[:, D:].reshape(DC, P, DC, P)
    r = np.empty((DC, P, DC, 2, P), dtype=f)
    r[:, :, :, 0, :] = lin.transpose(2, 1, 0, 3)
    r[:, :, :, 1, :] = gate.transpose(2, 1, 0, 3)
    m['ssm_glu_r'] = r.reshape(DC, P, DC * 256)
    return m


def attn_layout(inputs):
    f = np.float32
    m = {}
    w = inputs['attn_w_in'][0]
    wq = w[:, 0:1024].reshape(D, 16, 64)
    order = []
    for pair in range(2):
        for g in range(4):
            order += [(2 * pair) * 4 + g, (2 * pair + 1) * 4 + g]
    m['attn_wq_perm'] = np.ascontiguousarray(wq[:, order, :]).reshape(D, 1024)
    m['attn_wk'] = np.ascontiguousarray(w[:, 1024:1280])
    m['attn_wv'] = np.ascontiguousarray(w[:, 1280:1536])
    m['attn_wqi'] = np.ascontiguousarray(w[:, 1536:2048])
    ki = w[:, 2048:2112]
    m['attn_wki2'] = np.ascontiguousarray(np.concatenate([ki, ki], axis=1))
    m['attn_wwi'] = np.ascontiguousarray(w[:, 2112:2120])
    m['attn_wout_r'] = np.ascontiguousarray(inputs['attn_w_out'][0].reshape(16, 64, D).transpose(1, 0, 2))
    qg = inputs['attn_q_gain'][0]
    kg = inputs['attn_k_gain'][0]
    m['attn_gainT'] = np.ascontiguousarray(np.stack([np.tile(qg, 2), np.tile(kg, 2)], axis=1))
    rb = inputs['rel_bias']
    sl = np.arange(P)[:, None]
    tl = np.arange(P)[None, :]
    bt = np.empty((P, 32, P), dtype=f)
    for kind in range(2):
        dist = np.maximum(tl - sl + kind * P, 0)
        bk = rel_bucket_np(dist)
        for h in range(16):
            bt[:, kind * 16 + h, :] = rb[bk, h]
    m['attn_biasT'] = bt
    m['attn_b31B'] = np.ascontiguousarray(np.broadcast_to(rb[31][None, :], (P, 16)))
    cm = np.zeros((P, P), dtype=f)
    cm[np.arange(P)[None, :] > np.arange(P)[:, None]] = -1e30
    m['cmask'] = cm
    bo = np.zeros((P, P), dtype=f)
    bo[0:64, 0:64] = 1.0
    bo[64:128, 64:128] = 1.0
    m['blockones'] = bo
    sr = np.zeros((65, 64), dtype=f)
    sr[64, :] = 1.0
    m['selrow'] = sr
    return m


_shared_cache = {}


def shared_layout(inputs):
    f = np.float32
    m = {}
    gu = inputs['ffn_w_gu']
    g = gu[:, :, :DFF].reshape(2, DC, P, FC, P)
    u = gu[:, :, DFF:].reshape(2, DC, P, FC, P)
    gu_r = np.stack([g, u], axis=4)
    m['ffn_gu_r'] = np.ascontiguousarray(gu_r.transpose(0, 3, 2, 1, 4, 5)).reshape(2, FC, P, DC * 256)
    dn = inputs['ffn_w_down'].reshape(2, FC, P, DC, P)
    m['ffn_dn_r'] = np.ascontiguousarray(dn.transpose(0, 3, 2, 1, 4)).reshape(2, DC, P, FC * P)
    gu = inputs['moe_w_gu']
    g = gu[:, :, :, :DFF].reshape(2, NE, DC, P, FC, P)
    u = gu[:, :, :, DFF:].reshape(2, NE, DC, P, FC, P)
    r = np.empty((2, NE, FC, P, DC, 2, P), dtype=f)
    r[:, :, :, :, :, 0, :] = g.transpose(0, 1, 4, 3, 2, 5)
    r[:, :, :, :, :, 1, :] = u.transpose(0, 1, 4, 3, 2, 5)
    m['moe_gu_r'] = r.reshape(2, NE, FC, P, DC * 256)
    dn = inputs['moe_w_down'].reshape(2, NE, FC, P, DC, P)
    m['moe_dn_r'] = np.ascontiguousarray(dn.transpose(0, 1, 4, 3, 2, 5)).reshape(2, NE, DC, P, FC * P)
    m['moe_rwT'] = np.ascontiguousarray(inputs['moe_router_w'].reshape(2, DC, P, NE).transpose(2, 0, 1, 3))
    m['moe_rbB'] = np.ascontiguousarray(np.broadcast_to(inputs['moe_router_b'][None], (P, 2, NE)))
    m.update(attn_layout(inputs))
    m.update(ssm_layout(inputs))
    sel = np.zeros((NE, NE, P), dtype=f)
    for e in range(NE):
        sel[e, e, :] = 1.0
    m['sel8'] = sel
    return m


def kernel(**inputs):
    inputs = {k: np.asarray(v) for k, v in inputs.items()}
    b = Builder({'tail_split': True})
    nc = b.build()
    shared = shared_layout(inputs)
    in_maps = []
    for core in range(8):
        m = host_layout(inputs, core % 4)
        m['rankf'] = np.full((P, 1), float(core // 4), dtype=np.float32)
        m.update(shared)
        in_maps.append({k: m[k] for k in b.din})
    res = run_bass_kernel_spmd(nc, in_maps, core_ids=list(range(8)))
    out = np.stack([np.concatenate([res.results[i]['out'], res.results[i + 4]['out']], axis=0) for i in range(4)], axis=0)
    return out.astype(np.float32)
```
